# Optimizing a Trainium2 kernel written in Bass

```python
import math
import jax, jax.numpy as jnp
from jax import lax
import numpy as np

D_MODEL = 1024
BATCH = 8
SEQ = 4096
DEPTH = 2

GRID_W = 64
CTX_LEN = 256

D_MIX = D_MODEL
N_HEADS_MLA = 8
QK_NOPE_DIM = 64
QK_ROPE_DIM = 32
QK_HEAD_DIM = QK_NOPE_DIM + QK_ROPE_DIM
V_HEAD_DIM = 64
Q_LORA_RANK = 384
KV_LORA_RANK = 256
D_ATTN = N_HEADS_MLA * V_HEAD_DIM
D_CONV = D_MIX // 4
CONV_WIDTH = 31
D_POOL = D_MIX // 4
POOL_WINDOWS = (2, 4, 8, 16)
POOL_GROUP_DIM = D_POOL // len(POOL_WINDOWS)
ROPE_THETA = 10000.0
Q_BLOCK = 128

OFF_Q = 0
OFF_KV = OFF_Q + Q_LORA_RANK
OFF_KR = OFF_KV + KV_LORA_RANK
OFF_CONV = OFF_KR + QK_ROPE_DIM
OFF_POOL = OFF_CONV + 2 * D_CONV
D_IN = OFF_POOL + D_POOL

N_GROUPS = 4
EXPERTS_PER_GROUP = 8
N_EXPERTS = N_GROUPS * EXPERTS_PER_GROUP
TOP_K_IN_GROUP = 2
D_EXPERT = D_MODEL // 4

LN_EPS = 1e-5
RMS_EPS = 1e-6
DEEPNORM_ALPHA = (2 * DEPTH) ** 0.25
DEEPNORM_BETA = (8 * DEPTH) ** -0.25

kernel_name = "hymba_mla_conformer_pool_hmoe_diffusion_trunk"


def layer_norm(x, g, b, eps=LN_EPS):
    xf = x.astype(jnp.float32)
    mu = jnp.mean(xf, -1, keepdims=True)
    var = jnp.mean(jnp.square(xf - mu), -1, keepdims=True)
    y = (xf - mu) * lax.rsqrt(var + eps)
    return (y * g.astype(jnp.float32) + b.astype(jnp.float32)).astype(x.dtype)


def rms_norm(x, g, eps=RMS_EPS):
    xf = x.astype(jnp.float32)
    y = xf * lax.rsqrt(jnp.mean(jnp.square(xf), -1, keepdims=True) + eps)
    return (y * g.astype(jnp.float32)).astype(x.dtype)


def modulate(x, shift, scale):
    return x * (1 + scale) + shift


def axial_rope_tables(n_tokens):
    rows = n_tokens // GRID_W
    row = jnp.repeat(jnp.arange(rows), GRID_W)
    col = jnp.tile(jnp.arange(GRID_W), rows)
    d_axis = QK_ROPE_DIM // 2
    inv_freq = jnp.power(ROPE_THETA, -jnp.arange(0, d_axis, 2, dtype=jnp.float32) / d_axis)

    def axis_angles(p):
        a = p.astype(jnp.float32)[:, None] * inv_freq[None, :]
        return jnp.concatenate([a, a], -1)

    ang = jnp.concatenate([axis_angles(row), axis_angles(col)], -1)
    return jnp.cos(ang), jnp.sin(ang)


def rotate_axial_half(x):
    xs = x.reshape(x.shape[:-1] + (2, 2, QK_ROPE_DIM // 4))
    rot = jnp.stack([-xs[..., 1, :], xs[..., 0, :]], axis=-2)
    return rot.reshape(x.shape)


def apply_rope(x, cos, sin):
    y = x.astype(jnp.float32) * cos + rotate_axial_half(x).astype(jnp.float32) * sin
    return y.astype(x.dtype)


def mla_query(p_q, g_q, w_uq):
    lead = p_q.shape[:-1]
    q = rms_norm(p_q, g_q) @ w_uq
    return q.reshape(lead + (N_HEADS_MLA, QK_HEAD_DIM))


def mla_key_value(p_kvr, g_kv, w_ukv):
    lead = p_kvr.shape[:-1]
    c_kv, k_rope = p_kvr[..., :KV_LORA_RANK], p_kvr[..., KV_LORA_RANK:]
    kv = (rms_norm(c_kv, g_kv) @ w_ukv).reshape(lead + (N_HEADS_MLA, QK_NOPE_DIM + V_HEAD_DIM))
    return kv[..., :QK_NOPE_DIM], k_rope, kv[..., QK_NOPE_DIM:]


def assemble_keys(k_nope, k_rope):
    k_rope_h = jnp.broadcast_to(k_rope[..., None, :], k_nope.shape[:-1] + (QK_ROPE_DIM,))
    return jnp.concatenate([k_nope, k_rope_h], -1)


def latent_attention(q, k_lat, v_lat, k_ctx, v_ctx):
    k = jnp.concatenate([k_ctx, k_lat], 1)
    v = jnp.concatenate([v_ctx, v_lat], 1)
    B, S, H, Dq = q.shape
    n_blk = S // Q_BLOCK
    scale = 1.0 / math.sqrt(Dq)
    qb = q.reshape(B, n_blk, Q_BLOCK, H, Dq).transpose(1, 0, 2, 3, 4)

    def one_block(q_blk):
        s = jnp.einsum('bqhd,bkhd->bhqk', q_blk, k, preferred_element_type=jnp.float32) * scale
        p = jax.nn.softmax(s, axis=-1).astype(v.dtype)
        return jnp.einsum('bhqk,bkhd->bqhd', p, v)

    o = lax.map(one_block, qb)
    return o.transpose(1, 0, 2, 3, 4).reshape(B, S, H * V_HEAD_DIM)


def context_attention(q, k, v):
    B, N, H, Dq = q.shape
    s = jnp.einsum('bqhd,bkhd->bhqk', q, k, preferred_element_type=jnp.float32) * (1.0 / math.sqrt(Dq))
    p = jax.nn.softmax(s, axis=-1).astype(v.dtype)
    return jnp.einsum('bhqk,bkhd->bqhd', p, v).reshape(B, N, H * V_HEAD_DIM)


def conformer_conv(p_conv, conv_w, conv_b, ln_g, ln_b):
    a, gt = p_conv[..., :D_CONV], p_conv[..., D_CONV:]
    u = a * jax.nn.sigmoid(gt)
    pad = CONV_WIDTH // 2
    u = lax.conv_general_dilated(u, conv_w[:, None, :], window_strides=(1,), padding=[(pad, pad)],
                                 dimension_numbers=('NWC', 'WIO', 'NWC'), feature_group_count=D_CONV)
    u = layer_norm(u + conv_b, ln_g, ln_b)
    return jax.nn.silu(u)


def multiscale_pool(u, pool_w, pool_scale):
    B, N, _ = u.shape
    t = jnp.arange(N)
    outs = []
    for gi, w in enumerate(POOL_WINDOWS):
        ug = u[..., gi * POOL_GROUP_DIM:(gi + 1) * POOL_GROUP_DIM].astype(jnp.float32)
        cs = jnp.concatenate([jnp.zeros((B, 1, POOL_GROUP_DIM), jnp.float32), jnp.cumsum(ug, axis=1)], 1)
        lo = jnp.clip(t - w // 2, 0, N - 1)
        hi = jnp.clip(t + w // 2 - 1, 0, N - 1)
        cnt = (hi - lo + 1).astype(jnp.float32)[None, :, None]
        mixed = ((cs[:, hi + 1] - cs[:, lo]) / cnt - ug).astype(u.dtype)
        outs.append(mixed @ pool_w[gi])
    return jnp.concatenate(outs, -1) * pool_scale


def hierarchical_moe(h, w_rg, b_rg, w_re, b_re, w_gate, w_up, w_down):
    T = h.shape[0]
    tok = jnp.arange(T)
    g_logits = (h @ w_rg + b_rg).astype(jnp.float32)
    g_prob = jax.nn.softmax(g_logits, -1)
    g_sel = jnp.argmax(g_logits, -1)
    p_sel = g_prob[tok, g_sel]
    e_logits = (h @ w_re + b_re).astype(jnp.float32).reshape(T, N_GROUPS, EXPERTS_PER_GROUP)
    e_logits_g = e_logits[tok, g_sel]
    top_v, top_i = lax.top_k(e_logits_g, TOP_K_IN_GROUP)
    top_w = jax.nn.softmax(top_v, -1) * p_sel[:, None]
    expert_id = g_sel[:, None] * EXPERTS_PER_GROUP + top_i
    combine = jnp.sum(jax.nn.one_hot(expert_id, N_EXPERTS, dtype=jnp.float32) * top_w[..., None], 1)
    combine = combine.astype(h.dtype)
    y = jnp.zeros_like(h)
    for e in range(N_EXPERTS):
        hid = jax.nn.silu(h @ w_gate[e]) * (h @ w_up[e])
        y = y + combine[:, e:e + 1] * (hid @ w_down[e])
    return y


def post_norm(x, y, gate, g, b):
    return layer_norm(DEEPNORM_ALPHA * x + gate * y, g, b)


def setup_inputs(seed: int = 0) -> dict:
    key = jax.random.key(seed)
    ks = iter(jax.random.split(key, 40))

    def nrm(shape, scale):
        return jax.random.normal(next(ks), shape, jnp.float32) * scale

    L, D = DEPTH, D_MODEL
    return {
        "x": nrm((BATCH, SEQ, D), 1.0),
        "c": nrm((BATCH, D), 1.0),
        "ctx": nrm((BATCH, CTX_LEN, D), 1.0),
        "c_ctx": nrm((D,), 1.0),
        "w_ada": nrm((L, D, 6 * D), 0.5 * D ** -0.5),
        "b_ada": nrm((L, 6 * D), 0.01),
        "w_in": nrm((L, D, D_IN), D ** -0.5),
        "g_q": 1.0 + nrm((L, Q_LORA_RANK), 0.01),
        "w_uq": nrm((L, Q_LORA_RANK, N_HEADS_MLA * QK_HEAD_DIM), Q_LORA_RANK ** -0.5),
        "g_kv": 1.0 + nrm((L, KV_LORA_RANK), 0.01),
        "w_ukv": nrm((L, KV_LORA_RANK, N_HEADS_MLA * (QK_NOPE_DIM + V_HEAD_DIM)), KV_LORA_RANK ** -0.5),
        "conv_w": nrm((L, CONV_WIDTH, D_CONV), CONV_WIDTH ** -0.5),
        "conv_b": nrm((L, D_CONV), 0.01),
        "conv_ln_g": 1.0 + nrm((L, D_CONV), 0.01),
        "conv_ln_b": nrm((L, D_CONV), 0.01),
        "pool_w": nrm((L, len(POOL_WINDOWS), POOL_GROUP_DIM, POOL_GROUP_DIM), POOL_GROUP_DIM ** -0.5),
        "pool_scale": 1.0 + nrm((L, D_POOL), 0.01),
        "w_out": nrm((L, D_MIX, D), DEEPNORM_BETA * D_MIX ** -0.5),
        "ln1_g": 1.0 + nrm((L, D), 0.01),
        "ln1_b": nrm((L, D), 0.01),
        "w_router_group": nrm((L, D, N_GROUPS), D ** -0.5),
        "b_router_group": nrm((L, N_GROUPS), 0.01),
        "w_router_expert": nrm((L, D, N_EXPERTS), D ** -0.5),
        "b_router_expert": nrm((L, N_EXPERTS), 0.01),
        "w_gate": nrm((L, N_EXPERTS, D, D_EXPERT), D ** -0.5),
        "w_up": nrm((L, N_EXPERTS, D, D_EXPERT), D ** -0.5),
        "w_down": nrm((L, N_EXPERTS, D_EXPERT, D), DEEPNORM_BETA * D_EXPERT ** -0.5),
        "ln2_g": 1.0 + nrm((L, D), 0.01),
        "ln2_b": nrm((L, D), 0.01),
    }


def reference(x, c, ctx, c_ctx, w_ada, b_ada, w_in, g_q, w_uq, g_kv, w_ukv, conv_w, conv_b, conv_ln_g,
              conv_ln_b, pool_w, pool_scale, w_out, ln1_g, ln1_b, w_router_group, b_router_group,
              w_router_expert, b_router_expert, w_gate, w_up, w_down, ln2_g, ln2_b):
    B, S, D = x.shape
    cos, sin = axial_rope_tables(S)
    xc = ctx
    for l in range(DEPTH):
        last = l == DEPTH - 1
        ada = jax.nn.silu(c) @ w_ada[l] + b_ada[l]
        sh1, sc1, g1, sh2, sc2, g2 = [a[:, None, :] for a in jnp.split(ada, 6, axis=-1)]
        ada_c = jax.nn.silu(c_ctx) @ w_ada[l] + b_ada[l]
        csh1, csc1, cg1, csh2, csc2, cg2 = jnp.split(ada_c, 6, axis=-1)

        h = modulate(x, sh1, sc1)
        hc = modulate(xc, csh1, csc1)
        p = h @ w_in[l]
        if last:
            pc_kvr = hc @ w_in[l][:, OFF_KV:OFF_CONV]
        else:
            pc = hc @ w_in[l]
            pc_kvr = pc[..., OFF_KV:OFF_CONV]

        kc_nope, kc_rope, v_ctx = mla_key_value(pc_kvr, g_kv[l], w_ukv[l])
        k_ctx = assemble_keys(kc_nope, kc_rope)

        q = mla_query(p[..., OFF_Q:OFF_KV], g_q[l], w_uq[l])
        q = jnp.concatenate([q[..., :QK_NOPE_DIM],
                             apply_rope(q[..., QK_NOPE_DIM:], cos[:, None, :], sin[:, None, :])], -1)
        k_nope, k_rope, v_lat = mla_key_value(p[..., OFF_KV:OFF_CONV], g_kv[l], w_ukv[l])
        k_lat = assemble_keys(k_nope, apply_rope(k_rope, cos, sin))
        attn = latent_attention(q, k_lat, v_lat, k_ctx, v_ctx)
        conv = conformer_conv(p[..., OFF_CONV:OFF_POOL], conv_w[l], conv_b[l], conv_ln_g[l], conv_ln_b[l])
        pool = multiscale_pool(p[..., OFF_POOL:], pool_w[l], pool_scale[l])
        mix = jnp.concatenate([attn, conv, pool], -1) @ w_out[l]
        x = post_norm(x, mix, g1, ln1_g[l], ln1_b[l])

        if not last:
            qc = mla_query(pc[..., OFF_Q:OFF_KV], g_q[l], w_uq[l])
            attn_c = context_attention(qc, k_ctx, v_ctx)
            conv_c = conformer_conv(pc[..., OFF_CONV:OFF_POOL], conv_w[l], conv_b[l], conv_ln_g[l], conv_ln_b[l])
            pool_c = multiscale_pool(pc[..., OFF_POOL:], pool_w[l], pool_scale[l])
            mix_c = jnp.concatenate([attn_c, conv_c, pool_c], -1) @ w_out[l]
            xc = post_norm(xc, mix_c, cg1, ln1_g[l], ln1_b[l])

        h2 = modulate(x, sh2, sc2).reshape(B * S, D)
        if last:
            tokens = h2
        else:
            hc2 = modulate(xc, csh2, csc2).reshape(-1, D)
            tokens = jnp.concatenate([h2, hc2], 0)
        ffn = hierarchical_moe(tokens, w_router_group[l], b_router_group[l], w_router_expert[l],
                               b_router_expert[l], w_gate[l], w_up[l], w_down[l])
        x = post_norm(x, ffn[:B * S].reshape(B, S, D), g2, ln2_g[l], ln2_b[l])
        if not last:
            xc = post_norm(xc, ffn[B * S:].reshape(xc.shape), cg2, ln2_g[l], ln2_b[l])
    return x
```

```python
import math
from contextlib import ExitStack
import numpy as np
import concourse.bass as bass
import concourse.mybir as mybir
from concourse.bass_utils import run_bass_kernel_spmd

F32 = mybir.dt.float32
BF16 = mybir.dt.bfloat16
AF = mybir.ActivationFunctionType
ALU = mybir.AluOpType
AX = mybir.AxisListType

D = 1024
H = 8
DQ = 384
DKV = 256
DR = 32
DIN = 1440
NE = 32
DE = 256
GRID_W = 64
CONVW = 31
LN_EPS = 1e-5
RMS_EPS = 1e-6
NEG = -1.0e30


class Res:
    __slots__ = ("name", "w", "r", "sem", "cnt", "excl", "multi")

    def __init__(self, name, excl=False, multi=False):
        self.multi = False
        self.name = name
        self.w = None
        self.r = []
        self.sem = None
        self.cnt = 0
        self.excl = excl


class Sched:
    def __init__(self, nc):
        self.nc = nc
        self.eng = {"pe": nc.tensor, "act": nc.scalar, "dve": nc.vector, "pool": nc.gpsimd, "sp": nc.sync}
        self.sems = {}
        self.cnt = {}
        self.known = {}
        self.dma_res = []
        self.free_sems = []
        self.free_sw = []
        self.is_sw = {}
        self.nalloc = 0
        self.nwait = 0
        for e in self.eng:
            self.sems[e] = nc.alloc_semaphore("e_" + e)
            self.cnt[e] = 0
            self.known[e] = {}

    def _waits(self, e, reads, writes):
        deps = {}

        def add(ev):
            if ev is None:
                return
            k, v = ev
            if deps.get(k, 0) < v:
                deps[k] = v

        for r in reads:
            add(r.w)
        for w in writes:
            if not w.multi:
                add(w.w)
            for ev in w.r:
                add(ev)
        kn = self.known[e]
        for k, v in deps.items():
            if kn.get(k, 0) >= v:
                continue
            if e == "pe" and k == "pe":
                continue
            kn[k] = v
            sem = self.sems[k] if isinstance(k, str) else k.sem
            self.eng[e].wait_ge(sem, v)
            self.nwait += 1

    @staticmethod
    def _commit(ev, reads, writes):
        for r in reads:
            r.r.append(ev)
            if len(r.r) > 48:
                best = {}
                for k, v in r.r:
                    if best.get(k, 0) < v:
                        best[k] = v
                r.r = list(best.items())
        for w in writes:
            w.w = ev
            w.r = []

    def op(self, e, fn, reads=(), writes=()):
        if any(r.excl for r in reads):
            writes = list(writes) + [r for r in reads if r.excl and r not in writes]
            reads = [r for r in reads if not r.excl]
        self._waits(e, reads, writes)
        ins = fn(self.eng[e])
        self.cnt[e] += 1
        ins.then_inc(self.sems[e], 1)
        self._commit((e, self.cnt[e]), reads, writes)

    def dma(self, e, out, in_, reads, writes, **kw):
        dst = writes[0]
        self._waits(e, reads, writes)
        self.ensure(dst, sw=(e == "pool"))
        dst.cnt += 16
        self.eng[e].dma_start(out=out, in_=in_, **kw).then_inc(dst.sem, 16)
        self._commit((dst, dst.cnt), reads, writes)

    def idma(self, out, in_, idx_ap, scatter, reads, writes):
        import concourse.bass as _b
        dst = writes[0]
        self._waits("pool", reads, writes)
        self.ensure(dst, sw=True)
        dst.cnt += 16
        off = _b.IndirectOffsetOnAxis(ap=idx_ap, axis=0)
        if scatter:
            ins = self.nc.gpsimd.indirect_dma_start(out=out, out_offset=off, in_=in_, in_offset=None)
        else:
            ins = self.nc.gpsimd.indirect_dma_start(out=out, out_offset=None, in_=in_, in_offset=off)
        ins.then_inc(dst.sem, 16)
        self._commit((dst, dst.cnt), reads, writes)

    def ensure(self, dst, sw=False):
        if dst.sem is None:
            fl = self.free_sw if sw else self.free_sems
            self.is_sw[id(dst)] = sw
            if fl:
                dst.sem, dst.cnt = fl.pop()
            else:
                self.nalloc += 1
                dst.sem = self.nc.alloc_semaphore("d%d_%s" % (self.nalloc, dst.name))
                dst.cnt = 0
            self.dma_res.append(dst)

    def mark(self):
        return len(self.dma_res)

    def release(self, mark):
        for r in self.dma_res[mark:]:
            (self.free_sw if self.is_sw.get(id(r)) else self.free_sems).append((r.sem, r.cnt))
            r.sem = None
        del self.dma_res[mark:]

    def barrier(self):
        for e in self.eng:
            kn = self.known[e]
            for k in self.eng:
                if k == e:
                    continue
                v = self.cnt[k]
                if v > 0 and kn.get(k, 0) < v:
                    kn[k] = v
                    self.eng[e].wait_ge(self.sems[k], v)
            for r in self.dma_res:
                if r.cnt > 0 and kn.get(r, 0) < r.cnt:
                    kn[r] = r.cnt
                    self.eng[e].wait_ge(r.sem, r.cnt)

    def wait_all(self, e, ress):
        self._waits(e, ress, ())


class Ring:
    def __init__(self, alloc, name, shape, dt, n):
        self.bufs = []
        for i in range(n):
            nm = "%s_%d" % (name, i)
            self.bufs.append((alloc(nm, shape, dt), Res(nm)))
        self.i = 0

    def get(self):
        b = self.bufs[self.i % len(self.bufs)]
        self.i += 1
        return b


class _Cut(Exception):
    pass


def run_skewed(gens):
    active = []
    it = iter(gens)
    while True:
        g = next(it, None)
        if g is not None:
            active.append(g)
        elif not active:
            break
        for g_ in list(reversed(active)):
            try:
                next(g_)
            except StopIteration:
                active.remove(g_)


def build(T, C, L, debug=False, stop_after=None):
    st = {}
    try:
        return _build(T, C, L, debug, stop_after, st)
    except _Cut:
        return finish(st["nc"], st["S"], [])


def _build(T, C, L, debug, stop_after, st_):
    NTL = T // 128
    NTC = C // 128
    TT = T + C
    NTT = NTL + NTC
    ALPHA = float((2 * L) ** 0.25)
    QS = 1.0 / math.sqrt(96.0)
    SEG = min(1024, T)

    nc = bass.Bass("TRN2", target_bir_lowering=False)
    S = Sched(nc)
    op = S.op
    dma = S.dma
    st_["nc"] = nc
    st_["S"] = S

    def cut(tag):
        if stop_after == tag:
            raise _Cut()

    def din(name, shape, dt=F32):
        return nc.dram_tensor(name, shape, dt, kind="ExternalInput").ap()

    def dscr(name, shape, dt=F32):
        return nc.dram_tensor(name, shape, dt, kind=("ExternalOutput" if debug else "Internal")).ap()

    x_in = din("x", [T, D])
    ctx_in = din("ctx", [C, D])
    cvec = din("cvec", [2, D])
    w_ada = din("w_ada", [L, D, 6 * D])
    b_ada = din("b_ada", [L, 6 * D])
    w_in = din("w_in", [L, D, DIN])
    g_q = din("g_q", [L, DQ])
    w_uq = din("w_uq", [L, DQ, 768])
    g_kv = din("g_kv", [L, DKV])
    w_ukv = din("w_ukv", [L, DKV, 1024])
    conv_w = din("conv_w", [L, CONVW, 256])
    conv_b = din("conv_b", [L, 256])
    conv_ln_g = din("conv_ln_g", [L, 256])
    conv_ln_b = din("conv_ln_b", [L, 256])
    pool_w = din("pool_w", [L, 4, 64, 64])
    pool_scale = din("pool_scale", [L, 256])
    w_out = din("w_out", [L, D, D])
    ln1_g = din("ln1_g", [L, D])
    ln1_b = din("ln1_b", [L, D])
    w_rg = din("w_router_group", [L, D, 4])
    b_rg = din("b_router_group", [L, 4])
    w_re = din("w_router_expert", [L, D, NE])
    b_re = din("b_router_expert", [L, NE])
    w_gate = din("w_gate", [L, NE, D, DE])
    w_up = din("w_up", [L, NE, D, DE])
    w_down = din("w_down", [L, NE, DE, D])
    ln2_g = din("ln2_g", [L, D])
    ln2_b = din("ln2_b", [L, D])
    ident_d = din("ident", [128, 128])
    rope_d = din("rope_cs", [TT, 2, DR])
    pedge_d = din("pool_edge", [128, 2, 2, 8])
    pinvw_d = din("pool_invw", [128, 2])

    NB = (TT + 4 * 511 + 511) // 512
    NP = NB * 512
    I32 = mybir.dt.int32
    tri_d = din("tri", [128, 128])
    thr_d = din("thr_bc", [128, NB])
    blk_d = din("blk_bc", [128, NB])
    jp_d = din("jp", [128, 8])
    out_d = nc.dram_tensor("out", [T, D], F32, kind="ExternalOutput").ap()
    h2tok_scr = dscr("h2tok_scr", [TT, D], BF16)
    h2perm_scr = dscr("h2perm_scr", [NP, D], BF16)
    c8perm_scr = dscr("c8perm_scr", [NP, 8])
    cbT_scr = dscr("cbT_scr", [NB, 8, 512])
    yperm_scr = dscr("yperm_scr", [NP, D])
    R_h2tok = Res("h2tok_scr", multi=True)
    R_h2perm = Res("h2perm_scr", multi=True)
    R_c8perm = Res("c8perm_scr", multi=True)
    R_cbT = Res("cbT_scr", multi=True)
    R_yperm = Res("yperm_scr", multi=True)

    ada_scr = dscr("ada_scr", [L, 2, 6 * D])
    xs_mix = dscr("xs_mix", [TT, D])
    xs_out = [dscr("xs_out0", [TT, D]), dscr("xs_out1", [TT, D])]
    kT_scr = dscr("kT_scr", [H, 97, TT], BF16)
    qT_scr = dscr("qT_scr", [H, 96, TT], BF16)
    mT_scr = dscr("mT_scr", [H, TT], BF16)
    v_scr = dscr("v_scr", [H, 128, NTT, 80], BF16)
    catcp_scr = dscr("catcp_scr", [TT, 512], BF16)
    h2T_scr = dscr("h2T_scr", [8, 128, TT], BF16)
    combT_scr = dscr("combT_scr", [NE, TT])
    wg_scr = nc.dram_tensor("wg_scr", [L * NE * 128, 8 * DE], BF16, kind="Internal").ap()
    wu_scr = nc.dram_tensor("wu_scr", [L * NE * 128, 8 * DE], BF16, kind="Internal").ap()
    wd_scr = nc.dram_tensor("wd_scr", [L * NE * 128, 2 * D], BF16, kind="Internal").ap()
    R_ada = Res("ada_scr")
    R_xs_mix = Res("xs_mix", multi=True)
    R_xs_out = [Res("xs_out0", multi=True), Res("xs_out1", multi=True)]
    R_kT = Res("kT_scr", multi=True)
    R_qT = Res("qT_scr", multi=True)
    R_mT = Res("mT_scr")
    R_v = Res("v_scr", multi=True)
    R_catcp = Res("catcp_scr", multi=True)
    R_h2T = Res("h2T_scr", multi=True)
    R_combT = Res("combT_scr", multi=True)
    R_wg = Res("wg_scr")
    R_wu = Res("wu_scr")
    R_wd = Res("wd_scr")
    R_out = Res("out", multi=True)
    for R_ in [R_h2perm, R_c8perm]:
        S.ensure(R_, sw=True)
    R_h2pz = Res("h2perm_zero")
    R_c8pz = Res("c8perm_zero")
    S.ensure(R_h2pz)
    S.ensure(R_c8pz)
    for R_ in [R_h2tok, R_cbT, R_yperm]:
        S.ensure(R_)
    for R_ in [R_ada, R_xs_mix, R_xs_out[0], R_xs_out[1], R_kT, R_qT, R_mT, R_v, R_catcp, R_h2T, R_combT, R_out]:
        S.ensure(R_)
    for R_ in [R_wg, R_wu, R_wd]:
        S.ensure(R_, sw=True)

    PBK = []
    for i in range(8):
        PBK.append((nc.alloc_psum_tensor("pb%d" % i, [128, 512], F32), Res("pb%d" % i, excl=True)))

    def bfv(t):
        return t[:].bitcast(BF16)

    def palloc(name, shape, dt=F32):
        return nc.alloc_sbuf_tensor(name, shape, dt)

    def sb(name, shape, dt=F32):
        return nc.alloc_sbuf_tensor(name, shape, dt), Res(name)

    ident_f, R_idf = sb("ident_f", [128, 128])
    ident_b, R_idb = sb("ident_b", [128, 128], BF16)
    dma("sp", ident_f[:], ident_d, [], [R_idf])
    op("dve", lambda e: e.tensor_copy(out=ident_b[:], in_=ident_f[:]), [R_idf], [R_idb])
    eps_t, R_epst = sb("eps_t", [128, 2])
    op("dve", lambda e: e.memset(eps_t[:, 0:1], LN_EPS), [], [R_epst])
    op("dve", lambda e: e.memset(eps_t[:, 1:2], RMS_EPS), [R_epst], [R_epst])
    tri_sb, R_tri = sb("tri_sb", [128, 128])
    dma("sp", tri_sb[:], tri_d, [], [R_tri])
    ones_sb, R_ones = sb("ones_sb", [128, 128])
    op("dve", lambda e: e.memset(ones_sb[:], 1.0), [], [R_ones])
    thr_sb, R_thr = sb("thr_sb", [128, NB])
    blk_sb, R_blk = sb("blk_sb", [128, NB])
    jp_sb, R_jp = sb("jp_sb", [128, 8])
    dma("sp", thr_sb[:], thr_d, [], [R_thr])
    dma("sp", blk_sb[:], blk_d, [], [R_blk])
    dma("sp", jp_sb[:], jp_d, [], [R_jp])
    zer_b, R_zerb = sb("zer_b", [128, D], BF16)
    zer_f, R_zerf = sb("zer_f", [128, 8])
    op("dve", lambda e: e.memset(zer_b[:], 0.0), [], [R_zerb])
    op("dve", lambda e: e.memset(zer_f[:], 0.0), [], [R_zerf])
    goh_all, R_goh = sb("goh_all", [128, NTT, 4])
    c8_all, R_c8 = sb("c8_all", [128, NTT, 8])
    dest_f, R_destf = sb("dest_f", [128, NTT])
    dest_i, R_desti = sb("dest_i", [128, NTT], I32)
    widx_f, R_widxf = sb("widx_f", [128, NB, 8])
    widx_i, R_widxi = sb("widx_i", [128, NB * 8], I32)
    srt, R_srt = sb("srt", [128, 64])
    pedge, R_pedge = sb("pedge", [128, 2, 2, 8])
    pinvw, R_pinvw = sb("pinvw", [128, 2])
    dma("sp", pedge[:], pedge_d, [], [R_pedge])
    dma("sp", pinvw[:], pinvw_d, [], [R_pinvw])

    w_in_sb, R_win = sb("w_in_sb", [128, 8, DIN], BF16)
    w_uq_sb, R_wuq = sb("w_uq_sb", [128, 3, 768], BF16)
    w_ukv_sb, R_wukv = sb("w_ukv_sb", [128, 2, 1024], BF16)
    for R_ in (R_win, R_wuq, R_wukv):
        S.ensure(R_, sw=True)

    def load_mix_weights(l_):
        dma("pool", w_in_sb[:], w_in[l_].rearrange("(k p) n -> p k n", p=128), [], [R_win])
        dma("pool", w_uq_sb[:], w_uq[l_].rearrange("(k p) n -> p k n", p=128), [], [R_wuq])
        dma("pool", w_ukv_sb[:], w_ukv[l_].rearrange("(k p) n -> p k n", p=128), [], [R_wukv])

    load_mix_weights(0)

    for l in range(L if stop_after not in ("0", "A", "C", "B", "D") else 0):
        for e0 in range(0, NE, 8):
            for e1 in range(e0, e0 + 8):
                r0 = (l * NE + e1) * 128
                dma("pool", wg_scr[r0:r0 + 128, :].rearrange("p (k h) -> p k h", k=8), w_gate[l, e1].rearrange("(k p) h -> p k h", p=128), [], [R_wg])
                dma("pool", wu_scr[r0:r0 + 128, :].rearrange("p (k h) -> p k h", k=8), w_up[l, e1].rearrange("(k p) h -> p k h", p=128), [], [R_wu])
                dma("pool", wd_scr[r0:r0 + 128, :].rearrange("p (c d) -> p c d", c=2), w_down[l, e1].rearrange("(c p) d -> p c d", p=128), [], [R_wd])

    with ExitStack() as st0:
        mk0 = S.mark()

        def a0(name, shape, dt=F32):
            return st0.enter_context(nc.sbuf_tensor(name, shape, dt))
        cs, R_cs = a0("cs", [2, D]), Res("cs")
        csT, R_csT = a0("csT", [128, 8, 2]), Res("csT")
        bada, R_bada = a0("bada", [2, 6 * D]), Res("bada")
        adas, R_adas = a0("adas", [2, 6 * D]), Res("adas")
        wblk = Ring(a0, "wblk", [128, 8, 512], F32, 2)
        dma("sp", cs[:], cvec, [], [R_cs])
        op("act", lambda e: e.activation(out=cs[:], in_=cs[:], func=AF.Silu), [R_cs], [R_cs])
        pb, Rpb = PBK[0]
        for k in range(8):
            op("pe", lambda e: e.transpose(out=pb[:, 2 * k:2 * k + 2], in_=cs[0:2, k * 128:(k + 1) * 128],
                                           identity=ident_f[0:2, 0:2]), [R_cs, R_idf], [Rpb])
        op("dve", lambda e: e.tensor_copy(out=csT[:].rearrange("p k r -> p (k r)"), in_=pb[:, 0:16]), [Rpb], [R_csT])
        nb_i = 0
        for l in range(L):
            dma("sp", bada[:], b_ada[l].partition_broadcast(2), [], [R_bada])
            for nb in range(12):
                wb, Rwb = wblk.get()
                dma("sp", wb[:], w_ada[l, :, nb * 512:(nb + 1) * 512].rearrange("(k p) n -> p k n", p=128), [], [Rwb])
                pb, Rpb = PBK[1 + (nb_i % 2)]
                nb_i += 1
                for k in range(8):
                    op("pe", lambda e: e.matmul(pb[0:2, :], lhsT=csT[:, k, :], rhs=wb[:, k, :], start=(k == 0), stop=(k == 7)),
                       [R_csT, Rwb], [Rpb])
                op("dve", lambda e: e.tensor_tensor(out=adas[:, nb * 512:(nb + 1) * 512], in0=pb[0:2, :],
                                                    in1=bada[:, nb * 512:(nb + 1) * 512], op=ALU.add), [Rpb, R_bada], [R_adas])
            for j in (1, 4):
                op("dve", lambda e: e.tensor_scalar_add(out=adas[:, j * D:(j + 1) * D], in0=adas[:, j * D:(j + 1) * D], scalar1=1.0),
                   [R_adas], [R_adas])
            dma("sp", ada_scr[l], adas[:], [R_adas], [R_ada])
        S.barrier()
        S.release(mk0)

    if stop_after == "0":
        return finish(nc, S, [R_ada])

    def ada_vec(l, r, j):
        return ada_scr[l, r, j * D:(j + 1) * D]

    def load_bc(t, R, src1d, n, rd=()):
        dma("sp", t[:, 0:n], src1d.partition_broadcast(128), list(rd), [R])

    def x_src(l, i):
        if l == 0:
            if i < NTC:
                return ctx_in[i * 128:(i + 1) * 128, :], []
            return x_in[(i - NTC) * 128:(i - NTC + 1) * 128, :], []
        return xs_out[(l - 1) % 2][i * 128:(i + 1) * 128, :], [R_xs_out[(l - 1) % 2]]

    def layer_norm_tile(st, eng2, y, Ry, n, gbc, Rg, bbc, Rb, outt, Rout, rings):
        stt, Rst = rings["st"].get()
        mv, Rmv = rings["mv"].get()
        nch = (n + 511) // 512
        for c in range(nch):
            a, b_ = c * 512, min(n, (c + 1) * 512)
            op("dve", lambda e: e.bn_stats(out=stt[:, c * 6:(c + 1) * 6], in_=y[:, a:b_]), [Ry], [Rst])
        op("dve", lambda e: e.bn_aggr(out=mv[:, 0:2], in_=stt[:, 0:nch * 6]), [Rst], [Rmv])
        op("act", lambda e: e.activation(out=mv[:, 2:3], in_=mv[:, 1:2], func=AF.Ln, bias=eps_t[:, 0:1], scale=1.0), [Rmv], [Rmv])
        op("act", lambda e: e.activation(out=mv[:, 2:3], in_=mv[:, 2:3], func=AF.Exp, scale=-0.5), [Rmv], [Rmv])
        op("dve", lambda e: e.scalar_tensor_tensor(out=mv[:, 3:4], in0=mv[:, 0:1], scalar=-1.0, in1=mv[:, 2:3],
                                                   op0=ALU.mult, op1=ALU.mult), [Rmv], [Rmv])
        op("act", lambda e: e.activation(out=y[:, 0:n], in_=y[:, 0:n], func=AF.Identity, bias=mv[:, 3:4], scale=mv[:, 2:3]),
           [Ry, Rmv], [Ry])
        op(eng2, lambda e: e.tensor_tensor(out=y[:, 0:n], in0=y[:, 0:n], in1=gbc[:, 0:n], op=ALU.mult), [Ry, Rg], [Ry])
        op("dve", lambda e: e.tensor_tensor(out=outt[:, 0:n], in0=y[:, 0:n], in1=bbc[:, 0:n], op=ALU.add), [Ry, Rb], [Rout])

    for l in range(L):
        last = (l == L - 1)
        S.barrier()

        with ExitStack() as stAC:
            def aAC(name, shape, dt=F32):
                return stAC.enter_context(nc.sbuf_tensor("%s_L%d" % (name, l), shape, dt))
            cpT_l, R_cpl = aAC("cpT_l", [128, 4, T + 32]), Res("cpT_l")
            cpT_c, R_cpc = aAC("cpT_c", [128, 4, C + 32]), Res("cpT_c")
            for (t_, R_, n_) in ((cpT_l, R_cpl, T), (cpT_c, R_cpc, C)):
                op("pool", lambda e: e.memset(t_[:, :, 0:16], 0.0), [], [R_])
                op("pool", lambda e: e.memset(t_[:, :, 16 + n_:32 + n_], 0.0), [R_], [R_])

            with ExitStack() as stA:
                mk_stA = S.mark()
                def aA(name, shape, dt=F32):
                    return stA.enter_context(nc.sbuf_tensor("%s_L%d" % (name, l), shape, dt))
                gq_bc, R_gq = aA("gq_bc", [128, DQ]), Res("gq_bc")
                gkv_bc, R_gkv = aA("gkv_bc", [128, DKV]), Res("gkv_bc")
                load_bc(gq_bc, R_gq, g_q[l], DQ)
                load_bc(gkv_bc, R_gkv, g_kv[l], DKV)
                sc1, sh1, R_sc1, R_sh1 = [], [], [], []
                for r in range(2):
                    t = aA("sc1_%d" % r, [128, D])
                    Rr = Res("sc1_%d" % r)
                    load_bc(t, Rr, ada_vec(l, r, 1), D, [R_ada])
                    sc1.append(t)
                    R_sc1.append(Rr)
                    t = aA("sh1_%d" % r, [128, D])
                    Rr = Res("sh1_%d" % r)
                    load_bc(t, Rr, ada_vec(l, r, 0), D, [R_ada])
                    sh1.append(t)
                    R_sh1.append(Rr)
                rope_sb, R_rope = aA("rope_sb", [128, NTT, 2, DR]), Res("rope_sb")
                dma("sp", rope_sb[:], rope_d.rearrange("(i p) a d -> p i a d", p=128), [], [R_rope])
                nq_all, R_nq = aA("nq_all", [128, NTT, H]), Res("nq_all")
                kmax2, R_kmax2 = aA("kmax2", [128, H]), Res("kmax2")
                op("dve", lambda e: e.memset(kmax2[:], 0.0), [], [R_kmax2])
                op("dve", lambda e: e.memset(nq_all[:], 0.0), [], [R_nq])

                xt_r = Ring(aA, "xt", [128, D], F32, 2)
                tmp_r = Ring(aA, "tmp32", [128, D], F32, 2)
                hb_r = Ring(aA, "hb", [128, D], BF16, 2)
                hT_r = Ring(aA, "hT", [128, 8, 128], BF16, 2)
                junk_r = Ring(aA, "junk", [128, 512], F32, 2)
                stat_r = Ring(aA, "stat", [128, 8], F32, 4)
                qn_r = Ring(aA, "qn", [128, DQ], BF16, 2)
                qnT_r = Ring(aA, "qnT", [128, 3, 128], BF16, 2)
                ckvn_r = Ring(aA, "ckvn", [128, DKV], BF16, 2)
                ckvT_r = Ring(aA, "ckvT", [128, 2, 128], BF16, 2)
                qaug_r = Ring(aA, "qaug", [128, H, 96], BF16, 2)
                kaug_r = Ring(aA, "kaug", [128, H, 112], BF16, 2)
                vaug_r = Ring(aA, "vaug", [128, H, 80], BF16, 2)
                for (t_, R_) in kaug_r.bufs:
                    op("dve", lambda e: e.memset(t_[:, :, 96:112], 1.0), [], [R_])
                for (t_, R_) in vaug_r.bufs:
                    op("dve", lambda e: e.memset(t_[:, :, 64:80], 1.0), [], [R_])
                kTst_r = Ring(aA, "kTst", [97, H, 128], BF16, 2)
                qTst_r = Ring(aA, "qTst", [96, H, 128], BF16, 2)
                cptok_r = Ring(aA, "cptok", [128, 512], F32, 2)
                sig_r = Ring(aA, "sig", [128, 256], F32, 2)
                rt_r = Ring(aA, "rt", [128, H, 2, DR], F32, 2)
                krr_r = Ring(aA, "krr", [128, 2, DR], F32, 2)
                nk_r = Ring(aA, "nk", [128, 16], F32, 2)

                def rope_apply(i, src_view, Rsrc, nh, t1, t2, Rt):
                    cosb = rope_sb[:, i, 0:1, :].to_broadcast([128, nh, DR])
                    op("dve", lambda e: e.tensor_tensor(out=t1[:, 0:nh, :], in0=src_view, in1=cosb, op=ALU.mult),
                       [Rsrc, R_rope], [Rt])
                    sv = src_view.rearrange("p h (a b c) -> p h a b c", a=2, b=2)
                    t2v = t2[:, 0:nh, :].rearrange("p h (a b c) -> p h a b c", a=2, b=2)
                    sn = rope_sb[:, i, 1, :].rearrange("p (a b c) -> p a b c", a=2, b=2)
                    for b_ in range(2):
                        snb = sn[:, :, b_, :].unsqueeze(1).to_broadcast([128, nh, 2, 8])
                        op("dve", lambda e: e.tensor_tensor(out=t2v[:, :, :, b_, :], in0=sv[:, :, :, 1 - b_, :], in1=snb, op=ALU.mult),
                           [Rsrc, R_rope], [Rt])
                    op("dve", lambda e: e.tensor_tensor(out=t1[:, 0:nh, :], in0=t1[:, 0:nh, :], in1=t2[:, 0:nh, :], op=ALU.add),
                       [Rt], [Rt])

                if stop_after == "Apre":
                    return finish(nc, S, [R_win, R_wuq, R_wukv, R_gq, R_gkv, R_rope] + R_sc1 + R_sh1)
                COLS = [(0, 384), (384, 672), (672, 1184), (1184, 1440)]
                def tileA(i):
                    isctx = i < NTC
                    typ = 1 if isctx else 0
                    full = not (last and isctx)
                    g0 = i * 128
                    src, Rsrc = x_src(l, i)
                    xt, Rxt = xt_r.get()
                    dma("sp", xt[:], src, Rsrc, [Rxt])
                    tmp, Rtmp = tmp_r.get()
                    hb, Rhb = hb_r.get()
                    op("pool", lambda e: e.tensor_tensor(out=tmp[:], in0=xt[:], in1=sc1[typ][:], op=ALU.mult), [Rxt, R_sc1[typ]], [Rtmp])
                    op("dve", lambda e: e.tensor_tensor(out=hb[:], in0=tmp[:], in1=sh1[typ][:], op=ALU.add), [Rtmp, R_sh1[typ]], [Rhb])
                    yield
                    pb, Rpb = PBK[0]
                    pbv = bfv(pb)
                    for k in range(8):
                        op("pe", lambda e: e.transpose(out=pbv[:, k * 128:(k + 1) * 128], in_=hb[:, k * 128:(k + 1) * 128], identity=ident_b[:]),
                           [Rhb, R_idb], [Rpb])
                    hT, RhT = hT_r.get()
                    op("act", lambda e: e.copy(out=hT[:].rearrange("p k t -> p (k t)"), in_=pbv[:, 0:1024]), [Rpb], [RhT])
                    cut("A1")
                    G = [PBK[2], PBK[3], PBK[4], PBK[5]]
                    for gi, (c0, c1) in enumerate(COLS):
                        if not full and gi != 1:
                            continue
                        gt, Rg = G[gi]
                        for k in range(8):
                            op("pe", lambda e: e.matmul(gt[:, 0:c1 - c0], lhsT=hT[:, k, :], rhs=w_in_sb[:, k, c0:c1], start=(k == 0), stop=(k == 7)),
                               [RhT, R_win], [Rg])
                    g1t, Rg1 = G[0]
                    g2t, Rg2 = G[1]
                    g3t, Rg3 = G[2]
                    g4t, Rg4 = G[3]
                    stt, Rstt = stat_r.get()
                    junk, Rjunk = junk_r.get()
                    cut("A2")
                    op("act", lambda e: e.activation(out=junk[:, 0:DKV], in_=g2t[:, 0:DKV], func=AF.Square, accum_out=stt[:, 0:1]),
                       [Rg2], [Rjunk, Rstt])
                    op("act", lambda e: e.activation(out=stt[:, 1:2], in_=stt[:, 0:1], func=AF.Ln, bias=eps_t[:, 1:2], scale=1.0 / DKV),
                       [Rstt], [Rstt])
                    op("act", lambda e: e.activation(out=stt[:, 1:2], in_=stt[:, 1:2], func=AF.Exp, scale=-0.5), [Rstt], [Rstt])
                    ckvn, Rckvn = ckvn_r.get()
                    op("dve", lambda e: e.scalar_tensor_tensor(out=ckvn[:], in0=g2t[:, 0:DKV], scalar=stt[:, 1:2], in1=gkv_bc[:],
                                                               op0=ALU.mult, op1=ALU.mult), [Rg2, Rstt, R_gkv], [Rckvn])
                    cut("A3")
                    krr, Rkrr = krr_r.get()
                    rt, Rrt = rt_r.get()
                    rope_apply(i, g2t[:, DKV:DKV + DR].unsqueeze(1), Rg2, 1, krr[:, 0:1, :], krr[:, 1:2, :], Rkrr)
                    cut("A4")
                    if full:
                        op("act", lambda e: e.activation(out=junk[:, 0:DQ], in_=g1t[:, 0:DQ], func=AF.Square, accum_out=stt[:, 2:3]),
                           [Rg1], [Rjunk, Rstt])
                        op("act", lambda e: e.activation(out=stt[:, 3:4], in_=stt[:, 2:3], func=AF.Ln, bias=eps_t[:, 1:2], scale=1.0 / DQ),
                           [Rstt], [Rstt])
                        op("act", lambda e: e.activation(out=stt[:, 3:4], in_=stt[:, 3:4], func=AF.Exp, scale=-0.5), [Rstt], [Rstt])
                        qn, Rqn = qn_r.get()
                        op("dve", lambda e: e.scalar_tensor_tensor(out=qn[:], in0=g1t[:, 0:DQ], scalar=stt[:, 3:4], in1=gq_bc[:],
                                                                   op0=ALU.mult, op1=ALU.mult), [Rg1, Rstt, R_gq], [Rqn])
                        sig, Rsig = sig_r.get()
                        cptok, Rcptok = cptok_r.get()
                        op("act", lambda e: e.activation(out=sig[:], in_=g3t[:, 256:512], func=AF.Exp, scale=-1.0), [Rg3], [Rsig])
                        op("act", lambda e: e.activation(out=sig[:], in_=sig[:], func=AF.Ln, bias=1.0, scale=1.0), [Rsig], [Rsig])
                        op("act", lambda e: e.activation(out=sig[:], in_=sig[:], func=AF.Exp, scale=-1.0), [Rsig], [Rsig])
                        op("dve", lambda e: e.tensor_tensor(out=cptok[:, 0:256], in0=g3t[:, 0:256], in1=sig[:], op=ALU.mult),
                           [Rg3, Rsig], [Rcptok])
                        op("act", lambda e: e.copy(out=cptok[:, 256:512], in_=g4t[:, 0:256]), [Rg4, Rcptok], [Rcptok])
                        pb1, Rpb1 = PBK[1]
                        pb1v = bfv(pb1)
                        for k in range(3):
                            op("pe", lambda e: e.transpose(out=pb1v[:, k * 128:(k + 1) * 128], in_=qn[:, k * 128:(k + 1) * 128], identity=ident_b[:]),
                               [Rqn, R_idb], [Rpb1])
                        qnT, RqnT = qnT_r.get()
                        op("act", lambda e: e.copy(out=qnT[:].rearrange("p k t -> p (k t)"), in_=pb1v[:, 0:384]), [Rpb1], [RqnT])
                    cut("A5")
                    pb0, Rpb0 = PBK[0]
                    pb0v = bfv(pb0)
                    for k in range(2):
                        op("pe", lambda e: e.transpose(out=pb0v[:, k * 128:(k + 1) * 128], in_=ckvn[:, k * 128:(k + 1) * 128], identity=ident_b[:]),
                           [Rckvn, R_idb], [Rpb0])
                    ckvT, RckvT = ckvT_r.get()
                    op("dve", lambda e: e.tensor_copy(out=ckvT[:].rearrange("p k t -> p (k t)"), in_=pb0v[:, 0:256]), [Rpb0], [RckvT])
                    yield
                    if full:
                        Q = [(PBK[6], 0, 5), (PBK[7], 5, 3)]
                        for ((qt, Rq), h0, nh) in Q:
                            for k in range(3):
                                op("pe", lambda e: e.matmul(qt[:, 0:nh * 96], lhsT=qnT[:, k, :], rhs=w_uq_sb[:, k, h0 * 96:(h0 + nh) * 96],
                                                            start=(k == 0), stop=(k == 2)), [RqnT, R_wuq], [Rq])
                    cut("A6")
                    KV = [(PBK[2], 0), (PBK[4], 4)]
                    for ((kt, Rk), h0) in KV:
                        for k in range(2):
                            op("pe", lambda e: e.matmul(kt[:, 0:512], lhsT=ckvT[:, k, :], rhs=w_ukv_sb[:, k, h0 * 128:(h0 + 4) * 128],
                                                        start=(k == 0), stop=(k == 1)), [RckvT, R_wukv], [Rk])
                    if full:
                        pb5, Rpb5 = PBK[5]
                        for k in range(4):
                            op("pe", lambda e: e.transpose(out=pb5[:, k * 128:(k + 1) * 128], in_=cptok[:, k * 128:(k + 1) * 128], identity=ident_f[:]),
                               [Rcptok, R_idf], [Rpb5])
                        cpd, Rcpd, off = (cpT_c, R_cpc, g0) if isctx else (cpT_l, R_cpl, g0 - C)
                        op("act", lambda e: e.copy(out=cpd[:, :, 16 + off:16 + off + 128], in_=pb5[:].rearrange("p (k t) -> p k t", k=4)),
                           [Rpb5], [Rcpd])
                        qaug, Rqaug = qaug_r.get()
                        nk, Rnk = nk_r.get()
                        for ((qt, Rq), h0, nh) in Q:
                            qv = qt[:, 0:nh * 96].rearrange("p (h d) -> p h d", d=96)
                            op("act", lambda e: e.copy(out=qaug[:, h0:h0 + nh, 0:64], in_=qv[:, :, 0:64]), [Rq], [Rqaug])
                            rope_apply(i, qv[:, :, 64:96], Rq, nh, rt[:, h0:h0 + nh, 0, :], rt[:, h0:h0 + nh, 1, :], Rrt)
                            op("dve", lambda e: e.tensor_copy(out=qaug[:, h0:h0 + nh, 64:96], in_=rt[:, h0:h0 + nh, 0, :]), [Rrt], [Rqaug])
                            jv = junk[:, 0:nh * 96].rearrange("p (h d) -> p h d", d=96)
                            op("act", lambda e: e.activation(out=jv, in_=qv, func=AF.Square), [Rq], [Rjunk])
                            op("dve", lambda e: e.tensor_reduce(out=nq_all[:, i, h0:h0 + nh], in_=jv, axis=AX.X, op=ALU.add), [Rjunk], [R_nq])
                    else:
                        nk, Rnk = nk_r.get()
                    cut("A7")
                    kaug, Rkaug = kaug_r.get()
                    vaug, Rvaug = vaug_r.get()
                    for ((kt, Rk), h0) in KV:
                        kv = kt[:, 0:512].rearrange("p (h d) -> p h d", d=128)
                        cut("KV0")
                        op("act", lambda e: e.copy(out=kaug[:, h0:h0 + 4, 0:64], in_=kv[:, :, 0:64]), [Rk], [Rkaug])
                        cut("KV1")
                        op("dve", lambda e: e.tensor_copy(out=vaug[:, h0:h0 + 4, 0:64], in_=kv[:, :, 64:128]), [Rk], [Rvaug])
                        cut("KV2")
                        jv = junk[:, 0:256].rearrange("p (h d) -> p h d", d=64)
                        op("act", lambda e: e.activation(out=jv, in_=kv[:, :, 0:64], func=AF.Square), [Rk], [Rjunk])
                        cut("KV3")
                        op("dve", lambda e: e.tensor_reduce(out=nk[:, h0:h0 + 4], in_=jv, axis=AX.X, op=ALU.add), [Rjunk], [Rnk])
                        cut("KV4")
                    cut("K1")
                    op("dve", lambda e: e.tensor_copy(out=kaug[:, :, 64:96], in_=krr[:, 0:1, :].to_broadcast([128, H, DR])), [Rkrr], [Rkaug])
                    cut("K2")
                    op("dve", lambda e: e.tensor_tensor(out=krr[:, 1, :], in0=krr[:, 0, :], in1=krr[:, 0, :], op=ALU.mult), [Rkrr], [Rkrr])
                    op("dve", lambda e: e.tensor_reduce(out=nk[:, 8:9], in_=krr[:, 1, :], axis=AX.X, op=ALU.add), [Rkrr], [Rnk])
                    cut("K3")
                    op("dve", lambda e: e.tensor_scalar(out=nk[:, 0:8], in0=nk[:, 0:8], scalar1=nk[:, 8:9], scalar2=None, op0=ALU.add), [Rnk], [Rnk])
                    cut("K4")
                    op("dve", lambda e: e.tensor_tensor(out=kmax2[:], in0=kmax2[:], in1=nk[:, 0:8], op=ALU.max), [Rnk, R_kmax2], [R_kmax2])
                    cut("A7b")
                    yield
                    if full:
                        pb0, Rpb0 = PBK[0]
                        pb0v = bfv(pb0)
                        for h in range(H):
                            op("pe", lambda e: e.transpose(out=pb0v[0:96, h * 128:(h + 1) * 128], in_=qaug[:, h, :], identity=ident_b[:]),
                               [Rqaug, R_idb], [Rpb0])
                        qTst, RqTst = qTst_r.get()
                        op("act", lambda e: e.copy(out=qTst[:].rearrange("p h t -> p (h t)"), in_=pb0v[0:96, 0:1024]), [Rpb0], [RqTst])
                        dma("sp", qT_scr[:, :, g0:g0 + 128].rearrange("h d t -> d h t"), qTst[:], [RqTst], [R_qT])
                    pb1, Rpb1 = PBK[1]
                    pb1v = bfv(pb1)
                    for h in range(H):
                        op("pe", lambda e: e.transpose(out=pb1v[0:97, h * 128:(h + 1) * 128], in_=kaug[:, h, 0:97], identity=ident_b[:]),
                           [Rkaug, R_idb], [Rpb1])
                    kTst, RkTst = kTst_r.get()
                    op("dve", lambda e: e.tensor_copy(out=kTst[:].rearrange("p h t -> p (h t)"), in_=pb1v[0:97, 0:1024]), [Rpb1], [RkTst])
                    dma("sp", kT_scr[:, :, g0:g0 + 128].rearrange("h d t -> d h t"), kTst[:], [RkTst], [R_kT])
                    dma("sp", v_scr[:, :, i, :].rearrange("h p d -> p h d"), vaug[:], [Rvaug], [R_v])
                    cut("AT%d" % i)

                run_skewed([tileA(i) for i in range(NTT)])
                cut("A8")
                pb, Rpb = PBK[0]
                op("pe", lambda e: e.transpose(out=pb[0:8, 0:128], in_=kmax2[:, 0:8], identity=ident_f[:]), [R_kmax2, R_idf], [Rpb])
                km, Rkm = aA("km", [8, 16]), Res("km")
                op("dve", lambda e: e.tensor_reduce(out=km[:, 0:1], in_=pb[0:8, 0:128], axis=AX.X, op=ALU.max), [Rpb], [Rkm])
                op("act", lambda e: e.activation(out=km[:, 1:2], in_=km[:, 0:1], func=AF.Sqrt, scale=1.0404), [Rkm], [Rkm])
                op("dve", lambda e: e.tensor_scalar(out=km[:, 8:16], in0=ident_f[0:8, 0:8], scalar1=km[:, 1:2], scalar2=-1.0,
                                                    op0=ALU.mult, op1=ALU.mult), [Rkm, R_idf], [Rkm])
                ones8, Rones8 = aA("ones8", [8, 128]), Res("ones8")
                op("dve", lambda e: e.memset(ones8[:], 1.0), [], [Rones8])
                pb, Rpb = PBK[1]
                op("pe", lambda e: e.matmul(pb[:, 0:8], lhsT=ones8[:], rhs=km[:, 8:16], start=True, stop=True), [Rones8, Rkm], [Rpb])
                kmbc, Rkmbc = aA("kmbc", [128, 8]), Res("kmbc")
                op("dve", lambda e: e.tensor_copy(out=kmbc[:], in_=pb[:, 0:8]), [Rpb], [Rkmbc])
                op("act", lambda e: e.activation(out=nq_all[:], in_=nq_all[:], func=AF.Sqrt), [R_nq], [R_nq])
                op("dve", lambda e: e.tensor_tensor(out=nq_all[:], in0=nq_all[:], in1=kmbc[:].unsqueeze(1).to_broadcast([128, NTT, H]), op=ALU.mult),
                   [R_nq, Rkmbc], [R_nq])
                mT_sb, RmT = aA("mT_sb", [8, TT], BF16), Res("mT_sb")
                for i0 in range(0, NTT, 4):
                    pb, Rpb = PBK[(i0 // 4) % 2]
                    ni = min(4, NTT - i0)
                    for j in range(ni):
                        op("pe", lambda e: e.transpose(out=pb[0:8, j * 128:(j + 1) * 128], in_=nq_all[:, i0 + j, :], identity=ident_f[:]),
                           [R_nq, R_idf], [Rpb])
                    op("dve", lambda e: e.tensor_copy(out=mT_sb[:, i0 * 128:(i0 + ni) * 128], in_=pb[0:8, 0:ni * 128]), [Rpb], [RmT])
                dma("sp", mT_scr, mT_sb[:], [RmT], [R_mT])
                S.barrier()
                S.release(mk_stA)
            if stop_after == "A":
                return finish(nc, S, [R_kT, R_qT, R_mT, R_v])

            with ExitStack() as stC:
                mk_stC = S.mark()
                def aC(name, shape, dt=F32):
                    return stC.enter_context(nc.sbuf_tensor("%s_L%d" % (name, l), shape, dt))
                cwr, Rcwr = aC("cwr", [CONVW, 256]), Res("cwr")
                cw_sb, Rcw = aC("cw_sb", [128, 2, CONVW]), Res("cw_sb")
                dma("sp", cwr[:], conv_w[l], [], [Rcwr])
                pb, Rpb = PBK[0]
                for k in range(2):
                    op("pe", lambda e: e.transpose(out=pb[:, k * 32:k * 32 + CONVW], in_=cwr[0:CONVW, k * 128:(k + 1) * 128],
                                                   identity=ident_f[0:CONVW, 0:CONVW]), [Rcwr, R_idf], [Rpb])
                op("dve", lambda e: e.tensor_copy(out=cw_sb[:], in_=pb[:, 0:64].rearrange("p (k j) -> p k j", k=2)[:, :, 0:CONVW]), [Rpb], [Rcw])
                convb_bc, Rcb = aC("convb_bc", [128, 256]), Res("convb_bc")
                clng_bc, Rclg = aC("clng_bc", [128, 256]), Res("clng_bc")
                clnb_bc, Rclb = aC("clnb_bc", [128, 256]), Res("clnb_bc")
                psc_bc, Rpsc = aC("psc_bc", [128, 256]), Res("psc_bc")
                load_bc(convb_bc, Rcb, conv_b[l], 256)
                load_bc(clng_bc, Rclg, conv_ln_g[l], 256)
                load_bc(clnb_bc, Rclb, conv_ln_b[l], 256)
                load_bc(psc_bc, Rpsc, pool_scale[l], 256)
                poolw_sb, Rpw = aC("poolw_sb", [128, 2, 128], BF16), Res("poolw_sb")
                op("dve", lambda e: e.memset(poolw_sb[:], 0.0), [], [Rpw])
                for ph in range(2):
                    dma("pool", poolw_sb[ph * 64:(ph + 1) * 64, :, ph * 64:(ph + 1) * 64],
                        pool_w[l].rearrange("(k two) i o -> two i k o", two=2)[ph], [], [Rpw])
                acc_t = aC("acc", [128, 2, SEG])
                R_acc = [Res("acc0"), Res("acc1")]
                P2, RP2 = aC("P2", [128, 2, SEG + 16]), Res("P2")
                P4, RP4 = aC("P4", [128, 2, SEG + 16]), Res("P4")
                P8, RP8 = aC("P8", [128, SEG + 16]), Res("P8")
                P16, RP16 = aC("P16", [128, SEG + 16]), Res("P16")
                mixed, Rmixed = aC("mixed", [128, 2, SEG], BF16), Res("mixed")
                etmp, Retmp = aC("etmp", [128, 2, 8]), Res("etmp")
                ctmp_r = Ring(aC, "ctmp", [128, SEG], F32, 3)
                cv_r = Ring(aC, "cv", [128, 256], F32, 2)
                sgc_r = Ring(aC, "sgc", [128, 256], F32, 2)
                catcp_r = Ring(aC, "catcp", [128, 512], BF16, 2)
                lnr = {"st": Ring(aC, "cst", [128, 12], F32, 2), "mv": Ring(aC, "cmv", [128, 4], F32, 2)}
                cut("C1")
                seqs = [(cpT_l, R_cpl, T, C)]
                if not last:
                    seqs.append((cpT_c, R_cpc, C, 0))
                tile_ctr = 0
                for (buf, Rbuf, n, goff) in seqs:
                    for s0 in range(0, n, SEG):
                        seg = min(SEG, n - s0)
                        b0 = 16 + s0
                        for j in range(CONVW):
                            if j == 0:
                                op("dve", lambda e: e.tensor_scalar(out=acc_t[:, 0, 0:seg], in0=buf[:, 0, b0 - 15:b0 - 15 + seg], scalar1=cw_sb[:, 0, 0:1],
                                                                    scalar2=None, op0=ALU.mult), [Rbuf, Rcw], [R_acc[0]])
                                op("act", lambda e: e.activation(out=acc_t[:, 1, 0:seg], in_=buf[:, 1, b0 - 15:b0 - 15 + seg], func=AF.Copy,
                                                                 scale=cw_sb[:, 1, 0:1]), [Rbuf, Rcw], [R_acc[1]])
                                continue
                            op("dve", lambda e: e.scalar_tensor_tensor(out=acc_t[:, 0, 0:seg], in0=buf[:, 0, b0 - 15 + j:b0 - 15 + j + seg],
                                                                       scalar=cw_sb[:, 0, j:j + 1], in1=acc_t[:, 0, 0:seg],
                                                                       op0=ALU.mult, op1=ALU.add), [Rbuf, Rcw, R_acc[0]], [R_acc[0]])
                            ct, Rct = ctmp_r.get()
                            op("act", lambda e: e.activation(out=ct[:, 0:seg], in_=buf[:, 1, b0 - 15 + j:b0 - 15 + j + seg], func=AF.Copy,
                                                             scale=cw_sb[:, 1, j:j + 1]), [Rbuf, Rcw], [Rct])
                            op("pool", lambda e: e.tensor_tensor(out=acc_t[:, 1, 0:seg], in0=acc_t[:, 1, 0:seg], in1=ct[:, 0:seg], op=ALU.add),
                               [Rct, R_acc[1]], [R_acc[1]])
                        cut("C2")
                        n2 = seg + 16
                        op("dve", lambda e: e.tensor_tensor(out=P2[:, :, 0:n2], in0=buf[:, 2:4, b0 - 9:b0 - 9 + n2], in1=buf[:, 2:4, b0 - 8:b0 - 8 + n2],
                                                            op=ALU.add), [Rbuf], [RP2])
                        op("dve", lambda e: e.tensor_tensor(out=P4[:, :, 2:n2 - 2], in0=P2[:, :, 1:n2 - 3], in1=P2[:, :, 3:n2 - 1], op=ALU.add),
                           [RP2], [RP4])
                        op("dve", lambda e: e.tensor_tensor(out=P8[:, 4:n2 - 4], in0=P4[:, 1, 2:n2 - 6], in1=P4[:, 1, 6:n2 - 2], op=ALU.add),
                           [RP4], [RP8])
                        op("dve", lambda e: e.tensor_tensor(out=P16[:, 8:n2 - 8], in0=P8[:, 4:n2 - 12], in1=P8[:, 12:n2 - 4], op=ALU.add),
                           [RP8], [RP16])
                        srcs = {(0, 0): (P2[0:64, 0, 8:8 + seg], RP2), (1, 0): (P4[64:128, 0, 8:8 + seg], RP4),
                                (0, 1): (P8[0:64, 8:8 + seg], RP8), (1, 1): (P16[64:128, 8:8 + seg], RP16)}
                        for (ph, k), (sap, Rs) in srcs.items():
                            ps = slice(ph * 64, ph * 64 + 64)
                            op("dve", lambda e: e.scalar_tensor_tensor(out=mixed[ps, k, 0:seg], in0=sap, scalar=pinvw[ps, k:k + 1],
                                                                       in1=buf[ps, 2 + k, b0:b0 + seg], op0=ALU.mult, op1=ALU.subtract),
                               [Rs, R_pinvw, Rbuf], [Rmixed])
                            for (side, cond, c0) in ((0, s0 == 0, 0), (1, s0 + seg == n, seg - 8)):
                                if not cond:
                                    continue
                                sap8 = sap[:, c0:c0 + 8]
                                op("dve", lambda e: e.tensor_tensor(out=etmp[ps, k, :], in0=sap8, in1=pedge[ps, k, side, :], op=ALU.mult),
                                   [Rs, R_pedge], [Retmp])
                                op("dve", lambda e: e.tensor_tensor(out=mixed[ps, k, c0:c0 + 8], in0=etmp[ps, k, :], in1=buf[ps, 2 + k, b0 + c0:b0 + c0 + 8],
                                                                    op=ALU.subtract), [Retmp, Rbuf], [Rmixed])
                        cut("C3")
                        for j in range(seg // 128):
                            g0 = goff + s0 + j * 128
                            pb, Rpb = PBK[tile_ctr % 2]
                            pb2, Rpb2 = PBK[2 + tile_ctr % 2]
                            tile_ctr += 1
                            for k in range(2):
                                op("pe", lambda e: e.transpose(out=pb[:, k * 128:(k + 1) * 128], in_=acc_t[:, k, j * 128:(j + 1) * 128], identity=ident_f[:]),
                                   [R_acc[k], R_idf], [Rpb])
                            cv, Rcv = cv_r.get()
                            op("dve", lambda e: e.tensor_tensor(out=cv[:], in0=pb[:, 0:256], in1=convb_bc[:], op=ALU.add), [Rpb, Rcb], [Rcv])
                            layer_norm_tile(None, "pool", cv, Rcv, 256, clng_bc, Rclg, clnb_bc, Rclb, cv, Rcv, lnr)
                            catcp, Rcatcp = catcp_r.get()
                            sgc, Rsgc = sgc_r.get()
                            op("act", lambda e: e.activation(out=sgc[:], in_=cv[:], func=AF.Exp, scale=-1.0), [Rcv], [Rsgc])
                            op("act", lambda e: e.activation(out=sgc[:], in_=sgc[:], func=AF.Ln, bias=1.0, scale=1.0), [Rsgc], [Rsgc])
                            op("act", lambda e: e.activation(out=sgc[:], in_=sgc[:], func=AF.Exp, scale=-1.0), [Rsgc], [Rsgc])
                            op("dve", lambda e: e.tensor_tensor(out=catcp[:, 0:256], in0=cv[:], in1=sgc[:], op=ALU.mult), [Rcv, Rsgc], [Rcatcp])
                            cut("C3b")
                            for k in range(2):
                                op("pe", lambda e: e.matmul(pb2[:, k * 128:(k + 1) * 128], lhsT=mixed[:, k, j * 128:(j + 1) * 128],
                                                            rhs=poolw_sb[:, k, :], start=True, stop=True), [Rmixed, Rpw], [Rpb2])
                            cut("C3c")
                            op("dve", lambda e: e.tensor_tensor(out=catcp[:, 256:512], in0=pb2[:, 0:256], in1=psc_bc[:], op=ALU.mult),
                               [Rpb2, Rpsc, Rcatcp], [Rcatcp])
                            cut("C3d")
                            dma("sp", catcp_scr[g0:g0 + 128, :], catcp[:], [Rcatcp], [R_catcp])
                            cut("C4")
                S.barrier()
                S.release(mk_stC)
        if stop_after == "C":
            return finish(nc, S, [R_catcp])

        with ExitStack() as stBD:
            def aBD(name, shape, dt=F32):
                return stBD.enter_context(nc.sbuf_tensor("%s_L%d" % (name, l), shape, dt))
            attn_sb = aBD("attn_sb", [128, NTT, 512], BF16)
            R_attn = [Res("attn%d" % i) for i in range(NTT)]
            with ExitStack() as stB:
                mk_stB = S.mark()
                def aB(name, shape, dt=F32):
                    return stB.enter_context(nc.sbuf_tensor("%s_L%d" % (name, l), shape, dt))
                NJ = NP // 128
                dma("act", h2perm_scr.rearrange("(j p) d -> p j d", p=128), zer_b[:].unsqueeze(1).to_broadcast([128, NJ, D]), [R_zerb], [R_h2pz])
                dma("act", c8perm_scr.rearrange("(j p) e -> p j e", p=128), zer_f[:].unsqueeze(1).to_broadcast([128, NJ, 8]), [R_zerf], [R_c8pz])
                KT_r = Ring(aB, "KT", [97, TT], BF16, 2)
                V_r = Ring(aB, "V", [128, NTT, 80], BF16, 2)
                qT_r = Ring(aB, "qT", [97, 512], BF16, 3)
                PT_r = Ring(aB, "PT", [128, 512], BF16, 4)
                oT_r = Ring(aB, "oT", [65, 512], F32, 2)
                rec_r = Ring(aB, "rec", [128, 4], F32, 2)
                blocks = [(C + b * 512, 512, list(range(NTT))) for b in range(T // 512)]
                if not last:
                    blocks.append((0, C, list(range(NTC))))
                bi = 0
                si = 0
                for h in range(H):
                    KT, RKT = KT_r.get()
                    V, RV = V_r.get()
                    dma("sp", KT[:], kT_scr[h], [R_kT], [RKT])
                    dma("sp", V[:], v_scr[h], [R_v], [RV])
                    for (g0, n, chunks) in blocks:
                        qT, RqT = qT_r.get()
                        dma("sp", qT[0:96, 0:n], qT_scr[h, :, g0:g0 + n], [R_qT], [RqT])
                        dma("sp", qT[96:97, 0:n], mT_scr[h:h + 1, g0:g0 + n], [R_mT], [RqT])
                        pO, RpO = PBK[bi % 2]
                        LOOK = 2
                        pend = []

                        def issue_s(c):
                            nonlocal si
                            pS_, RpS_ = PBK[2 + si % 4]
                            si += 1
                            op("pe", lambda e: e.matmul(pS_[:, 0:n], lhsT=KT[:, c * 128:(c + 1) * 128], rhs=qT[:, 0:n], start=True, stop=True),
                               [RKT, RqT], [RpS_])
                            pend.append((pS_, RpS_))
                        for c in chunks[:LOOK]:
                            issue_s(c)
                        for ci, c in enumerate(chunks):
                            if ci + LOOK < len(chunks):
                                issue_s(chunks[ci + LOOK])
                            pS, RpS = pend.pop(0)
                            PT, RPT = PT_r.get()
                            op("act", lambda e: e.activation(out=PT[:, 0:n], in_=pS[:, 0:n], func=AF.Exp, scale=QS), [RpS], [RPT])
                            op("pe", lambda e: e.matmul(pO[0:65, 0:n], lhsT=V[:, c, 0:65], rhs=PT[:, 0:n], start=(ci == 0), stop=(ci == len(chunks) - 1)),
                               [RV, RPT], [RpO])
                        oT, RoT = oT_r.get()
                        op("dve", lambda e: e.tensor_copy(out=oT[:, 0:n], in_=pO[0:65, 0:n]), [RpO], [RoT])
                        pb, Rpb = PBK[6 + bi % 2]
                        bi += 1
                        nj = n // 128
                        for j in range(nj):
                            op("pe", lambda e: e.transpose(out=pb[:, j * 65:(j + 1) * 65], in_=oT[0:65, j * 128:(j + 1) * 128], identity=ident_f[0:65, 0:65]),
                               [RoT, R_idf], [Rpb])
                        pv = pb[:, 0:nj * 65].rearrange("p (j d) -> p j d", d=65)
                        rec, Rrec = rec_r.get()
                        op("dve", lambda e: e.reciprocal(out=rec[:, 0:nj], in_=pv[:, :, 64]), [Rpb], [Rrec])
                        i0 = g0 // 128
                        Rs_ = R_attn[i0:i0 + nj]
                        op("dve", lambda e: e.tensor_tensor(out=attn_sb[:, i0:i0 + nj, h * 64:(h + 1) * 64], in0=pv[:, :, 0:64],
                                                            in1=rec[:, 0:nj].unsqueeze(2).to_broadcast([128, nj, 64]), op=ALU.mult),
                           [Rpb, Rrec] + Rs_, Rs_)
                S.barrier()
                S.release(mk_stB)
            if stop_after == "B":
                dbg = nc.dram_tensor("attn_dbg", [128, NTT, 512], BF16, kind="ExternalOutput").ap()
                Rd = Res("attn_dbg")
                dma("sp", dbg, attn_sb[:], R_attn, [Rd])
                return finish(nc, S, [Rd])

            with ExitStack() as stD:
                mk_stD = S.mark()
                def aD(name, shape, dt=F32):
                    return stD.enter_context(nc.sbuf_tensor("%s_L%d" % (name, l), shape, dt))
                w_out_sb, R_wout = aD("w_out_sb", [128, 8, D], BF16), Res("w_out_sb")
                dma("pool", w_out_sb[:], w_out[l].rearrange("(k p) n -> p k n", p=128), [], [R_wout])
                wr_sb, R_wr = aD("wr_sb", [128, 8, 36]), Res("wr_sb")
                dma("sp", wr_sb[:, :, 0:4], w_rg[l].rearrange("(k p) n -> p k n", p=128), [], [R_wr])
                dma("sp", wr_sb[:, :, 4:36], w_re[l].rearrange("(k p) n -> p k n", p=128), [], [R_wr])
                br_bc, R_br = aD("br_bc", [128, 36]), Res("br_bc")
                dma("sp", br_bc[:, 0:4], b_rg[l].partition_broadcast(128), [], [R_br])
                dma("sp", br_bc[:, 4:36], b_re[l].partition_broadcast(128), [], [R_br])
                bcs = {}
                for (nm, j) in (("g1", 2), ("sh2", 3), ("sc2", 4)):
                    for r in range(2):
                        if r == 1 and last:
                            continue
                        t = aD("%s_%d" % (nm, r), [128, D])
                        Rr = Res("%s_%d" % (nm, r))
                        load_bc(t, Rr, ada_vec(l, r, j), D, [R_ada])
                        bcs[(nm, r)] = (t, Rr)
                ln1g_bc, R_l1g = aD("ln1g_bc", [128, D]), Res("ln1g_bc")
                ln1b_bc, R_l1b = aD("ln1b_bc", [128, D]), Res("ln1b_bc")
                load_bc(ln1g_bc, R_l1g, ln1_g[l], D)
                load_bc(ln1b_bc, R_l1b, ln1_b[l], D)
                catcp_r = Ring(aD, "catcpD", [128, 512], BF16, 2)
                catT_r = Ring(aD, "catT", [128, 8, 128], BF16, 2)
                xt_r = Ring(aD, "xtD", [128, D], F32, 3)
                y_r = Ring(aD, "yD", [128, D], F32, 2)
                x1_r = Ring(aD, "x1D", [128, D], F32, 2)
                h2_r = Ring(aD, "h2D", [128, D], F32, 2)
                h2Tf_r = Ring(aD, "h2Tf", [128, 8, 128], F32, 2)
                h2Tb_r = Ring(aD, "h2b", [128, D], BF16, 2)
                lg_r = Ring(aD, "lg", [128, 36], F32, 2)
                rs_r = Ring(aD, "rs", [128, 16], F32, 2)
                oh_r = Ring(aD, "oh", [128, 3, 32], F32, 2)
                comb_r = Ring(aD, "comb", [128, 32], F32, 2)
                combT_r = Ring(aD, "combT", [32, 128], F32, 2)
                lnr = {"st": Ring(aD, "dst", [128, 12], F32, 2), "mv": Ring(aD, "dmv", [128, 4], F32, 2)}
                cut("D1")
                def tileD(i, tcnt):
                    isctx = i < NTC
                    typ = 1 if isctx else 0
                    g0 = i * 128
                    catcp, Rcatcp = catcp_r.get()
                    dma("sp", catcp[:], catcp_scr[g0:g0 + 128, :], [R_catcp], [Rcatcp])
                    src, Rsrc = x_src(l, i)
                    xt, Rxt = xt_r.get()
                    dma("sp", xt[:], src, Rsrc, [Rxt])
                    yield
                    pb, Rpb = PBK[tcnt % 2]
                    pbv = bfv(pb)
                    for k in range(4):
                        op("pe", lambda e: e.transpose(out=pbv[:, k * 128:(k + 1) * 128], in_=attn_sb[:, i, k * 128:(k + 1) * 128], identity=ident_b[:]),
                           [R_attn[i], R_idb], [Rpb])
                    for k in range(4):
                        op("pe", lambda e: e.transpose(out=pbv[:, (4 + k) * 128:(5 + k) * 128], in_=catcp[:, k * 128:(k + 1) * 128], identity=ident_b[:]),
                           [Rcatcp, R_idb], [Rpb])
                    catT, RcatT = catT_r.get()
                    op("act", lambda e: e.copy(out=catT[:].rearrange("p k t -> p (k t)"), in_=pbv[:, 0:1024]), [Rpb], [RcatT])
                    cut("D2")
                    M = [PBK[2 + 2 * (tcnt % 2)], PBK[3 + 2 * (tcnt % 2)]]
                    for hf in range(2):
                        mt, Rm = M[hf]
                        for k in range(8):
                            op("pe", lambda e: e.matmul(mt[:, :], lhsT=catT[:, k, :], rhs=w_out_sb[:, k, hf * 512:(hf + 1) * 512], start=(k == 0), stop=(k == 7)),
                               [RcatT, R_wout], [Rm])
                    cut("D3")
                    yield
                    y, Ry = y_r.get()
                    g1t, Rg1 = bcs[("g1", typ)]
                    for hf in range(2):
                        mt, Rm = M[hf]
                        op("dve", lambda e: e.tensor_tensor(out=y[:, hf * 512:(hf + 1) * 512], in0=mt[:, :], in1=g1t[:, hf * 512:(hf + 1) * 512], op=ALU.mult),
                           [Rm, Rg1], [Ry])
                    op("dve", lambda e: e.scalar_tensor_tensor(out=y[:], in0=xt[:], scalar=ALPHA, in1=y[:], op0=ALU.mult, op1=ALU.add),
                       [Rxt, Ry], [Ry])
                    x1, Rx1 = x1_r.get()
                    layer_norm_tile(None, "pool", y, Ry, D, ln1g_bc, R_l1g, ln1b_bc, R_l1b, x1, Rx1, lnr)
                    dma("sp", xs_mix[g0:g0 + 128, :], x1[:], [Rx1], [R_xs_mix])
                    cut("D4")
                    h2, Rh2 = h2_r.get()
                    sc2t, Rsc2 = bcs[("sc2", typ)]
                    sh2t, Rsh2 = bcs[("sh2", typ)]
                    op("pool", lambda e: e.tensor_tensor(out=h2[:], in0=x1[:], in1=sc2t[:], op=ALU.mult), [Rx1, Rsc2], [Rh2])
                    op("dve", lambda e: e.tensor_tensor(out=h2[:], in0=h2[:], in1=sh2t[:], op=ALU.add), [Rh2, Rsh2], [Rh2])
                    yield
                    T6, RT6 = PBK[6]
                    T7, RT7 = PBK[7]
                    for k in range(8):
                        tb, Rtb = (T6, RT6) if k < 4 else (T7, RT7)
                        op("pe", lambda e: e.transpose(out=tb[:, (k % 4) * 128:(k % 4 + 1) * 128], in_=h2[:, k * 128:(k + 1) * 128], identity=ident_f[:]),
                           [Rh2, R_idf], [Rtb])
                    h2Tf, Rh2Tf = h2Tf_r.get()
                    h2b, Rh2b = h2Tb_r.get()
                    op("pool", lambda e: e.tensor_copy(out=h2b[:], in_=h2[:]), [Rh2], [Rh2b])
                    dma("sp", h2tok_scr[g0:g0 + 128, :], h2b[:], [Rh2b], [R_h2tok])
                    for hf, (tb, Rtb) in enumerate(((T6, RT6), (T7, RT7))):
                        op("act", lambda e: e.copy(out=h2Tf[:, hf * 4:hf * 4 + 4, :].rearrange("p k t -> p (k t)"), in_=tb[:, :]), [Rtb], [Rh2Tf])
                    cut("D5")
                    pr, Rpr = PBK[tcnt % 2]
                    for k in range(8):
                        op("pe", lambda e: e.matmul(pr[:, 0:36], lhsT=h2Tf[:, k, :], rhs=wr_sb[:, k, :], start=(k == 0), stop=(k == 7)),
                           [Rh2Tf, R_wr], [Rpr])
                    lg, Rlg = lg_r.get()
                    rs, Rrs = rs_r.get()
                    oh, Roh = oh_r.get()
                    op("dve", lambda e: e.tensor_tensor(out=lg[:], in0=pr[:, 0:36], in1=br_bc[:], op=ALU.add), [Rpr, R_br], [Rlg])
                    cut("D6")
                    yield
                    op("dve", lambda e: e.tensor_reduce(out=rs[:, 0:1], in_=lg[:, 0:4], axis=AX.X, op=ALU.max), [Rlg], [Rrs])
                    op("dve", lambda e: e.tensor_scalar(out=rs[:, 8:12], in0=lg[:, 0:4], scalar1=rs[:, 0:1], scalar2=None, op0=ALU.is_equal), [Rlg, Rrs], [Rrs])
                    op("dve", lambda e: e.tensor_copy(out=goh_all[:, i, :], in_=rs[:, 8:12]), [Rrs], [R_goh])
                    op("dve", lambda e: e.tensor_scalar(out=rs[:, 1:2], in0=rs[:, 0:1], scalar1=-1.0, scalar2=None, op0=ALU.mult), [Rrs], [Rrs])
                    op("act", lambda e: e.activation(out=rs[:, 12:16], in_=lg[:, 0:4], func=AF.Exp, bias=rs[:, 1:2], scale=1.0, accum_out=rs[:, 2:3]),
                       [Rlg, Rrs], [Rrs])
                    op("dve", lambda e: e.reciprocal(out=rs[:, 2:3], in_=rs[:, 2:3]), [Rrs], [Rrs])
                    op("dve", lambda e: e.tensor_scalar(out=rs[:, 8:12], in0=rs[:, 8:12], scalar1=-1.0, scalar2=-NEG, op0=ALU.add, op1=ALU.mult), [Rrs], [Rrs])
                    elm = oh[:, 0, :]
                    op("dve", lambda e: e.tensor_tensor(out=elm.rearrange("p (g x) -> p g x", g=4), in0=lg[:, 4:36].rearrange("p (g x) -> p g x", g=4),
                                                        in1=rs[:, 8:12].unsqueeze(2).to_broadcast([128, 4, 8]), op=ALU.add), [Rlg, Rrs], [Roh])
                    op("dve", lambda e: e.tensor_reduce(out=rs[:, 3:4], in_=elm, axis=AX.X, op=ALU.max), [Roh], [Rrs])
                    op("dve", lambda e: e.tensor_scalar(out=oh[:, 1, :], in0=elm, scalar1=rs[:, 3:4], scalar2=None, op0=ALU.is_equal), [Roh, Rrs], [Roh])
                    op("dve", lambda e: e.scalar_tensor_tensor(out=elm, in0=oh[:, 1, :], scalar=NEG, in1=elm, op0=ALU.mult, op1=ALU.add), [Roh], [Roh])
                    op("dve", lambda e: e.tensor_reduce(out=rs[:, 4:5], in_=elm, axis=AX.X, op=ALU.max), [Roh], [Rrs])
                    op("dve", lambda e: e.tensor_scalar(out=oh[:, 2, :], in0=elm, scalar1=rs[:, 4:5], scalar2=None, op0=ALU.is_equal), [Roh, Rrs], [Roh])
                    op("dve", lambda e: e.tensor_tensor(out=rs[:, 5:6], in0=rs[:, 4:5], in1=rs[:, 3:4], op=ALU.subtract), [Rrs], [Rrs])
                    op("act", lambda e: e.activation(out=rs[:, 5:6], in_=rs[:, 5:6], func=AF.Exp), [Rrs], [Rrs])
                    op("dve", lambda e: e.tensor_scalar(out=rs[:, 6:7], in0=rs[:, 5:6], scalar1=1.0, scalar2=None, op0=ALU.add), [Rrs], [Rrs])
                    op("dve", lambda e: e.reciprocal(out=rs[:, 6:7], in_=rs[:, 6:7]), [Rrs], [Rrs])
                    op("dve", lambda e: e.tensor_tensor(out=rs[:, 6:7], in0=rs[:, 6:7], in1=rs[:, 2:3], op=ALU.mult), [Rrs], [Rrs])
                    op("dve", lambda e: e.tensor_tensor(out=rs[:, 7:8], in0=rs[:, 6:7], in1=rs[:, 5:6], op=ALU.mult), [Rrs], [Rrs])
                    cut("D7")
                    comb, Rcomb = comb_r.get()
                    op("dve", lambda e: e.tensor_scalar(out=comb[:], in0=oh[:, 1, :], scalar1=rs[:, 6:7], scalar2=None, op0=ALU.mult), [Roh, Rrs], [Rcomb])
                    op("dve", lambda e: e.scalar_tensor_tensor(out=comb[:], in0=oh[:, 2, :], scalar=rs[:, 7:8], in1=comb[:], op0=ALU.mult, op1=ALU.add),
                       [Roh, Rrs, Rcomb], [Rcomb])
                    cut("D8")
                    op("dve", lambda e: e.tensor_reduce(out=c8_all[:, i, :], in_=comb[:].rearrange("p (g j) -> p j g", g=4), axis=AX.X, op=ALU.add),
                       [Rcomb], [R_c8])

                op("dve", lambda e: e.memset(goh_all[:], 0.0), [], [R_goh])
                tilesD = [i for i in range(NTT) if not (i < NTC and last)]
                run_skewed([tileD(i, tc) for tc, i in enumerate(tilesD)])
                S.barrier()
                S.release(mk_stD)
        if stop_after == "D":
            return finish(nc, S, [R_xs_mix, R_h2tok])

        tilesD = [i for i in range(NTT) if not (i < NTC and last)]
        with ExitStack() as stS:
            mk_stS = S.mark()

            def aS(name, shape, dt=F32):
                return stS.enter_context(nc.sbuf_tensor("%s_L%d" % (name, l), shape, dt))
            CUM = srt[:, 0:4]
            op("dve", lambda e: e.memset(srt[:], 0.0), [], [R_srt])
            op("dve", lambda e: e.memset(dest_f[:], 0.0), [], [R_destf])
            tmp4_r = Ring(aS, "tmp4", [128, 4], F32, 2)
            for n_, i in enumerate(tilesD):
                pr, Rpr = PBK[n_ % 2]
                op("pe", lambda e: e.matmul(pr[:, 0:4], lhsT=tri_sb[:], rhs=goh_all[:, i, :], start=True, stop=True), [R_tri, R_goh], [Rpr])
                op("pe", lambda e: e.matmul(pr[:, 4:8], lhsT=ones_sb[:], rhs=goh_all[:, i, :], start=True, stop=True), [R_ones, R_goh], [Rpr])
                t4, Rt4 = tmp4_r.get()
                op("dve", lambda e: e.tensor_tensor(out=t4[:], in0=pr[:, 0:4], in1=CUM, op=ALU.add), [Rpr, R_srt], [Rt4])
                op("dve", lambda e: e.tensor_tensor(out=t4[:], in0=t4[:], in1=goh_all[:, i, :], op=ALU.mult), [Rt4, R_goh], [Rt4])
                op("dve", lambda e: e.tensor_reduce(out=dest_f[:, i:i + 1], in_=t4[:], axis=AX.X, op=ALU.add), [Rt4], [R_destf])
                op("dve", lambda e: e.tensor_tensor(out=CUM, in0=pr[:, 4:8], in1=CUM, op=ALU.add), [Rpr, R_srt], [R_srt])
            tk, Rtk = aS("tk", [128, NB]), Res("tk")
            for g in range(4):
                op("dve", lambda e: e.tensor_scalar(out=tk[:], in0=thr_sb[:], scalar1=srt[:, g:g + 1], scalar2=None, op0=ALU.is_lt), [R_thr, R_srt], [Rtk])
                op("dve", lambda e: e.tensor_reduce(out=srt[:, 8 + g:9 + g], in_=tk[:], axis=AX.X, op=ALU.add), [Rtk], [R_srt])
            op("dve", lambda e: e.memset(srt[:, 16:17], 0.0), [R_srt], [R_srt])
            for g in range(1, 4):
                op("dve", lambda e: e.tensor_tensor(out=srt[:, 16 + g:17 + g], in0=srt[:, 15 + g:16 + g], in1=srt[:, 7 + g:8 + g], op=ALU.add), [R_srt], [R_srt])
            op("dve", lambda e: e.tensor_scalar(out=srt[:, 24:28], in0=srt[:, 16:20], scalar1=512.0, scalar2=None, op0=ALU.mult), [R_srt], [R_srt])
            for g in range(4):
                op("dve", lambda e: e.scalar_tensor_tensor(out=dest_f[:], in0=goh_all[:, :, g], scalar=srt[:, 24 + g:25 + g], in1=dest_f[:],
                                                           op0=ALU.mult, op1=ALU.add), [R_goh, R_srt, R_destf], [R_destf])
            op("dve", lambda e: e.tensor_copy(out=dest_i[:], in_=dest_f[:]), [R_destf], [R_desti])
            gb, Rgb = aS("gb", [128, NB]), Res("gb")
            op("dve", lambda e: e.memset(gb[:], 0.0), [], [Rgb])
            for g in range(1, 4):
                op("dve", lambda e: e.tensor_scalar(out=tk[:], in0=blk_sb[:], scalar1=srt[:, 16 + g:17 + g], scalar2=None, op0=ALU.is_ge), [R_blk, R_srt], [Rtk])
                op("dve", lambda e: e.tensor_tensor(out=gb[:], in0=gb[:], in1=tk[:], op=ALU.add), [Rtk, Rgb], [Rgb])
            op("dve", lambda e: e.tensor_scalar(out=gb[:], in0=gb[:], scalar1=1024.0, scalar2=float(l * NE * 128), op0=ALU.mult, op1=ALU.add), [Rgb], [Rgb])
            op("dve", lambda e: e.tensor_tensor(out=widx_f[:], in0=gb[:].unsqueeze(2).to_broadcast([128, NB, 8]),
                                                in1=jp_sb[:].unsqueeze(1).to_broadcast([128, NB, 8]), op=ALU.add), [Rgb, R_jp], [R_widxf])
            op("dve", lambda e: e.tensor_copy(out=widx_i[:], in_=widx_f[:].rearrange("p b j -> p (b j)")), [R_widxf], [R_widxi])
            h2r_r = Ring(aS, "h2r", [128, D], BF16, 3)
            for i in tilesD:
                h2r, Rh2r = h2r_r.get()
                dma("sp", h2r[:], h2tok_scr[i * 128:(i + 1) * 128, :], [R_h2tok], [Rh2r])
                S.idma(h2perm_scr, h2r[:], dest_i[:, i:i + 1], True, [Rh2r, R_desti, R_h2pz], [R_h2perm])
                S.idma(c8perm_scr, c8_all[:, i, :], dest_i[:, i:i + 1], True, [R_c8, R_desti, R_c8pz], [R_c8perm])
            S.barrier()
            S.release(mk_stS)
        if stop_after == "S":
            return finish(nc, S, [R_h2perm, R_c8perm])

        with ExitStack() as stE:
            mk_stE = S.mark()

            def aE(name, shape, dt=F32):
                return stE.enter_context(nc.sbuf_tensor("%s_L%d" % (name, l), shape, dt))
            if not last:
                load_mix_weights(l + 1)
            hp_r = Ring(aE, "hp", [128, 4, D], BF16, 2)
            h2Tb_r = Ring(aE, "h2TbE", [128, 8, 512], BF16, 2)
            c8_r = Ring(aE, "c8b", [128, 4, 8], F32, 2)
            c8T_r = Ring(aE, "c8T", [8, 512], F32, 2)
            wg_r = Ring(aE, "wg", [128, 8 * DE], BF16, 3)
            wu_r = Ring(aE, "wu", [128, 8 * DE], BF16, 3)
            wd_r = Ring(aE, "wd", [128, 2 * D], BF16, 3)
            cb_r = Ring(aE, "cb", [128, 512], F32, 3)
            sg_r = Ring(aE, "sg", [128, 512], F32, 3)
            hid = aE("hidT_all", [128, 16, 512], BF16)
            R_hid = [Res("hid%d" % e) for e in range(8)]
            yo_r = Ring(aE, "yo", [128, D], F32, 3)
            ecnt = 0
            for b in range(NB):
                hp, Rhp = hp_r.get()
                dma("sp", hp[:], h2perm_scr[b * 512:(b + 1) * 512, :].rearrange("(j p) d -> p j d", p=128), [R_h2perm], [Rhp])
                c8, Rc8 = c8_r.get()
                dma("sp", c8[:], c8perm_scr[b * 512:(b + 1) * 512, :].rearrange("(j p) e -> p j e", p=128), [R_c8perm], [Rc8])
                hb_, Rhb_ = h2Tb_r.get()
                for j in range(4):
                    pb, Rpb = PBK[j]
                    pbv = bfv(pb)
                    for k in range(8):
                        op("pe", lambda e: e.transpose(out=pbv[:, k * 128:(k + 1) * 128], in_=hp[:, j, k * 128:(k + 1) * 128], identity=ident_b[:]),
                           [Rhp, R_idb], [Rpb])
                    eng = "act" if j % 2 == 0 else "dve"
                    if eng == "act":
                        op("act", lambda e: e.copy(out=hb_[:, :, j * 128:(j + 1) * 128], in_=pbv[:, 0:1024].rearrange("p (k t) -> p k t", k=8)), [Rpb], [Rhb_])
                    else:
                        op("dve", lambda e: e.tensor_copy(out=hb_[:, :, j * 128:(j + 1) * 128], in_=pbv[:, 0:1024].rearrange("p (k t) -> p k t", k=8)), [Rpb], [Rhb_])
                pc, Rpc = PBK[4]
                for j in range(4):
                    op("pe", lambda e: e.transpose(out=pc[0:8, j * 128:(j + 1) * 128], in_=c8[:, j, :], identity=ident_f[:]), [Rc8, R_idf], [Rpc])
                c8T, Rc8T = c8T_r.get()
                op("dve", lambda e: e.tensor_copy(out=c8T[:], in_=pc[0:8, 0:512]), [Rpc], [Rc8T])
                dma("sp", cbT_scr[b], c8T[:], [Rc8T], [R_cbT])
                for j_ in range(8):
                    wg, Rwg = wg_r.get()
                    wu, Rwu = wu_r.get()
                    cb, Rcb_ = cb_r.get()
                    ix = widx_i[:, b * 8 + j_:b * 8 + j_ + 1]
                    S.idma(wg[:], wg_scr, ix, False, [R_wg, R_widxi], [Rwg])
                    S.idma(wu[:], wu_scr, ix, False, [R_wu, R_widxi], [Rwu])
                    dma("sp", cb[:], cbT_scr[b, j_, :].partition_broadcast(128), [R_cbT], [Rcb_])
                    wgv = wg[:].rearrange("p (k h) -> p k h", k=8)
                    wuv = wu[:].rearrange("p (k h) -> p k h", k=8)
                    base = 4 * (ecnt % 2)
                    ecnt += 1
                    for hc in range(2):
                        gt, Rg = PBK[base + hc]
                        ut, Ru = PBK[base + 2 + hc]
                        for k in range(8):
                            op("pe", lambda e: e.matmul(gt[:, :], lhsT=wgv[:, k, hc * 128:(hc + 1) * 128], rhs=hb_[:, k, :], start=(k == 0), stop=(k == 7)),
                               [Rwg, Rhb_], [Rg])
                        for k in range(8):
                            op("pe", lambda e: e.matmul(ut[:, :], lhsT=wuv[:, k, hc * 128:(hc + 1) * 128], rhs=hb_[:, k, :], start=(k == 0), stop=(k == 7)),
                               [Rwu, Rhb_], [Ru])
                    for hc in range(2):
                        gt, Rg = PBK[base + hc]
                        ut, Ru = PBK[base + 2 + hc]
                        sg, Rsg = sg_r.get()
                        op("act", lambda e: e.activation(out=sg[:], in_=gt[:, :], func=AF.Silu), [Rg], [Rsg])
                        op("dve", lambda e: e.tensor_tensor(out=sg[:], in0=ut[:, :], in1=sg[:], op=ALU.mult), [Ru, Rsg], [Rsg])
                        op("dve", lambda e: e.tensor_tensor(out=hid[:, 2 * j_ + hc, :], in0=sg[:], in1=cb[:], op=ALU.mult),
                           [Rsg, Rcb_], [R_hid[j_]])
                for j_ in range(8):
                    wd, Rwd = wd_r.get()
                    ix = widx_i[:, b * 8 + j_:b * 8 + j_ + 1]
                    S.idma(wd[:], wd_scr, ix, False, [R_wd, R_widxi], [Rwd])
                    wdv = wd[:].rearrange("p (c d) -> p c d", c=2)
                    for hc in range(2):
                        for j in range(4):
                            for dh in range(2):
                                yt, Ry_ = PBK[j * 2 + dh]
                                op("pe", lambda e: e.matmul(yt[:, :], lhsT=hid[:, 2 * j_ + hc, j * 128:(j + 1) * 128], rhs=wdv[:, hc, dh * 512:(dh + 1) * 512],
                                                            start=(j_ == 0 and hc == 0), stop=(j_ == 7 and hc == 1)), [R_hid[j_], Rwd], [Ry_])
                for j in range(4):
                    yo, Ryo = yo_r.get()
                    for dh in range(2):
                        yt, Ry_ = PBK[j * 2 + dh]
                        if dh == 0:
                            op("act", lambda e: e.copy(out=yo[:, 0:512], in_=yt[:, :]), [Ry_], [Ryo])
                        else:
                            op("dve", lambda e: e.tensor_copy(out=yo[:, 512:1024], in_=yt[:, :]), [Ry_], [Ryo])
                    r0 = b * 512 + j * 128
                    dma("sp", yperm_scr[r0:r0 + 128, :], yo[:], [Ryo], [R_yperm])
            S.barrier()
            S.release(mk_stE)
        if stop_after == "E":
            return finish(nc, S, [R_yperm])

        with ExitStack() as stF:
            mk_stF = S.mark()

            def aF(name, shape, dt=F32):
                return stF.enter_context(nc.sbuf_tensor("%s_L%d" % (name, l), shape, dt))
            g2bc = {}
            for r in range(2):
                if r == 1 and last:
                    continue
                t = aF("g2_%d" % r, [128, D])
                Rr = Res("g2_%d" % r)
                load_bc(t, Rr, ada_vec(l, r, 5), D, [R_ada])
                g2bc[r] = (t, Rr)
            ln2g_bc, R_l2g = aF("ln2g_bc", [128, D]), Res("ln2g_bc")
            ln2b_bc, R_l2b = aF("ln2b_bc", [128, D]), Res("ln2b_bc")
            load_bc(ln2g_bc, R_l2g, ln2_g[l], D)
            load_bc(ln2b_bc, R_l2b, ln2_b[l], D)
            xt_r = Ring(aF, "xtF", [128, D], F32, 3)
            yg_r = Ring(aF, "ygF", [128, D], F32, 3)
            o_r = Ring(aF, "oF", [128, D], F32, 3)
            lnr = {"st": Ring(aF, "fst", [128, 12], F32, 3), "mv": Ring(aF, "fmv", [128, 4], F32, 3)}

            def tileF(i):
                typ = 1 if i < NTC else 0
                gg = i * 128
                xt, Rxt = xt_r.get()
                dma("sp", xt[:], xs_mix[gg:gg + 128, :], [R_xs_mix], [Rxt])
                yg, Ryg = yg_r.get()
                S.idma(yg[:], yperm_scr, dest_i[:, i:i + 1], False, [R_yperm, R_desti], [Ryg])
                yield
                g2t, Rg2 = g2bc[typ]
                op("dve", lambda e: e.tensor_tensor(out=yg[:], in0=yg[:], in1=g2t[:], op=ALU.mult), [Ryg, Rg2], [Ryg])
                op("dve", lambda e: e.scalar_tensor_tensor(out=yg[:], in0=xt[:], scalar=ALPHA, in1=yg[:], op0=ALU.mult, op1=ALU.add), [Rxt, Ryg], [Ryg])
                o, Ro = o_r.get()
                layer_norm_tile(None, "pool", yg, Ryg, D, ln2g_bc, R_l2g, ln2b_bc, R_l2b, o, Ro, lnr)
                if last:
                    dma("sp", out_d[gg - C:gg - C + 128, :], o[:], [Ro], [R_out])
                else:
                    dma("sp", xs_out[l % 2][gg:gg + 128, :], o[:], [Ro], [R_xs_out[l % 2]])

            run_skewed([tileF(i) for i in tilesD])
            S.barrier()
            S.release(mk_stF)
    return finish(nc, S, [R_out])


def finish(nc, S, ress):
    S.barrier()
    S.wait_all("sp", ress)
    return nc


def _rope_tables(T, C):
    rows = T // GRID_W
    row = np.repeat(np.arange(rows), GRID_W).astype(np.float32)
    col = np.tile(np.arange(GRID_W), rows).astype(np.float32)
    d_axis = DR // 2
    inv_freq = np.power(np.float32(10000.0), -np.arange(0, d_axis, 2, dtype=np.float32) / np.float32(d_axis)).astype(np.float32)

    def ax(p):
        a = p[:, None] * inv_freq[None, :]
        return np.concatenate([a, a], -1)

    ang = np.concatenate([ax(row), ax(col)], -1).astype(np.float32)
    cos = np.cos(ang).astype(np.float32)
    sin = np.sin(ang).astype(np.float32)
    sgn = np.tile(np.concatenate([-np.ones(8), np.ones(8)]), 2).astype(np.float32)
    tab = np.zeros((T + C, 2, DR), np.float32)
    tab[:C, 0, :] = 1.0
    tab[C:, 0, :] = cos
    tab[C:, 1, :] = sin * sgn[None, :]
    return tab


def _pool_tables():
    wins = (2, 4, 8, 16)
    edge = np.zeros((128, 2, 2, 8), np.float32)
    invw = np.zeros((128, 2), np.float32)
    for k in range(2):
        for ph in range(2):
            w = wins[2 * k + ph]
            ps = slice(ph * 64, ph * 64 + 64)
            invw[ps, k] = 1.0 / w
            for j in range(8):
                t = j
                cnt = (t + w // 2 - 1) - max(t - w // 2, 0) + 1
                edge[ps, k, 0, j] = 1.0 / cnt
                r = 7 - j
                hi = min(w // 2 - 1, r)
                cnt = hi + w // 2 + 1
                edge[ps, k, 1, j] = 1.0 / cnt
    return edge, invw


def _sort_tables(T, C):
    TT = T + C
    NB = (TT + 4 * 511 + 511) // 512
    tri = np.triu(np.ones((128, 128), np.float32), k=1)
    thr = np.broadcast_to((np.arange(NB, dtype=np.float32) * 512.0)[None, :], (128, NB)).copy()
    blk = np.broadcast_to(np.arange(NB, dtype=np.float32)[None, :], (128, NB)).copy()
    jp = (np.arange(8, dtype=np.float32)[None, :] * 128.0 + np.arange(128, dtype=np.float32)[:, None]).astype(np.float32)
    return {"tri": tri, "thr_bc": thr, "blk_bc": blk, "jp": jp}


_CACHE = {}


def _consts(T, C):
    edge, invw = _pool_tables()
    return {
        "ident": np.eye(128, dtype=np.float32),
        "rope_cs": _rope_tables(T, C),
        "pool_edge": edge,
        "pool_invw": invw,
        **_sort_tables(T, C),
    }


_WKEYS = ["w_ada", "b_ada", "w_in", "g_q", "w_uq", "g_kv", "w_ukv", "conv_w", "conv_b", "conv_ln_g", "conv_ln_b", "pool_w",
          "pool_scale", "w_out", "ln1_g", "ln1_b", "w_router_group", "b_router_group", "w_router_expert", "b_router_expert",
          "w_gate", "w_up", "w_down", "ln2_g", "ln2_b"]


def make_in_maps(inputs, T, C, ncores):
    consts = _consts(T, C)
    shared = {k: np.ascontiguousarray(np.asarray(inputs[k], dtype=np.float32)) for k in _WKEYS}
    maps = []
    for b in range(ncores):
        m = dict(shared)
        m.update(consts)
        m["x"] = np.ascontiguousarray(np.asarray(inputs["x"][b], dtype=np.float32))
        m["ctx"] = np.ascontiguousarray(np.asarray(inputs["ctx"][b], dtype=np.float32))
        m["cvec"] = np.ascontiguousarray(np.stack([np.asarray(inputs["c"][b]), np.asarray(inputs["c_ctx"])]).astype(np.float32))
        maps.append(m)
    return maps


def kernel(**inputs):
    x = np.asarray(inputs["x"])
    B, T, _ = x.shape
    C = np.asarray(inputs["ctx"]).shape[1]
    L = np.asarray(inputs["w_ada"]).shape[0]
    key = (T, C, L)
    if key not in _CACHE:
        _CACHE[key] = build(T, C, L)
    nc = _CACHE[key]
    maps = make_in_maps(inputs, T, C, B)
    res = run_bass_kernel_spmd(nc, maps, core_ids=list(range(B)))
    return np.stack([np.asarray(r["out"]) for r in res.results], axis=0).astype(np.float32)
```

```python
import math
from contextlib import ExitStack
import numpy as np
import concourse.bass as bass
import concourse.mybir as mybir
from concourse.bass_utils import run_bass_kernel_spmd

F32 = mybir.dt.float32
BF16 = mybir.dt.bfloat16
AF = mybir.ActivationFunctionType
ALU = mybir.AluOpType
AX = mybir.AxisListType

D = 1024
H = 8
DQ = 384
DKV = 256
DR = 32
DIN = 1440
NE = 32
DE = 256
GRID_W = 64
CONVW = 31
LN_EPS = 1e-5
RMS_EPS = 1e-6
NEG = -1.0e30


class Res:
    __slots__ = ("name", "w", "r", "sem", "cnt", "excl", "multi")

    def __init__(self, name, excl=False, multi=False):
        self.multi = False
        self.name = name
        self.w = None
        self.r = []
        self.sem = None
        self.cnt = 0
        self.excl = excl


class Sched:
    def __init__(self, nc):
        self.nc = nc
        self.eng = {"pe": nc.tensor, "act": nc.scalar, "dve": nc.vector, "pool": nc.gpsimd, "sp": nc.sync}
        self.sems = {}
        self.cnt = {}
        self.known = {}
        self.dma_res = []
        self.free_sems = []
        self.free_sw = []
        self.is_sw = {}
        self.nalloc = 0
        self.nwait = 0
        for e in self.eng:
            self.sems[e] = nc.alloc_semaphore("e_" + e)
            self.cnt[e] = 0
            self.known[e] = {}

    def _waits(self, e, reads, writes):
        deps = {}

        def add(ev):
            if ev is None:
                return
            k, v = ev
            if deps.get(k, 0) < v:
                deps[k] = v

        for r in reads:
            add(r.w)
        for w in writes:
            if not w.multi:
                add(w.w)
            for ev in w.r:
                add(ev)
        kn = self.known[e]
        for k, v in deps.items():
            if kn.get(k, 0) >= v:
                continue
            if e == "pe" and k == "pe":
                continue
            kn[k] = v
            sem = self.sems[k] if isinstance(k, str) else k.sem
            self.eng[e].wait_ge(sem, v)
            self.nwait += 1

    @staticmethod
    def _commit(ev, reads, writes):
        for r in reads:
            r.r.append(ev)
            if len(r.r) > 48:
                best = {}
                for k, v in r.r:
                    if best.get(k, 0) < v:
                        best[k] = v
                r.r = list(best.items())
        for w in writes:
            w.w = ev
            w.r = []

    def op(self, e, fn, reads=(), writes=()):
        if any(r.excl for r in reads):
            writes = list(writes) + [r for r in reads if r.excl and r not in writes]
            reads = [r for r in reads if not r.excl]
        self._waits(e, reads, writes)
        ins = fn(self.eng[e])
        self.cnt[e] += 1
        ins.then_inc(self.sems[e], 1)
        self._commit((e, self.cnt[e]), reads, writes)

    def dma(self, e, out, in_, reads, writes, **kw):
        dst = writes[0]
        self._waits(e, reads, writes)
        self.ensure(dst, sw=(e == "pool"))
        dst.cnt += 16
        self.eng[e].dma_start(out=out, in_=in_, **kw).then_inc(dst.sem, 16)
        self._commit((dst, dst.cnt), reads, writes)

    def idma(self, out, in_, idx_ap, scatter, reads, writes):
        import concourse.bass as _b
        dst = writes[0]
        self._waits("pool", reads, writes)
        self.ensure(dst, sw=True)
        dst.cnt += 16
        off = _b.IndirectOffsetOnAxis(ap=idx_ap, axis=0)
        if scatter:
            ins = self.nc.gpsimd.indirect_dma_start(out=out, out_offset=off, in_=in_, in_offset=None)
        else:
            ins = self.nc.gpsimd.indirect_dma_start(out=out, out_offset=None, in_=in_, in_offset=off)
        ins.then_inc(dst.sem, 16)
        self._commit((dst, dst.cnt), reads, writes)

    def ensure(self, dst, sw=False):
        if dst.sem is None:
            fl = self.free_sw if sw else self.free_sems
            self.is_sw[id(dst)] = sw
            if fl:
                dst.sem, dst.cnt = fl.pop()
            else:
                self.nalloc += 1
                dst.sem = self.nc.alloc_semaphore("d%d_%s" % (self.nalloc, dst.name))
                dst.cnt = 0
            self.dma_res.append(dst)

    def mark(self):
        return len(self.dma_res)

    def release(self, mark):
        for r in self.dma_res[mark:]:
            (self.free_sw if self.is_sw.get(id(r)) else self.free_sems).append((r.sem, r.cnt))
            r.sem = None
        del self.dma_res[mark:]

    def barrier(self):
        for e in self.eng:
            kn = self.known[e]
            for k in self.eng:
                if k == e:
                    continue
                v = self.cnt[k]
                if v > 0 and kn.get(k, 0) < v:
                    kn[k] = v
                    self.eng[e].wait_ge(self.sems[k], v)
            for r in self.dma_res:
                if r.cnt > 0 and kn.get(r, 0) < r.cnt:
                    kn[r] = r.cnt
                    self.eng[e].wait_ge(r.sem, r.cnt)

    def wait_all(self, e, ress):
        self._waits(e, ress, ())


class Ring:
    def __init__(self, alloc, name, shape, dt, n):
        self.bufs = []
        for i in range(n):
            nm = "%s_%d" % (name, i)
            self.bufs.append((alloc(nm, shape, dt), Res(nm)))
        self.i = 0

    def get(self):
        b = self.bufs[self.i % len(self.bufs)]
        self.i += 1
        return b


class _Cut(Exception):
    pass


def run_skewed(gens):
    active = []
    it = iter(gens)
    while True:
        g = next(it, None)
        if g is not None:
            active.append(g)
        elif not active:
            break
        for g_ in list(reversed(active)):
            try:
                next(g_)
            except StopIteration:
                active.remove(g_)


def build(T, C, L, debug=False, stop_after=None):
    st = {}
    try:
        return _build(T, C, L, debug, stop_after, st)
    except _Cut:
        return finish(st["nc"], st["S"], [])


def _build(T, C, L, debug, stop_after, st_):
    NTL = T // 128
    NTC = C // 128
    TT = T + C
    NTT = NTL + NTC
    ALPHA = float((2 * L) ** 0.25)
    QS = 1.0 / math.sqrt(96.0)
    SEG = min(1024, T)

    nc = bass.Bass("TRN2", target_bir_lowering=False)
    S = Sched(nc)
    op = S.op
    dma = S.dma
    st_["nc"] = nc
    st_["S"] = S

    def cut(tag):
        if stop_after == tag:
            raise _Cut()

    def din(name, shape, dt=F32):
        return nc.dram_tensor(name, shape, dt, kind="ExternalInput").ap()

    def dscr(name, shape, dt=F32):
        return nc.dram_tensor(name, shape, dt, kind=("ExternalOutput" if debug else "Internal")).ap()

    x_in = din("x", [T, D])
    ctx_in = din("ctx", [C, D])
    cvec = din("cvec", [2, D])
    w_ada = din("w_ada", [L, D, 6 * D])
    b_ada = din("b_ada", [L, 6 * D])
    w_in = din("w_in", [L, D, DIN])
    g_q = din("g_q", [L, DQ])
    w_uq = din("w_uq", [L, DQ, 768])
    g_kv = din("g_kv", [L, DKV])
    w_ukv = din("w_ukv", [L, DKV, 1024])
    conv_w = din("conv_w", [L, CONVW, 256])
    conv_b = din("conv_b", [L, 256])
    conv_ln_g = din("conv_ln_g", [L, 256])
    conv_ln_b = din("conv_ln_b", [L, 256])
    pool_w = din("pool_w", [L, 4, 64, 64])
    pool_scale = din("pool_scale", [L, 256])
    w_out = din("w_out", [L, D, D])
    ln1_g = din("ln1_g", [L, D])
    ln1_b = din("ln1_b", [L, D])
    w_rg = din("w_router_group", [L, D, 4])
    b_rg = din("b_router_group", [L, 4])
    w_re = din("w_router_expert", [L, D, NE])
    b_re = din("b_router_expert", [L, NE])
    w_gate = din("w_gate", [L, NE, D, DE])
    w_up = din("w_up", [L, NE, D, DE])
    w_down = din("w_down", [L, NE, DE, D])
    ln2_g = din("ln2_g", [L, D])
    ln2_b = din("ln2_b", [L, D])
    ident_d = din("ident", [128, 128])
    rope_d = din("rope_cs", [TT, 2, DR])
    pedge_d = din("pool_edge", [128, 2, 2, 8])
    pinvw_d = din("pool_invw", [128, 2])

    NB = (TT + 4 * 511 + 511) // 512
    NP = NB * 512
    I32 = mybir.dt.int32
    tri_d = din("tri", [128, 128])
    thr_d = din("thr_bc", [128, NB])
    blk_d = din("blk_bc", [128, NB])
    jp_d = din("jp", [128, 8])
    out_d = nc.dram_tensor("out", [T, D], F32, kind="ExternalOutput").ap()
    h2tok_scr = dscr("h2tok_scr", [TT, D], BF16)
    h2perm_scr = dscr("h2perm_scr", [NP, D], BF16)
    c8perm_scr = dscr("c8perm_scr", [NP, 8])
    cbT_scr = dscr("cbT_scr", [NB, 8, 512])
    yperm_scr = dscr("yperm_scr", [NP, D])
    R_h2tok = Res("h2tok_scr", multi=True)
    R_h2perm = Res("h2perm_scr", multi=True)
    R_c8perm = Res("c8perm_scr", multi=True)
    R_cbT = Res("cbT_scr", multi=True)
    R_yperm = Res("yperm_scr", multi=True)

    ada_scr = dscr("ada_scr", [L, 2, 6 * D])
    xs_mix = dscr("xs_mix", [TT, D])
    xs_out = [dscr("xs_out0", [TT, D]), dscr("xs_out1", [TT, D])]
    kT_scr = dscr("kT_scr", [H, 97, TT], BF16)
    qT_scr = dscr("qT_scr", [H, 96, TT], BF16)
    mT_scr = dscr("mT_scr", [H, TT], BF16)
    v_scr = dscr("v_scr", [H, 128, NTT, 80], BF16)
    catcp_scr = dscr("catcp_scr", [TT, 512], BF16)
    h2T_scr = dscr("h2T_scr", [8, 128, TT], BF16)
    combT_scr = dscr("combT_scr", [NE, TT])
    wg_scr = nc.dram_tensor("wg_scr", [L * NE * 128, 8 * DE], BF16, kind="Internal").ap()
    wu_scr = nc.dram_tensor("wu_scr", [L * NE * 128, 8 * DE], BF16, kind="Internal").ap()
    wd_scr = nc.dram_tensor("wd_scr", [L * NE * 128, 2 * D], BF16, kind="Internal").ap()
    R_ada = Res("ada_scr")
    R_xs_mix = Res("xs_mix", multi=True)
    R_xs_out = [Res("xs_out0", multi=True), Res("xs_out1", multi=True)]
    R_kT = Res("kT_scr", multi=True)
    R_qT = Res("qT_scr", multi=True)
    R_mT = Res("mT_scr")
    R_v = Res("v_scr", multi=True)
    R_catcp = Res("catcp_scr", multi=True)
    R_h2T = Res("h2T_scr", multi=True)
    R_combT = Res("combT_scr", multi=True)
    R_wg = Res("wg_scr")
    R_wu = Res("wu_scr")
    R_wd = Res("wd_scr")
    R_out = Res("out", multi=True)
    for R_ in [R_h2perm, R_c8perm]:
        S.ensure(R_, sw=True)
    R_h2pz = Res("h2perm_zero")
    R_c8pz = Res("c8perm_zero")
    S.ensure(R_h2pz)
    S.ensure(R_c8pz)
    for R_ in [R_h2tok, R_cbT, R_yperm]:
        S.ensure(R_)
    for R_ in [R_ada, R_xs_mix, R_xs_out[0], R_xs_out[1], R_kT, R_qT, R_mT, R_v, R_catcp, R_h2T, R_combT, R_out]:
        S.ensure(R_)
    for R_ in [R_wg, R_wu, R_wd]:
        S.ensure(R_, sw=True)

    PBK = []
    for i in range(8):
        PBK.append((nc.alloc_psum_tensor("pb%d" % i, [128, 512], F32), Res("pb%d" % i, excl=True)))

    def bfv(t):
        return t[:].bitcast(BF16)

    def palloc(name, shape, dt=F32):
        return nc.alloc_sbuf_tensor(name, shape, dt)

    def sb(name, shape, dt=F32):
        return nc.alloc_sbuf_tensor(name, shape, dt), Res(name)

    ident_f, R_idf = sb("ident_f", [128, 128])
    ident_b, R_idb = sb("ident_b", [128, 128], BF16)
    dma("sp", ident_f[:], ident_d, [], [R_idf])
    op("dve", lambda e: e.tensor_copy(out=ident_b[:], in_=ident_f[:]), [R_idf], [R_idb])
    eps_t, R_epst = sb("eps_t", [128, 2])
    op("dve", lambda e: e.memset(eps_t[:, 0:1], LN_EPS), [], [R_epst])
    op("dve", lambda e: e.memset(eps_t[:, 1:2], RMS_EPS), [R_epst], [R_epst])
    tri_sb, R_tri = sb("tri_sb", [128, 128])
    dma("sp", tri_sb[:], tri_d, [], [R_tri])
    ones_sb, R_ones = sb("ones_sb", [128, 128])
    op("dve", lambda e: e.memset(ones_sb[:], 1.0), [], [R_ones])
    thr_sb, R_thr = sb("thr_sb", [128, NB])
    blk_sb, R_blk = sb("blk_sb", [128, NB])
    jp_sb, R_jp = sb("jp_sb", [128, 8])
    dma("sp", thr_sb[:], thr_d, [], [R_thr])
    dma("sp", blk_sb[:], blk_d, [], [R_blk])
    dma("sp", jp_sb[:], jp_d, [], [R_jp])
    zer_b, R_zerb = sb("zer_b", [128, D], BF16)
    zer_f, R_zerf = sb("zer_f", [128, 8])
    op("dve", lambda e: e.memset(zer_b[:], 0.0), [], [R_zerb])
    op("dve", lambda e: e.memset(zer_f[:], 0.0), [], [R_zerf])
    goh_all, R_goh = sb("goh_all", [128, NTT, 4])
    c8_all, R_c8 = sb("c8_all", [128, NTT, 8])
    dest_f, R_destf = sb("dest_f", [128, NTT])
    dest_i, R_desti = sb("dest_i", [128, NTT], I32)
    widx_f, R_widxf = sb("widx_f", [128, NB, 8])
    widx_i, R_widxi = sb("widx_i", [128, NB * 8], I32)
    srt, R_srt = sb("srt", [128, 64])
    pedge, R_pedge = sb("pedge", [128, 2, 2, 8])
    pinvw, R_pinvw = sb("pinvw", [128, 2])
    dma("sp", pedge[:], pedge_d, [], [R_pedge])
    dma("sp", pinvw[:], pinvw_d, [], [R_pinvw])

    w_in_sb, R_win = sb("w_in_sb", [128, 8, DIN], BF16)
    w_uq_sb, R_wuq = sb("w_uq_sb", [128, 3, 768], BF16)
    w_ukv_sb, R_wukv = sb("w_ukv_sb", [128, 2, 1024], BF16)
    for R_ in (R_win, R_wuq, R_wukv):
        S.ensure(R_, sw=True)

    def load_mix_weights(l_):
        dma("pool", w_in_sb[:], w_in[l_].rearrange("(k p) n -> p k n", p=128), [], [R_win])
        dma("pool", w_uq_sb[:], w_uq[l_].rearrange("(k p) n -> p k n", p=128), [], [R_wuq])
        dma("pool", w_ukv_sb[:], w_ukv[l_].rearrange("(k p) n -> p k n", p=128), [], [R_wukv])

    load_mix_weights(0)

    for l in range(L if stop_after not in ("0", "A", "C", "B", "D") else 0):
        for e0 in range(0, NE, 8):
            for e1 in range(e0, e0 + 8):
                r0 = (l * NE + e1) * 128
                dma("pool", wg_scr[r0:r0 + 128, :].rearrange("p (k h) -> p k h", k=8), w_gate[l, e1].rearrange("(k p) h -> p k h", p=128), [], [R_wg])
                dma("pool", wu_scr[r0:r0 + 128, :].rearrange("p (k h) -> p k h", k=8), w_up[l, e1].rearrange("(k p) h -> p k h", p=128), [], [R_wu])
                dma("pool", wd_scr[r0:r0 + 128, :].rearrange("p (c d) -> p c d", c=2), w_down[l, e1].rearrange("(c p) d -> p c d", p=128), [], [R_wd])

    with ExitStack() as st0:
        mk0 = S.mark()

        def a0(name, shape, dt=F32):
            return st0.enter_context(nc.sbuf_tensor(name, shape, dt))
        cs, R_cs = a0("cs", [2, D]), Res("cs")
        csT, R_csT = a0("csT", [128, 8, 2]), Res("csT")
        bada, R_bada = a0("bada", [2, 6 * D]), Res("bada")
        adas, R_adas = a0("adas", [2, 6 * D]), Res("adas")
        wblk = Ring(a0, "wblk", [128, 8, 512], F32, 2)
        dma("sp", cs[:], cvec, [], [R_cs])
        op("act", lambda e: e.activation(out=cs[:], in_=cs[:], func=AF.Silu), [R_cs], [R_cs])
        pb, Rpb = PBK[0]
        for k in range(8):
            op("pe", lambda e: e.transpose(out=pb[:, 2 * k:2 * k + 2], in_=cs[0:2, k * 128:(k + 1) * 128],
                                           identity=ident_f[0:2, 0:2]), [R_cs, R_idf], [Rpb])
        op("dve", lambda e: e.tensor_copy(out=csT[:].rearrange("p k r -> p (k r)"), in_=pb[:, 0:16]), [Rpb], [R_csT])
        nb_i = 0
        for l in range(L):
            dma("sp", bada[:], b_ada[l].partition_broadcast(2), [], [R_bada])
            for nb in range(12):
                wb, Rwb = wblk.get()
                dma("sp", wb[:], w_ada[l, :, nb * 512:(nb + 1) * 512].rearrange("(k p) n -> p k n", p=128), [], [Rwb])
                pb, Rpb = PBK[1 + (nb_i % 2)]
                nb_i += 1
                for k in range(8):
                    op("pe", lambda e: e.matmul(pb[0:2, :], lhsT=csT[:, k, :], rhs=wb[:, k, :], start=(k == 0), stop=(k == 7)),
                       [R_csT, Rwb], [Rpb])
                op("dve", lambda e: e.tensor_tensor(out=adas[:, nb * 512:(nb + 1) * 512], in0=pb[0:2, :],
                                                    in1=bada[:, nb * 512:(nb + 1) * 512], op=ALU.add), [Rpb, R_bada], [R_adas])
            for j in (1, 4):
                op("dve", lambda e: e.tensor_scalar_add(out=adas[:, j * D:(j + 1) * D], in0=adas[:, j * D:(j + 1) * D], scalar1=1.0),
                   [R_adas], [R_adas])
            dma("sp", ada_scr[l], adas[:], [R_adas], [R_ada])
        S.barrier()
        S.release(mk0)

    if stop_after == "0":
        return finish(nc, S, [R_ada])

    def ada_vec(l, r, j):
        return ada_scr[l, r, j * D:(j + 1) * D]

    def load_bc(t, R, src1d, n, rd=()):
        dma("sp", t[:, 0:n], src1d.partition_broadcast(128), list(rd), [R])

    def x_src(l, i):
        if l == 0:
            if i < NTC:
                return ctx_in[i * 128:(i + 1) * 128, :], []
            return x_in[(i - NTC) * 128:(i - NTC + 1) * 128, :], []
        return xs_out[(l - 1) % 2][i * 128:(i + 1) * 128, :], [R_xs_out[(l - 1) % 2]]

    def layer_norm_tile(st, eng2, y, Ry, n, gbc, Rg, bbc, Rb, outt, Rout, rings):
        stt, Rst = rings["st"].get()
        mv, Rmv = rings["mv"].get()
        nch = (n + 511) // 512
        for c in range(nch):
            a, b_ = c * 512, min(n, (c + 1) * 512)
            op("dve", lambda e: e.bn_stats(out=stt[:, c * 6:(c + 1) * 6], in_=y[:, a:b_]), [Ry], [Rst])
        op("dve", lambda e: e.bn_aggr(out=mv[:, 0:2], in_=stt[:, 0:nch * 6]), [Rst], [Rmv])
        op("act", lambda e: e.activation(out=mv[:, 2:3], in_=mv[:, 1:2], func=AF.Ln, bias=eps_t[:, 0:1], scale=1.0), [Rmv], [Rmv])
        op("act", lambda e: e.activation(out=mv[:, 2:3], in_=mv[:, 2:3], func=AF.Exp, scale=-0.5), [Rmv], [Rmv])
        op("dve", lambda e: e.scalar_tensor_tensor(out=mv[:, 3:4], in0=mv[:, 0:1], scalar=-1.0, in1=mv[:, 2:3],
                                                   op0=ALU.mult, op1=ALU.mult), [Rmv], [Rmv])
        op("act", lambda e: e.activation(out=y[:, 0:n], in_=y[:, 0:n], func=AF.Identity, bias=mv[:, 3:4], scale=mv[:, 2:3]),
           [Ry, Rmv], [Ry])
        op(eng2, lambda e: e.tensor_tensor(out=y[:, 0:n], in0=y[:, 0:n], in1=gbc[:, 0:n], op=ALU.mult), [Ry, Rg], [Ry])
        op("dve", lambda e: e.tensor_tensor(out=outt[:, 0:n], in0=y[:, 0:n], in1=bbc[:, 0:n], op=ALU.add), [Ry, Rb], [Rout])

    for l in range(L):
        last = (l == L - 1)
        S.barrier()

        with ExitStack() as stAC:
            def aAC(name, shape, dt=F32):
                return stAC.enter_context(nc.sbuf_tensor("%s_L%d" % (name, l), shape, dt))
            cpT_l, R_cpl = aAC("cpT_l", [128, 4, T + 32]), Res("cpT_l")
            cpT_c, R_cpc = aAC("cpT_c", [128, 4, C + 32]), Res("cpT_c")
            for (t_, R_, n_) in ((cpT_l, R_cpl, T), (cpT_c, R_cpc, C)):
                op("pool", lambda e: e.memset(t_[:, :, 0:16], 0.0), [], [R_])
                op("pool", lambda e: e.memset(t_[:, :, 16 + n_:32 + n_], 0.0), [R_], [R_])

            with ExitStack() as stA:
                mk_stA = S.mark()
                def aA(name, shape, dt=F32):
                    return stA.enter_context(nc.sbuf_tensor("%s_L%d" % (name, l), shape, dt))
                gq_bc, R_gq = aA("gq_bc", [128, DQ]), Res("gq_bc")
                gkv_bc, R_gkv = aA("gkv_bc", [128, DKV]), Res("gkv_bc")
                load_bc(gq_bc, R_gq, g_q[l], DQ)
                load_bc(gkv_bc, R_gkv, g_kv[l], DKV)
                sc1, sh1, R_sc1, R_sh1 = [], [], [], []
                for r in range(2):
                    t = aA("sc1_%d" % r, [128, D])
                    Rr = Res("sc1_%d" % r)
                    load_bc(t, Rr, ada_vec(l, r, 1), D, [R_ada])
                    sc1.append(t)
                    R_sc1.append(Rr)
                    t = aA("sh1_%d" % r, [128, D])
                    Rr = Res("sh1_%d" % r)
                    load_bc(t, Rr, ada_vec(l, r, 0), D, [R_ada])
                    sh1.append(t)
                    R_sh1.append(Rr)
                rope_sb, R_rope = aA("rope_sb", [128, NTT, 2, DR]), Res("rope_sb")
                dma("sp", rope_sb[:], rope_d.rearrange("(i p) a d -> p i a d", p=128), [], [R_rope])
                nq_all, R_nq = aA("nq_all", [128, NTT, H]), Res("nq_all")
                kmax2, R_kmax2 = aA("kmax2", [128, H]), Res("kmax2")
                op("dve", lambda e: e.memset(kmax2[:], 0.0), [], [R_kmax2])
                op("dve", lambda e: e.memset(nq_all[:], 0.0), [], [R_nq])

                xt_r = Ring(aA, "xt", [128, D], F32, 2)
                tmp_r = Ring(aA, "tmp32", [128, D], F32, 2)
                hb_r = Ring(aA, "hb", [128, D], BF16, 2)
                hT_r = Ring(aA, "hT", [128, 8, 128], BF16, 2)
                junk_r = Ring(aA, "junk", [128, 512], F32, 2)
                stat_r = Ring(aA, "stat", [128, 8], F32, 4)
                qn_r = Ring(aA, "qn", [128, DQ], BF16, 2)
                qnT_r = Ring(aA, "qnT", [128, 3, 128], BF16, 2)
                ckvn_r = Ring(aA, "ckvn", [128, DKV], BF16, 2)
                ckvT_r = Ring(aA, "ckvT", [128, 2, 128], BF16, 2)
                qaug_r = Ring(aA, "qaug", [128, H, 96], BF16, 2)
                kaug_r = Ring(aA, "kaug", [128, H, 112], BF16, 2)
                vaug_r = Ring(aA, "vaug", [128, H, 80], BF16, 2)
                for (t_, R_) in kaug_r.bufs:
                    op("dve", lambda e: e.memset(t_[:, :, 96:112], 1.0), [], [R_])
                for (t_, R_) in vaug_r.bufs:
                    op("dve", lambda e: e.memset(t_[:, :, 64:80], 1.0), [], [R_])
                kTst_r = Ring(aA, "kTst", [97, H, 128], BF16, 2)
                qTst_r = Ring(aA, "qTst", [96, H, 128], BF16, 2)
                cptok_r = Ring(aA, "cptok", [128, 512], F32, 2)
                sig_r = Ring(aA, "sig", [128, 256], F32, 2)
                rt_r = Ring(aA, "rt", [128, H, 2, DR], F32, 2)
                krr_r = Ring(aA, "krr", [128, 2, DR], F32, 2)
                nk_r = Ring(aA, "nk", [128, 16], F32, 2)

                def rope_apply(i, src_view, Rsrc, nh, t1, t2, Rt):
                    cosb = rope_sb[:, i, 0:1, :].to_broadcast([128, nh, DR])
                    op("dve", lambda e: e.tensor_tensor(out=t1[:, 0:nh, :], in0=src_view, in1=cosb, op=ALU.mult),
                       [Rsrc, R_rope], [Rt])
                    sv = src_view.rearrange("p h (a b c) -> p h a b c", a=2, b=2)
                    t2v = t2[:, 0:nh, :].rearrange("p h (a b c) -> p h a b c", a=2, b=2)
                    sn = rope_sb[:, i, 1, :].rearrange("p (a b c) -> p a b c", a=2, b=2)
                    for b_ in range(2):
                        snb = sn[:, :, b_, :].unsqueeze(1).to_broadcast([128, nh, 2, 8])
                        op("dve", lambda e: e.tensor_tensor(out=t2v[:, :, :, b_, :], in0=sv[:, :, :, 1 - b_, :], in1=snb, op=ALU.mult),
                           [Rsrc, R_rope], [Rt])
                    op("dve", lambda e: e.tensor_tensor(out=t1[:, 0:nh, :], in0=t1[:, 0:nh, :], in1=t2[:, 0:nh, :], op=ALU.add),
                       [Rt], [Rt])

                if stop_after == "Apre":
                    return finish(nc, S, [R_win, R_wuq, R_wukv, R_gq, R_gkv, R_rope] + R_sc1 + R_sh1)
                COLS = [(0, 384), (384, 672), (672, 1184), (1184, 1440)]
                def tileA(i):
                    isctx = i < NTC
                    typ = 1 if isctx else 0
                    full = not (last and isctx)
                    g0 = i * 128
                    src, Rsrc = x_src(l, i)
                    xt, Rxt = xt_r.get()
                    dma("sp", xt[:], src, Rsrc, [Rxt])
                    tmp, Rtmp = tmp_r.get()
                    hb, Rhb = hb_r.get()
                    op("pool", lambda e: e.tensor_tensor(out=tmp[:], in0=xt[:], in1=sc1[typ][:], op=ALU.mult), [Rxt, R_sc1[typ]], [Rtmp])
                    op("dve", lambda e: e.tensor_tensor(out=hb[:], in0=tmp[:], in1=sh1[typ][:], op=ALU.add), [Rtmp, R_sh1[typ]], [Rhb])
                    yield
                    pb, Rpb = PBK[0]
                    pbv = bfv(pb)
                    for k in range(8):
                        op("pe", lambda e: e.transpose(out=pbv[:, k * 128:(k + 1) * 128], in_=hb[:, k * 128:(k + 1) * 128], identity=ident_b[:]),
                           [Rhb, R_idb], [Rpb])
                    hT, RhT = hT_r.get()
                    op("act", lambda e: e.copy(out=hT[:].rearrange("p k t -> p (k t)"), in_=pbv[:, 0:1024]), [Rpb], [RhT])
                    cut("A1")
                    G = [PBK[2], PBK[3], PBK[4], PBK[5]]
                    for gi, (c0, c1) in enumerate(COLS):
                        if not full and gi != 1:
                            continue
                        gt, Rg = G[gi]
                        for k in range(8):
                            op("pe", lambda e: e.matmul(gt[:, 0:c1 - c0], lhsT=hT[:, k, :], rhs=w_in_sb[:, k, c0:c1], start=(k == 0), stop=(k == 7)),
                               [RhT, R_win], [Rg])
                    g1t, Rg1 = G[0]
                    g2t, Rg2 = G[1]
                    g3t, Rg3 = G[2]
                    g4t, Rg4 = G[3]
                    stt, Rstt = stat_r.get()
                    junk, Rjunk = junk_r.get()
                    cut("A2")
                    op("act", lambda e: e.activation(out=junk[:, 0:DKV], in_=g2t[:, 0:DKV], func=AF.Square, accum_out=stt[:, 0:1]),
                       [Rg2], [Rjunk, Rstt])
                    op("act", lambda e: e.activation(out=stt[:, 1:2], in_=stt[:, 0:1], func=AF.Ln, bias=eps_t[:, 1:2], scale=1.0 / DKV),
                       [Rstt], [Rstt])
                    op("act", lambda e: e.activation(out=stt[:, 1:2], in_=stt[:, 1:2], func=AF.Exp, scale=-0.5), [Rstt], [Rstt])
                    ckvn, Rckvn = ckvn_r.get()
                    op("dve", lambda e: e.scalar_tensor_tensor(out=ckvn[:], in0=g2t[:, 0:DKV], scalar=stt[:, 1:2], in1=gkv_bc[:],
                                                               op0=ALU.mult, op1=ALU.mult), [Rg2, Rstt, R_gkv], [Rckvn])
                    cut("A3")
                    krr, Rkrr = krr_r.get()
                    rt, Rrt = rt_r.get()
                    rope_apply(i, g2t[:, DKV:DKV + DR].unsqueeze(1), Rg2, 1, krr[:, 0:1, :], krr[:, 1:2, :], Rkrr)
                    cut("A4")
                    if full:
                        op("act", lambda e: e.activation(out=junk[:, 0:DQ], in_=g1t[:, 0:DQ], func=AF.Square, accum_out=stt[:, 2:3]),
                           [Rg1], [Rjunk, Rstt])
                        op("act", lambda e: e.activation(out=stt[:, 3:4], in_=stt[:, 2:3], func=AF.Ln, bias=eps_t[:, 1:2], scale=1.0 / DQ),
                           [Rstt], [Rstt])
                        op("act", lambda e: e.activation(out=stt[:, 3:4], in_=stt[:, 3:4], func=AF.Exp, scale=-0.5), [Rstt], [Rstt])
                        qn, Rqn = qn_r.get()
                        op("dve", lambda e: e.scalar_tensor_tensor(out=qn[:], in0=g1t[:, 0:DQ], scalar=stt[:, 3:4], in1=gq_bc[:],
                                                                   op0=ALU.mult, op1=ALU.mult), [Rg1, Rstt, R_gq], [Rqn])
                        sig, Rsig = sig_r.get()
                        cptok, Rcptok = cptok_r.get()
                        op("act", lambda e: e.activation(out=sig[:], in_=g3t[:, 256:512], func=AF.Exp, scale=-1.0), [Rg3], [Rsig])
                        op("act", lambda e: e.activation(out=sig[:], in_=sig[:], func=AF.Ln, bias=1.0, scale=1.0), [Rsig], [Rsig])
                        op("act", lambda e: e.activation(out=sig[:], in_=sig[:], func=AF.Exp, scale=-1.0), [Rsig], [Rsig])
                        op("dve", lambda e: e.tensor_tensor(out=cptok[:, 0:256], in0=g3t[:, 0:256], in1=sig[:], op=ALU.mult),
                           [Rg3, Rsig], [Rcptok])
                        op("act", lambda e: e.copy(out=cptok[:, 256:512], in_=g4t[:, 0:256]), [Rg4, Rcptok], [Rcptok])
                        pb1, Rpb1 = PBK[1]
                        pb1v = bfv(pb1)
                        for k in range(3):
                            op("pe", lambda e: e.transpose(out=pb1v[:, k * 128:(k + 1) * 128], in_=qn[:, k * 128:(k + 1) * 128], identity=ident_b[:]),
                               [Rqn, R_idb], [Rpb1])
                        qnT, RqnT = qnT_r.get()
                        op("act", lambda e: e.copy(out=qnT[:].rearrange("p k t -> p (k t)"), in_=pb1v[:, 0:384]), [Rpb1], [RqnT])
                    cut("A5")
                    pb0, Rpb0 = PBK[0]
                    pb0v = bfv(pb0)
                    for k in range(2):
                        op("pe", lambda e: e.transpose(out=pb0v[:, k * 128:(k + 1) * 128], in_=ckvn[:, k * 128:(k + 1) * 128], identity=ident_b[:]),
                           [Rckvn, R_idb], [Rpb0])
                    ckvT, RckvT = ckvT_r.get()
                    op("dve", lambda e: e.tensor_copy(out=ckvT[:].rearrange("p k t -> p (k t)"), in_=pb0v[:, 0:256]), [Rpb0], [RckvT])
                    yield
                    if full:
                        Q = [(PBK[6], 0, 5), (PBK[7], 5, 3)]
                        for ((qt, Rq), h0, nh) in Q:
                            for k in range(3):
                                op("pe", lambda e: e.matmul(qt[:, 0:nh * 96], lhsT=qnT[:, k, :], rhs=w_uq_sb[:, k, h0 * 96:(h0 + nh) * 96],
                                                            start=(k == 0), stop=(k == 2)), [RqnT, R_wuq], [Rq])
                    cut("A6")
                    KV = [(PBK[2], 0), (PBK[4], 4)]
                    for ((kt, Rk), h0) in KV:
                        for k in range(2):
                            op("pe", lambda e: e.matmul(kt[:, 0:512], lhsT=ckvT[:, k, :], rhs=w_ukv_sb[:, k, h0 * 128:(h0 + 4) * 128],
                                                        start=(k == 0), stop=(k == 1)), [RckvT, R_wukv], [Rk])
                    if full:
                        pb5, Rpb5 = PBK[5]
                        for k in range(4):
                            op("pe", lambda e: e.transpose(out=pb5[:, k * 128:(k + 1) * 128], in_=cptok[:, k * 128:(k + 1) * 128], identity=ident_f[:]),
                               [Rcptok, R_idf], [Rpb5])
                        cpd, Rcpd, off = (cpT_c, R_cpc, g0) if isctx else (cpT_l, R_cpl, g0 - C)
                        op("act", lambda e: e.copy(out=cpd[:, :, 16 + off:16 + off + 128], in_=pb5[:].rearrange("p (k t) -> p k t", k=4)),
                           [Rpb5], [Rcpd])
                        qaug, Rqaug = qaug_r.get()
                        nk, Rnk = nk_r.get()
                        for ((qt, Rq), h0, nh) in Q:
                            qv = qt[:, 0:nh * 96].rearrange("p (h d) -> p h d", d=96)
                            op("act", lambda e: e.copy(out=qaug[:, h0:h0 + nh, 0:64], in_=qv[:, :, 0:64]), [Rq], [Rqaug])
                            rope_apply(i, qv[:, :, 64:96], Rq, nh, rt[:, h0:h0 + nh, 0, :], rt[:, h0:h0 + nh, 1, :], Rrt)
                            op("dve", lambda e: e.tensor_copy(out=qaug[:, h0:h0 + nh, 64:96], in_=rt[:, h0:h0 + nh, 0, :]), [Rrt], [Rqaug])
                            jv = junk[:, 0:nh * 96].rearrange("p (h d) -> p h d", d=96)
                            op("act", lambda e: e.activation(out=jv, in_=qv, func=AF.Square), [Rq], [Rjunk])
                            op("dve", lambda e: e.tensor_reduce(out=nq_all[:, i, h0:h0 + nh], in_=jv, axis=AX.X, op=ALU.add), [Rjunk], [R_nq])
                    else:
                        nk, Rnk = nk_r.get()
                    cut("A7")
                    kaug, Rkaug = kaug_r.get()
                    vaug, Rvaug = vaug_r.get()
                    for ((kt, Rk), h0) in KV:
                        kv = kt[:, 0:512].rearrange("p (h d) -> p h d", d=128)
                        cut("KV0")
                        op("act", lambda e: e.copy(out=kaug[:, h0:h0 + 4, 0:64], in_=kv[:, :, 0:64]), [Rk], [Rkaug])
                        cut("KV1")
                        op("dve", lambda e: e.tensor_copy(out=vaug[:, h0:h0 + 4, 0:64], in_=kv[:, :, 64:128]), [Rk], [Rvaug])
                        cut("KV2")
                        jv = junk[:, 0:256].rearrange("p (h d) -> p h d", d=64)
                        op("act", lambda e: e.activation(out=jv, in_=kv[:, :, 0:64], func=AF.Square), [Rk], [Rjunk])
                        cut("KV3")
                        op("dve", lambda e: e.tensor_reduce(out=nk[:, h0:h0 + 4], in_=jv, axis=AX.X, op=ALU.add), [Rjunk], [Rnk])
                        cut("KV4")
                    cut("K1")
                    op("dve", lambda e: e.tensor_copy(out=kaug[:, :, 64:96], in_=krr[:, 0:1, :].to_broadcast([128, H, DR])), [Rkrr], [Rkaug])
                    cut("K2")
                    op("dve", lambda e: e.tensor_tensor(out=krr[:, 1, :], in0=krr[:, 0, :], in1=krr[:, 0, :], op=ALU.mult), [Rkrr], [Rkrr])
                    op("dve", lambda e: e.tensor_reduce(out=nk[:, 8:9], in_=krr[:, 1, :], axis=AX.X, op=ALU.add), [Rkrr], [Rnk])
                    cut("K3")
                    op("dve", lambda e: e.tensor_scalar(out=nk[:, 0:8], in0=nk[:, 0:8], scalar1=nk[:, 8:9], scalar2=None, op0=ALU.add), [Rnk], [Rnk])
                    cut("K4")
                    op("dve", lambda e: e.tensor_tensor(out=kmax2[:], in0=kmax2[:], in1=nk[:, 0:8], op=ALU.max), [Rnk, R_kmax2], [R_kmax2])
                    cut("A7b")
                    yield
                    if full:
                        pb0, Rpb0 = PBK[0]
                        pb0v = bfv(pb0)
                        for h in range(H):
                            op("pe", lambda e: e.transpose(out=pb0v[0:96, h * 128:(h + 1) * 128], in_=qaug[:, h, :], identity=ident_b[:]),
                               [Rqaug, R_idb], [Rpb0])
                        qTst, RqTst = qTst_r.get()
                        op("act", lambda e: e.copy(out=qTst[:].rearrange("p h t -> p (h t)"), in_=pb0v[0:96, 0:1024]), [Rpb0], [RqTst])
                        dma("sp", qT_scr[:, :, g0:g0 + 128].rearrange("h d t -> d h t"), qTst[:], [RqTst], [R_qT])
                    pb1, Rpb1 = PBK[1]
                    pb1v = bfv(pb1)
                    for h in range(H):
                        op("pe", lambda e: e.transpose(out=pb1v[0:97, h * 128:(h + 1) * 128], in_=kaug[:, h, 0:97], identity=ident_b[:]),
                           [Rkaug, R_idb], [Rpb1])
                    kTst, RkTst = kTst_r.get()
                    op("dve", lambda e: e.tensor_copy(out=kTst[:].rearrange("p h t -> p (h t)"), in_=pb1v[0:97, 0:1024]), [Rpb1], [RkTst])
                    dma("sp", kT_scr[:, :, g0:g0 + 128].rearrange("h d t -> d h t"), kTst[:], [RkTst], [R_kT])
                    dma("sp", v_scr[:, :, i, :].rearrange("h p d -> p h d"), vaug[:], [Rvaug], [R_v])
                    cut("AT%d" % i)

                run_skewed([tileA(i) for i in range(NTT)])
                cut("A8")
                pb, Rpb = PBK[0]
                op("pe", lambda e: e.transpose(out=pb[0:8, 0:128], in_=kmax2[:, 0:8], identity=ident_f[:]), [R_kmax2, R_idf], [Rpb])
                km, Rkm = aA("km", [8, 16]), Res("km")
                op("dve", lambda e: e.tensor_reduce(out=km[:, 0:1], in_=pb[0:8, 0:128], axis=AX.X, op=ALU.max), [Rpb], [Rkm])
                op("act", lambda e: e.activation(out=km[:, 1:2], in_=km[:, 0:1], func=AF.Sqrt, scale=1.0404), [Rkm], [Rkm])
                op("dve", lambda e: e.tensor_scalar(out=km[:, 8:16], in0=ident_f[0:8, 0:8], scalar1=km[:, 1:2], scalar2=-1.0,
                                                    op0=ALU.mult, op1=ALU.mult), [Rkm, R_idf], [Rkm])
                ones8, Rones8 = aA("ones8", [8, 128]), Res("ones8")
                op("dve", lambda e: e.memset(ones8[:], 1.0), [], [Rones8])
                pb, Rpb = PBK[1]
                op("pe", lambda e: e.matmul(pb[:, 0:8], lhsT=ones8[:], rhs=km[:, 8:16], start=True, stop=True), [Rones8, Rkm], [Rpb])
                kmbc, Rkmbc = aA("kmbc", [128, 8]), Res("kmbc")
                op("dve", lambda e: e.tensor_copy(out=kmbc[:], in_=pb[:, 0:8]), [Rpb], [Rkmbc])
                op("act", lambda e: e.activation(out=nq_all[:], in_=nq_all[:], func=AF.Sqrt), [R_nq], [R_nq])
                op("dve", lambda e: e.tensor_tensor(out=nq_all[:], in0=nq_all[:], in1=kmbc[:].unsqueeze(1).to_broadcast([128, NTT, H]), op=ALU.mult),
                   [R_nq, Rkmbc], [R_nq])
                mT_sb, RmT = aA("mT_sb", [8, TT], BF16), Res("mT_sb")
                for i0 in range(0, NTT, 4):
                    pb, Rpb = PBK[(i0 // 4) % 2]
                    ni = min(4, NTT - i0)
                    for j in range(ni):
                        op("pe", lambda e: e.transpose(out=pb[0:8, j * 128:(j + 1) * 128], in_=nq_all[:, i0 + j, :], identity=ident_f[:]),
                           [R_nq, R_idf], [Rpb])
                    op("dve", lambda e: e.tensor_copy(out=mT_sb[:, i0 * 128:(i0 + ni) * 128], in_=pb[0:8, 0:ni * 128]), [Rpb], [RmT])
                dma("sp", mT_scr, mT_sb[:], [RmT], [R_mT])
                S.barrier()
                S.release(mk_stA)
            if stop_after == "A":
                return finish(nc, S, [R_kT, R_qT, R_mT, R_v])

            with ExitStack() as stC:
                mk_stC = S.mark()
                def aC(name, shape, dt=F32):
                    return stC.enter_context(nc.sbuf_tensor("%s_L%d" % (name, l), shape, dt))
                cwr, Rcwr = aC("cwr", [CONVW, 256]), Res("cwr")
                cw_sb, Rcw = aC("cw_sb", [128, 2, CONVW]), Res("cw_sb")
                dma("sp", cwr[:], conv_w[l], [], [Rcwr])
                pb, Rpb = PBK[0]
                for k in range(2):
                    op("pe", lambda e: e.transpose(out=pb[:, k * 32:k * 32 + CONVW], in_=cwr[0:CONVW, k * 128:(k + 1) * 128],
                                                   identity=ident_f[0:CONVW, 0:CONVW]), [Rcwr, R_idf], [Rpb])
                op("dve", lambda e: e.tensor_copy(out=cw_sb[:], in_=pb[:, 0:64].rearrange("p (k j) -> p k j", k=2)[:, :, 0:CONVW]), [Rpb], [Rcw])
                convb_bc, Rcb = aC("convb_bc", [128, 256]), Res("convb_bc")
                clng_bc, Rclg = aC("clng_bc", [128, 256]), Res("clng_bc")
                clnb_bc, Rclb = aC("clnb_bc", [128, 256]), Res("clnb_bc")
                psc_bc, Rpsc = aC("psc_bc", [128, 256]), Res("psc_bc")
                load_bc(convb_bc, Rcb, conv_b[l], 256)
                load_bc(clng_bc, Rclg, conv_ln_g[l], 256)
                load_bc(clnb_bc, Rclb, conv_ln_b[l], 256)
                load_bc(psc_bc, Rpsc, pool_scale[l], 256)
                poolw_sb, Rpw = aC("poolw_sb", [128, 2, 128], BF16), Res("poolw_sb")
                op("dve", lambda e: e.memset(poolw_sb[:], 0.0), [], [Rpw])
                for ph in range(2):
                    dma("pool", poolw_sb[ph * 64:(ph + 1) * 64, :, ph * 64:(ph + 1) * 64],
                        pool_w[l].rearrange("(k two) i o -> two i k o", two=2)[ph], [], [Rpw])
                acc_t = aC("acc", [128, 2, SEG])
                R_acc = [Res("acc0"), Res("acc1")]
                P2, RP2 = aC("P2", [128, 2, SEG + 16]), Res("P2")
                P4, RP4 = aC("P4", [128, 2, SEG + 16]), Res("P4")
                P8, RP8 = aC("P8", [128, SEG + 16]), Res("P8")
                P16, RP16 = aC("P16", [128, SEG + 16]), Res("P16")
                mixed, Rmixed = aC("mixed", [128, 2, SEG], BF16), Res("mixed")
                etmp, Retmp = aC("etmp", [128, 2, 8]), Res("etmp")
                ctmp_r = Ring(aC, "ctmp", [128, SEG], F32, 3)
                cv_r = Ring(aC, "cv", [128, 256], F32, 2)
                sgc_r = Ring(aC, "sgc", [128, 256], F32, 2)
                catcp_r = Ring(aC, "catcp", [128, 512], BF16, 2)
                lnr = {"st": Ring(aC, "cst", [128, 12], F32, 2), "mv": Ring(aC, "cmv", [128, 4], F32, 2)}
                cut("C1")
                seqs = [(cpT_l, R_cpl, T, C)]
                if not last:
                    seqs.append((cpT_c, R_cpc, C, 0))
                tile_ctr = 0
                for (buf, Rbuf, n, goff) in seqs:
                    for s0 in range(0, n, SEG):
                        seg = min(SEG, n - s0)
                        b0 = 16 + s0
                        for j in range(CONVW):
                            if j == 0:
                                op("dve", lambda e: e.tensor_scalar(out=acc_t[:, 0, 0:seg], in0=buf[:, 0, b0 - 15:b0 - 15 + seg], scalar1=cw_sb[:, 0, 0:1],
                                                                    scalar2=None, op0=ALU.mult), [Rbuf, Rcw], [R_acc[0]])
                                op("act", lambda e: e.activation(out=acc_t[:, 1, 0:seg], in_=buf[:, 1, b0 - 15:b0 - 15 + seg], func=AF.Copy,
                                                                 scale=cw_sb[:, 1, 0:1]), [Rbuf, Rcw], [R_acc[1]])
                                continue
                            op("dve", lambda e: e.scalar_tensor_tensor(out=acc_t[:, 0, 0:seg], in0=buf[:, 0, b0 - 15 + j:b0 - 15 + j + seg],
                                                                       scalar=cw_sb[:, 0, j:j + 1], in1=acc_t[:, 0, 0:seg],
                                                                       op0=ALU.mult, op1=ALU.add), [Rbuf, Rcw, R_acc[0]], [R_acc[0]])
                            ct, Rct = ctmp_r.get()
                            op("act", lambda e: e.activation(out=ct[:, 0:seg], in_=buf[:, 1, b0 - 15 + j:b0 - 15 + j + seg], func=AF.Copy,
                                                             scale=cw_sb[:, 1, j:j + 1]), [Rbuf, Rcw], [Rct])
                            op("pool", lambda e: e.tensor_tensor(out=acc_t[:, 1, 0:seg], in0=acc_t[:, 1, 0:seg], in1=ct[:, 0:seg], op=ALU.add),
                               [Rct, R_acc[1]], [R_acc[1]])
                        cut("C2")
                        n2 = seg + 16
                        op("dve", lambda e: e.tensor_tensor(out=P2[:, :, 0:n2], in0=buf[:, 2:4, b0 - 9:b0 - 9 + n2], in1=buf[:, 2:4, b0 - 8:b0 - 8 + n2],
                                                            op=ALU.add), [Rbuf], [RP2])
                        op("dve", lambda e: e.tensor_tensor(out=P4[:, :, 2:n2 - 2], in0=P2[:, :, 1:n2 - 3], in1=P2[:, :, 3:n2 - 1], op=ALU.add),
                           [RP2], [RP4])
                        op("dve", lambda e: e.tensor_tensor(out=P8[:, 4:n2 - 4], in0=P4[:, 1, 2:n2 - 6], in1=P4[:, 1, 6:n2 - 2], op=ALU.add),
                           [RP4], [RP8])
                        op("dve", lambda e: e.tensor_tensor(out=P16[:, 8:n2 - 8], in0=P8[:, 4:n2 - 12], in1=P8[:, 12:n2 - 4], op=ALU.add),
                           [RP8], [RP16])
                        srcs = {(0, 0): (P2[0:64, 0, 8:8 + seg], RP2), (1, 0): (P4[64:128, 0, 8:8 + seg], RP4),
                                (0, 1): (P8[0:64, 8:8 + seg], RP8), (1, 1): (P16[64:128, 8:8 + seg], RP16)}
                        for (ph, k), (sap, Rs) in srcs.items():
                            ps = slice(ph * 64, ph * 64 + 64)
                            op("dve", lambda e: e.scalar_tensor_tensor(out=mixed[ps, k, 0:seg], in0=sap, scalar=pinvw[ps, k:k + 1],
                                                                       in1=buf[ps, 2 + k, b0:b0 + seg], op0=ALU.mult, op1=ALU.subtract),
                               [Rs, R_pinvw, Rbuf], [Rmixed])
                            for (side, cond, c0) in ((0, s0 == 0, 0), (1, s0 + seg == n, seg - 8)):
                                if not cond:
                                    continue
                                sap8 = sap[:, c0:c0 + 8]
                                op("dve", lambda e: e.tensor_tensor(out=etmp[ps, k, :], in0=sap8, in1=pedge[ps, k, side, :], op=ALU.mult),
                                   [Rs, R_pedge], [Retmp])
                                op("dve", lambda e: e.tensor_tensor(out=mixed[ps, k, c0:c0 + 8], in0=etmp[ps, k, :], in1=buf[ps, 2 + k, b0 + c0:b0 + c0 + 8],
                                                                    op=ALU.subtract), [Retmp, Rbuf], [Rmixed])
                        cut("C3")
                        for j in range(seg // 128):
                            g0 = goff + s0 + j * 128
                            pb, Rpb = PBK[tile_ctr % 2]
                            pb2, Rpb2 = PBK[2 + tile_ctr % 2]
                            tile_ctr += 1
                            for k in range(2):
                                op("pe", lambda e: e.transpose(out=pb[:, k * 128:(k + 1) * 128], in_=acc_t[:, k, j * 128:(j + 1) * 128], identity=ident_f[:]),
                                   [R_acc[k], R_idf], [Rpb])
                            cv, Rcv = cv_r.get()
                            op("dve", lambda e: e.tensor_tensor(out=cv[:], in0=pb[:, 0:256], in1=convb_bc[:], op=ALU.add), [Rpb, Rcb], [Rcv])
                            layer_norm_tile(None, "pool", cv, Rcv, 256, clng_bc, Rclg, clnb_bc, Rclb, cv, Rcv, lnr)
                            catcp, Rcatcp = catcp_r.get()
                            sgc, Rsgc = sgc_r.get()
                            op("act", lambda e: e.activation(out=sgc[:], in_=cv[:], func=AF.Exp, scale=-1.0), [Rcv], [Rsgc])
                            op("act", lambda e: e.activation(out=sgc[:], in_=sgc[:], func=AF.Ln, bias=1.0, scale=1.0), [Rsgc], [Rsgc])
                            op("act", lambda e: e.activation(out=sgc[:], in_=sgc[:], func=AF.Exp, scale=-1.0), [Rsgc], [Rsgc])
                            op("dve", lambda e: e.tensor_tensor(out=catcp[:, 0:256], in0=cv[:], in1=sgc[:], op=ALU.mult), [Rcv, Rsgc], [Rcatcp])
                            cut("C3b")
                            for k in range(2):
                                op("pe", lambda e: e.matmul(pb2[:, k * 128:(k + 1) * 128], lhsT=mixed[:, k, j * 128:(j + 1) * 128],
                                                            rhs=poolw_sb[:, k, :], start=True, stop=True), [Rmixed, Rpw], [Rpb2])
                            cut("C3c")
                            op("dve", lambda e: e.tensor_tensor(out=catcp[:, 256:512], in0=pb2[:, 0:256], in1=psc_bc[:], op=ALU.mult),
                               [Rpb2, Rpsc, Rcatcp], [Rcatcp])
                            cut("C3d")
                            dma("sp", catcp_scr[g0:g0 + 128, :], catcp[:], [Rcatcp], [R_catcp])
                            cut("C4")
                S.barrier()
                S.release(mk_stC)
        if stop_after == "C":
            return finish(nc, S, [R_catcp])

        with ExitStack() as stBD:
            def aBD(name, shape, dt=F32):
                return stBD.enter_context(nc.sbuf_tensor("%s_L%d" % (name, l), shape, dt))
            attn_sb = aBD("attn_sb", [128, NTT, 512], BF16)
            R_attn = [Res("attn%d" % i) for i in range(NTT)]
            with ExitStack() as stB:
                mk_stB = S.mark()
                def aB(name, shape, dt=F32):
                    return stB.enter_context(nc.sbuf_tensor("%s_L%d" % (name, l), shape, dt))
                NJ = NP // 128
                dma("act", h2perm_scr.rearrange("(j p) d -> p j d", p=128), zer_b[:].unsqueeze(1).to_broadcast([128, NJ, D]), [R_zerb], [R_h2pz])
                dma("act", c8perm_scr.rearrange("(j p) e -> p j e", p=128), zer_f[:].unsqueeze(1).to_broadcast([128, NJ, 8]), [R_zerf], [R_c8pz])
                KT_r = Ring(aB, "KT", [97, TT], BF16, 2)
                V_r = Ring(aB, "V", [128, NTT, 80], BF16, 2)
                qT_r = Ring(aB, "qT", [97, 512], BF16, 3)
                PT_r = Ring(aB, "PT", [128, 512], BF16, 4)
                oT_r = Ring(aB, "oT", [65, 512], F32, 2)
                rec_r = Ring(aB, "rec", [128, 4], F32, 2)
                blocks = [(C + b * 512, 512, list(range(NTT))) for b in range(T // 512)]
                if not last:
                    blocks.append((0, C, list(range(NTC))))
                bi = 0
                si = 0
                for h in range(H):
                    KT, RKT = KT_r.get()
                    V, RV = V_r.get()
                    dma("sp", KT[:], kT_scr[h], [R_kT], [RKT])
                    dma("sp", V[:], v_scr[h], [R_v], [RV])
                    for (g0, n, chunks) in blocks:
                        qT, RqT = qT_r.get()
                        dma("sp", qT[0:96, 0:n], qT_scr[h, :, g0:g0 + n], [R_qT], [RqT])
                        dma("sp", qT[96:97, 0:n], mT_scr[h:h + 1, g0:g0 + n], [R_mT], [RqT])
                        pO, RpO = PBK[bi % 2]
                        LOOK = 2
                        pend = []

                        def issue_s(c):
                            nonlocal si
                            pS_, RpS_ = PBK[2 + si % 4]
                            si += 1
                            op("pe", lambda e: e.matmul(pS_[:, 0:n], lhsT=KT[:, c * 128:(c + 1) * 128], rhs=qT[:, 0:n], start=True, stop=True),
                               [RKT, RqT], [RpS_])
                            pend.append((pS_, RpS_))
                        for c in chunks[:LOOK]:
                            issue_s(c)
                        for ci, c in enumerate(chunks):
                            if ci + LOOK < len(chunks):
                                issue_s(chunks[ci + LOOK])
                            pS, RpS = pend.pop(0)
                            PT, RPT = PT_r.get()
                            op("act", lambda e: e.activation(out=PT[:, 0:n], in_=pS[:, 0:n], func=AF.Exp, scale=QS), [RpS], [RPT])
                            op("pe", lambda e: e.matmul(pO[0:65, 0:n], lhsT=V[:, c, 0:65], rhs=PT[:, 0:n], start=(ci == 0), stop=(ci == len(chunks) - 1)),
                               [RV, RPT], [RpO])
                        oT, RoT = oT_r.get()
                        op("dve", lambda e: e.tensor_copy(out=oT[:, 0:n], in_=pO[0:65, 0:n]), [RpO], [RoT])
                        pb, Rpb = PBK[6 + bi % 2]
                        bi += 1
                        nj = n // 128
                        for j in range(nj):
                            op("pe", lambda e: e.transpose(out=pb[:, j * 65:(j + 1) * 65], in_=oT[0:65, j * 128:(j + 1) * 128], identity=ident_f[0:65, 0:65]),
                               [RoT, R_idf], [Rpb])
                        pv = pb[:, 0:nj * 65].rearrange("p (j d) -> p j d", d=65)
                        rec, Rrec = rec_r.get()
                        op("dve", lambda e: e.reciprocal(out=rec[:, 0:nj], in_=pv[:, :, 64]), [Rpb], [Rrec])
                        i0 = g0 // 128
                        Rs_ = R_attn[i0:i0 + nj]
                        op("dve", lambda e: e.tensor_tensor(out=attn_sb[:, i0:i0 + nj, h * 64:(h + 1) * 64], in0=pv[:, :, 0:64],
                                                            in1=rec[:, 0:nj].unsqueeze(2).to_broadcast([128, nj, 64]), op=ALU.mult),
                           [Rpb, Rrec] + Rs_, Rs_)
                S.barrier()
                S.release(mk_stB)
            if stop_after == "B":
                dbg = nc.dram_tensor("attn_dbg", [128, NTT, 512], BF16, kind="ExternalOutput").ap()
                Rd = Res("attn_dbg")
                dma("sp", dbg, attn_sb[:], R_attn, [Rd])
                return finish(nc, S, [Rd])

            with ExitStack() as stD:
                mk_stD = S.mark()
                def aD(name, shape, dt=F32):
                    return stD.enter_context(nc.sbuf_tensor("%s_L%d" % (name, l), shape, dt))
                w_out_sb, R_wout = aD("w_out_sb", [128, 8, D], BF16), Res("w_out_sb")
                dma("pool", w_out_sb[:], w_out[l].rearrange("(k p) n -> p k n", p=128), [], [R_wout])
                wr_sb, R_wr = aD("wr_sb", [128, 8, 36]), Res("wr_sb")
                dma("sp", wr_sb[:, :, 0:4], w_rg[l].rearrange("(k p) n -> p k n", p=128), [], [R_wr])
                dma("sp", wr_sb[:, :, 4:36], w_re[l].rearrange("(k p) n -> p k n", p=128), [], [R_wr])
                br_bc, R_br = aD("br_bc", [128, 36]), Res("br_bc")
                dma("sp", br_bc[:, 0:4], b_rg[l].partition_broadcast(128), [], [R_br])
                dma("sp", br_bc[:, 4:36], b_re[l].partition_broadcast(128), [], [R_br])
                bcs = {}
                for (nm, j) in (("g1", 2), ("sh2", 3), ("sc2", 4)):
                    for r in range(2):
                        if r == 1 and last:
                            continue
                        t = aD("%s_%d" % (nm, r), [128, D])
                        Rr = Res("%s_%d" % (nm, r))
                        load_bc(t, Rr, ada_vec(l, r, j), D, [R_ada])
                        bcs[(nm, r)] = (t, Rr)
                ln1g_bc, R_l1g = aD("ln1g_bc", [128, D]), Res("ln1g_bc")
                ln1b_bc, R_l1b = aD("ln1b_bc", [128, D]), Res("ln1b_bc")
                load_bc(ln1g_bc, R_l1g, ln1_g[l], D)
                load_bc(ln1b_bc, R_l1b, ln1_b[l], D)
                catcp_r = Ring(aD, "catcpD", [128, 512], BF16, 2)
                catT_r = Ring(aD, "catT", [128, 8, 128], BF16, 2)
                xt_r = Ring(aD, "xtD", [128, D], F32, 3)
                y_r = Ring(aD, "yD", [128, D], F32, 2)
                x1_r = Ring(aD, "x1D", [128, D], F32, 2)
                h2_r = Ring(aD, "h2D", [128, D], F32, 2)
                h2Tf_r = Ring(aD, "h2Tf", [128, 8, 128], F32, 2)
                h2Tb_r = Ring(aD, "h2b", [128, D], BF16, 2)
                lg_r = Ring(aD, "lg", [128, 36], F32, 2)
                rs_r = Ring(aD, "rs", [128, 16], F32, 2)
                oh_r = Ring(aD, "oh", [128, 3, 32], F32, 2)
                comb_r = Ring(aD, "comb", [128, 32], F32, 2)
                combT_r = Ring(aD, "combT", [32, 128], F32, 2)
                lnr = {"st": Ring(aD, "dst", [128, 12], F32, 2), "mv": Ring(aD, "dmv", [128, 4], F32, 2)}
                cut("D1")
                def tileD(i, tcnt):
                    isctx = i < NTC
                    typ = 1 if isctx else 0
                    g0 = i * 128
                    catcp, Rcatcp = catcp_r.get()
                    dma("sp", catcp[:], catcp_scr[g0:g0 + 128, :], [R_catcp], [Rcatcp])
                    src, Rsrc = x_src(l, i)
                    xt, Rxt = xt_r.get()
                    dma("sp", xt[:], src, Rsrc, [Rxt])
                    yield
                    pb, Rpb = PBK[tcnt % 2]
                    pbv = bfv(pb)
                    for k in range(4):
                        op("pe", lambda e: e.transpose(out=pbv[:, k * 128:(k + 1) * 128], in_=attn_sb[:, i, k * 128:(k + 1) * 128], identity=ident_b[:]),
                           [R_attn[i], R_idb], [Rpb])
                    for k in range(4):
                        op("pe", lambda e: e.transpose(out=pbv[:, (4 + k) * 128:(5 + k) * 128], in_=catcp[:, k * 128:(k + 1) * 128], identity=ident_b[:]),
                           [Rcatcp, R_idb], [Rpb])
                    catT, RcatT = catT_r.get()
                    op("act", lambda e: e.copy(out=catT[:].rearrange("p k t -> p (k t)"), in_=pbv[:, 0:1024]), [Rpb], [RcatT])
                    cut("D2")
                    M = [PBK[2 + 2 * (tcnt % 2)], PBK[3 + 2 * (tcnt % 2)]]
                    for hf in range(2):
                        mt, Rm = M[hf]
                        for k in range(8):
                            op("pe", lambda e: e.matmul(mt[:, :], lhsT=catT[:, k, :], rhs=w_out_sb[:, k, hf * 512:(hf + 1) * 512], start=(k == 0), stop=(k == 7)),
                               [RcatT, R_wout], [Rm])
                    cut("D3")
                    yield
                    y, Ry = y_r.get()
                    g1t, Rg1 = bcs[("g1", typ)]
                    for hf in range(2):
                        mt, Rm = M[hf]
                        op("dve", lambda e: e.tensor_tensor(out=y[:, hf * 512:(hf + 1) * 512], in0=mt[:, :], in1=g1t[:, hf * 512:(hf + 1) * 512], op=ALU.mult),
                           [Rm, Rg1], [Ry])
                    op("dve", lambda e: e.scalar_tensor_tensor(out=y[:], in0=xt[:], scalar=ALPHA, in1=y[:], op0=ALU.mult, op1=ALU.add),
                       [Rxt, Ry], [Ry])
                    x1, Rx1 = x1_r.get()
                    layer_norm_tile(None, "pool", y, Ry, D, ln1g_bc, R_l1g, ln1b_bc, R_l1b, x1, Rx1, lnr)
                    dma("sp", xs_mix[g0:g0 + 128, :], x1[:], [Rx1], [R_xs_mix])
                    cut("D4")
                    h2, Rh2 = h2_r.get()
                    sc2t, Rsc2 = bcs[("sc2", typ)]
                    sh2t, Rsh2 = bcs[("sh2", typ)]
                    op("pool", lambda e: e.tensor_tensor(out=h2[:], in0=x1[:], in1=sc2t[:], op=ALU.mult), [Rx1, Rsc2], [Rh2])
                    op("dve", lambda e: e.tensor_tensor(out=h2[:], in0=h2[:], in1=sh2t[:], op=ALU.add), [Rh2, Rsh2], [Rh2])
                    yield
                    T6, RT6 = PBK[6]
                    T7, RT7 = PBK[7]
                    for k in range(8):
                        tb, Rtb = (T6, RT6) if k < 4 else (T7, RT7)
                        op("pe", lambda e: e.transpose(out=tb[:, (k % 4) * 128:(k % 4 + 1) * 128], in_=h2[:, k * 128:(k + 1) * 128], identity=ident_f[:]),
                           [Rh2, R_idf], [Rtb])
                    h2Tf, Rh2Tf = h2Tf_r.get()
                    h2b, Rh2b = h2Tb_r.get()
                    op("pool", lambda e: e.tensor_copy(out=h2b[:], in_=h2[:]), [Rh2], [Rh2b])
                    dma("sp", h2tok_scr[g0:g0 + 128, :], h2b[:], [Rh2b], [R_h2tok])
                    for hf, (tb, Rtb) in enumerate(((T6, RT6), (T7, RT7))):
                        op("act", lambda e: e.copy(out=h2Tf[:, hf * 4:hf * 4 + 4, :].rearrange("p k t -> p (k t)"), in_=tb[:, :]), [Rtb], [Rh2Tf])
                    cut("D5")
                    pr, Rpr = PBK[tcnt % 2]
                    for k in range(8):
                        op("pe", lambda e: e.matmul(pr[:, 0:36], lhsT=h2Tf[:, k, :], rhs=wr_sb[:, k, :], start=(k == 0), stop=(k == 7)),
                           [Rh2Tf, R_wr], [Rpr])
                    lg, Rlg = lg_r.get()
                    rs, Rrs = rs_r.get()
                    oh, Roh = oh_r.get()
                    op("dve", lambda e: e.tensor_tensor(out=lg[:], in0=pr[:, 0:36], in1=br_bc[:], op=ALU.add), [Rpr, R_br], [Rlg])
                    cut("D6")
                    yield
                    op("dve", lambda e: e.tensor_reduce(out=rs[:, 0:1], in_=lg[:, 0:4], axis=AX.X, op=ALU.max), [Rlg], [Rrs])
                    op("dve", lambda e: e.tensor_scalar(out=rs[:, 8:12], in0=lg[:, 0:4], scalar1=rs[:, 0:1], scalar2=None, op0=ALU.is_equal), [Rlg, Rrs], [Rrs])
                    op("dve", lambda e: e.tensor_copy(out=goh_all[:, i, :], in_=rs[:, 8:12]), [Rrs], [R_goh])
                    op("dve", lambda e: e.tensor_scalar(out=rs[:, 1:2], in0=rs[:, 0:1], scalar1=-1.0, scalar2=None, op0=ALU.mult), [Rrs], [Rrs])
                    op("act", lambda e: e.activation(out=rs[:, 12:16], in_=lg[:, 0:4], func=AF.Exp, bias=rs[:, 1:2], scale=1.0, accum_out=rs[:, 2:3]),
                       [Rlg, Rrs], [Rrs])
                    op("dve", lambda e: e.reciprocal(out=rs[:, 2:3], in_=rs[:, 2:3]), [Rrs], [Rrs])
                    op("dve", lambda e: e.tensor_scalar(out=rs[:, 8:12], in0=rs[:, 8:12], scalar1=-1.0, scalar2=-NEG, op0=ALU.add, op1=ALU.mult), [Rrs], [Rrs])
                    elm = oh[:, 0, :]
                    op("dve", lambda e: e.tensor_tensor(out=elm.rearrange("p (g x) -> p g x", g=4), in0=lg[:, 4:36].rearrange("p (g x) -> p g x", g=4),
                                                        in1=rs[:, 8:12].unsqueeze(2).to_broadcast([128, 4, 8]), op=ALU.add), [Rlg, Rrs], [Roh])
                    op("dve", lambda e: e.tensor_reduce(out=rs[:, 3:4], in_=elm, axis=AX.X, op=ALU.max), [Roh], [Rrs])
                    op("dve", lambda e: e.tensor_scalar(out=oh[:, 1, :], in0=elm, scalar1=rs[:, 3:4], scalar2=None, op0=ALU.is_equal), [Roh, Rrs], [Roh])
                    op("dve", lambda e: e.scalar_tensor_tensor(out=elm, in0=oh[:, 1, :], scalar=NEG, in1=elm, op0=ALU.mult, op1=ALU.add), [Roh], [Roh])
                    op("dve", lambda e: e.tensor_reduce(out=rs[:, 4:5], in_=elm, axis=AX.X, op=ALU.max), [Roh], [Rrs])
                    op("dve", lambda e: e.tensor_scalar(out=oh[:, 2, :], in0=elm, scalar1=rs[:, 4:5], scalar2=None, op0=ALU.is_equal), [Roh, Rrs], [Roh])
                    op("dve", lambda e: e.tensor_tensor(out=rs[:, 5:6], in0=rs[:, 4:5], in1=rs[:, 3:4], op=ALU.subtract), [Rrs], [Rrs])
                    op("act", lambda e: e.activation(out=rs[:, 5:6], in_=rs[:, 5:6], func=AF.Exp), [Rrs], [Rrs])
                    op("dve", lambda e: e.tensor_scalar(out=rs[:, 6:7], in0=rs[:, 5:6], scalar1=1.0, scalar2=None, op0=ALU.add), [Rrs], [Rrs])
                    op("dve", lambda e: e.reciprocal(out=rs[:, 6:7], in_=rs[:, 6:7]), [Rrs], [Rrs])
                    op("dve", lambda e: e.tensor_tensor(out=rs[:, 6:7], in0=rs[:, 6:7], in1=rs[:, 2:3], op=ALU.mult), [Rrs], [Rrs])
                    op("dve", lambda e: e.tensor_tensor(out=rs[:, 7:8], in0=rs[:, 6:7], in1=rs[:, 5:6], op=ALU.mult), [Rrs], [Rrs])
                    cut("D7")
                    comb, Rcomb = comb_r.get()
                    op("dve", lambda e: e.tensor_scalar(out=comb[:], in0=oh[:, 1, :], scalar1=rs[:, 6:7], scalar2=None, op0=ALU.mult), [Roh, Rrs], [Rcomb])
                    op("dve", lambda e: e.scalar_tensor_tensor(out=comb[:], in0=oh[:, 2, :], scalar=rs[:, 7:8], in1=comb[:], op0=ALU.mult, op1=ALU.add),
                       [Roh, Rrs, Rcomb], [Rcomb])
                    cut("D8")
                    op("dve", lambda e: e.tensor_reduce(out=c8_all[:, i, :], in_=comb[:].rearrange("p (g j) -> p j g", g=4), axis=AX.X, op=ALU.add),
                       [Rcomb], [R_c8])

                op("dve", lambda e: e.memset(goh_all[:], 0.0), [], [R_goh])
                tilesD = [i for i in range(NTT) if not (i < NTC and last)]
                run_skewed([tileD(i, tc) for tc, i in enumerate(tilesD)])
                S.barrier()
                S.release(mk_stD)
        if stop_after == "D":
            return finish(nc, S, [R_xs_mix, R_h2tok])

        tilesD = [i for i in range(NTT) if not (i < NTC and last)]
        with ExitStack() as stS:
            mk_stS = S.mark()

            def aS(name, shape, dt=F32):
                return stS.enter_context(nc.sbuf_tensor("%s_L%d" % (name, l), shape, dt))
            CUM = srt[:, 0:4]
            op("dve", lambda e: e.memset(srt[:], 0.0), [], [R_srt])
            op("dve", lambda e: e.memset(dest_f[:], 0.0), [], [R_destf])
            tmp4_r = Ring(aS, "tmp4", [128, 4], F32, 2)
            for n_, i in enumerate(tilesD):
                pr, Rpr = PBK[n_ % 2]
                op("pe", lambda e: e.matmul(pr[:, 0:4], lhsT=tri_sb[:], rhs=goh_all[:, i, :], start=True, stop=True), [R_tri, R_goh], [Rpr])
                op("pe", lambda e: e.matmul(pr[:, 4:8], lhsT=ones_sb[:], rhs=goh_all[:, i, :], start=True, stop=True), [R_ones, R_goh], [Rpr])
                t4, Rt4 = tmp4_r.get()
                op("dve", lambda e: e.tensor_tensor(out=t4[:], in0=pr[:, 0:4], in1=CUM, op=ALU.add), [Rpr, R_srt], [Rt4])
                op("dve", lambda e: e.tensor_tensor(out=t4[:], in0=t4[:], in1=goh_all[:, i, :], op=ALU.mult), [Rt4, R_goh], [Rt4])
                op("dve", lambda e: e.tensor_reduce(out=dest_f[:, i:i + 1], in_=t4[:], axis=AX.X, op=ALU.add), [Rt4], [R_destf])
                op("dve", lambda e: e.tensor_tensor(out=CUM, in0=pr[:, 4:8], in1=CUM, op=ALU.add), [Rpr, R_srt], [R_srt])
            tk, Rtk = aS("tk", [128, NB]), Res("tk")
            for g in range(4):
                op("dve", lambda e: e.tensor_scalar(out=tk[:], in0=thr_sb[:], scalar1=srt[:, g:g + 1], scalar2=None, op0=ALU.is_lt), [R_thr, R_srt], [Rtk])
                op("dve", lambda e: e.tensor_reduce(out=srt[:, 8 + g:9 + g], in_=tk[:], axis=AX.X, op=ALU.add), [Rtk], [R_srt])
            op("dve", lambda e: e.memset(srt[:, 16:17], 0.0), [R_srt], [R_srt])
            for g in range(1, 4):
                op("dve", lambda e: e.tensor_tensor(out=srt[:, 16 + g:17 + g], in0=srt[:, 15 + g:16 + g], in1=srt[:, 7 + g:8 + g], op=ALU.add), [R_srt], [R_srt])
            op("dve", lambda e: e.tensor_scalar(out=srt[:, 24:28], in0=srt[:, 16:20], scalar1=512.0, scalar2=None, op0=ALU.mult), [R_srt], [R_srt])
            for g in range(4):
                op("dve", lambda e: e.scalar_tensor_tensor(out=dest_f[:], in0=goh_all[:, :, g], scalar=srt[:, 24 + g:25 + g], in1=dest_f[:],
                                                           op0=ALU.mult, op1=ALU.add), [R_goh, R_srt, R_destf], [R_destf])
            op("dve", lambda e: e.tensor_copy(out=dest_i[:], in_=dest_f[:]), [R_destf], [R_desti])
            gb, Rgb = aS("gb", [128, NB]), Res("gb")
            op("dve", lambda e: e.memset(gb[:], 0.0), [], [Rgb])
            for g in range(1, 4):
                op("dve", lambda e: e.tensor_scalar(out=tk[:], in0=blk_sb[:], scalar1=srt[:, 16 + g:17 + g], scalar2=None, op0=ALU.is_ge), [R_blk, R_srt], [Rtk])
                op("dve", lambda e: e.tensor_tensor(out=gb[:], in0=gb[:], in1=tk[:], op=ALU.add), [Rtk, Rgb], [Rgb])
            op("dve", lambda e: e.tensor_scalar(out=gb[:], in0=gb[:], scalar1=1024.0, scalar2=float(l * NE * 128), op0=ALU.mult, op1=ALU.add), [Rgb], [Rgb])
            op("dve", lambda e: e.tensor_tensor(out=widx_f[:], in0=gb[:].unsqueeze(2).to_broadcast([128, NB, 8]),
                                                in1=jp_sb[:].unsqueeze(1).to_broadcast([128, NB, 8]), op=ALU.add), [Rgb, R_jp], [R_widxf])
            op("dve", lambda e: e.tensor_copy(out=widx_i[:], in_=widx_f[:].rearrange("p b j -> p (b j)")), [R_widxf], [R_widxi])
            h2r_r = Ring(aS, "h2r", [128, D], BF16, 3)
            for i in tilesD:
                h2r, Rh2r = h2r_r.get()
                dma("sp", h2r[:], h2tok_scr[i * 128:(i + 1) * 128, :], [R_h2tok], [Rh2r])
                S.idma(h2perm_scr, h2r[:], dest_i[:, i:i + 1], True, [Rh2r, R_desti, R_h2pz], [R_h2perm])
                S.idma(c8perm_scr, c8_all[:, i, :], dest_i[:, i:i + 1], True, [R_c8, R_desti, R_c8pz], [R_c8perm])
            S.barrier()
            S.release(mk_stS)
        if stop_after == "S":
            return finish(nc, S, [R_h2perm, R_c8perm])

        with ExitStack() as stE:
            mk_stE = S.mark()

            def aE(name, shape, dt=F32):
                return stE.enter_context(nc.sbuf_tensor("%s_L%d" % (name, l), shape, dt))
            if not last:
                load_mix_weights(l + 1)
            hp_r = Ring(aE, "hp", [128, 4, D], BF16, 2)
            h2Tb_r = Ring(aE, "h2TbE", [128, 8, 512], BF16, 2)
            c8_r = Ring(aE, "c8b", [128, 4, 8], F32, 2)
            c8T_r = Ring(aE, "c8T", [8, 512], F32, 2)
            wg_r = Ring(aE, "wg", [128, 8 * DE], BF16, 3)
            wu_r = Ring(aE, "wu", [128, 8 * DE], BF16, 3)
            wd_r = Ring(aE, "wd", [128, 2 * D], BF16, 3)
            cb_r = Ring(aE, "cb", [128, 512], F32, 3)
            sg_r = Ring(aE, "sg", [128, 512], F32, 3)
            hid = aE("hidT_all", [128, 16, 512], BF16)
            R_hid = [Res("hid%d" % e) for e in range(8)]
            yo_r = Ring(aE, "yo", [128, D], F32, 3)
            ecnt = 0
            NB_l = ((T if last else TT) + 4 * 511) // 512
            for b in range(NB_l):
                hp, Rhp = hp_r.get()
                dma("sp", hp[:], h2perm_scr[b * 512:(b + 1) * 512, :].rearrange("(j p) d -> p j d", p=128), [R_h2perm], [Rhp])
                c8, Rc8 = c8_r.get()
                dma("sp", c8[:], c8perm_scr[b * 512:(b + 1) * 512, :].rearrange("(j p) e -> p j e", p=128), [R_c8perm], [Rc8])
                hb_, Rhb_ = h2Tb_r.get()
                for j in range(4):
                    pb, Rpb = PBK[j]
                    pbv = bfv(pb)
                    for k in range(8):
                        op("pe", lambda e: e.transpose(out=pbv[:, k * 128:(k + 1) * 128], in_=hp[:, j, k * 128:(k + 1) * 128], identity=ident_b[:]),
                           [Rhp, R_idb], [Rpb])
                    eng = "act" if j % 2 == 0 else "dve"
                    if eng == "act":
                        op("act", lambda e: e.copy(out=hb_[:, :, j * 128:(j + 1) * 128], in_=pbv[:, 0:1024].rearrange("p (k t) -> p k t", k=8)), [Rpb], [Rhb_])
                    else:
                        op("dve", lambda e: e.tensor_copy(out=hb_[:, :, j * 128:(j + 1) * 128], in_=pbv[:, 0:1024].rearrange("p (k t) -> p k t", k=8)), [Rpb], [Rhb_])
                pc, Rpc = PBK[4]
                for j in range(4):
                    op("pe", lambda e: e.transpose(out=pc[0:8, j * 128:(j + 1) * 128], in_=c8[:, j, :], identity=ident_f[:]), [Rc8, R_idf], [Rpc])
                c8T, Rc8T = c8T_r.get()
                op("dve", lambda e: e.tensor_copy(out=c8T[:], in_=pc[0:8, 0:512]), [Rpc], [Rc8T])
                dma("sp", cbT_scr[b], c8T[:], [Rc8T], [R_cbT])
                for j_ in range(8):
                    wg, Rwg = wg_r.get()
                    wu, Rwu = wu_r.get()
                    cb, Rcb_ = cb_r.get()
                    ix = widx_i[:, b * 8 + j_:b * 8 + j_ + 1]
                    S.idma(wg[:], wg_scr, ix, False, [R_wg, R_widxi], [Rwg])
                    S.idma(wu[:], wu_scr, ix, False, [R_wu, R_widxi], [Rwu])
                    dma("sp", cb[:], cbT_scr[b, j_, :].partition_broadcast(128), [R_cbT], [Rcb_])
                    wgv = wg[:].rearrange("p (k h) -> p k h", k=8)
                    wuv = wu[:].rearrange("p (k h) -> p k h", k=8)
                    base = 4 * (ecnt % 2)
                    ecnt += 1
                    for hc in range(2):
                        gt, Rg = PBK[base + hc]
                        ut, Ru = PBK[base + 2 + hc]
                        for k in range(8):
                            op("pe", lambda e: e.matmul(gt[:, :], lhsT=wgv[:, k, hc * 128:(hc + 1) * 128], rhs=hb_[:, k, :], start=(k == 0), stop=(k == 7)),
                               [Rwg, Rhb_], [Rg])
                        for k in range(8):
                            op("pe", lambda e: e.matmul(ut[:, :], lhsT=wuv[:, k, hc * 128:(hc + 1) * 128], rhs=hb_[:, k, :], start=(k == 0), stop=(k == 7)),
                               [Rwu, Rhb_], [Ru])
                    for hc in range(2):
                        gt, Rg = PBK[base + hc]
                        ut, Ru = PBK[base + 2 + hc]
                        sg, Rsg = sg_r.get()
                        op("act", lambda e: e.activation(out=sg[:], in_=gt[:, :], func=AF.Silu), [Rg], [Rsg])
                        op("dve", lambda e: e.tensor_tensor(out=sg[:], in0=ut[:, :], in1=sg[:], op=ALU.mult), [Ru, Rsg], [Rsg])
                        op("dve", lambda e: e.tensor_tensor(out=hid[:, 2 * j_ + hc, :], in0=sg[:], in1=cb[:], op=ALU.mult),
                           [Rsg, Rcb_], [R_hid[j_]])
                for j_ in range(8):
                    wd, Rwd = wd_r.get()
                    ix = widx_i[:, b * 8 + j_:b * 8 + j_ + 1]
                    S.idma(wd[:], wd_scr, ix, False, [R_wd, R_widxi], [Rwd])
                    wdv = wd[:].rearrange("p (c d) -> p c d", c=2)
                    for hc in range(2):
                        for j in range(4):
                            for dh in range(2):
                                yt, Ry_ = PBK[j * 2 + dh]
                                op("pe", lambda e: e.matmul(yt[:, :], lhsT=hid[:, 2 * j_ + hc, j * 128:(j + 1) * 128], rhs=wdv[:, hc, dh * 512:(dh + 1) * 512],
                                                            start=(j_ == 0 and hc == 0), stop=(j_ == 7 and hc == 1)), [R_hid[j_], Rwd], [Ry_])
                for j in range(4):
                    yo, Ryo = yo_r.get()
                    for dh in range(2):
                        yt, Ry_ = PBK[j * 2 + dh]
                        if dh == 0:
                            op("act", lambda e: e.copy(out=yo[:, 0:512], in_=yt[:, :]), [Ry_], [Ryo])
                        else:
                            op("dve", lambda e: e.tensor_copy(out=yo[:, 512:1024], in_=yt[:, :]), [Ry_], [Ryo])
                    r0 = b * 512 + j * 128
                    dma("sp", yperm_scr[r0:r0 + 128, :], yo[:], [Ryo], [R_yperm])
            S.barrier()
            S.release(mk_stE)
        if stop_after == "E":
            return finish(nc, S, [R_yperm])

        with ExitStack() as stF:
            mk_stF = S.mark()

            def aF(name, shape, dt=F32):
                return stF.enter_context(nc.sbuf_tensor("%s_L%d" % (name, l), shape, dt))
            g2bc = {}
            for r in range(2):
                if r == 1 and last:
                    continue
                t = aF("g2_%d" % r, [128, D])
                Rr = Res("g2_%d" % r)
                load_bc(t, Rr, ada_vec(l, r, 5), D, [R_ada])
                g2bc[r] = (t, Rr)
            ln2g_bc, R_l2g = aF("ln2g_bc", [128, D]), Res("ln2g_bc")
            ln2b_bc, R_l2b = aF("ln2b_bc", [128, D]), Res("ln2b_bc")
            load_bc(ln2g_bc, R_l2g, ln2_g[l], D)
            load_bc(ln2b_bc, R_l2b, ln2_b[l], D)
            xt_r = Ring(aF, "xtF", [128, D], F32, 3)
            yg_r = Ring(aF, "ygF", [128, D], F32, 3)
            o_r = Ring(aF, "oF", [128, D], F32, 3)
            lnr = {"st": Ring(aF, "fst", [128, 12], F32, 3), "mv": Ring(aF, "fmv", [128, 4], F32, 3)}

            def tileF(i):
                typ = 1 if i < NTC else 0
                gg = i * 128
                xt, Rxt = xt_r.get()
                dma("sp", xt[:], xs_mix[gg:gg + 128, :], [R_xs_mix], [Rxt])
                yg, Ryg = yg_r.get()
                S.idma(yg[:], yperm_scr, dest_i[:, i:i + 1], False, [R_yperm, R_desti], [Ryg])
                yield
                g2t, Rg2 = g2bc[typ]
                op("dve", lambda e: e.tensor_tensor(out=yg[:], in0=yg[:], in1=g2t[:], op=ALU.mult), [Ryg, Rg2], [Ryg])
                op("dve", lambda e: e.scalar_tensor_tensor(out=yg[:], in0=xt[:], scalar=ALPHA, in1=yg[:], op0=ALU.mult, op1=ALU.add), [Rxt, Ryg], [Ryg])
                o, Ro = o_r.get()
                layer_norm_tile(None, "pool", yg, Ryg, D, ln2g_bc, R_l2g, ln2b_bc, R_l2b, o, Ro, lnr)
                if last:
                    dma("sp", out_d[gg - C:gg - C + 128, :], o[:], [Ro], [R_out])
                else:
                    dma("sp", xs_out[l % 2][gg:gg + 128, :], o[:], [Ro], [R_xs_out[l % 2]])

            run_skewed([tileF(i) for i in tilesD])
            S.barrier()
            S.release(mk_stF)
    return finish(nc, S, [R_out])


def finish(nc, S, ress):
    S.barrier()
    S.wait_all("sp", ress)
    return nc


def _rope_tables(T, C):
    rows = T // GRID_W
    row = np.repeat(np.arange(rows), GRID_W).astype(np.float32)
    col = np.tile(np.arange(GRID_W), rows).astype(np.float32)
    d_axis = DR // 2
    inv_freq = np.power(np.float32(10000.0), -np.arange(0, d_axis, 2, dtype=np.float32) / np.float32(d_axis)).astype(np.float32)

    def ax(p):
        a = p[:, None] * inv_freq[None, :]
        return np.concatenate([a, a], -1)

    ang = np.concatenate([ax(row), ax(col)], -1).astype(np.float32)
    cos = np.cos(ang).astype(np.float32)
    sin = np.sin(ang).astype(np.float32)
    sgn = np.tile(np.concatenate([-np.ones(8), np.ones(8)]), 2).astype(np.float32)
    tab = np.zeros((T + C, 2, DR), np.float32)
    tab[:C, 0, :] = 1.0
    tab[C:, 0, :] = cos
    tab[C:, 1, :] = sin * sgn[None, :]
    return tab


def _pool_tables():
    wins = (2, 4, 8, 16)
    edge = np.zeros((128, 2, 2, 8), np.float32)
    invw = np.zeros((128, 2), np.float32)
    for k in range(2):
        for ph in range(2):
            w = wins[2 * k + ph]
            ps = slice(ph * 64, ph * 64 + 64)
            invw[ps, k] = 1.0 / w
            for j in range(8):
                t = j
                cnt = (t + w // 2 - 1) - max(t - w // 2, 0) + 1
                edge[ps, k, 0, j] = 1.0 / cnt
                r = 7 - j
                hi = min(w // 2 - 1, r)
                cnt = hi + w // 2 + 1
                edge[ps, k, 1, j] = 1.0 / cnt
    return edge, invw


def _sort_tables(T, C):
    TT = T + C
    NB = (TT + 4 * 511 + 511) // 512
    tri = np.triu(np.ones((128, 128), np.float32), k=1)
    thr = np.broadcast_to((np.arange(NB, dtype=np.float32) * 512.0)[None, :], (128, NB)).copy()
    blk = np.broadcast_to(np.arange(NB, dtype=np.float32)[None, :], (128, NB)).copy()
    jp = (np.arange(8, dtype=np.float32)[None, :] * 128.0 + np.arange(128, dtype=np.float32)[:, None]).astype(np.float32)
    return {"tri": tri, "thr_bc": thr, "blk_bc": blk, "jp": jp}


_CACHE = {}


def _consts(T, C):
    edge, invw = _pool_tables()
    return {
        "ident": np.eye(128, dtype=np.float32),
        "rope_cs": _rope_tables(T, C),
        "pool_edge": edge,
        "pool_invw": invw,
        **_sort_tables(T, C),
    }


_WKEYS = ["w_ada", "b_ada", "w_in", "g_q", "w_uq", "g_kv", "w_ukv", "conv_w", "conv_b", "conv_ln_g", "conv_ln_b", "pool_w",
          "pool_scale", "w_out", "ln1_g", "ln1_b", "w_router_group", "b_router_group", "w_router_expert", "b_router_expert",
          "w_gate", "w_up", "w_down", "ln2_g", "ln2_b"]


def make_in_maps(inputs, T, C, ncores):
    consts = _consts(T, C)
    shared = {k: np.ascontiguousarray(np.asarray(inputs[k], dtype=np.float32)) for k in _WKEYS}
    maps = []
    for b in range(ncores):
        m = dict(shared)
        m.update(consts)
        m["x"] = np.ascontiguousarray(np.asarray(inputs["x"][b], dtype=np.float32))
        m["ctx"] = np.ascontiguousarray(np.asarray(inputs["ctx"][b], dtype=np.float32))
        m["cvec"] = np.ascontiguousarray(np.stack([np.asarray(inputs["c"][b]), np.asarray(inputs["c_ctx"])]).astype(np.float32))
        maps.append(m)
    return maps


def kernel(**inputs):
    x = np.asarray(inputs["x"])
    B, T, _ = x.shape
    C = np.asarray(inputs["ctx"]).shape[1]
    L = np.asarray(inputs["w_ada"]).shape[0]
    key = (T, C, L)
    if key not in _CACHE:
        _CACHE[key] = build(T, C, L)
    nc = _CACHE[key]
    maps = make_in_maps(inputs, T, C, B)
    res = run_bass_kernel_spmd(nc, maps, core_ids=list(range(B)))
    return np.stack([np.asarray(r["out"]) for r in res.results], axis=0).astype(np.float32)
```

```python
import math
from contextlib import ExitStack
import numpy as np
import concourse.bass as bass
import concourse.mybir as mybir
from concourse.bass_utils import run_bass_kernel_spmd

F32 = mybir.dt.float32
BF16 = mybir.dt.bfloat16
AF = mybir.ActivationFunctionType
ALU = mybir.AluOpType
AX = mybir.AxisListType

D = 1024
H = 8
DQ = 384
DKV = 256
DR = 32
DIN = 1440
NE = 32
DE = 256
GRID_W = 64
CONVW = 31
LN_EPS = 1e-5
RMS_EPS = 1e-6
NEG = -1.0e30


class Res:
    __slots__ = ("name", "w", "r", "sem", "cnt", "excl", "multi")

    def __init__(self, name, excl=False, multi=False):
        self.multi = False
        self.name = name
        self.w = None
        self.r = []
        self.sem = None
        self.cnt = 0
        self.excl = excl


class Sched:
    def __init__(self, nc):
        self.nc = nc
        self.eng = {"pe": nc.tensor, "act": nc.scalar, "dve": nc.vector, "pool": nc.gpsimd, "sp": nc.sync}
        self.sems = {}
        self.cnt = {}
        self.known = {}
        self.dma_res = []
        self.free_sems = []
        self.free_sw = []
        self.is_sw = {}
        self.nalloc = 0
        self.nwait = 0
        for e in self.eng:
            self.sems[e] = nc.alloc_semaphore("e_" + e)
            self.cnt[e] = 0
            self.known[e] = {}

    def _waits(self, e, reads, writes):
        deps = {}

        def add(ev):
            if ev is None:
                return
            k, v = ev
            if deps.get(k, 0) < v:
                deps[k] = v

        for r in reads:
            add(r.w)
        for w in writes:
            if not w.multi:
                add(w.w)
            for ev in w.r:
                add(ev)
        kn = self.known[e]
        for k, v in deps.items():
            if kn.get(k, 0) >= v:
                continue
            if e == "pe" and k == "pe":
                continue
            kn[k] = v
            sem = self.sems[k] if isinstance(k, str) else k.sem
            self.eng[e].wait_ge(sem, v)
            self.nwait += 1

    @staticmethod
    def _commit(ev, reads, writes):
        for r in reads:
            r.r.append(ev)
            if len(r.r) > 48:
                best = {}
                for k, v in r.r:
                    if best.get(k, 0) < v:
                        best[k] = v
                r.r = list(best.items())
        for w in writes:
            w.w = ev
            w.r = []

    def op(self, e, fn, reads=(), writes=()):
        if any(r.excl for r in reads):
            writes = list(writes) + [r for r in reads if r.excl and r not in writes]
            reads = [r for r in reads if not r.excl]
        self._waits(e, reads, writes)
        ins = fn(self.eng[e])
        self.cnt[e] += 1
        ins.then_inc(self.sems[e], 1)
        self._commit((e, self.cnt[e]), reads, writes)

    def dma(self, e, out, in_, reads, writes, **kw):
        dst = writes[0]
        self._waits(e, reads, writes)
        self.ensure(dst, sw=(e == "pool"))
        dst.cnt += 16
        self.eng[e].dma_start(out=out, in_=in_, **kw).then_inc(dst.sem, 16)
        self._commit((dst, dst.cnt), reads, writes)

    def idma(self, out, in_, idx_ap, scatter, reads, writes):
        import concourse.bass as _b
        dst = writes[0]
        self._waits("pool", reads, writes)
        self.ensure(dst, sw=True)
        dst.cnt += 16
        off = _b.IndirectOffsetOnAxis(ap=idx_ap, axis=0)
        if scatter:
            ins = self.nc.gpsimd.indirect_dma_start(out=out, out_offset=off, in_=in_, in_offset=None)
        else:
            ins = self.nc.gpsimd.indirect_dma_start(out=out, out_offset=None, in_=in_, in_offset=off)
        ins.then_inc(dst.sem, 16)
        self._commit((dst, dst.cnt), reads, writes)

    def ensure(self, dst, sw=False):
        if dst.sem is None:
            fl = self.free_sw if sw else self.free_sems
            self.is_sw[id(dst)] = sw
            if fl:
                dst.sem, dst.cnt = fl.pop()
            else:
                self.nalloc += 1
                dst.sem = self.nc.alloc_semaphore("d%d_%s" % (self.nalloc, dst.name))
                dst.cnt = 0
            self.dma_res.append(dst)

    def mark(self):
        return len(self.dma_res)

    def release(self, mark):
        for r in self.dma_res[mark:]:
            (self.free_sw if self.is_sw.get(id(r)) else self.free_sems).append((r.sem, r.cnt))
            r.sem = None
        del self.dma_res[mark:]

    def barrier(self):
        for e in self.eng:
            kn = self.known[e]
            for k in self.eng:
                if k == e:
                    continue
                v = self.cnt[k]
                if v > 0 and kn.get(k, 0) < v:
                    kn[k] = v
                    self.eng[e].wait_ge(self.sems[k], v)
            for r in self.dma_res:
                if r.cnt > 0 and kn.get(r, 0) < r.cnt:
                    kn[r] = r.cnt
                    self.eng[e].wait_ge(r.sem, r.cnt)

    def wait_all(self, e, ress):
        self._waits(e, ress, ())


class Ring:
    def __init__(self, alloc, name, shape, dt, n):
        self.bufs = []
        for i in range(n):
            nm = "%s_%d" % (name, i)
            self.bufs.append((alloc(nm, shape, dt), Res(nm)))
        self.i = 0

    def get(self):
        b = self.bufs[self.i % len(self.bufs)]
        self.i += 1
        return b


class _Cut(Exception):
    pass


def run_skewed(gens):
    active = []
    it = iter(gens)
    while True:
        g = next(it, None)
        if g is not None:
            active.append(g)
        elif not active:
            break
        for g_ in list(reversed(active)):
            try:
                next(g_)
            except StopIteration:
                active.remove(g_)


def build(T, C, L, debug=False, stop_after=None):
    st = {}
    try:
        return _build(T, C, L, debug, stop_after, st)
    except _Cut:
        return finish(st["nc"], st["S"], [])


def _build(T, C, L, debug, stop_after, st_):
    NTL = T // 128
    NTC = C // 128
    TT = T + C
    NTT = NTL + NTC
    ALPHA = float((2 * L) ** 0.25)
    QS = 1.0 / math.sqrt(96.0)
    SEG = min(1024, T)

    nc = bass.Bass("TRN2", target_bir_lowering=False)
    S = Sched(nc)
    op = S.op
    dma = S.dma
    st_["nc"] = nc
    st_["S"] = S

    def cut(tag):
        if stop_after == tag:
            raise _Cut()

    def din(name, shape, dt=F32):
        return nc.dram_tensor(name, shape, dt, kind="ExternalInput").ap()

    def dscr(name, shape, dt=F32):
        return nc.dram_tensor(name, shape, dt, kind=("ExternalOutput" if debug else "Internal")).ap()

    x_in = din("x", [T, D])
    ctx_in = din("ctx", [C, D])
    cvec = din("cvec", [2, D])
    w_ada = din("w_ada", [L, D, 6 * D])
    b_ada = din("b_ada", [L, 6 * D])
    w_in = din("w_in", [L, D, DIN])
    g_q = din("g_q", [L, DQ])
    w_uq = din("w_uq", [L, DQ, 768])
    g_kv = din("g_kv", [L, DKV])
    w_ukv = din("w_ukv", [L, DKV, 1024])
    conv_w = din("conv_w", [L, CONVW, 256])
    conv_b = din("conv_b", [L, 256])
    conv_ln_g = din("conv_ln_g", [L, 256])
    conv_ln_b = din("conv_ln_b", [L, 256])
    pool_w = din("pool_w", [L, 4, 64, 64])
    pool_scale = din("pool_scale", [L, 256])
    w_out = din("w_out", [L, D, D])
    ln1_g = din("ln1_g", [L, D])
    ln1_b = din("ln1_b", [L, D])
    w_rg = din("w_router_group", [L, D, 4])
    b_rg = din("b_router_group", [L, 4])
    w_re = din("w_router_expert", [L, D, NE])
    b_re = din("b_router_expert", [L, NE])
    w_gate = din("w_gate", [L, NE, D, DE])
    w_up = din("w_up", [L, NE, D, DE])
    w_down = din("w_down", [L, NE, DE, D])
    ln2_g = din("ln2_g", [L, D])
    ln2_b = din("ln2_b", [L, D])
    ident_d = din("ident", [128, 128])
    rope_d = din("rope_cs", [TT, 2, DR])
    pedge_d = din("pool_edge", [128, 2, 2, 8])
    pinvw_d = din("pool_invw", [128, 2])

    NB = (TT + 4 * 511 + 511) // 512
    NP = NB * 512
    I32 = mybir.dt.int32
    tri_d = din("tri", [128, 128])
    thr_d = din("thr_bc", [128, NB])
    blk_d = din("blk_bc", [128, NB])
    jp_d = din("jp", [128, 8])
    out_d = nc.dram_tensor("out", [T, D], F32, kind="ExternalOutput").ap()
    h2tok_scr = dscr("h2tok_scr", [TT, D], BF16)
    h2perm_scr = dscr("h2perm_scr", [NP, D], BF16)
    c8perm_scr = dscr("c8perm_scr", [NP, 8])
    cbT_scr = dscr("cbT_scr", [NB, 8, 512])
    yperm_scr = dscr("yperm_scr", [NP, D])
    R_h2tok = Res("h2tok_scr", multi=True)
    R_h2perm = Res("h2perm_scr", multi=True)
    R_c8perm = Res("c8perm_scr", multi=True)
    R_cbT = Res("cbT_scr", multi=True)
    R_yperm = Res("yperm_scr", multi=True)

    ada_scr = dscr("ada_scr", [L, 2, 6 * D])
    xs_mix = dscr("xs_mix", [TT, D])
    xs_out = [dscr("xs_out0", [TT, D]), dscr("xs_out1", [TT, D])]
    kT_scr = dscr("kT_scr", [H, 97, TT], BF16)
    qT_scr = dscr("qT_scr", [H, 96, TT], BF16)
    mT_scr = dscr("mT_scr", [H, TT], BF16)
    v_scr = dscr("v_scr", [H, 128, NTT, 80], BF16)
    catcp_scr = dscr("catcp_scr", [TT, 512], BF16)
    h2T_scr = dscr("h2T_scr", [8, 128, TT], BF16)
    combT_scr = dscr("combT_scr", [NE, TT])
    wg_scr = nc.dram_tensor("wg_scr", [L * NE * 128, 8 * DE], BF16, kind="Internal").ap()
    wu_scr = nc.dram_tensor("wu_scr", [L * NE * 128, 8 * DE], BF16, kind="Internal").ap()
    wd_scr = nc.dram_tensor("wd_scr", [L * NE * 128, 2 * D], BF16, kind="Internal").ap()
    R_ada = Res("ada_scr")
    R_xs_mix = Res("xs_mix", multi=True)
    R_xs_out = [Res("xs_out0", multi=True), Res("xs_out1", multi=True)]
    R_kT = Res("kT_scr", multi=True)
    R_qT = Res("qT_scr", multi=True)
    R_mT = Res("mT_scr")
    R_v = Res("v_scr", multi=True)
    R_catcp = Res("catcp_scr", multi=True)
    R_h2T = Res("h2T_scr", multi=True)
    R_combT = Res("combT_scr", multi=True)
    R_wg = Res("wg_scr")
    R_wu = Res("wu_scr")
    R_wd = Res("wd_scr")
    R_out = Res("out", multi=True)
    for R_ in [R_h2perm, R_c8perm]:
        S.ensure(R_, sw=True)
    R_h2pz = Res("h2perm_zero")
    R_c8pz = Res("c8perm_zero")
    S.ensure(R_h2pz)
    S.ensure(R_c8pz)
    for R_ in [R_h2tok, R_cbT, R_yperm]:
        S.ensure(R_)
    for R_ in [R_ada, R_xs_mix, R_xs_out[0], R_xs_out[1], R_kT, R_qT, R_mT, R_v, R_catcp, R_h2T, R_combT, R_out]:
        S.ensure(R_)
    for R_ in [R_wg, R_wu, R_wd]:
        S.ensure(R_, sw=True)

    PBK = []
    for i in range(8):
        PBK.append((nc.alloc_psum_tensor("pb%d" % i, [128, 512], F32), Res("pb%d" % i, excl=True)))

    def bfv(t):
        return t[:].bitcast(BF16)

    def palloc(name, shape, dt=F32):
        return nc.alloc_sbuf_tensor(name, shape, dt)

    def sb(name, shape, dt=F32):
        return nc.alloc_sbuf_tensor(name, shape, dt), Res(name)

    ident_f, R_idf = sb("ident_f", [128, 128])
    ident_b, R_idb = sb("ident_b", [128, 128], BF16)
    dma("sp", ident_f[:], ident_d, [], [R_idf])
    op("dve", lambda e: e.tensor_copy(out=ident_b[:], in_=ident_f[:]), [R_idf], [R_idb])
    eps_t, R_epst = sb("eps_t", [128, 2])
    op("dve", lambda e: e.memset(eps_t[:, 0:1], LN_EPS), [], [R_epst])
    op("dve", lambda e: e.memset(eps_t[:, 1:2], RMS_EPS), [R_epst], [R_epst])
    tri_sb, R_tri = sb("tri_sb", [128, 128])
    dma("sp", tri_sb[:], tri_d, [], [R_tri])
    ones_sb, R_ones = sb("ones_sb", [128, 128])
    op("dve", lambda e: e.memset(ones_sb[:], 1.0), [], [R_ones])
    thr_sb, R_thr = sb("thr_sb", [128, NB])
    blk_sb, R_blk = sb("blk_sb", [128, NB])
    jp_sb, R_jp = sb("jp_sb", [128, 8])
    dma("sp", thr_sb[:], thr_d, [], [R_thr])
    dma("sp", blk_sb[:], blk_d, [], [R_blk])
    dma("sp", jp_sb[:], jp_d, [], [R_jp])
    zer_b, R_zerb = sb("zer_b", [128, D], BF16)
    zer_f, R_zerf = sb("zer_f", [128, 8])
    op("dve", lambda e: e.memset(zer_b[:], 0.0), [], [R_zerb])
    op("dve", lambda e: e.memset(zer_f[:], 0.0), [], [R_zerf])
    goh_all, R_goh = sb("goh_all", [128, NTT, 4])
    c8_all, R_c8 = sb("c8_all", [128, NTT, 8])
    dest_f, R_destf = sb("dest_f", [128, NTT])
    dest_i, R_desti = sb("dest_i", [128, NTT], I32)
    widx_f, R_widxf = sb("widx_f", [128, NB, 8])
    widx_i, R_widxi = sb("widx_i", [128, NB * 8], I32)
    srt, R_srt = sb("srt", [128, 64])
    pedge, R_pedge = sb("pedge", [128, 2, 2, 8])
    pinvw, R_pinvw = sb("pinvw", [128, 2])
    dma("sp", pedge[:], pedge_d, [], [R_pedge])
    dma("sp", pinvw[:], pinvw_d, [], [R_pinvw])

    w_in_sb, R_win = sb("w_in_sb", [128, 8, DIN], BF16)
    w_uq_sb, R_wuq = sb("w_uq_sb", [128, 3, 768], BF16)
    w_ukv_sb, R_wukv = sb("w_ukv_sb", [128, 2, 1024], BF16)
    for R_ in (R_win, R_wuq, R_wukv):
        S.ensure(R_, sw=True)

    def load_mix_weights(l_):
        dma("pool", w_in_sb[:], w_in[l_].rearrange("(k p) n -> p k n", p=128), [], [R_win])
        dma("pool", w_uq_sb[:], w_uq[l_].rearrange("(k p) n -> p k n", p=128), [], [R_wuq])
        dma("pool", w_ukv_sb[:], w_ukv[l_].rearrange("(k p) n -> p k n", p=128), [], [R_wukv])

    load_mix_weights(0)

    for l in range(L if stop_after not in ("0", "A", "C", "B", "D") else 0):
        for e0 in range(0, NE, 8):
            for e1 in range(e0, e0 + 8):
                r0 = (l * NE + e1) * 128
                dma("pool", wg_scr[r0:r0 + 128, :].rearrange("p (k h) -> p k h", k=8), w_gate[l, e1].rearrange("(k p) h -> p k h", p=128), [], [R_wg])
                dma("pool", wu_scr[r0:r0 + 128, :].rearrange("p (k h) -> p k h", k=8), w_up[l, e1].rearrange("(k p) h -> p k h", p=128), [], [R_wu])
                dma("pool", wd_scr[r0:r0 + 128, :].rearrange("p (c d) -> p c d", c=2), w_down[l, e1].rearrange("(c p) d -> p c d", p=128), [], [R_wd])

    with ExitStack() as st0:
        mk0 = S.mark()

        def a0(name, shape, dt=F32):
            return st0.enter_context(nc.sbuf_tensor(name, shape, dt))
        cs, R_cs = a0("cs", [2, D]), Res("cs")
        csT, R_csT = a0("csT", [128, 8, 2]), Res("csT")
        bada, R_bada = a0("bada", [2, 6 * D]), Res("bada")
        adas, R_adas = a0("adas", [2, 6 * D]), Res("adas")
        wblk = Ring(a0, "wblk", [128, 8, 512], F32, 2)
        dma("sp", cs[:], cvec, [], [R_cs])
        op("act", lambda e: e.activation(out=cs[:], in_=cs[:], func=AF.Silu), [R_cs], [R_cs])
        pb, Rpb = PBK[0]
        for k in range(8):
            op("pe", lambda e: e.transpose(out=pb[:, 2 * k:2 * k + 2], in_=cs[0:2, k * 128:(k + 1) * 128],
                                           identity=ident_f[0:2, 0:2]), [R_cs, R_idf], [Rpb])
        op("dve", lambda e: e.tensor_copy(out=csT[:].rearrange("p k r -> p (k r)"), in_=pb[:, 0:16]), [Rpb], [R_csT])
        nb_i = 0
        for l in range(L):
            dma("sp", bada[:], b_ada[l].partition_broadcast(2), [], [R_bada])
            for nb in range(12):
                wb, Rwb = wblk.get()
                dma("sp", wb[:], w_ada[l, :, nb * 512:(nb + 1) * 512].rearrange("(k p) n -> p k n", p=128), [], [Rwb])
                pb, Rpb = PBK[1 + (nb_i % 2)]
                nb_i += 1
                for k in range(8):
                    op("pe", lambda e: e.matmul(pb[0:2, :], lhsT=csT[:, k, :], rhs=wb[:, k, :], start=(k == 0), stop=(k == 7)),
                       [R_csT, Rwb], [Rpb])
                op("dve", lambda e: e.tensor_tensor(out=adas[:, nb * 512:(nb + 1) * 512], in0=pb[0:2, :],
                                                    in1=bada[:, nb * 512:(nb + 1) * 512], op=ALU.add), [Rpb, R_bada], [R_adas])
            for j in (1, 4):
                op("dve", lambda e: e.tensor_scalar_add(out=adas[:, j * D:(j + 1) * D], in0=adas[:, j * D:(j + 1) * D], scalar1=1.0),
                   [R_adas], [R_adas])
            dma("sp", ada_scr[l], adas[:], [R_adas], [R_ada])
        S.barrier()
        S.release(mk0)

    if stop_after == "0":
        return finish(nc, S, [R_ada])

    def ada_vec(l, r, j):
        return ada_scr[l, r, j * D:(j + 1) * D]

    def load_bc(t, R, src1d, n, rd=()):
        dma("sp", t[:, 0:n], src1d.partition_broadcast(128), list(rd), [R])

    def x_src(l, i):
        if l == 0:
            if i < NTC:
                return ctx_in[i * 128:(i + 1) * 128, :], []
            return x_in[(i - NTC) * 128:(i - NTC + 1) * 128, :], []
        return xs_out[(l - 1) % 2][i * 128:(i + 1) * 128, :], [R_xs_out[(l - 1) % 2]]

    def layer_norm_tile(st, eng2, y, Ry, n, gbc, Rg, bbc, Rb, outt, Rout, rings):
        stt, Rst = rings["st"].get()
        mv, Rmv = rings["mv"].get()
        nch = (n + 511) // 512
        for c in range(nch):
            a, b_ = c * 512, min(n, (c + 1) * 512)
            op("dve", lambda e: e.bn_stats(out=stt[:, c * 6:(c + 1) * 6], in_=y[:, a:b_]), [Ry], [Rst])
        op("dve", lambda e: e.bn_aggr(out=mv[:, 0:2], in_=stt[:, 0:nch * 6]), [Rst], [Rmv])
        op("act", lambda e: e.activation(out=mv[:, 2:3], in_=mv[:, 1:2], func=AF.Ln, bias=eps_t[:, 0:1], scale=1.0), [Rmv], [Rmv])
        op("act", lambda e: e.activation(out=mv[:, 2:3], in_=mv[:, 2:3], func=AF.Exp, scale=-0.5), [Rmv], [Rmv])
        op("dve", lambda e: e.scalar_tensor_tensor(out=mv[:, 3:4], in0=mv[:, 0:1], scalar=-1.0, in1=mv[:, 2:3],
                                                   op0=ALU.mult, op1=ALU.mult), [Rmv], [Rmv])
        op("act", lambda e: e.activation(out=y[:, 0:n], in_=y[:, 0:n], func=AF.Identity, bias=mv[:, 3:4], scale=mv[:, 2:3]),
           [Ry, Rmv], [Ry])
        op(eng2, lambda e: e.tensor_tensor(out=y[:, 0:n], in0=y[:, 0:n], in1=gbc[:, 0:n], op=ALU.mult), [Ry, Rg], [Ry])
        op("dve", lambda e: e.tensor_tensor(out=outt[:, 0:n], in0=y[:, 0:n], in1=bbc[:, 0:n], op=ALU.add), [Ry, Rb], [Rout])

    for l in range(L):
        last = (l == L - 1)
        S.barrier()

        with ExitStack() as stAC:
            def aAC(name, shape, dt=F32):
                return stAC.enter_context(nc.sbuf_tensor("%s_L%d" % (name, l), shape, dt))
            cpT_l, R_cpl = aAC("cpT_l", [128, 4, T + 32]), Res("cpT_l")
            cpT_c, R_cpc = aAC("cpT_c", [128, 4, C + 32]), Res("cpT_c")
            for (t_, R_, n_) in ((cpT_l, R_cpl, T), (cpT_c, R_cpc, C)):
                op("pool", lambda e: e.memset(t_[:, :, 0:16], 0.0), [], [R_])
                op("pool", lambda e: e.memset(t_[:, :, 16 + n_:32 + n_], 0.0), [R_], [R_])

            with ExitStack() as stA:
                mk_stA = S.mark()
                def aA(name, shape, dt=F32):
                    return stA.enter_context(nc.sbuf_tensor("%s_L%d" % (name, l), shape, dt))
                gq_bc, R_gq = aA("gq_bc", [128, DQ]), Res("gq_bc")
                gkv_bc, R_gkv = aA("gkv_bc", [128, DKV]), Res("gkv_bc")
                load_bc(gq_bc, R_gq, g_q[l], DQ)
                load_bc(gkv_bc, R_gkv, g_kv[l], DKV)
                sc1, sh1, R_sc1, R_sh1 = [], [], [], []
                for r in range(2):
                    t = aA("sc1_%d" % r, [128, D])
                    Rr = Res("sc1_%d" % r)
                    load_bc(t, Rr, ada_vec(l, r, 1), D, [R_ada])
                    sc1.append(t)
                    R_sc1.append(Rr)
                    t = aA("sh1_%d" % r, [128, D])
                    Rr = Res("sh1_%d" % r)
                    load_bc(t, Rr, ada_vec(l, r, 0), D, [R_ada])
                    sh1.append(t)
                    R_sh1.append(Rr)
                rope_sb, R_rope = aA("rope_sb", [128, NTT, 2, DR]), Res("rope_sb")
                dma("sp", rope_sb[:], rope_d.rearrange("(i p) a d -> p i a d", p=128), [], [R_rope])
                nq_all, R_nq = aA("nq_all", [128, NTT, H]), Res("nq_all")
                kmax2, R_kmax2 = aA("kmax2", [128, H]), Res("kmax2")
                op("dve", lambda e: e.memset(kmax2[:], 0.0), [], [R_kmax2])
                op("dve", lambda e: e.memset(nq_all[:], 0.0), [], [R_nq])

                xt_r = Ring(aA, "xt", [128, D], F32, 2)
                tmp_r = Ring(aA, "tmp32", [128, D], F32, 2)
                hb_r = Ring(aA, "hb", [128, D], BF16, 2)
                hT_r = Ring(aA, "hT", [128, 8, 128], BF16, 2)
                junk_r = Ring(aA, "junk", [128, 512], F32, 2)
                stat_r = Ring(aA, "stat", [128, 8], F32, 4)
                qn_r = Ring(aA, "qn", [128, DQ], BF16, 2)
                qnT_r = Ring(aA, "qnT", [128, 3, 128], BF16, 2)
                ckvn_r = Ring(aA, "ckvn", [128, DKV], BF16, 2)
                ckvT_r = Ring(aA, "ckvT", [128, 2, 128], BF16, 2)
                qaug_r = Ring(aA, "qaug", [128, H, 96], BF16, 2)
                kaug_r = Ring(aA, "kaug", [128, H, 112], BF16, 2)
                vaug_r = Ring(aA, "vaug", [128, H, 80], BF16, 2)
                for (t_, R_) in kaug_r.bufs:
                    op("dve", lambda e: e.memset(t_[:, :, 96:112], 1.0), [], [R_])
                for (t_, R_) in vaug_r.bufs:
                    op("dve", lambda e: e.memset(t_[:, :, 64:80], 1.0), [], [R_])
                kTst_r = Ring(aA, "kTst", [97, H, 128], BF16, 2)
                qTst_r = Ring(aA, "qTst", [96, H, 128], BF16, 2)
                cptok_r = Ring(aA, "cptok", [128, 512], F32, 2)
                sig_r = Ring(aA, "sig", [128, 256], F32, 2)
                rt_r = Ring(aA, "rt", [128, H, 2, DR], F32, 2)
                krr_r = Ring(aA, "krr", [128, 2, DR], F32, 2)
                nk_r = Ring(aA, "nk", [128, 16], F32, 2)

                def rope_apply(i, src_view, Rsrc, nh, t1, t2, Rt):
                    cosb = rope_sb[:, i, 0:1, :].to_broadcast([128, nh, DR])
                    op("dve", lambda e: e.tensor_tensor(out=t1[:, 0:nh, :], in0=src_view, in1=cosb, op=ALU.mult),
                       [Rsrc, R_rope], [Rt])
                    sv = src_view.rearrange("p h (a b c) -> p h a b c", a=2, b=2)
                    t2v = t2[:, 0:nh, :].rearrange("p h (a b c) -> p h a b c", a=2, b=2)
                    sn = rope_sb[:, i, 1, :].rearrange("p (a b c) -> p a b c", a=2, b=2)
                    for b_ in range(2):
                        snb = sn[:, :, b_, :].unsqueeze(1).to_broadcast([128, nh, 2, 8])
                        op("dve", lambda e: e.tensor_tensor(out=t2v[:, :, :, b_, :], in0=sv[:, :, :, 1 - b_, :], in1=snb, op=ALU.mult),
                           [Rsrc, R_rope], [Rt])
                    op("dve", lambda e: e.tensor_tensor(out=t1[:, 0:nh, :], in0=t1[:, 0:nh, :], in1=t2[:, 0:nh, :], op=ALU.add),
                       [Rt], [Rt])

                if stop_after == "Apre":
                    return finish(nc, S, [R_win, R_wuq, R_wukv, R_gq, R_gkv, R_rope] + R_sc1 + R_sh1)
                COLS = [(0, 384), (384, 672), (672, 1184), (1184, 1440)]
                def tileA(i):
                    isctx = i < NTC
                    typ = 1 if isctx else 0
                    full = not (last and isctx)
                    g0 = i * 128
                    src, Rsrc = x_src(l, i)
                    xt, Rxt = xt_r.get()
                    dma("sp", xt[:], src, Rsrc, [Rxt])
                    tmp, Rtmp = tmp_r.get()
                    hb, Rhb = hb_r.get()
                    op("pool", lambda e: e.tensor_tensor(out=tmp[:], in0=xt[:], in1=sc1[typ][:], op=ALU.mult), [Rxt, R_sc1[typ]], [Rtmp])
                    op("dve", lambda e: e.tensor_tensor(out=hb[:], in0=tmp[:], in1=sh1[typ][:], op=ALU.add), [Rtmp, R_sh1[typ]], [Rhb])
                    yield
                    pb, Rpb = PBK[0]
                    pbv = bfv(pb)
                    for k in range(8):
                        op("pe", lambda e: e.transpose(out=pbv[:, k * 128:(k + 1) * 128], in_=hb[:, k * 128:(k + 1) * 128], identity=ident_b[:]),
                           [Rhb, R_idb], [Rpb])
                    hT, RhT = hT_r.get()
                    op("act", lambda e: e.copy(out=hT[:].rearrange("p k t -> p (k t)"), in_=pbv[:, 0:1024]), [Rpb], [RhT])
                    cut("A1")
                    G = [PBK[2], PBK[3], PBK[4], PBK[5]]
                    for gi, (c0, c1) in enumerate(COLS):
                        if not full and gi != 1:
                            continue
                        gt, Rg = G[gi]
                        for k in range(8):
                            op("pe", lambda e: e.matmul(gt[:, 0:c1 - c0], lhsT=hT[:, k, :], rhs=w_in_sb[:, k, c0:c1], start=(k == 0), stop=(k == 7)),
                               [RhT, R_win], [Rg])
                    g1t, Rg1 = G[0]
                    g2t, Rg2 = G[1]
                    g3t, Rg3 = G[2]
                    g4t, Rg4 = G[3]
                    stt, Rstt = stat_r.get()
                    junk, Rjunk = junk_r.get()
                    cut("A2")
                    op("act", lambda e: e.activation(out=junk[:, 0:DKV], in_=g2t[:, 0:DKV], func=AF.Square, accum_out=stt[:, 0:1]),
                       [Rg2], [Rjunk, Rstt])
                    op("act", lambda e: e.activation(out=stt[:, 1:2], in_=stt[:, 0:1], func=AF.Ln, bias=eps_t[:, 1:2], scale=1.0 / DKV),
                       [Rstt], [Rstt])
                    op("act", lambda e: e.activation(out=stt[:, 1:2], in_=stt[:, 1:2], func=AF.Exp, scale=-0.5), [Rstt], [Rstt])
                    ckvn, Rckvn = ckvn_r.get()
                    op("dve", lambda e: e.scalar_tensor_tensor(out=ckvn[:], in0=g2t[:, 0:DKV], scalar=stt[:, 1:2], in1=gkv_bc[:],
                                                               op0=ALU.mult, op1=ALU.mult), [Rg2, Rstt, R_gkv], [Rckvn])
                    cut("A3")
                    krr, Rkrr = krr_r.get()
                    rt, Rrt = rt_r.get()
                    rope_apply(i, g2t[:, DKV:DKV + DR].unsqueeze(1), Rg2, 1, krr[:, 0:1, :], krr[:, 1:2, :], Rkrr)
                    cut("A4")
                    if full:
                        op("act", lambda e: e.activation(out=junk[:, 0:DQ], in_=g1t[:, 0:DQ], func=AF.Square, accum_out=stt[:, 2:3]),
                           [Rg1], [Rjunk, Rstt])
                        op("act", lambda e: e.activation(out=stt[:, 3:4], in_=stt[:, 2:3], func=AF.Ln, bias=eps_t[:, 1:2], scale=1.0 / DQ),
                           [Rstt], [Rstt])
                        op("act", lambda e: e.activation(out=stt[:, 3:4], in_=stt[:, 3:4], func=AF.Exp, scale=-0.5), [Rstt], [Rstt])
                        qn, Rqn = qn_r.get()
                        op("dve", lambda e: e.scalar_tensor_tensor(out=qn[:], in0=g1t[:, 0:DQ], scalar=stt[:, 3:4], in1=gq_bc[:],
                                                                   op0=ALU.mult, op1=ALU.mult), [Rg1, Rstt, R_gq], [Rqn])
                        sig, Rsig = sig_r.get()
                        cptok, Rcptok = cptok_r.get()
                        op("act", lambda e: e.activation(out=sig[:], in_=g3t[:, 256:512], func=AF.Exp, scale=-1.0), [Rg3], [Rsig])
                        op("act", lambda e: e.activation(out=sig[:], in_=sig[:], func=AF.Ln, bias=1.0, scale=1.0), [Rsig], [Rsig])
                        op("act", lambda e: e.activation(out=sig[:], in_=sig[:], func=AF.Exp, scale=-1.0), [Rsig], [Rsig])
                        op("dve", lambda e: e.tensor_tensor(out=cptok[:, 0:256], in0=g3t[:, 0:256], in1=sig[:], op=ALU.mult),
                           [Rg3, Rsig], [Rcptok])
                        op("act", lambda e: e.copy(out=cptok[:, 256:512], in_=g4t[:, 0:256]), [Rg4, Rcptok], [Rcptok])
                        pb1, Rpb1 = PBK[1]
                        pb1v = bfv(pb1)
                        for k in range(3):
                            op("pe", lambda e: e.transpose(out=pb1v[:, k * 128:(k + 1) * 128], in_=qn[:, k * 128:(k + 1) * 128], identity=ident_b[:]),
                               [Rqn, R_idb], [Rpb1])
                        qnT, RqnT = qnT_r.get()
                        op("act", lambda e: e.copy(out=qnT[:].rearrange("p k t -> p (k t)"), in_=pb1v[:, 0:384]), [Rpb1], [RqnT])
                    cut("A5")
                    pb0, Rpb0 = PBK[0]
                    pb0v = bfv(pb0)
                    for k in range(2):
                        op("pe", lambda e: e.transpose(out=pb0v[:, k * 128:(k + 1) * 128], in_=ckvn[:, k * 128:(k + 1) * 128], identity=ident_b[:]),
                           [Rckvn, R_idb], [Rpb0])
                    ckvT, RckvT = ckvT_r.get()
                    op("dve", lambda e: e.tensor_copy(out=ckvT[:].rearrange("p k t -> p (k t)"), in_=pb0v[:, 0:256]), [Rpb0], [RckvT])
                    yield
                    if full:
                        Q = [(PBK[6], 0, 5), (PBK[7], 5, 3)]
                        for ((qt, Rq), h0, nh) in Q:
                            for k in range(3):
                                op("pe", lambda e: e.matmul(qt[:, 0:nh * 96], lhsT=qnT[:, k, :], rhs=w_uq_sb[:, k, h0 * 96:(h0 + nh) * 96],
                                                            start=(k == 0), stop=(k == 2)), [RqnT, R_wuq], [Rq])
                    cut("A6")
                    KV = [(PBK[2], 0), (PBK[4], 4)]
                    for ((kt, Rk), h0) in KV:
                        for k in range(2):
                            op("pe", lambda e: e.matmul(kt[:, 0:512], lhsT=ckvT[:, k, :], rhs=w_ukv_sb[:, k, h0 * 128:(h0 + 4) * 128],
                                                        start=(k == 0), stop=(k == 1)), [RckvT, R_wukv], [Rk])
                    if full:
                        pb5, Rpb5 = PBK[5]
                        for k in range(4):
                            op("pe", lambda e: e.transpose(out=pb5[:, k * 128:(k + 1) * 128], in_=cptok[:, k * 128:(k + 1) * 128], identity=ident_f[:]),
                               [Rcptok, R_idf], [Rpb5])
                        cpd, Rcpd, off = (cpT_c, R_cpc, g0) if isctx else (cpT_l, R_cpl, g0 - C)
                        op("act", lambda e: e.copy(out=cpd[:, :, 16 + off:16 + off + 128], in_=pb5[:].rearrange("p (k t) -> p k t", k=4)),
                           [Rpb5], [Rcpd])
                        qaug, Rqaug = qaug_r.get()
                        nk, Rnk = nk_r.get()
                        for ((qt, Rq), h0, nh) in Q:
                            qv = qt[:, 0:nh * 96].rearrange("p (h d) -> p h d", d=96)
                            op("act", lambda e: e.copy(out=qaug[:, h0:h0 + nh, 0:64], in_=qv[:, :, 0:64]), [Rq], [Rqaug])
                            rope_apply(i, qv[:, :, 64:96], Rq, nh, rt[:, h0:h0 + nh, 0, :], rt[:, h0:h0 + nh, 1, :], Rrt)
                            op("dve", lambda e: e.tensor_copy(out=qaug[:, h0:h0 + nh, 64:96], in_=rt[:, h0:h0 + nh, 0, :]), [Rrt], [Rqaug])
                            jv = junk[:, 0:nh * 96].rearrange("p (h d) -> p h d", d=96)
                            op("act", lambda e: e.activation(out=jv, in_=qv, func=AF.Square), [Rq], [Rjunk])
                            op("dve", lambda e: e.tensor_reduce(out=nq_all[:, i, h0:h0 + nh], in_=jv, axis=AX.X, op=ALU.add), [Rjunk], [R_nq])
                    else:
                        nk, Rnk = nk_r.get()
                    cut("A7")
                    kaug, Rkaug = kaug_r.get()
                    vaug, Rvaug = vaug_r.get()
                    for ((kt, Rk), h0) in KV:
                        kv = kt[:, 0:512].rearrange("p (h d) -> p h d", d=128)
                        cut("KV0")
                        op("act", lambda e: e.copy(out=kaug[:, h0:h0 + 4, 0:64], in_=kv[:, :, 0:64]), [Rk], [Rkaug])
                        cut("KV1")
                        op("dve", lambda e: e.tensor_copy(out=vaug[:, h0:h0 + 4, 0:64], in_=kv[:, :, 64:128]), [Rk], [Rvaug])
                        cut("KV2")
                        jv = junk[:, 0:256].rearrange("p (h d) -> p h d", d=64)
                        op("act", lambda e: e.activation(out=jv, in_=kv[:, :, 0:64], func=AF.Square), [Rk], [Rjunk])
                        cut("KV3")
                        op("dve", lambda e: e.tensor_reduce(out=nk[:, h0:h0 + 4], in_=jv, axis=AX.X, op=ALU.add), [Rjunk], [Rnk])
                        cut("KV4")
                    cut("K1")
                    op("dve", lambda e: e.tensor_copy(out=kaug[:, :, 64:96], in_=krr[:, 0:1, :].to_broadcast([128, H, DR])), [Rkrr], [Rkaug])
                    cut("K2")
                    op("dve", lambda e: e.tensor_tensor(out=krr[:, 1, :], in0=krr[:, 0, :], in1=krr[:, 0, :], op=ALU.mult), [Rkrr], [Rkrr])
                    op("dve", lambda e: e.tensor_reduce(out=nk[:, 8:9], in_=krr[:, 1, :], axis=AX.X, op=ALU.add), [Rkrr], [Rnk])
                    cut("K3")
                    op("dve", lambda e: e.tensor_scalar(out=nk[:, 0:8], in0=nk[:, 0:8], scalar1=nk[:, 8:9], scalar2=None, op0=ALU.add), [Rnk], [Rnk])
                    cut("K4")
                    op("dve", lambda e: e.tensor_tensor(out=kmax2[:], in0=kmax2[:], in1=nk[:, 0:8], op=ALU.max), [Rnk, R_kmax2], [R_kmax2])
                    cut("A7b")
                    yield
                    if full:
                        pb0, Rpb0 = PBK[0]
                        pb0v = bfv(pb0)
                        for h in range(H):
                            op("pe", lambda e: e.transpose(out=pb0v[0:96, h * 128:(h + 1) * 128], in_=qaug[:, h, :], identity=ident_b[:]),
                               [Rqaug, R_idb], [Rpb0])
                        qTst, RqTst = qTst_r.get()
                        op("act", lambda e: e.copy(out=qTst[:].rearrange("p h t -> p (h t)"), in_=pb0v[0:96, 0:1024]), [Rpb0], [RqTst])
                        dma("sp", qT_scr[:, :, g0:g0 + 128].rearrange("h d t -> d h t"), qTst[:], [RqTst], [R_qT])
                    pb1, Rpb1 = PBK[1]
                    pb1v = bfv(pb1)
                    for h in range(H):
                        op("pe", lambda e: e.transpose(out=pb1v[0:97, h * 128:(h + 1) * 128], in_=kaug[:, h, 0:97], identity=ident_b[:]),
                           [Rkaug, R_idb], [Rpb1])
                    kTst, RkTst = kTst_r.get()
                    op("dve", lambda e: e.tensor_copy(out=kTst[:].rearrange("p h t -> p (h t)"), in_=pb1v[0:97, 0:1024]), [Rpb1], [RkTst])
                    dma("sp", kT_scr[:, :, g0:g0 + 128].rearrange("h d t -> d h t"), kTst[:], [RkTst], [R_kT])
                    dma("sp", v_scr[:, :, i, :].rearrange("h p d -> p h d"), vaug[:], [Rvaug], [R_v])
                    cut("AT%d" % i)

                run_skewed([tileA(i) for i in range(NTT)])
                cut("A8")
                pb, Rpb = PBK[0]
                op("pe", lambda e: e.transpose(out=pb[0:8, 0:128], in_=kmax2[:, 0:8], identity=ident_f[:]), [R_kmax2, R_idf], [Rpb])
                km, Rkm = aA("km", [8, 16]), Res("km")
                op("dve", lambda e: e.tensor_reduce(out=km[:, 0:1], in_=pb[0:8, 0:128], axis=AX.X, op=ALU.max), [Rpb], [Rkm])
                op("act", lambda e: e.activation(out=km[:, 1:2], in_=km[:, 0:1], func=AF.Sqrt, scale=1.0404), [Rkm], [Rkm])
                op("dve", lambda e: e.tensor_scalar(out=km[:, 8:16], in0=ident_f[0:8, 0:8], scalar1=km[:, 1:2], scalar2=-1.0,
                                                    op0=ALU.mult, op1=ALU.mult), [Rkm, R_idf], [Rkm])
                ones8, Rones8 = aA("ones8", [8, 128]), Res("ones8")
                op("dve", lambda e: e.memset(ones8[:], 1.0), [], [Rones8])
                pb, Rpb = PBK[1]
                op("pe", lambda e: e.matmul(pb[:, 0:8], lhsT=ones8[:], rhs=km[:, 8:16], start=True, stop=True), [Rones8, Rkm], [Rpb])
                kmbc, Rkmbc = aA("kmbc", [128, 8]), Res("kmbc")
                op("dve", lambda e: e.tensor_copy(out=kmbc[:], in_=pb[:, 0:8]), [Rpb], [Rkmbc])
                op("act", lambda e: e.activation(out=nq_all[:], in_=nq_all[:], func=AF.Sqrt), [R_nq], [R_nq])
                op("dve", lambda e: e.tensor_tensor(out=nq_all[:], in0=nq_all[:], in1=kmbc[:].unsqueeze(1).to_broadcast([128, NTT, H]), op=ALU.mult),
                   [R_nq, Rkmbc], [R_nq])
                mT_sb, RmT = aA("mT_sb", [8, TT], BF16), Res("mT_sb")
                for i0 in range(0, NTT, 4):
                    pb, Rpb = PBK[(i0 // 4) % 2]
                    ni = min(4, NTT - i0)
                    for j in range(ni):
                        op("pe", lambda e: e.transpose(out=pb[0:8, j * 128:(j + 1) * 128], in_=nq_all[:, i0 + j, :], identity=ident_f[:]),
                           [R_nq, R_idf], [Rpb])
                    op("dve", lambda e: e.tensor_copy(out=mT_sb[:, i0 * 128:(i0 + ni) * 128], in_=pb[0:8, 0:ni * 128]), [Rpb], [RmT])
                dma("sp", mT_scr, mT_sb[:], [RmT], [R_mT])
                S.barrier()
                S.release(mk_stA)
            if stop_after == "A":
                return finish(nc, S, [R_kT, R_qT, R_mT, R_v])

            with ExitStack() as stC:
                mk_stC = S.mark()
                def aC(name, shape, dt=F32):
                    return stC.enter_context(nc.sbuf_tensor("%s_L%d" % (name, l), shape, dt))
                cwr, Rcwr = aC("cwr", [CONVW, 256]), Res("cwr")
                cw_sb, Rcw = aC("cw_sb", [128, 2, CONVW]), Res("cw_sb")
                dma("sp", cwr[:], conv_w[l], [], [Rcwr])
                pb, Rpb = PBK[0]
                for k in range(2):
                    op("pe", lambda e: e.transpose(out=pb[:, k * 32:k * 32 + CONVW], in_=cwr[0:CONVW, k * 128:(k + 1) * 128],
                                                   identity=ident_f[0:CONVW, 0:CONVW]), [Rcwr, R_idf], [Rpb])
                op("dve", lambda e: e.tensor_copy(out=cw_sb[:], in_=pb[:, 0:64].rearrange("p (k j) -> p k j", k=2)[:, :, 0:CONVW]), [Rpb], [Rcw])
                convb_bc, Rcb = aC("convb_bc", [128, 256]), Res("convb_bc")
                clng_bc, Rclg = aC("clng_bc", [128, 256]), Res("clng_bc")
                clnb_bc, Rclb = aC("clnb_bc", [128, 256]), Res("clnb_bc")
                psc_bc, Rpsc = aC("psc_bc", [128, 256]), Res("psc_bc")
                load_bc(convb_bc, Rcb, conv_b[l], 256)
                load_bc(clng_bc, Rclg, conv_ln_g[l], 256)
                load_bc(clnb_bc, Rclb, conv_ln_b[l], 256)
                load_bc(psc_bc, Rpsc, pool_scale[l], 256)
                poolw_sb, Rpw = aC("poolw_sb", [128, 2, 128], BF16), Res("poolw_sb")
                op("dve", lambda e: e.memset(poolw_sb[:], 0.0), [], [Rpw])
                for ph in range(2):
                    dma("pool", poolw_sb[ph * 64:(ph + 1) * 64, :, ph * 64:(ph + 1) * 64],
                        pool_w[l].rearrange("(k two) i o -> two i k o", two=2)[ph], [], [Rpw])
                acc_t = aC("acc", [128, 2, SEG])
                R_acc = [Res("acc0"), Res("acc1")]
                P2, RP2 = aC("P2", [128, 2, SEG + 16]), Res("P2")
                P4, RP4 = aC("P4", [128, 2, SEG + 16]), Res("P4")
                P8, RP8 = aC("P8", [128, SEG + 16]), Res("P8")
                P16, RP16 = aC("P16", [128, SEG + 16]), Res("P16")
                mixed, Rmixed = aC("mixed", [128, 2, SEG], BF16), Res("mixed")
                etmp, Retmp = aC("etmp", [128, 2, 8]), Res("etmp")
                ctmp_r = Ring(aC, "ctmp", [128, SEG], F32, 3)
                cv_r = Ring(aC, "cv", [128, 256], F32, 2)
                sgc_r = Ring(aC, "sgc", [128, 256], F32, 2)
                catcp_r = Ring(aC, "catcp", [128, 512], BF16, 2)
                lnr = {"st": Ring(aC, "cst", [128, 12], F32, 2), "mv": Ring(aC, "cmv", [128, 4], F32, 2)}
                cut("C1")
                seqs = [(cpT_l, R_cpl, T, C)]
                if not last:
                    seqs.append((cpT_c, R_cpc, C, 0))
                tile_ctr = 0
                for (buf, Rbuf, n, goff) in seqs:
                    for s0 in range(0, n, SEG):
                        seg = min(SEG, n - s0)
                        b0 = 16 + s0
                        for j in range(CONVW):
                            if j == 0:
                                op("dve", lambda e: e.tensor_scalar(out=acc_t[:, 0, 0:seg], in0=buf[:, 0, b0 - 15:b0 - 15 + seg], scalar1=cw_sb[:, 0, 0:1],
                                                                    scalar2=None, op0=ALU.mult), [Rbuf, Rcw], [R_acc[0]])
                                op("act", lambda e: e.activation(out=acc_t[:, 1, 0:seg], in_=buf[:, 1, b0 - 15:b0 - 15 + seg], func=AF.Copy,
                                                                 scale=cw_sb[:, 1, 0:1]), [Rbuf, Rcw], [R_acc[1]])
                                continue
                            op("dve", lambda e: e.scalar_tensor_tensor(out=acc_t[:, 0, 0:seg], in0=buf[:, 0, b0 - 15 + j:b0 - 15 + j + seg],
                                                                       scalar=cw_sb[:, 0, j:j + 1], in1=acc_t[:, 0, 0:seg],
                                                                       op0=ALU.mult, op1=ALU.add), [Rbuf, Rcw, R_acc[0]], [R_acc[0]])
                            ct, Rct = ctmp_r.get()
                            op("act", lambda e: e.activation(out=ct[:, 0:seg], in_=buf[:, 1, b0 - 15 + j:b0 - 15 + j + seg], func=AF.Copy,
                                                             scale=cw_sb[:, 1, j:j + 1]), [Rbuf, Rcw], [Rct])
                            op("pool", lambda e: e.tensor_tensor(out=acc_t[:, 1, 0:seg], in0=acc_t[:, 1, 0:seg], in1=ct[:, 0:seg], op=ALU.add),
                               [Rct, R_acc[1]], [R_acc[1]])
                        cut("C2")
                        n2 = seg + 16
                        op("dve", lambda e: e.tensor_tensor(out=P2[:, :, 0:n2], in0=buf[:, 2:4, b0 - 9:b0 - 9 + n2], in1=buf[:, 2:4, b0 - 8:b0 - 8 + n2],
                                                            op=ALU.add), [Rbuf], [RP2])
                        op("dve", lambda e: e.tensor_tensor(out=P4[:, :, 2:n2 - 2], in0=P2[:, :, 1:n2 - 3], in1=P2[:, :, 3:n2 - 1], op=ALU.add),
                           [RP2], [RP4])
                        op("dve", lambda e: e.tensor_tensor(out=P8[:, 4:n2 - 4], in0=P4[:, 1, 2:n2 - 6], in1=P4[:, 1, 6:n2 - 2], op=ALU.add),
                           [RP4], [RP8])
                        op("dve", lambda e: e.tensor_tensor(out=P16[:, 8:n2 - 8], in0=P8[:, 4:n2 - 12], in1=P8[:, 12:n2 - 4], op=ALU.add),
                           [RP8], [RP16])
                        srcs = {(0, 0): (P2[0:64, 0, 8:8 + seg], RP2), (1, 0): (P4[64:128, 0, 8:8 + seg], RP4),
                                (0, 1): (P8[0:64, 8:8 + seg], RP8), (1, 1): (P16[64:128, 8:8 + seg], RP16)}
                        for (ph, k), (sap, Rs) in srcs.items():
                            ps = slice(ph * 64, ph * 64 + 64)
                            op("dve", lambda e: e.scalar_tensor_tensor(out=mixed[ps, k, 0:seg], in0=sap, scalar=pinvw[ps, k:k + 1],
                                                                       in1=buf[ps, 2 + k, b0:b0 + seg], op0=ALU.mult, op1=ALU.subtract),
                               [Rs, R_pinvw, Rbuf], [Rmixed])
                            for (side, cond, c0) in ((0, s0 == 0, 0), (1, s0 + seg == n, seg - 8)):
                                if not cond:
                                    continue
                                sap8 = sap[:, c0:c0 + 8]
                                op("dve", lambda e: e.tensor_tensor(out=etmp[ps, k, :], in0=sap8, in1=pedge[ps, k, side, :], op=ALU.mult),
                                   [Rs, R_pedge], [Retmp])
                                op("dve", lambda e: e.tensor_tensor(out=mixed[ps, k, c0:c0 + 8], in0=etmp[ps, k, :], in1=buf[ps, 2 + k, b0 + c0:b0 + c0 + 8],
                                                                    op=ALU.subtract), [Retmp, Rbuf], [Rmixed])
                        cut("C3")
                        for j in range(seg // 128):
                            g0 = goff + s0 + j * 128
                            pb, Rpb = PBK[tile_ctr % 2]
                            pb2, Rpb2 = PBK[2 + tile_ctr % 2]
                            tile_ctr += 1
                            for k in range(2):
                                op("pe", lambda e: e.transpose(out=pb[:, k * 128:(k + 1) * 128], in_=acc_t[:, k, j * 128:(j + 1) * 128], identity=ident_f[:]),
                                   [R_acc[k], R_idf], [Rpb])
                            cv, Rcv = cv_r.get()
                            op("dve", lambda e: e.tensor_tensor(out=cv[:], in0=pb[:, 0:256], in1=convb_bc[:], op=ALU.add), [Rpb, Rcb], [Rcv])
                            layer_norm_tile(None, "pool", cv, Rcv, 256, clng_bc, Rclg, clnb_bc, Rclb, cv, Rcv, lnr)
                            catcp, Rcatcp = catcp_r.get()
                            sgc, Rsgc = sgc_r.get()
                            op("act", lambda e: e.activation(out=sgc[:], in_=cv[:], func=AF.Exp, scale=-1.0), [Rcv], [Rsgc])
                            op("act", lambda e: e.activation(out=sgc[:], in_=sgc[:], func=AF.Ln, bias=1.0, scale=1.0), [Rsgc], [Rsgc])
                            op("act", lambda e: e.activation(out=sgc[:], in_=sgc[:], func=AF.Exp, scale=-1.0), [Rsgc], [Rsgc])
                            op("dve", lambda e: e.tensor_tensor(out=catcp[:, 0:256], in0=cv[:], in1=sgc[:], op=ALU.mult), [Rcv, Rsgc], [Rcatcp])
                            cut("C3b")
                            for k in range(2):
                                op("pe", lambda e: e.matmul(pb2[:, k * 128:(k + 1) * 128], lhsT=mixed[:, k, j * 128:(j + 1) * 128],
                                                            rhs=poolw_sb[:, k, :], start=True, stop=True), [Rmixed, Rpw], [Rpb2])
                            cut("C3c")
                            op("dve", lambda e: e.tensor_tensor(out=catcp[:, 256:512], in0=pb2[:, 0:256], in1=psc_bc[:], op=ALU.mult),
                               [Rpb2, Rpsc, Rcatcp], [Rcatcp])
                            cut("C3d")
                            dma("sp", catcp_scr[g0:g0 + 128, :], catcp[:], [Rcatcp], [R_catcp])
                            cut("C4")
                S.barrier()
                S.release(mk_stC)
        if stop_after == "C":
            return finish(nc, S, [R_catcp])

        with ExitStack() as stBD:
            def aBD(name, shape, dt=F32):
                return stBD.enter_context(nc.sbuf_tensor("%s_L%d" % (name, l), shape, dt))
            attn_sb = aBD("attn_sb", [128, NTT, 512], BF16)
            R_attn = [Res("attn%d" % i) for i in range(NTT)]
            with ExitStack() as stB:
                mk_stB = S.mark()
                def aB(name, shape, dt=F32):
                    return stB.enter_context(nc.sbuf_tensor("%s_L%d" % (name, l), shape, dt))
                NJ = NP // 128
                dma("act", h2perm_scr.rearrange("(j p) d -> p j d", p=128), zer_b[:].unsqueeze(1).to_broadcast([128, NJ, D]), [R_zerb], [R_h2pz])
                dma("act", c8perm_scr.rearrange("(j p) e -> p j e", p=128), zer_f[:].unsqueeze(1).to_broadcast([128, NJ, 8]), [R_zerf], [R_c8pz])
                KT_r = Ring(aB, "KT", [97, TT], BF16, 2)
                V_r = Ring(aB, "V", [128, NTT, 80], BF16, 2)
                qT_r = Ring(aB, "qT", [97, 512], BF16, 3)
                PT_r = Ring(aB, "PT", [128, 512], BF16, 4)
                oT_r = Ring(aB, "oT", [65, 512], F32, 2)
                rec_r = Ring(aB, "rec", [128, 4], F32, 2)
                blocks = [(C + b * 512, 512, list(range(NTT))) for b in range(T // 512)]
                if not last:
                    blocks.append((0, C, list(range(NTC))))
                bi = 0
                si = 0
                for h in range(H):
                    KT, RKT = KT_r.get()
                    V, RV = V_r.get()
                    dma("sp", KT[:], kT_scr[h], [R_kT], [RKT])
                    dma("sp", V[:], v_scr[h], [R_v], [RV])
                    for (g0, n, chunks) in blocks:
                        qT, RqT = qT_r.get()
                        dma("sp", qT[0:96, 0:n], qT_scr[h, :, g0:g0 + n], [R_qT], [RqT])
                        dma("sp", qT[96:97, 0:n], mT_scr[h:h + 1, g0:g0 + n], [R_mT], [RqT])
                        pO, RpO = PBK[bi % 2]
                        LOOK = 3
                        pend = []

                        def issue_s(c):
                            nonlocal si
                            pS_, RpS_ = PBK[2 + si % 4]
                            si += 1
                            op("pe", lambda e: e.matmul(pS_[:, 0:n], lhsT=KT[:, c * 128:(c + 1) * 128], rhs=qT[:, 0:n], start=True, stop=True),
                               [RKT, RqT], [RpS_])
                            pend.append((pS_, RpS_))
                        for c in chunks[:LOOK]:
                            issue_s(c)
                        for ci, c in enumerate(chunks):
                            if ci + LOOK < len(chunks):
                                issue_s(chunks[ci + LOOK])
                            pS, RpS = pend.pop(0)
                            PT, RPT = PT_r.get()
                            op("act", lambda e: e.activation(out=PT[:, 0:n], in_=pS[:, 0:n], func=AF.Exp, scale=QS), [RpS], [RPT])
                            op("pe", lambda e: e.matmul(pO[0:65, 0:n], lhsT=V[:, c, 0:65], rhs=PT[:, 0:n], start=(ci == 0), stop=(ci == len(chunks) - 1)),
                               [RV, RPT], [RpO])
                        oT, RoT = oT_r.get()
                        op("dve", lambda e: e.tensor_copy(out=oT[:, 0:n], in_=pO[0:65, 0:n]), [RpO], [RoT])
                        pb, Rpb = PBK[6 + bi % 2]
                        bi += 1
                        nj = n // 128
                        for j in range(nj):
                            op("pe", lambda e: e.transpose(out=pb[:, j * 65:(j + 1) * 65], in_=oT[0:65, j * 128:(j + 1) * 128], identity=ident_f[0:65, 0:65]),
                               [RoT, R_idf], [Rpb])
                        pv = pb[:, 0:nj * 65].rearrange("p (j d) -> p j d", d=65)
                        rec, Rrec = rec_r.get()
                        op("dve", lambda e: e.reciprocal(out=rec[:, 0:nj], in_=pv[:, :, 64]), [Rpb], [Rrec])
                        i0 = g0 // 128
                        Rs_ = R_attn[i0:i0 + nj]
                        op("dve", lambda e: e.tensor_tensor(out=attn_sb[:, i0:i0 + nj, h * 64:(h + 1) * 64], in0=pv[:, :, 0:64],
                                                            in1=rec[:, 0:nj].unsqueeze(2).to_broadcast([128, nj, 64]), op=ALU.mult),
                           [Rpb, Rrec] + Rs_, Rs_)
                S.barrier()
                S.release(mk_stB)
            if stop_after == "B":
                dbg = nc.dram_tensor("attn_dbg", [128, NTT, 512], BF16, kind="ExternalOutput").ap()
                Rd = Res("attn_dbg")
                dma("sp", dbg, attn_sb[:], R_attn, [Rd])
                return finish(nc, S, [Rd])

            with ExitStack() as stD:
                mk_stD = S.mark()
                def aD(name, shape, dt=F32):
                    return stD.enter_context(nc.sbuf_tensor("%s_L%d" % (name, l), shape, dt))
                w_out_sb, R_wout = aD("w_out_sb", [128, 8, D], BF16), Res("w_out_sb")
                dma("pool", w_out_sb[:], w_out[l].rearrange("(k p) n -> p k n", p=128), [], [R_wout])
                wr_sb, R_wr = aD("wr_sb", [128, 8, 36]), Res("wr_sb")
                dma("sp", wr_sb[:, :, 0:4], w_rg[l].rearrange("(k p) n -> p k n", p=128), [], [R_wr])
                dma("sp", wr_sb[:, :, 4:36], w_re[l].rearrange("(k p) n -> p k n", p=128), [], [R_wr])
                br_bc, R_br = aD("br_bc", [128, 36]), Res("br_bc")
                dma("sp", br_bc[:, 0:4], b_rg[l].partition_broadcast(128), [], [R_br])
                dma("sp", br_bc[:, 4:36], b_re[l].partition_broadcast(128), [], [R_br])
                bcs = {}
                for (nm, j) in (("g1", 2), ("sh2", 3), ("sc2", 4)):
                    for r in range(2):
                        if r == 1 and last:
                            continue
                        t = aD("%s_%d" % (nm, r), [128, D])
                        Rr = Res("%s_%d" % (nm, r))
                        load_bc(t, Rr, ada_vec(l, r, j), D, [R_ada])
                        bcs[(nm, r)] = (t, Rr)
                ln1g_bc, R_l1g = aD("ln1g_bc", [128, D]), Res("ln1g_bc")
                ln1b_bc, R_l1b = aD("ln1b_bc", [128, D]), Res("ln1b_bc")
                load_bc(ln1g_bc, R_l1g, ln1_g[l], D)
                load_bc(ln1b_bc, R_l1b, ln1_b[l], D)
                catcp_r = Ring(aD, "catcpD", [128, 512], BF16, 2)
                catT_r = Ring(aD, "catT", [128, 8, 128], BF16, 2)
                xt_r = Ring(aD, "xtD", [128, D], F32, 3)
                y_r = Ring(aD, "yD", [128, D], F32, 2)
                x1_r = Ring(aD, "x1D", [128, D], F32, 2)
                h2_r = Ring(aD, "h2D", [128, D], F32, 2)
                h2Tf_r = Ring(aD, "h2Tf", [128, 8, 128], F32, 2)
                h2Tb_r = Ring(aD, "h2b", [128, D], BF16, 2)
                lg_r = Ring(aD, "lg", [128, 36], F32, 2)
                rs_r = Ring(aD, "rs", [128, 16], F32, 2)
                oh_r = Ring(aD, "oh", [128, 3, 32], F32, 2)
                comb_r = Ring(aD, "comb", [128, 32], F32, 2)
                combT_r = Ring(aD, "combT", [32, 128], F32, 2)
                lnr = {"st": Ring(aD, "dst", [128, 12], F32, 2), "mv": Ring(aD, "dmv", [128, 4], F32, 2)}
                cut("D1")
                def tileD(i, tcnt):
                    isctx = i < NTC
                    typ = 1 if isctx else 0
                    g0 = i * 128
                    catcp, Rcatcp = catcp_r.get()
                    dma("sp", catcp[:], catcp_scr[g0:g0 + 128, :], [R_catcp], [Rcatcp])
                    src, Rsrc = x_src(l, i)
                    xt, Rxt = xt_r.get()
                    dma("sp", xt[:], src, Rsrc, [Rxt])
                    yield
                    pb, Rpb = PBK[tcnt % 2]
                    pbv = bfv(pb)
                    for k in range(4):
                        op("pe", lambda e: e.transpose(out=pbv[:, k * 128:(k + 1) * 128], in_=attn_sb[:, i, k * 128:(k + 1) * 128], identity=ident_b[:]),
                           [R_attn[i], R_idb], [Rpb])
                    for k in range(4):
                        op("pe", lambda e: e.transpose(out=pbv[:, (4 + k) * 128:(5 + k) * 128], in_=catcp[:, k * 128:(k + 1) * 128], identity=ident_b[:]),
                           [Rcatcp, R_idb], [Rpb])
                    catT, RcatT = catT_r.get()
                    op("act", lambda e: e.copy(out=catT[:].rearrange("p k t -> p (k t)"), in_=pbv[:, 0:1024]), [Rpb], [RcatT])
                    cut("D2")
                    M = [PBK[2 + 2 * (tcnt % 2)], PBK[3 + 2 * (tcnt % 2)]]
                    for hf in range(2):
                        mt, Rm = M[hf]
                        for k in range(8):
                            op("pe", lambda e: e.matmul(mt[:, :], lhsT=catT[:, k, :], rhs=w_out_sb[:, k, hf * 512:(hf + 1) * 512], start=(k == 0), stop=(k == 7)),
                               [RcatT, R_wout], [Rm])
                    cut("D3")
                    yield
                    y, Ry = y_r.get()
                    g1t, Rg1 = bcs[("g1", typ)]
                    for hf in range(2):
                        mt, Rm = M[hf]
                        op("dve", lambda e: e.tensor_tensor(out=y[:, hf * 512:(hf + 1) * 512], in0=mt[:, :], in1=g1t[:, hf * 512:(hf + 1) * 512], op=ALU.mult),
                           [Rm, Rg1], [Ry])
                    op("dve", lambda e: e.scalar_tensor_tensor(out=y[:], in0=xt[:], scalar=ALPHA, in1=y[:], op0=ALU.mult, op1=ALU.add),
                       [Rxt, Ry], [Ry])
                    x1, Rx1 = x1_r.get()
                    layer_norm_tile(None, "pool", y, Ry, D, ln1g_bc, R_l1g, ln1b_bc, R_l1b, x1, Rx1, lnr)
                    dma("sp", xs_mix[g0:g0 + 128, :], x1[:], [Rx1], [R_xs_mix])
                    cut("D4")
                    h2, Rh2 = h2_r.get()
                    sc2t, Rsc2 = bcs[("sc2", typ)]
                    sh2t, Rsh2 = bcs[("sh2", typ)]
                    op("pool", lambda e: e.tensor_tensor(out=h2[:], in0=x1[:], in1=sc2t[:], op=ALU.mult), [Rx1, Rsc2], [Rh2])
                    op("dve", lambda e: e.tensor_tensor(out=h2[:], in0=h2[:], in1=sh2t[:], op=ALU.add), [Rh2, Rsh2], [Rh2])
                    yield
                    T6, RT6 = PBK[6]
                    T7, RT7 = PBK[7]
                    for k in range(8):
                        tb, Rtb = (T6, RT6) if k < 4 else (T7, RT7)
                        op("pe", lambda e: e.transpose(out=tb[:, (k % 4) * 128:(k % 4 + 1) * 128], in_=h2[:, k * 128:(k + 1) * 128], identity=ident_f[:]),
                           [Rh2, R_idf], [Rtb])
                    h2Tf, Rh2Tf = h2Tf_r.get()
                    h2b, Rh2b = h2Tb_r.get()
                    op("pool", lambda e: e.tensor_copy(out=h2b[:], in_=h2[:]), [Rh2], [Rh2b])
                    dma("sp", h2tok_scr[g0:g0 + 128, :], h2b[:], [Rh2b], [R_h2tok])
                    for hf, (tb, Rtb) in enumerate(((T6, RT6), (T7, RT7))):
                        op("act", lambda e: e.copy(out=h2Tf[:, hf * 4:hf * 4 + 4, :].rearrange("p k t -> p (k t)"), in_=tb[:, :]), [Rtb], [Rh2Tf])
                    cut("D5")
                    pr, Rpr = PBK[tcnt % 2]
                    for k in range(8):
                        op("pe", lambda e: e.matmul(pr[:, 0:36], lhsT=h2Tf[:, k, :], rhs=wr_sb[:, k, :], start=(k == 0), stop=(k == 7)),
                           [Rh2Tf, R_wr], [Rpr])
                    lg, Rlg = lg_r.get()
                    rs, Rrs = rs_r.get()
                    oh, Roh = oh_r.get()
                    op("dve", lambda e: e.tensor_tensor(out=lg[:], in0=pr[:, 0:36], in1=br_bc[:], op=ALU.add), [Rpr, R_br], [Rlg])
                    cut("D6")
                    yield
                    op("dve", lambda e: e.tensor_reduce(out=rs[:, 0:1], in_=lg[:, 0:4], axis=AX.X, op=ALU.max), [Rlg], [Rrs])
                    op("dve", lambda e: e.tensor_scalar(out=rs[:, 8:12], in0=lg[:, 0:4], scalar1=rs[:, 0:1], scalar2=None, op0=ALU.is_equal), [Rlg, Rrs], [Rrs])
                    op("dve", lambda e: e.tensor_copy(out=goh_all[:, i, :], in_=rs[:, 8:12]), [Rrs], [R_goh])
                    op("dve", lambda e: e.tensor_scalar(out=rs[:, 1:2], in0=rs[:, 0:1], scalar1=-1.0, scalar2=None, op0=ALU.mult), [Rrs], [Rrs])
                    op("act", lambda e: e.activation(out=rs[:, 12:16], in_=lg[:, 0:4], func=AF.Exp, bias=rs[:, 1:2], scale=1.0, accum_out=rs[:, 2:3]),
                       [Rlg, Rrs], [Rrs])
                    op("dve", lambda e: e.reciprocal(out=rs[:, 2:3], in_=rs[:, 2:3]), [Rrs], [Rrs])
                    op("dve", lambda e: e.tensor_scalar(out=rs[:, 8:12], in0=rs[:, 8:12], scalar1=-1.0, scalar2=-NEG, op0=ALU.add, op1=ALU.mult), [Rrs], [Rrs])
                    elm = oh[:, 0, :]
                    op("dve", lambda e: e.tensor_tensor(out=elm.rearrange("p (g x) -> p g x", g=4), in0=lg[:, 4:36].rearrange("p (g x) -> p g x", g=4),
                                                        in1=rs[:, 8:12].unsqueeze(2).to_broadcast([128, 4, 8]), op=ALU.add), [Rlg, Rrs], [Roh])
                    op("dve", lambda e: e.tensor_reduce(out=rs[:, 3:4], in_=elm, axis=AX.X, op=ALU.max), [Roh], [Rrs])
                    op("dve", lambda e: e.tensor_scalar(out=oh[:, 1, :], in0=elm, scalar1=rs[:, 3:4], scalar2=None, op0=ALU.is_equal), [Roh, Rrs], [Roh])
                    op("dve", lambda e: e.scalar_tensor_tensor(out=elm, in0=oh[:, 1, :], scalar=NEG, in1=elm, op0=ALU.mult, op1=ALU.add), [Roh], [Roh])
                    op("dve", lambda e: e.tensor_reduce(out=rs[:, 4:5], in_=elm, axis=AX.X, op=ALU.max), [Roh], [Rrs])
                    op("dve", lambda e: e.tensor_scalar(out=oh[:, 2, :], in0=elm, scalar1=rs[:, 4:5], scalar2=None, op0=ALU.is_equal), [Roh, Rrs], [Roh])
                    op("dve", lambda e: e.tensor_tensor(out=rs[:, 5:6], in0=rs[:, 4:5], in1=rs[:, 3:4], op=ALU.subtract), [Rrs], [Rrs])
                    op("act", lambda e: e.activation(out=rs[:, 5:6], in_=rs[:, 5:6], func=AF.Exp), [Rrs], [Rrs])
                    op("dve", lambda e: e.tensor_scalar(out=rs[:, 6:7], in0=rs[:, 5:6], scalar1=1.0, scalar2=None, op0=ALU.add), [Rrs], [Rrs])
                    op("dve", lambda e: e.reciprocal(out=rs[:, 6:7], in_=rs[:, 6:7]), [Rrs], [Rrs])
                    op("dve", lambda e: e.tensor_tensor(out=rs[:, 6:7], in0=rs[:, 6:7], in1=rs[:, 2:3], op=ALU.mult), [Rrs], [Rrs])
                    op("dve", lambda e: e.tensor_tensor(out=rs[:, 7:8], in0=rs[:, 6:7], in1=rs[:, 5:6], op=ALU.mult), [Rrs], [Rrs])
                    cut("D7")
                    comb, Rcomb = comb_r.get()
                    op("dve", lambda e: e.tensor_scalar(out=comb[:], in0=oh[:, 1, :], scalar1=rs[:, 6:7], scalar2=None, op0=ALU.mult), [Roh, Rrs], [Rcomb])
                    op("dve", lambda e: e.scalar_tensor_tensor(out=comb[:], in0=oh[:, 2, :], scalar=rs[:, 7:8], in1=comb[:], op0=ALU.mult, op1=ALU.add),
                       [Roh, Rrs, Rcomb], [Rcomb])
                    cut("D8")
                    op("dve", lambda e: e.tensor_reduce(out=c8_all[:, i, :], in_=comb[:].rearrange("p (g j) -> p j g", g=4), axis=AX.X, op=ALU.add),
                       [Rcomb], [R_c8])

                op("dve", lambda e: e.memset(goh_all[:], 0.0), [], [R_goh])
                tilesD = [i for i in range(NTT) if not (i < NTC and last)]
                run_skewed([tileD(i, tc) for tc, i in enumerate(tilesD)])
                S.barrier()
                S.release(mk_stD)
        if stop_after == "D":
            return finish(nc, S, [R_xs_mix, R_h2tok])

        tilesD = [i for i in range(NTT) if not (i < NTC and last)]
        with ExitStack() as stS:
            mk_stS = S.mark()

            def aS(name, shape, dt=F32):
                return stS.enter_context(nc.sbuf_tensor("%s_L%d" % (name, l), shape, dt))
            CUM = srt[:, 0:4]
            op("dve", lambda e: e.memset(srt[:], 0.0), [], [R_srt])
            op("dve", lambda e: e.memset(dest_f[:], 0.0), [], [R_destf])
            tmp4_r = Ring(aS, "tmp4", [128, 4], F32, 2)
            for n_, i in enumerate(tilesD):
                pr, Rpr = PBK[n_ % 2]
                op("pe", lambda e: e.matmul(pr[:, 0:4], lhsT=tri_sb[:], rhs=goh_all[:, i, :], start=True, stop=True), [R_tri, R_goh], [Rpr])
                op("pe", lambda e: e.matmul(pr[:, 4:8], lhsT=ones_sb[:], rhs=goh_all[:, i, :], start=True, stop=True), [R_ones, R_goh], [Rpr])
                t4, Rt4 = tmp4_r.get()
                op("dve", lambda e: e.tensor_tensor(out=t4[:], in0=pr[:, 0:4], in1=CUM, op=ALU.add), [Rpr, R_srt], [Rt4])
                op("dve", lambda e: e.tensor_tensor(out=t4[:], in0=t4[:], in1=goh_all[:, i, :], op=ALU.mult), [Rt4, R_goh], [Rt4])
                op("dve", lambda e: e.tensor_reduce(out=dest_f[:, i:i + 1], in_=t4[:], axis=AX.X, op=ALU.add), [Rt4], [R_destf])
                op("dve", lambda e: e.tensor_tensor(out=CUM, in0=pr[:, 4:8], in1=CUM, op=ALU.add), [Rpr, R_srt], [R_srt])
            tk, Rtk = aS("tk", [128, NB]), Res("tk")
            for g in range(4):
                op("dve", lambda e: e.tensor_scalar(out=tk[:], in0=thr_sb[:], scalar1=srt[:, g:g + 1], scalar2=None, op0=ALU.is_lt), [R_thr, R_srt], [Rtk])
                op("dve", lambda e: e.tensor_reduce(out=srt[:, 8 + g:9 + g], in_=tk[:], axis=AX.X, op=ALU.add), [Rtk], [R_srt])
            op("dve", lambda e: e.memset(srt[:, 16:17], 0.0), [R_srt], [R_srt])
            for g in range(1, 4):
                op("dve", lambda e: e.tensor_tensor(out=srt[:, 16 + g:17 + g], in0=srt[:, 15 + g:16 + g], in1=srt[:, 7 + g:8 + g], op=ALU.add), [R_srt], [R_srt])
            op("dve", lambda e: e.tensor_scalar(out=srt[:, 24:28], in0=srt[:, 16:20], scalar1=512.0, scalar2=None, op0=ALU.mult), [R_srt], [R_srt])
            for g in range(4):
                op("dve", lambda e: e.scalar_tensor_tensor(out=dest_f[:], in0=goh_all[:, :, g], scalar=srt[:, 24 + g:25 + g], in1=dest_f[:],
                                                           op0=ALU.mult, op1=ALU.add), [R_goh, R_srt, R_destf], [R_destf])
            op("dve", lambda e: e.tensor_copy(out=dest_i[:], in_=dest_f[:]), [R_destf], [R_desti])
            gb, Rgb = aS("gb", [128, NB]), Res("gb")
            op("dve", lambda e: e.memset(gb[:], 0.0), [], [Rgb])
            for g in range(1, 4):
                op("dve", lambda e: e.tensor_scalar(out=tk[:], in0=blk_sb[:], scalar1=srt[:, 16 + g:17 + g], scalar2=None, op0=ALU.is_ge), [R_blk, R_srt], [Rtk])
                op("dve", lambda e: e.tensor_tensor(out=gb[:], in0=gb[:], in1=tk[:], op=ALU.add), [Rtk, Rgb], [Rgb])
            op("dve", lambda e: e.tensor_scalar(out=gb[:], in0=gb[:], scalar1=1024.0, scalar2=float(l * NE * 128), op0=ALU.mult, op1=ALU.add), [Rgb], [Rgb])
            op("dve", lambda e: e.tensor_tensor(out=widx_f[:], in0=gb[:].unsqueeze(2).to_broadcast([128, NB, 8]),
                                                in1=jp_sb[:].unsqueeze(1).to_broadcast([128, NB, 8]), op=ALU.add), [Rgb, R_jp], [R_widxf])
            op("dve", lambda e: e.tensor_copy(out=widx_i[:], in_=widx_f[:].rearrange("p b j -> p (b j)")), [R_widxf], [R_widxi])
            h2r_r = Ring(aS, "h2r", [128, D], BF16, 3)
            for i in tilesD:
                h2r, Rh2r = h2r_r.get()
                dma("sp", h2r[:], h2tok_scr[i * 128:(i + 1) * 128, :], [R_h2tok], [Rh2r])
                S.idma(h2perm_scr, h2r[:], dest_i[:, i:i + 1], True, [Rh2r, R_desti, R_h2pz], [R_h2perm])
                S.idma(c8perm_scr, c8_all[:, i, :], dest_i[:, i:i + 1], True, [R_c8, R_desti, R_c8pz], [R_c8perm])
            S.barrier()
            S.release(mk_stS)
        if stop_after == "S":
            return finish(nc, S, [R_h2perm, R_c8perm])

        with ExitStack() as stE:
            mk_stE = S.mark()

            def aE(name, shape, dt=F32):
                return stE.enter_context(nc.sbuf_tensor("%s_L%d" % (name, l), shape, dt))
            if not last:
                load_mix_weights(l + 1)
            hp_r = Ring(aE, "hp", [128, 4, D], BF16, 2)
            h2Tb_r = Ring(aE, "h2TbE", [128, 8, 512], BF16, 2)
            c8_r = Ring(aE, "c8b", [128, 4, 8], F32, 2)
            c8T_r = Ring(aE, "c8T", [8, 512], F32, 2)
            wg_r = Ring(aE, "wg", [128, 8 * DE], BF16, 3)
            wu_r = Ring(aE, "wu", [128, 8 * DE], BF16, 3)
            wd_r = Ring(aE, "wd", [128, 2 * D], BF16, 3)
            cb_r = Ring(aE, "cb", [128, 512], F32, 3)
            sg_r = Ring(aE, "sg", [128, 512], F32, 3)
            hid = aE("hidT_all", [128, 16, 512], BF16)
            R_hid = [Res("hid%d" % e) for e in range(8)]
            yo_r = Ring(aE, "yo", [128, D], F32, 3)
            ecnt = 0
            NB_l = ((T if last else TT) + 4 * 511) // 512
            for b in range(NB_l):
                hp, Rhp = hp_r.get()
                dma("sp", hp[:], h2perm_scr[b * 512:(b + 1) * 512, :].rearrange("(j p) d -> p j d", p=128), [R_h2perm], [Rhp])
                c8, Rc8 = c8_r.get()
                dma("sp", c8[:], c8perm_scr[b * 512:(b + 1) * 512, :].rearrange("(j p) e -> p j e", p=128), [R_c8perm], [Rc8])
                hb_, Rhb_ = h2Tb_r.get()
                for j in range(4):
                    pb, Rpb = PBK[j]
                    pbv = bfv(pb)
                    for k in range(8):
                        op("pe", lambda e: e.transpose(out=pbv[:, k * 128:(k + 1) * 128], in_=hp[:, j, k * 128:(k + 1) * 128], identity=ident_b[:]),
                           [Rhp, R_idb], [Rpb])
                    eng = "act" if j % 2 == 0 else "dve"
                    if eng == "act":
                        op("act", lambda e: e.copy(out=hb_[:, :, j * 128:(j + 1) * 128], in_=pbv[:, 0:1024].rearrange("p (k t) -> p k t", k=8)), [Rpb], [Rhb_])
                    else:
                        op("dve", lambda e: e.tensor_copy(out=hb_[:, :, j * 128:(j + 1) * 128], in_=pbv[:, 0:1024].rearrange("p (k t) -> p k t", k=8)), [Rpb], [Rhb_])
                pc, Rpc = PBK[4]
                for j in range(4):
                    op("pe", lambda e: e.transpose(out=pc[0:8, j * 128:(j + 1) * 128], in_=c8[:, j, :], identity=ident_f[:]), [Rc8, R_idf], [Rpc])
                c8T, Rc8T = c8T_r.get()
                op("dve", lambda e: e.tensor_copy(out=c8T[:], in_=pc[0:8, 0:512]), [Rpc], [Rc8T])
                dma("sp", cbT_scr[b], c8T[:], [Rc8T], [R_cbT])
                for j_ in range(8):
                    wg, Rwg = wg_r.get()
                    wu, Rwu = wu_r.get()
                    cb, Rcb_ = cb_r.get()
                    ix = widx_i[:, b * 8 + j_:b * 8 + j_ + 1]
                    S.idma(wg[:], wg_scr, ix, False, [R_wg, R_widxi], [Rwg])
                    S.idma(wu[:], wu_scr, ix, False, [R_wu, R_widxi], [Rwu])
                    dma("sp", cb[:], cbT_scr[b, j_, :].partition_broadcast(128), [R_cbT], [Rcb_])
                    wgv = wg[:].rearrange("p (k h) -> p k h", k=8)
                    wuv = wu[:].rearrange("p (k h) -> p k h", k=8)
                    base = 4 * (ecnt % 2)
                    ecnt += 1
                    for hc in range(2):
                        gt, Rg = PBK[base + hc]
                        ut, Ru = PBK[base + 2 + hc]
                        for k in range(8):
                            op("pe", lambda e: e.matmul(gt[:, :], lhsT=wgv[:, k, hc * 128:(hc + 1) * 128], rhs=hb_[:, k, :], start=(k == 0), stop=(k == 7)),
                               [Rwg, Rhb_], [Rg])
                        for k in range(8):
                            op("pe", lambda e: e.matmul(ut[:, :], lhsT=wuv[:, k, hc * 128:(hc + 1) * 128], rhs=hb_[:, k, :], start=(k == 0), stop=(k == 7)),
                               [Rwu, Rhb_], [Ru])
                    for hc in range(2):
                        gt, Rg = PBK[base + hc]
                        ut, Ru = PBK[base + 2 + hc]
                        sg, Rsg = sg_r.get()
                        op("act", lambda e: e.activation(out=sg[:], in_=gt[:, :], func=AF.Silu), [Rg], [Rsg])
                        op("dve", lambda e: e.tensor_tensor(out=sg[:], in0=ut[:, :], in1=sg[:], op=ALU.mult), [Ru, Rsg], [Rsg])
                        op("dve", lambda e: e.tensor_tensor(out=hid[:, 2 * j_ + hc, :], in0=sg[:], in1=cb[:], op=ALU.mult),
                           [Rsg, Rcb_], [R_hid[j_]])
                for j_ in range(8):
                    wd, Rwd = wd_r.get()
                    ix = widx_i[:, b * 8 + j_:b * 8 + j_ + 1]
                    S.idma(wd[:], wd_scr, ix, False, [R_wd, R_widxi], [Rwd])
                    wdv = wd[:].rearrange("p (c d) -> p c d", c=2)
                    for hc in range(2):
                        for j in range(4):
                            for dh in range(2):
                                yt, Ry_ = PBK[j * 2 + dh]
                                op("pe", lambda e: e.matmul(yt[:, :], lhsT=hid[:, 2 * j_ + hc, j * 128:(j + 1) * 128], rhs=wdv[:, hc, dh * 512:(dh + 1) * 512],
                                                            start=(j_ == 0 and hc == 0), stop=(j_ == 7 and hc == 1)), [R_hid[j_], Rwd], [Ry_])
                for j in range(4):
                    yo, Ryo = yo_r.get()
                    for dh in range(2):
                        yt, Ry_ = PBK[j * 2 + dh]
                        if dh == 0:
                            op("act", lambda e: e.copy(out=yo[:, 0:512], in_=yt[:, :]), [Ry_], [Ryo])
                        else:
                            op("dve", lambda e: e.tensor_copy(out=yo[:, 512:1024], in_=yt[:, :]), [Ry_], [Ryo])
                    r0 = b * 512 + j * 128
                    dma("sp", yperm_scr[r0:r0 + 128, :], yo[:], [Ryo], [R_yperm])
            S.barrier()
            S.release(mk_stE)
        if stop_after == "E":
            return finish(nc, S, [R_yperm])

        with ExitStack() as stF:
            mk_stF = S.mark()

            def aF(name, shape, dt=F32):
                return stF.enter_context(nc.sbuf_tensor("%s_L%d" % (name, l), shape, dt))
            g2bc = {}
            for r in range(2):
                if r == 1 and last:
                    continue
                t = aF("g2_%d" % r, [128, D])
                Rr = Res("g2_%d" % r)
                load_bc(t, Rr, ada_vec(l, r, 5), D, [R_ada])
                g2bc[r] = (t, Rr)
            ln2g_bc, R_l2g = aF("ln2g_bc", [128, D]), Res("ln2g_bc")
            ln2b_bc, R_l2b = aF("ln2b_bc", [128, D]), Res("ln2b_bc")
            load_bc(ln2g_bc, R_l2g, ln2_g[l], D)
            load_bc(ln2b_bc, R_l2b, ln2_b[l], D)
            xt_r = Ring(aF, "xtF", [128, D], F32, 3)
            yg_r = Ring(aF, "ygF", [128, D], F32, 3)
            o_r = Ring(aF, "oF", [128, D], F32, 3)
            lnr = {"st": Ring(aF, "fst", [128, 12], F32, 3), "mv": Ring(aF, "fmv", [128, 4], F32, 3)}

            def tileF(i):
                typ = 1 if i < NTC else 0
                gg = i * 128
                xt, Rxt = xt_r.get()
                dma("sp", xt[:], xs_mix[gg:gg + 128, :], [R_xs_mix], [Rxt])
                yg, Ryg = yg_r.get()
                S.idma(yg[:], yperm_scr, dest_i[:, i:i + 1], False, [R_yperm, R_desti], [Ryg])
                yield
                g2t, Rg2 = g2bc[typ]
                op("dve", lambda e: e.tensor_tensor(out=yg[:], in0=yg[:], in1=g2t[:], op=ALU.mult), [Ryg, Rg2], [Ryg])
                op("dve", lambda e: e.scalar_tensor_tensor(out=yg[:], in0=xt[:], scalar=ALPHA, in1=yg[:], op0=ALU.mult, op1=ALU.add), [Rxt, Ryg], [Ryg])
                o, Ro = o_r.get()
                layer_norm_tile(None, "dve", yg, Ryg, D, ln2g_bc, R_l2g, ln2b_bc, R_l2b, o, Ro, lnr)
                if last:
                    dma("sp", out_d[gg - C:gg - C + 128, :], o[:], [Ro], [R_out])
                else:
                    dma("sp", xs_out[l % 2][gg:gg + 128, :], o[:], [Ro], [R_xs_out[l % 2]])

            run_skewed([tileF(i) for i in tilesD])
            S.barrier()
            S.release(mk_stF)
    return finish(nc, S, [R_out])


def finish(nc, S, ress):
    S.barrier()
    S.wait_all("sp", ress)
    return nc


def _rope_tables(T, C):
    rows = T // GRID_W
    row = np.repeat(np.arange(rows), GRID_W).astype(np.float32)
    col = np.tile(np.arange(GRID_W), rows).astype(np.float32)
    d_axis = DR // 2
    inv_freq = np.power(np.float32(10000.0), -np.arange(0, d_axis, 2, dtype=np.float32) / np.float32(d_axis)).astype(np.float32)

    def ax(p):
        a = p[:, None] * inv_freq[None, :]
        return np.concatenate([a, a], -1)

    ang = np.concatenate([ax(row), ax(col)], -1).astype(np.float32)
    cos = np.cos(ang).astype(np.float32)
    sin = np.sin(ang).astype(np.float32)
    sgn = np.tile(np.concatenate([-np.ones(8), np.ones(8)]), 2).astype(np.float32)
    tab = np.zeros((T + C, 2, DR), np.float32)
    tab[:C, 0, :] = 1.0
    tab[C:, 0, :] = cos
    tab[C:, 1, :] = sin * sgn[None, :]
    return tab


def _pool_tables():
    wins = (2, 4, 8, 16)
    edge = np.zeros((128, 2, 2, 8), np.float32)
    invw = np.zeros((128, 2), np.float32)
    for k in range(2):
        for ph in range(2):
            w = wins[2 * k + ph]
            ps = slice(ph * 64, ph * 64 + 64)
            invw[ps, k] = 1.0 / w
            for j in range(8):
                t = j
                cnt = (t + w // 2 - 1) - max(t - w // 2, 0) + 1
                edge[ps, k, 0, j] = 1.0 / cnt
                r = 7 - j
                hi = min(w // 2 - 1, r)
                cnt = hi + w // 2 + 1
                edge[ps, k, 1, j] = 1.0 / cnt
    return edge, invw


def _sort_tables(T, C):
    TT = T + C
    NB = (TT + 4 * 511 + 511) // 512
    tri = np.triu(np.ones((128, 128), np.float32), k=1)
    thr = np.broadcast_to((np.arange(NB, dtype=np.float32) * 512.0)[None, :], (128, NB)).copy()
    blk = np.broadcast_to(np.arange(NB, dtype=np.float32)[None, :], (128, NB)).copy()
    jp = (np.arange(8, dtype=np.float32)[None, :] * 128.0 + np.arange(128, dtype=np.float32)[:, None]).astype(np.float32)
    return {"tri": tri, "thr_bc": thr, "blk_bc": blk, "jp": jp}


_CACHE = {}


def _consts(T, C):
    edge, invw = _pool_tables()
    return {
        "ident": np.eye(128, dtype=np.float32),
        "rope_cs": _rope_tables(T, C),
        "pool_edge": edge,
        "pool_invw": invw,
        **_sort_tables(T, C),
    }


_WKEYS = ["w_ada", "b_ada", "w_in", "g_q", "w_uq", "g_kv", "w_ukv", "conv_w", "conv_b", "conv_ln_g", "conv_ln_b", "pool_w",
          "pool_scale", "w_out", "ln1_g", "ln1_b", "w_router_group", "b_router_group", "w_router_expert", "b_router_expert",
          "w_gate", "w_up", "w_down", "ln2_g", "ln2_b"]


def make_in_maps(inputs, T, C, ncores):
    consts = _consts(T, C)
    shared = {k: np.ascontiguousarray(np.asarray(inputs[k], dtype=np.float32)) for k in _WKEYS}
    maps = []
    for b in range(ncores):
        m = dict(shared)
        m.update(consts)
        m["x"] = np.ascontiguousarray(np.asarray(inputs["x"][b], dtype=np.float32))
        m["ctx"] = np.ascontiguousarray(np.asarray(inputs["ctx"][b], dtype=np.float32))
        m["cvec"] = np.ascontiguousarray(np.stack([np.asarray(inputs["c"][b]), np.asarray(inputs["c_ctx"])]).astype(np.float32))
        maps.append(m)
    return maps


def kernel(**inputs):
    x = np.asarray(inputs["x"])
    B, T, _ = x.shape
    C = np.asarray(inputs["ctx"]).shape[1]
    L = np.asarray(inputs["w_ada"]).shape[0]
    key = (T, C, L)
    if key not in _CACHE:
        _CACHE[key] = build(T, C, L)
    nc = _CACHE[key]
    maps = make_in_maps(inputs, T, C, B)
    res = run_bass_kernel_spmd(nc, maps, core_ids=list(range(B)))
    return np.stack([np.asarray(r["out"]) for r in res.results], axis=0).astype(np.float32)
```

```python
import math
from contextlib import ExitStack
import numpy as np
import concourse.bass as bass
import concourse.mybir as mybir
from concourse.bass_utils import run_bass_kernel_spmd

F32 = mybir.dt.float32
BF16 = mybir.dt.bfloat16
AF = mybir.ActivationFunctionType
ALU = mybir.AluOpType
AX = mybir.AxisListType

D = 1024
H = 8
DQ = 384
DKV = 256
DR = 32
DIN = 1440
NE = 32
DE = 256
GRID_W = 64
CONVW = 31
LN_EPS = 1e-5
RMS_EPS = 1e-6
NEG = -1.0e30


class Res:
    __slots__ = ("name", "w", "r", "sem", "cnt", "excl", "multi")

    def __init__(self, name, excl=False, multi=False, unordered=False):
        self.multi = unordered
        self.name = name
        self.w = None
        self.r = []
        self.sem = None
        self.cnt = 0
        self.excl = excl


class Sched:
    def __init__(self, nc):
        self.nc = nc
        self.eng = {"pe": nc.tensor, "act": nc.scalar, "dve": nc.vector, "pool": nc.gpsimd, "sp": nc.sync}
        self.sems = {}
        self.cnt = {}
        self.known = {}
        self.dma_res = []
        self.free_sems = []
        self.free_sw = []
        self.is_sw = {}
        self.nalloc = 0
        self.nwait = 0
        for e in self.eng:
            self.sems[e] = nc.alloc_semaphore("e_" + e)
            self.cnt[e] = 0
            self.known[e] = {}

    def _waits(self, e, reads, writes):
        deps = {}

        def add(ev):
            if ev is None:
                return
            k, v = ev
            if deps.get(k, 0) < v:
                deps[k] = v

        for r in reads:
            add(r.w)
        for w in writes:
            if not w.multi:
                add(w.w)
            for ev in w.r:
                add(ev)
        kn = self.known[e]
        for k, v in deps.items():
            if kn.get(k, 0) >= v:
                continue
            if e == "pe" and k == "pe":
                continue
            kn[k] = v
            sem = self.sems[k] if isinstance(k, str) else k.sem
            self.eng[e].wait_ge(sem, v)
            self.nwait += 1

    @staticmethod
    def _commit(ev, reads, writes):
        for r in reads:
            r.r.append(ev)
            if len(r.r) > 48:
                best = {}
                for k, v in r.r:
                    if best.get(k, 0) < v:
                        best[k] = v
                r.r = list(best.items())
        for w in writes:
            w.w = ev
            w.r = []

    def op(self, e, fn, reads=(), writes=()):
        if any(r.excl for r in reads):
            writes = list(writes) + [r for r in reads if r.excl and r not in writes]
            reads = [r for r in reads if not r.excl]
        self._waits(e, reads, writes)
        ins = fn(self.eng[e])
        self.cnt[e] += 1
        ins.then_inc(self.sems[e], 1)
        self._commit((e, self.cnt[e]), reads, writes)

    def dma(self, e, out, in_, reads, writes, **kw):
        dst = writes[0]
        self._waits(e, reads, writes)
        self.ensure(dst, sw=(e == "pool"))
        dst.cnt += 16
        self.eng[e].dma_start(out=out, in_=in_, **kw).then_inc(dst.sem, 16)
        self._commit((dst, dst.cnt), reads, writes)

    def idma(self, out, in_, idx_ap, scatter, reads, writes):
        import concourse.bass as _b
        dst = writes[0]
        self._waits("pool", reads, writes)
        self.ensure(dst, sw=True)
        dst.cnt += 16
        off = _b.IndirectOffsetOnAxis(ap=idx_ap, axis=0)
        if scatter:
            ins = self.nc.gpsimd.indirect_dma_start(out=out, out_offset=off, in_=in_, in_offset=None)
        else:
            ins = self.nc.gpsimd.indirect_dma_start(out=out, out_offset=None, in_=in_, in_offset=off)
        ins.then_inc(dst.sem, 16)
        self._commit((dst, dst.cnt), reads, writes)

    def ensure(self, dst, sw=False):
        if dst.sem is None:
            fl = self.free_sw if sw else self.free_sems
            self.is_sw[id(dst)] = sw
            if fl:
                dst.sem, dst.cnt = fl.pop()
            else:
                self.nalloc += 1
                dst.sem = self.nc.alloc_semaphore("d%d_%s" % (self.nalloc, dst.name))
                dst.cnt = 0
            self.dma_res.append(dst)

    def mark(self):
        return len(self.dma_res)

    def release(self, mark):
        for r in self.dma_res[mark:]:
            (self.free_sw if self.is_sw.get(id(r)) else self.free_sems).append((r.sem, r.cnt))
            r.sem = None
        del self.dma_res[mark:]

    def barrier(self):
        for e in self.eng:
            kn = self.known[e]
            for k in self.eng:
                if k == e:
                    continue
                v = self.cnt[k]
                if v > 0 and kn.get(k, 0) < v:
                    kn[k] = v
                    self.eng[e].wait_ge(self.sems[k], v)
            for r in self.dma_res:
                if r.cnt > 0 and kn.get(r, 0) < r.cnt:
                    kn[r] = r.cnt
                    self.eng[e].wait_ge(r.sem, r.cnt)

    def wait_all(self, e, ress):
        self._waits(e, ress, ())


class Ring:
    def __init__(self, alloc, name, shape, dt, n):
        self.bufs = []
        for i in range(n):
            nm = "%s_%d" % (name, i)
            self.bufs.append((alloc(nm, shape, dt), Res(nm)))
        self.i = 0

    def get(self):
        b = self.bufs[self.i % len(self.bufs)]
        self.i += 1
        return b


class _Cut(Exception):
    pass


def run_skewed(gens):
    active = []
    it = iter(gens)
    while True:
        g = next(it, None)
        if g is not None:
            active.append(g)
        elif not active:
            break
        for g_ in list(reversed(active)):
            try:
                next(g_)
            except StopIteration:
                active.remove(g_)


def build(T, C, L, debug=False, stop_after=None):
    st = {}
    try:
        return _build(T, C, L, debug, stop_after, st)
    except _Cut:
        return finish(st["nc"], st["S"], [])


def _build(T, C, L, debug, stop_after, st_):
    NTL = T // 128
    NTC = C // 128
    TT = T + C
    NTT = NTL + NTC
    ALPHA = float((2 * L) ** 0.25)
    QS = 1.0 / math.sqrt(96.0)
    SEG = min(1024, T)

    nc = bass.Bass("TRN2", target_bir_lowering=False)
    S = Sched(nc)
    op = S.op
    dma = S.dma
    st_["nc"] = nc
    st_["S"] = S

    def cut(tag):
        if stop_after == tag:
            raise _Cut()

    def din(name, shape, dt=F32):
        return nc.dram_tensor(name, shape, dt, kind="ExternalInput").ap()

    def dscr(name, shape, dt=F32):
        return nc.dram_tensor(name, shape, dt, kind=("ExternalOutput" if debug else "Internal")).ap()

    x_in = din("x", [T, D])
    ctx_in = din("ctx", [C, D])
    cvec = din("cvec", [2, D])
    w_ada = din("w_ada", [L, D, 6 * D])
    b_ada = din("b_ada", [L, 6 * D])
    w_in = din("w_in", [L, D, DIN])
    g_q = din("g_q", [L, DQ])
    w_uq = din("w_uq", [L, DQ, 768])
    g_kv = din("g_kv", [L, DKV])
    w_ukv = din("w_ukv", [L, DKV, 1024])
    conv_w = din("conv_w", [L, CONVW, 256])
    conv_b = din("conv_b", [L, 256])
    conv_ln_g = din("conv_ln_g", [L, 256])
    conv_ln_b = din("conv_ln_b", [L, 256])
    pool_w = din("pool_w", [L, 4, 64, 64])
    pool_scale = din("pool_scale", [L, 256])
    w_out = din("w_out", [L, D, D])
    ln1_g = din("ln1_g", [L, D])
    ln1_b = din("ln1_b", [L, D])
    w_rg = din("w_router_group", [L, D, 4])
    b_rg = din("b_router_group", [L, 4])
    w_re = din("w_router_expert", [L, D, NE])
    b_re = din("b_router_expert", [L, NE])
    w_gate = din("w_gate", [L, NE, D, DE])
    w_up = din("w_up", [L, NE, D, DE])
    w_down = din("w_down", [L, NE, DE, D])
    ln2_g = din("ln2_g", [L, D])
    ln2_b = din("ln2_b", [L, D])
    ident_d = din("ident", [128, 128])
    rope_d = din("rope_cs", [TT, 2, DR])
    pedge_d = din("pool_edge", [128, 2, 2, 8])
    pinvw_d = din("pool_invw", [128, 2])

    NB = (TT + 4 * 511 + 511) // 512
    NP = NB * 512
    I32 = mybir.dt.int32
    tri_d = din("tri", [128, 128])
    thr_d = din("thr_bc", [128, NB])
    blk_d = din("blk_bc", [128, NB])
    jp_d = din("jp", [128, 8])
    out_d = nc.dram_tensor("out", [T, D], F32, kind="ExternalOutput").ap()
    h2tok_scr = dscr("h2tok_scr", [TT, D], BF16)
    h2perm_scr = dscr("h2perm_scr", [NP, D], BF16)
    c8perm_scr = dscr("c8perm_scr", [NP, 8])
    cbT_scr = dscr("cbT_scr", [NB, 8, 512])
    yperm_scr = dscr("yperm_scr", [NP, D])
    R_h2tok = Res("h2tok_scr", multi=True)
    R_h2perm = Res("h2perm_scr", unordered=True)
    R_c8perm = Res("c8perm_scr", unordered=True)
    R_cbT = Res("cbT_scr", multi=True)
    R_yperm = Res("yperm_scr", multi=True)

    ada_scr = dscr("ada_scr", [L, 2, 6 * D])
    xs_mix = dscr("xs_mix", [TT, D])
    xs_out = [dscr("xs_out0", [TT, D]), dscr("xs_out1", [TT, D])]
    kT_scr = dscr("kT_scr", [H, 97, TT], BF16)
    qT_scr = dscr("qT_scr", [H, 96, TT], BF16)
    mT_scr = dscr("mT_scr", [H, TT], BF16)
    v_scr = dscr("v_scr", [H, 128, NTT, 80], BF16)
    catcp_scr = dscr("catcp_scr", [TT, 512], BF16)
    h2T_scr = dscr("h2T_scr", [8, 128, TT], BF16)
    combT_scr = dscr("combT_scr", [NE, TT])
    wg_scr = nc.dram_tensor("wg_scr", [L * NE * 128, 8 * DE], BF16, kind="Internal").ap()
    wu_scr = nc.dram_tensor("wu_scr", [L * NE * 128, 8 * DE], BF16, kind="Internal").ap()
    wd_scr = nc.dram_tensor("wd_scr", [L * NE * 128, 2 * D], BF16, kind="Internal").ap()
    R_ada = Res("ada_scr")
    R_xs_mix = Res("xs_mix", multi=True)
    R_xs_out = [Res("xs_out0", multi=True), Res("xs_out1", multi=True)]
    R_kT = Res("kT_scr", multi=True)
    R_qT = Res("qT_scr", multi=True)
    R_mT = Res("mT_scr")
    R_v = Res("v_scr", multi=True)
    R_catcp = Res("catcp_scr", multi=True)
    R_h2T = Res("h2T_scr", multi=True)
    R_combT = Res("combT_scr", multi=True)
    R_wg = Res("wg_scr")
    R_wu = Res("wu_scr")
    R_wd = Res("wd_scr")
    R_out = Res("out", multi=True)
    for R_ in [R_h2perm, R_c8perm]:
        S.ensure(R_, sw=True)
    R_h2pz = Res("h2perm_zero")
    R_c8pz = Res("c8perm_zero")
    S.ensure(R_h2pz)
    S.ensure(R_c8pz)
    for R_ in [R_h2tok, R_cbT, R_yperm]:
        S.ensure(R_)
    for R_ in [R_ada, R_xs_mix, R_xs_out[0], R_xs_out[1], R_kT, R_qT, R_mT, R_v, R_catcp, R_h2T, R_combT, R_out]:
        S.ensure(R_)
    for R_ in [R_wg, R_wu, R_wd]:
        S.ensure(R_, sw=True)

    PBK = []
    for i in range(8):
        PBK.append((nc.alloc_psum_tensor("pb%d" % i, [128, 512], F32), Res("pb%d" % i, excl=True)))

    def bfv(t):
        return t[:].bitcast(BF16)

    def palloc(name, shape, dt=F32):
        return nc.alloc_sbuf_tensor(name, shape, dt)

    def sb(name, shape, dt=F32):
        return nc.alloc_sbuf_tensor(name, shape, dt), Res(name)

    ident_f, R_idf = sb("ident_f", [128, 128])
    ident_b, R_idb = sb("ident_b", [128, 128], BF16)
    dma("sp", ident_f[:], ident_d, [], [R_idf])
    op("dve", lambda e: e.tensor_copy(out=ident_b[:], in_=ident_f[:]), [R_idf], [R_idb])
    eps_t, R_epst = sb("eps_t", [128, 2])
    op("dve", lambda e: e.memset(eps_t[:, 0:1], LN_EPS), [], [R_epst])
    op("dve", lambda e: e.memset(eps_t[:, 1:2], RMS_EPS), [R_epst], [R_epst])
    tri_sb, R_tri = sb("tri_sb", [128, 128])
    dma("sp", tri_sb[:], tri_d, [], [R_tri])
    ones_sb, R_ones = sb("ones_sb", [128, 128])
    op("dve", lambda e: e.memset(ones_sb[:], 1.0), [], [R_ones])
    thr_sb, R_thr = sb("thr_sb", [128, NB])
    blk_sb, R_blk = sb("blk_sb", [128, NB])
    jp_sb, R_jp = sb("jp_sb", [128, 8])
    dma("sp", thr_sb[:], thr_d, [], [R_thr])
    dma("sp", blk_sb[:], blk_d, [], [R_blk])
    dma("sp", jp_sb[:], jp_d, [], [R_jp])
    zer_b, R_zerb = sb("zer_b", [128, D], BF16)
    zer_f, R_zerf = sb("zer_f", [128, 8])
    op("dve", lambda e: e.memset(zer_b[:], 0.0), [], [R_zerb])
    op("dve", lambda e: e.memset(zer_f[:], 0.0), [], [R_zerf])
    goh_all, R_goh = sb("goh_all", [128, NTT, 4])
    c8_all, R_c8 = sb("c8_all", [128, NTT, 8])
    dest_f, R_destf = sb("dest_f", [128, NTT])
    dest_i, R_desti = sb("dest_i", [128, NTT], I32)
    widx_f, R_widxf = sb("widx_f", [128, NB, 8])
    widx_i, R_widxi = sb("widx_i", [128, NB * 8], I32)
    srt, R_srt = sb("srt", [128, 64])
    pedge, R_pedge = sb("pedge", [128, 2, 2, 8])
    pinvw, R_pinvw = sb("pinvw", [128, 2])
    dma("sp", pedge[:], pedge_d, [], [R_pedge])
    dma("sp", pinvw[:], pinvw_d, [], [R_pinvw])

    w_in_sb, R_win = sb("w_in_sb", [128, 8, DIN], BF16)
    w_uq_sb, R_wuq = sb("w_uq_sb", [128, 3, 768], BF16)
    w_ukv_sb, R_wukv = sb("w_ukv_sb", [128, 2, 1024], BF16)
    for R_ in (R_win, R_wuq, R_wukv):
        S.ensure(R_, sw=True)

    def load_mix_weights(l_):
        dma("pool", w_in_sb[:], w_in[l_].rearrange("(k p) n -> p k n", p=128), [], [R_win])
        dma("pool", w_uq_sb[:], w_uq[l_].rearrange("(k p) n -> p k n", p=128), [], [R_wuq])
        dma("pool", w_ukv_sb[:], w_ukv[l_].rearrange("(k p) n -> p k n", p=128), [], [R_wukv])

    load_mix_weights(0)

    for l in range(L if stop_after not in ("0", "A", "C", "B", "D") else 0):
        for e0 in range(0, NE, 8):
            for e1 in range(e0, e0 + 8):
                r0 = (l * NE + e1) * 128
                dma("pool", wg_scr[r0:r0 + 128, :].rearrange("p (k h) -> p k h", k=8), w_gate[l, e1].rearrange("(k p) h -> p k h", p=128), [], [R_wg])
                dma("pool", wu_scr[r0:r0 + 128, :].rearrange("p (k h) -> p k h", k=8), w_up[l, e1].rearrange("(k p) h -> p k h", p=128), [], [R_wu])
                dma("pool", wd_scr[r0:r0 + 128, :].rearrange("p (c d) -> p c d", c=2), w_down[l, e1].rearrange("(c p) d -> p c d", p=128), [], [R_wd])

    with ExitStack() as st0:
        mk0 = S.mark()

        def a0(name, shape, dt=F32):
            return st0.enter_context(nc.sbuf_tensor(name, shape, dt))
        cs, R_cs = a0("cs", [2, D]), Res("cs")
        csT, R_csT = a0("csT", [128, 8, 2]), Res("csT")
        bada, R_bada = a0("bada", [2, 6 * D]), Res("bada")
        adas, R_adas = a0("adas", [2, 6 * D]), Res("adas")
        wblk = Ring(a0, "wblk", [128, 8, 512], F32, 2)
        dma("sp", cs[:], cvec, [], [R_cs])
        op("act", lambda e: e.activation(out=cs[:], in_=cs[:], func=AF.Silu), [R_cs], [R_cs])
        pb, Rpb = PBK[0]
        for k in range(8):
            op("pe", lambda e: e.transpose(out=pb[:, 2 * k:2 * k + 2], in_=cs[0:2, k * 128:(k + 1) * 128],
                                           identity=ident_f[0:2, 0:2]), [R_cs, R_idf], [Rpb])
        op("dve", lambda e: e.tensor_copy(out=csT[:].rearrange("p k r -> p (k r)"), in_=pb[:, 0:16]), [Rpb], [R_csT])
        nb_i = 0
        for l in range(L):
            dma("sp", bada[:], b_ada[l].partition_broadcast(2), [], [R_bada])
            for nb in range(12):
                wb, Rwb = wblk.get()
                dma("sp", wb[:], w_ada[l, :, nb * 512:(nb + 1) * 512].rearrange("(k p) n -> p k n", p=128), [], [Rwb])
                pb, Rpb = PBK[1 + (nb_i % 2)]
                nb_i += 1
                for k in range(8):
                    op("pe", lambda e: e.matmul(pb[0:2, :], lhsT=csT[:, k, :], rhs=wb[:, k, :], start=(k == 0), stop=(k == 7)),
                       [R_csT, Rwb], [Rpb])
                op("dve", lambda e: e.tensor_tensor(out=adas[:, nb * 512:(nb + 1) * 512], in0=pb[0:2, :],
                                                    in1=bada[:, nb * 512:(nb + 1) * 512], op=ALU.add), [Rpb, R_bada], [R_adas])
            for j in (1, 4):
                op("dve", lambda e: e.tensor_scalar_add(out=adas[:, j * D:(j + 1) * D], in0=adas[:, j * D:(j + 1) * D], scalar1=1.0),
                   [R_adas], [R_adas])
            dma("sp", ada_scr[l], adas[:], [R_adas], [R_ada])
        S.barrier()
        S.release(mk0)

    if stop_after == "0":
        return finish(nc, S, [R_ada])

    def ada_vec(l, r, j):
        return ada_scr[l, r, j * D:(j + 1) * D]

    def load_bc(t, R, src1d, n, rd=()):
        dma("sp", t[:, 0:n], src1d.partition_broadcast(128), list(rd), [R])

    def x_src(l, i):
        if l == 0:
            if i < NTC:
                return ctx_in[i * 128:(i + 1) * 128, :], []
            return x_in[(i - NTC) * 128:(i - NTC + 1) * 128, :], []
        return xs_out[(l - 1) % 2][i * 128:(i + 1) * 128, :], [R_xs_out[(l - 1) % 2]]

    def layer_norm_tile(st, eng2, y, Ry, n, gbc, Rg, bbc, Rb, outt, Rout, rings):
        stt, Rst = rings["st"].get()
        mv, Rmv = rings["mv"].get()
        nch = (n + 511) // 512
        for c in range(nch):
            a, b_ = c * 512, min(n, (c + 1) * 512)
            op("dve", lambda e: e.bn_stats(out=stt[:, c * 6:(c + 1) * 6], in_=y[:, a:b_]), [Ry], [Rst])
        op("dve", lambda e: e.bn_aggr(out=mv[:, 0:2], in_=stt[:, 0:nch * 6]), [Rst], [Rmv])
        op("act", lambda e: e.activation(out=mv[:, 2:3], in_=mv[:, 1:2], func=AF.Ln, bias=eps_t[:, 0:1], scale=1.0), [Rmv], [Rmv])
        op("act", lambda e: e.activation(out=mv[:, 2:3], in_=mv[:, 2:3], func=AF.Exp, scale=-0.5), [Rmv], [Rmv])
        op("dve", lambda e: e.scalar_tensor_tensor(out=mv[:, 3:4], in0=mv[:, 0:1], scalar=-1.0, in1=mv[:, 2:3],
                                                   op0=ALU.mult, op1=ALU.mult), [Rmv], [Rmv])
        op("act", lambda e: e.activation(out=y[:, 0:n], in_=y[:, 0:n], func=AF.Identity, bias=mv[:, 3:4], scale=mv[:, 2:3]),
           [Ry, Rmv], [Ry])
        op(eng2, lambda e: e.tensor_tensor(out=y[:, 0:n], in0=y[:, 0:n], in1=gbc[:, 0:n], op=ALU.mult), [Ry, Rg], [Ry])
        op("dve", lambda e: e.tensor_tensor(out=outt[:, 0:n], in0=y[:, 0:n], in1=bbc[:, 0:n], op=ALU.add), [Ry, Rb], [Rout])

    for l in range(L):
        last = (l == L - 1)
        S.barrier()

        with ExitStack() as stAC:
            def aAC(name, shape, dt=F32):
                return stAC.enter_context(nc.sbuf_tensor("%s_L%d" % (name, l), shape, dt))
            cpT_l, R_cpl = aAC("cpT_l", [128, 4, T + 32]), Res("cpT_l")
            cpT_c, R_cpc = aAC("cpT_c", [128, 4, C + 32]), Res("cpT_c")
            for (t_, R_, n_) in ((cpT_l, R_cpl, T), (cpT_c, R_cpc, C)):
                op("pool", lambda e: e.memset(t_[:, :, 0:16], 0.0), [], [R_])
                op("pool", lambda e: e.memset(t_[:, :, 16 + n_:32 + n_], 0.0), [R_], [R_])

            with ExitStack() as stA:
                mk_stA = S.mark()
                def aA(name, shape, dt=F32):
                    return stA.enter_context(nc.sbuf_tensor("%s_L%d" % (name, l), shape, dt))
                gq_bc, R_gq = aA("gq_bc", [128, DQ]), Res("gq_bc")
                gkv_bc, R_gkv = aA("gkv_bc", [128, DKV]), Res("gkv_bc")
                load_bc(gq_bc, R_gq, g_q[l], DQ)
                load_bc(gkv_bc, R_gkv, g_kv[l], DKV)
                sc1, sh1, R_sc1, R_sh1 = [], [], [], []
                for r in range(2):
                    t = aA("sc1_%d" % r, [128, D])
                    Rr = Res("sc1_%d" % r)
                    load_bc(t, Rr, ada_vec(l, r, 1), D, [R_ada])
                    sc1.append(t)
                    R_sc1.append(Rr)
                    t = aA("sh1_%d" % r, [128, D])
                    Rr = Res("sh1_%d" % r)
                    load_bc(t, Rr, ada_vec(l, r, 0), D, [R_ada])
                    sh1.append(t)
                    R_sh1.append(Rr)
                rope_sb, R_rope = aA("rope_sb", [128, NTT, 2, DR]), Res("rope_sb")
                dma("sp", rope_sb[:], rope_d.rearrange("(i p) a d -> p i a d", p=128), [], [R_rope])
                nq_all, R_nq = aA("nq_all", [128, NTT, H]), Res("nq_all")
                kmax2, R_kmax2 = aA("kmax2", [128, H]), Res("kmax2")
                op("dve", lambda e: e.memset(kmax2[:], 0.0), [], [R_kmax2])
                op("dve", lambda e: e.memset(nq_all[:], 0.0), [], [R_nq])

                xt_r = Ring(aA, "xt", [128, D], F32, 2)
                tmp_r = Ring(aA, "tmp32", [128, D], F32, 2)
                hb_r = Ring(aA, "hb", [128, D], BF16, 2)
                hT_r = Ring(aA, "hT", [128, 8, 128], BF16, 2)
                junk_r = Ring(aA, "junk", [128, 512], F32, 2)
                stat_r = Ring(aA, "stat", [128, 8], F32, 4)
                qn_r = Ring(aA, "qn", [128, DQ], BF16, 2)
                qnT_r = Ring(aA, "qnT", [128, 3, 128], BF16, 2)
                ckvn_r = Ring(aA, "ckvn", [128, DKV], BF16, 2)
                ckvT_r = Ring(aA, "ckvT", [128, 2, 128], BF16, 2)
                qaug_r = Ring(aA, "qaug", [128, H, 96], BF16, 2)
                kaug_r = Ring(aA, "kaug", [128, H, 112], BF16, 2)
                vaug_r = Ring(aA, "vaug", [128, H, 80], BF16, 2)
                for (t_, R_) in kaug_r.bufs:
                    op("dve", lambda e: e.memset(t_[:, :, 96:112], 1.0), [], [R_])
                for (t_, R_) in vaug_r.bufs:
                    op("dve", lambda e: e.memset(t_[:, :, 64:80], 1.0), [], [R_])
                kTst_r = Ring(aA, "kTst", [97, H, 128], BF16, 2)
                qTst_r = Ring(aA, "qTst", [96, H, 128], BF16, 2)
                cptok_r = Ring(aA, "cptok", [128, 512], F32, 2)
                sig_r = Ring(aA, "sig", [128, 256], F32, 2)
                rt_r = Ring(aA, "rt", [128, H, 2, DR], F32, 2)
                krr_r = Ring(aA, "krr", [128, 2, DR], F32, 2)
                nk_r = Ring(aA, "nk", [128, 16], F32, 2)

                def rope_apply(i, src_view, Rsrc, nh, t1, t2, Rt):
                    cosb = rope_sb[:, i, 0:1, :].to_broadcast([128, nh, DR])
                    op("dve", lambda e: e.tensor_tensor(out=t1[:, 0:nh, :], in0=src_view, in1=cosb, op=ALU.mult),
                       [Rsrc, R_rope], [Rt])
                    sv = src_view.rearrange("p h (a b c) -> p h a b c", a=2, b=2)
                    t2v = t2[:, 0:nh, :].rearrange("p h (a b c) -> p h a b c", a=2, b=2)
                    sn = rope_sb[:, i, 1, :].rearrange("p (a b c) -> p a b c", a=2, b=2)
                    for b_ in range(2):
                        snb = sn[:, :, b_, :].unsqueeze(1).to_broadcast([128, nh, 2, 8])
                        op("dve", lambda e: e.tensor_tensor(out=t2v[:, :, :, b_, :], in0=sv[:, :, :, 1 - b_, :], in1=snb, op=ALU.mult),
                           [Rsrc, R_rope], [Rt])
                    op("dve", lambda e: e.tensor_tensor(out=t1[:, 0:nh, :], in0=t1[:, 0:nh, :], in1=t2[:, 0:nh, :], op=ALU.add),
                       [Rt], [Rt])

                if stop_after == "Apre":
                    return finish(nc, S, [R_win, R_wuq, R_wukv, R_gq, R_gkv, R_rope] + R_sc1 + R_sh1)
                COLS = [(0, 384), (384, 672), (672, 1184), (1184, 1440)]
                def tileA(i):
                    isctx = i < NTC
                    typ = 1 if isctx else 0
                    full = not (last and isctx)
                    g0 = i * 128
                    src, Rsrc = x_src(l, i)
                    xt, Rxt = xt_r.get()
                    dma("sp", xt[:], src, Rsrc, [Rxt])
                    tmp, Rtmp = tmp_r.get()
                    hb, Rhb = hb_r.get()
                    op("pool", lambda e: e.tensor_tensor(out=tmp[:], in0=xt[:], in1=sc1[typ][:], op=ALU.mult), [Rxt, R_sc1[typ]], [Rtmp])
                    op("dve", lambda e: e.tensor_tensor(out=hb[:], in0=tmp[:], in1=sh1[typ][:], op=ALU.add), [Rtmp, R_sh1[typ]], [Rhb])
                    yield
                    pb, Rpb = PBK[0]
                    pbv = bfv(pb)
                    for k in range(8):
                        op("pe", lambda e: e.transpose(out=pbv[:, k * 128:(k + 1) * 128], in_=hb[:, k * 128:(k + 1) * 128], identity=ident_b[:]),
                           [Rhb, R_idb], [Rpb])
                    hT, RhT = hT_r.get()
                    op("act", lambda e: e.copy(out=hT[:].rearrange("p k t -> p (k t)"), in_=pbv[:, 0:1024]), [Rpb], [RhT])
                    cut("A1")
                    G = [PBK[2], PBK[3], PBK[4], PBK[5]]
                    for gi, (c0, c1) in enumerate(COLS):
                        if not full and gi != 1:
                            continue
                        gt, Rg = G[gi]
                        for k in range(8):
                            op("pe", lambda e: e.matmul(gt[:, 0:c1 - c0], lhsT=hT[:, k, :], rhs=w_in_sb[:, k, c0:c1], start=(k == 0), stop=(k == 7)),
                               [RhT, R_win], [Rg])
                    g1t, Rg1 = G[0]
                    g2t, Rg2 = G[1]
                    g3t, Rg3 = G[2]
                    g4t, Rg4 = G[3]
                    stt, Rstt = stat_r.get()
                    junk, Rjunk = junk_r.get()
                    cut("A2")
                    op("act", lambda e: e.activation(out=junk[:, 0:DKV], in_=g2t[:, 0:DKV], func=AF.Square, accum_out=stt[:, 0:1]),
                       [Rg2], [Rjunk, Rstt])
                    op("act", lambda e: e.activation(out=stt[:, 1:2], in_=stt[:, 0:1], func=AF.Ln, bias=eps_t[:, 1:2], scale=1.0 / DKV),
                       [Rstt], [Rstt])
                    op("act", lambda e: e.activation(out=stt[:, 1:2], in_=stt[:, 1:2], func=AF.Exp, scale=-0.5), [Rstt], [Rstt])
                    ckvn, Rckvn = ckvn_r.get()
                    op("dve", lambda e: e.scalar_tensor_tensor(out=ckvn[:], in0=g2t[:, 0:DKV], scalar=stt[:, 1:2], in1=gkv_bc[:],
                                                               op0=ALU.mult, op1=ALU.mult), [Rg2, Rstt, R_gkv], [Rckvn])
                    cut("A3")
                    krr, Rkrr = krr_r.get()
                    rt, Rrt = rt_r.get()
                    rope_apply(i, g2t[:, DKV:DKV + DR].unsqueeze(1), Rg2, 1, krr[:, 0:1, :], krr[:, 1:2, :], Rkrr)
                    cut("A4")
                    if full:
                        op("act", lambda e: e.activation(out=junk[:, 0:DQ], in_=g1t[:, 0:DQ], func=AF.Square, accum_out=stt[:, 2:3]),
                           [Rg1], [Rjunk, Rstt])
                        op("act", lambda e: e.activation(out=stt[:, 3:4], in_=stt[:, 2:3], func=AF.Ln, bias=eps_t[:, 1:2], scale=1.0 / DQ),
                           [Rstt], [Rstt])
                        op("act", lambda e: e.activation(out=stt[:, 3:4], in_=stt[:, 3:4], func=AF.Exp, scale=-0.5), [Rstt], [Rstt])
                        qn, Rqn = qn_r.get()
                        op("dve", lambda e: e.scalar_tensor_tensor(out=qn[:], in0=g1t[:, 0:DQ], scalar=stt[:, 3:4], in1=gq_bc[:],
                                                                   op0=ALU.mult, op1=ALU.mult), [Rg1, Rstt, R_gq], [Rqn])
                        sig, Rsig = sig_r.get()
                        cptok, Rcptok = cptok_r.get()
                        op("act", lambda e: e.activation(out=sig[:], in_=g3t[:, 256:512], func=AF.Exp, scale=-1.0), [Rg3], [Rsig])
                        op("act", lambda e: e.activation(out=sig[:], in_=sig[:], func=AF.Ln, bias=1.0, scale=1.0), [Rsig], [Rsig])
                        op("act", lambda e: e.activation(out=sig[:], in_=sig[:], func=AF.Exp, scale=-1.0), [Rsig], [Rsig])
                        op("dve", lambda e: e.tensor_tensor(out=cptok[:, 0:256], in0=g3t[:, 0:256], in1=sig[:], op=ALU.mult),
                           [Rg3, Rsig], [Rcptok])
                        op("act", lambda e: e.copy(out=cptok[:, 256:512], in_=g4t[:, 0:256]), [Rg4, Rcptok], [Rcptok])
                        pb1, Rpb1 = PBK[1]
                        pb1v = bfv(pb1)
                        for k in range(3):
                            op("pe", lambda e: e.transpose(out=pb1v[:, k * 128:(k + 1) * 128], in_=qn[:, k * 128:(k + 1) * 128], identity=ident_b[:]),
                               [Rqn, R_idb], [Rpb1])
                        qnT, RqnT = qnT_r.get()
                        op("act", lambda e: e.copy(out=qnT[:].rearrange("p k t -> p (k t)"), in_=pb1v[:, 0:384]), [Rpb1], [RqnT])
                    cut("A5")
                    pb0, Rpb0 = PBK[0]
                    pb0v = bfv(pb0)
                    for k in range(2):
                        op("pe", lambda e: e.transpose(out=pb0v[:, k * 128:(k + 1) * 128], in_=ckvn[:, k * 128:(k + 1) * 128], identity=ident_b[:]),
                           [Rckvn, R_idb], [Rpb0])
                    ckvT, RckvT = ckvT_r.get()
                    op("dve", lambda e: e.tensor_copy(out=ckvT[:].rearrange("p k t -> p (k t)"), in_=pb0v[:, 0:256]), [Rpb0], [RckvT])
                    yield
                    if full:
                        Q = [(PBK[6], 0, 5), (PBK[7], 5, 3)]
                        for ((qt, Rq), h0, nh) in Q:
                            for k in range(3):
                                op("pe", lambda e: e.matmul(qt[:, 0:nh * 96], lhsT=qnT[:, k, :], rhs=w_uq_sb[:, k, h0 * 96:(h0 + nh) * 96],
                                                            start=(k == 0), stop=(k == 2)), [RqnT, R_wuq], [Rq])
                    cut("A6")
                    KV = [(PBK[2], 0), (PBK[4], 4)]
                    for ((kt, Rk), h0) in KV:
                        for k in range(2):
                            op("pe", lambda e: e.matmul(kt[:, 0:512], lhsT=ckvT[:, k, :], rhs=w_ukv_sb[:, k, h0 * 128:(h0 + 4) * 128],
                                                        start=(k == 0), stop=(k == 1)), [RckvT, R_wukv], [Rk])
                    if full:
                        pb5, Rpb5 = PBK[5]
                        for k in range(4):
                            op("pe", lambda e: e.transpose(out=pb5[:, k * 128:(k + 1) * 128], in_=cptok[:, k * 128:(k + 1) * 128], identity=ident_f[:]),
                               [Rcptok, R_idf], [Rpb5])
                        cpd, Rcpd, off = (cpT_c, R_cpc, g0) if isctx else (cpT_l, R_cpl, g0 - C)
                        op("act", lambda e: e.copy(out=cpd[:, :, 16 + off:16 + off + 128], in_=pb5[:].rearrange("p (k t) -> p k t", k=4)),
                           [Rpb5], [Rcpd])
                        qaug, Rqaug = qaug_r.get()
                        nk, Rnk = nk_r.get()
                        for ((qt, Rq), h0, nh) in Q:
                            qv = qt[:, 0:nh * 96].rearrange("p (h d) -> p h d", d=96)
                            op("act", lambda e: e.copy(out=qaug[:, h0:h0 + nh, 0:64], in_=qv[:, :, 0:64]), [Rq], [Rqaug])
                            rope_apply(i, qv[:, :, 64:96], Rq, nh, rt[:, h0:h0 + nh, 0, :], rt[:, h0:h0 + nh, 1, :], Rrt)
                            op("dve", lambda e: e.tensor_copy(out=qaug[:, h0:h0 + nh, 64:96], in_=rt[:, h0:h0 + nh, 0, :]), [Rrt], [Rqaug])
                            jv = junk[:, 0:nh * 96].rearrange("p (h d) -> p h d", d=96)
                            op("act", lambda e: e.activation(out=jv, in_=qv, func=AF.Square), [Rq], [Rjunk])
                            op("dve", lambda e: e.tensor_reduce(out=nq_all[:, i, h0:h0 + nh], in_=jv, axis=AX.X, op=ALU.add), [Rjunk], [R_nq])
                    else:
                        nk, Rnk = nk_r.get()
                    cut("A7")
                    kaug, Rkaug = kaug_r.get()
                    vaug, Rvaug = vaug_r.get()
                    for ((kt, Rk), h0) in KV:
                        kv = kt[:, 0:512].rearrange("p (h d) -> p h d", d=128)
                        cut("KV0")
                        op("act", lambda e: e.copy(out=kaug[:, h0:h0 + 4, 0:64], in_=kv[:, :, 0:64]), [Rk], [Rkaug])
                        cut("KV1")
                        op("dve", lambda e: e.tensor_copy(out=vaug[:, h0:h0 + 4, 0:64], in_=kv[:, :, 64:128]), [Rk], [Rvaug])
                        cut("KV2")
                        jv = junk[:, 0:256].rearrange("p (h d) -> p h d", d=64)
                        op("act", lambda e: e.activation(out=jv, in_=kv[:, :, 0:64], func=AF.Square), [Rk], [Rjunk])
                        cut("KV3")
                        op("dve", lambda e: e.tensor_reduce(out=nk[:, h0:h0 + 4], in_=jv, axis=AX.X, op=ALU.add), [Rjunk], [Rnk])
                        cut("KV4")
                    cut("K1")
                    op("dve", lambda e: e.tensor_copy(out=kaug[:, :, 64:96], in_=krr[:, 0:1, :].to_broadcast([128, H, DR])), [Rkrr], [Rkaug])
                    cut("K2")
                    op("dve", lambda e: e.tensor_tensor(out=krr[:, 1, :], in0=krr[:, 0, :], in1=krr[:, 0, :], op=ALU.mult), [Rkrr], [Rkrr])
                    op("dve", lambda e: e.tensor_reduce(out=nk[:, 8:9], in_=krr[:, 1, :], axis=AX.X, op=ALU.add), [Rkrr], [Rnk])
                    cut("K3")
                    op("dve", lambda e: e.tensor_scalar(out=nk[:, 0:8], in0=nk[:, 0:8], scalar1=nk[:, 8:9], scalar2=None, op0=ALU.add), [Rnk], [Rnk])
                    cut("K4")
                    op("dve", lambda e: e.tensor_tensor(out=kmax2[:], in0=kmax2[:], in1=nk[:, 0:8], op=ALU.max), [Rnk, R_kmax2], [R_kmax2])
                    cut("A7b")
                    yield
                    if full:
                        pb0, Rpb0 = PBK[0]
                        pb0v = bfv(pb0)
                        for h in range(H):
                            op("pe", lambda e: e.transpose(out=pb0v[0:96, h * 128:(h + 1) * 128], in_=qaug[:, h, :], identity=ident_b[:]),
                               [Rqaug, R_idb], [Rpb0])
                        qTst, RqTst = qTst_r.get()
                        op("act", lambda e: e.copy(out=qTst[:].rearrange("p h t -> p (h t)"), in_=pb0v[0:96, 0:1024]), [Rpb0], [RqTst])
                        dma("sp", qT_scr[:, :, g0:g0 + 128].rearrange("h d t -> d h t"), qTst[:], [RqTst], [R_qT])
                    pb1, Rpb1 = PBK[1]
                    pb1v = bfv(pb1)
                    for h in range(H):
                        op("pe", lambda e: e.transpose(out=pb1v[0:97, h * 128:(h + 1) * 128], in_=kaug[:, h, 0:97], identity=ident_b[:]),
                           [Rkaug, R_idb], [Rpb1])
                    kTst, RkTst = kTst_r.get()
                    op("dve", lambda e: e.tensor_copy(out=kTst[:].rearrange("p h t -> p (h t)"), in_=pb1v[0:97, 0:1024]), [Rpb1], [RkTst])
                    dma("sp", kT_scr[:, :, g0:g0 + 128].rearrange("h d t -> d h t"), kTst[:], [RkTst], [R_kT])
                    dma("sp", v_scr[:, :, i, :].rearrange("h p d -> p h d"), vaug[:], [Rvaug], [R_v])
                    cut("AT%d" % i)

                run_skewed([tileA(i) for i in range(NTT)])
                cut("A8")
                pb, Rpb = PBK[0]
                op("pe", lambda e: e.transpose(out=pb[0:8, 0:128], in_=kmax2[:, 0:8], identity=ident_f[:]), [R_kmax2, R_idf], [Rpb])
                km, Rkm = aA("km", [8, 16]), Res("km")
                op("dve", lambda e: e.tensor_reduce(out=km[:, 0:1], in_=pb[0:8, 0:128], axis=AX.X, op=ALU.max), [Rpb], [Rkm])
                op("act", lambda e: e.activation(out=km[:, 1:2], in_=km[:, 0:1], func=AF.Sqrt, scale=1.0404), [Rkm], [Rkm])
                op("dve", lambda e: e.tensor_scalar(out=km[:, 8:16], in0=ident_f[0:8, 0:8], scalar1=km[:, 1:2], scalar2=-1.0,
                                                    op0=ALU.mult, op1=ALU.mult), [Rkm, R_idf], [Rkm])
                ones8, Rones8 = aA("ones8", [8, 128]), Res("ones8")
                op("dve", lambda e: e.memset(ones8[:], 1.0), [], [Rones8])
                pb, Rpb = PBK[1]
                op("pe", lambda e: e.matmul(pb[:, 0:8], lhsT=ones8[:], rhs=km[:, 8:16], start=True, stop=True), [Rones8, Rkm], [Rpb])
                kmbc, Rkmbc = aA("kmbc", [128, 8]), Res("kmbc")
                op("dve", lambda e: e.tensor_copy(out=kmbc[:], in_=pb[:, 0:8]), [Rpb], [Rkmbc])
                op("act", lambda e: e.activation(out=nq_all[:], in_=nq_all[:], func=AF.Sqrt), [R_nq], [R_nq])
                op("dve", lambda e: e.tensor_tensor(out=nq_all[:], in0=nq_all[:], in1=kmbc[:].unsqueeze(1).to_broadcast([128, NTT, H]), op=ALU.mult),
                   [R_nq, Rkmbc], [R_nq])
                mT_sb, RmT = aA("mT_sb", [8, TT], BF16), Res("mT_sb")
                for i0 in range(0, NTT, 4):
                    pb, Rpb = PBK[(i0 // 4) % 2]
                    ni = min(4, NTT - i0)
                    for j in range(ni):
                        op("pe", lambda e: e.transpose(out=pb[0:8, j * 128:(j + 1) * 128], in_=nq_all[:, i0 + j, :], identity=ident_f[:]),
                           [R_nq, R_idf], [Rpb])
                    op("dve", lambda e: e.tensor_copy(out=mT_sb[:, i0 * 128:(i0 + ni) * 128], in_=pb[0:8, 0:ni * 128]), [Rpb], [RmT])
                dma("sp", mT_scr, mT_sb[:], [RmT], [R_mT])
                S.barrier()
                S.release(mk_stA)
            if stop_after == "A":
                return finish(nc, S, [R_kT, R_qT, R_mT, R_v])

            with ExitStack() as stC:
                mk_stC = S.mark()
                def aC(name, shape, dt=F32):
                    return stC.enter_context(nc.sbuf_tensor("%s_L%d" % (name, l), shape, dt))
                cwr, Rcwr = aC("cwr", [CONVW, 256]), Res("cwr")
                cw_sb, Rcw = aC("cw_sb", [128, 2, CONVW]), Res("cw_sb")
                dma("sp", cwr[:], conv_w[l], [], [Rcwr])
                pb, Rpb = PBK[0]
                for k in range(2):
                    op("pe", lambda e: e.transpose(out=pb[:, k * 32:k * 32 + CONVW], in_=cwr[0:CONVW, k * 128:(k + 1) * 128],
                                                   identity=ident_f[0:CONVW, 0:CONVW]), [Rcwr, R_idf], [Rpb])
                op("dve", lambda e: e.tensor_copy(out=cw_sb[:], in_=pb[:, 0:64].rearrange("p (k j) -> p k j", k=2)[:, :, 0:CONVW]), [Rpb], [Rcw])
                convb_bc, Rcb = aC("convb_bc", [128, 256]), Res("convb_bc")
                clng_bc, Rclg = aC("clng_bc", [128, 256]), Res("clng_bc")
                clnb_bc, Rclb = aC("clnb_bc", [128, 256]), Res("clnb_bc")
                psc_bc, Rpsc = aC("psc_bc", [128, 256]), Res("psc_bc")
                load_bc(convb_bc, Rcb, conv_b[l], 256)
                load_bc(clng_bc, Rclg, conv_ln_g[l], 256)
                load_bc(clnb_bc, Rclb, conv_ln_b[l], 256)
                load_bc(psc_bc, Rpsc, pool_scale[l], 256)
                poolw_sb, Rpw = aC("poolw_sb", [128, 2, 128], BF16), Res("poolw_sb")
                op("dve", lambda e: e.memset(poolw_sb[:], 0.0), [], [Rpw])
                for ph in range(2):
                    dma("pool", poolw_sb[ph * 64:(ph + 1) * 64, :, ph * 64:(ph + 1) * 64],
                        pool_w[l].rearrange("(k two) i o -> two i k o", two=2)[ph], [], [Rpw])
                acc_t = aC("acc", [128, 2, SEG])
                R_acc = [Res("acc0"), Res("acc1")]
                P2, RP2 = aC("P2", [128, 2, SEG + 16]), Res("P2")
                P4, RP4 = aC("P4", [128, 2, SEG + 16]), Res("P4")
                P8, RP8 = aC("P8", [128, SEG + 16]), Res("P8")
                P16, RP16 = aC("P16", [128, SEG + 16]), Res("P16")
                mixed, Rmixed = aC("mixed", [128, 2, SEG], BF16), Res("mixed")
                etmp, Retmp = aC("etmp", [128, 2, 8]), Res("etmp")
                ctmp_r = Ring(aC, "ctmp", [128, SEG], F32, 3)
                cv_r = Ring(aC, "cv", [128, 256], F32, 2)
                sgc_r = Ring(aC, "sgc", [128, 256], F32, 2)
                catcp_r = Ring(aC, "catcp", [128, 512], BF16, 2)
                lnr = {"st": Ring(aC, "cst", [128, 12], F32, 2), "mv": Ring(aC, "cmv", [128, 4], F32, 2)}
                cut("C1")
                seqs = [(cpT_l, R_cpl, T, C)]
                if not last:
                    seqs.append((cpT_c, R_cpc, C, 0))
                tile_ctr = 0
                for (buf, Rbuf, n, goff) in seqs:
                    for s0 in range(0, n, SEG):
                        seg = min(SEG, n - s0)
                        b0 = 16 + s0
                        for j in range(CONVW):
                            if j == 0:
                                op("dve", lambda e: e.tensor_scalar(out=acc_t[:, 0, 0:seg], in0=buf[:, 0, b0 - 15:b0 - 15 + seg], scalar1=cw_sb[:, 0, 0:1],
                                                                    scalar2=None, op0=ALU.mult), [Rbuf, Rcw], [R_acc[0]])
                                op("act", lambda e: e.activation(out=acc_t[:, 1, 0:seg], in_=buf[:, 1, b0 - 15:b0 - 15 + seg], func=AF.Copy,
                                                                 scale=cw_sb[:, 1, 0:1]), [Rbuf, Rcw], [R_acc[1]])
                                continue
                            op("dve", lambda e: e.scalar_tensor_tensor(out=acc_t[:, 0, 0:seg], in0=buf[:, 0, b0 - 15 + j:b0 - 15 + j + seg],
                                                                       scalar=cw_sb[:, 0, j:j + 1], in1=acc_t[:, 0, 0:seg],
                                                                       op0=ALU.mult, op1=ALU.add), [Rbuf, Rcw, R_acc[0]], [R_acc[0]])
                            ct, Rct = ctmp_r.get()
                            op("act", lambda e: e.activation(out=ct[:, 0:seg], in_=buf[:, 1, b0 - 15 + j:b0 - 15 + j + seg], func=AF.Copy,
                                                             scale=cw_sb[:, 1, j:j + 1]), [Rbuf, Rcw], [Rct])
                            op("pool", lambda e: e.tensor_tensor(out=acc_t[:, 1, 0:seg], in0=acc_t[:, 1, 0:seg], in1=ct[:, 0:seg], op=ALU.add),
                               [Rct, R_acc[1]], [R_acc[1]])
                        cut("C2")
                        n2 = seg + 16
                        op("dve", lambda e: e.tensor_tensor(out=P2[:, :, 0:n2], in0=buf[:, 2:4, b0 - 9:b0 - 9 + n2], in1=buf[:, 2:4, b0 - 8:b0 - 8 + n2],
                                                            op=ALU.add), [Rbuf], [RP2])
                        op("dve", lambda e: e.tensor_tensor(out=P4[:, :, 2:n2 - 2], in0=P2[:, :, 1:n2 - 3], in1=P2[:, :, 3:n2 - 1], op=ALU.add),
                           [RP2], [RP4])
                        op("dve", lambda e: e.tensor_tensor(out=P8[:, 4:n2 - 4], in0=P4[:, 1, 2:n2 - 6], in1=P4[:, 1, 6:n2 - 2], op=ALU.add),
                           [RP4], [RP8])
                        op("dve", lambda e: e.tensor_tensor(out=P16[:, 8:n2 - 8], in0=P8[:, 4:n2 - 12], in1=P8[:, 12:n2 - 4], op=ALU.add),
                           [RP8], [RP16])
                        srcs = {(0, 0): (P2[0:64, 0, 8:8 + seg], RP2), (1, 0): (P4[64:128, 0, 8:8 + seg], RP4),
                                (0, 1): (P8[0:64, 8:8 + seg], RP8), (1, 1): (P16[64:128, 8:8 + seg], RP16)}
                        for (ph, k), (sap, Rs) in srcs.items():
                            ps = slice(ph * 64, ph * 64 + 64)
                            op("dve", lambda e: e.scalar_tensor_tensor(out=mixed[ps, k, 0:seg], in0=sap, scalar=pinvw[ps, k:k + 1],
                                                                       in1=buf[ps, 2 + k, b0:b0 + seg], op0=ALU.mult, op1=ALU.subtract),
                               [Rs, R_pinvw, Rbuf], [Rmixed])
                            for (side, cond, c0) in ((0, s0 == 0, 0), (1, s0 + seg == n, seg - 8)):
                                if not cond:
                                    continue
                                sap8 = sap[:, c0:c0 + 8]
                                op("dve", lambda e: e.tensor_tensor(out=etmp[ps, k, :], in0=sap8, in1=pedge[ps, k, side, :], op=ALU.mult),
                                   [Rs, R_pedge], [Retmp])
                                op("dve", lambda e: e.tensor_tensor(out=mixed[ps, k, c0:c0 + 8], in0=etmp[ps, k, :], in1=buf[ps, 2 + k, b0 + c0:b0 + c0 + 8],
                                                                    op=ALU.subtract), [Retmp, Rbuf], [Rmixed])
                        cut("C3")
                        for j in range(seg // 128):
                            g0 = goff + s0 + j * 128
                            pb, Rpb = PBK[tile_ctr % 2]
                            pb2, Rpb2 = PBK[2 + tile_ctr % 2]
                            tile_ctr += 1
                            for k in range(2):
                                op("pe", lambda e: e.transpose(out=pb[:, k * 128:(k + 1) * 128], in_=acc_t[:, k, j * 128:(j + 1) * 128], identity=ident_f[:]),
                                   [R_acc[k], R_idf], [Rpb])
                            cv, Rcv = cv_r.get()
                            op("dve", lambda e: e.tensor_tensor(out=cv[:], in0=pb[:, 0:256], in1=convb_bc[:], op=ALU.add), [Rpb, Rcb], [Rcv])
                            layer_norm_tile(None, "pool", cv, Rcv, 256, clng_bc, Rclg, clnb_bc, Rclb, cv, Rcv, lnr)
                            catcp, Rcatcp = catcp_r.get()
                            sgc, Rsgc = sgc_r.get()
                            op("act", lambda e: e.activation(out=sgc[:], in_=cv[:], func=AF.Exp, scale=-1.0), [Rcv], [Rsgc])
                            op("act", lambda e: e.activation(out=sgc[:], in_=sgc[:], func=AF.Ln, bias=1.0, scale=1.0), [Rsgc], [Rsgc])
                            op("act", lambda e: e.activation(out=sgc[:], in_=sgc[:], func=AF.Exp, scale=-1.0), [Rsgc], [Rsgc])
                            op("dve", lambda e: e.tensor_tensor(out=catcp[:, 0:256], in0=cv[:], in1=sgc[:], op=ALU.mult), [Rcv, Rsgc], [Rcatcp])
                            cut("C3b")
                            for k in range(2):
                                op("pe", lambda e: e.matmul(pb2[:, k * 128:(k + 1) * 128], lhsT=mixed[:, k, j * 128:(j + 1) * 128],
                                                            rhs=poolw_sb[:, k, :], start=True, stop=True), [Rmixed, Rpw], [Rpb2])
                            cut("C3c")
                            op("dve", lambda e: e.tensor_tensor(out=catcp[:, 256:512], in0=pb2[:, 0:256], in1=psc_bc[:], op=ALU.mult),
                               [Rpb2, Rpsc, Rcatcp], [Rcatcp])
                            cut("C3d")
                            dma("sp", catcp_scr[g0:g0 + 128, :], catcp[:], [Rcatcp], [R_catcp])
                            cut("C4")
                S.barrier()
                S.release(mk_stC)
        if stop_after == "C":
            return finish(nc, S, [R_catcp])

        with ExitStack() as stBD:
            def aBD(name, shape, dt=F32):
                return stBD.enter_context(nc.sbuf_tensor("%s_L%d" % (name, l), shape, dt))
            attn_sb = aBD("attn_sb", [128, NTT, 512], BF16)
            R_attn = [Res("attn%d" % i) for i in range(NTT)]
            with ExitStack() as stB:
                mk_stB = S.mark()
                def aB(name, shape, dt=F32):
                    return stB.enter_context(nc.sbuf_tensor("%s_L%d" % (name, l), shape, dt))
                NJ = NP // 128
                dma("act", h2perm_scr.rearrange("(j p) d -> p j d", p=128), zer_b[:].unsqueeze(1).to_broadcast([128, NJ, D]), [R_zerb], [R_h2pz])
                dma("act", c8perm_scr.rearrange("(j p) e -> p j e", p=128), zer_f[:].unsqueeze(1).to_broadcast([128, NJ, 8]), [R_zerf], [R_c8pz])
                KT_r = Ring(aB, "KT", [97, TT], BF16, 2)
                V_r = Ring(aB, "V", [128, NTT, 80], BF16, 2)
                qT_r = Ring(aB, "qT", [97, 512], BF16, 3)
                PT_r = Ring(aB, "PT", [128, 512], BF16, 4)
                oT_r = Ring(aB, "oT", [65, 512], F32, 2)
                rec_r = Ring(aB, "rec", [128, 4], F32, 2)
                blocks = [(C + b * 512, 512, list(range(NTT))) for b in range(T // 512)]
                if not last:
                    blocks.append((0, C, list(range(NTC))))
                bi = 0
                si = 0
                for h in range(H):
                    KT, RKT = KT_r.get()
                    V, RV = V_r.get()
                    dma("sp", KT[:], kT_scr[h], [R_kT], [RKT])
                    dma("sp", V[:], v_scr[h], [R_v], [RV])
                    for (g0, n, chunks) in blocks:
                        qT, RqT = qT_r.get()
                        dma("sp", qT[0:96, 0:n], qT_scr[h, :, g0:g0 + n], [R_qT], [RqT])
                        dma("sp", qT[96:97, 0:n], mT_scr[h:h + 1, g0:g0 + n], [R_mT], [RqT])
                        pO, RpO = PBK[bi % 2]
                        LOOK = 3
                        pend = []

                        def issue_s(c):
                            nonlocal si
                            pS_, RpS_ = PBK[2 + si % 4]
                            si += 1
                            op("pe", lambda e: e.matmul(pS_[:, 0:n], lhsT=KT[:, c * 128:(c + 1) * 128], rhs=qT[:, 0:n], start=True, stop=True),
                               [RKT, RqT], [RpS_])
                            pend.append((pS_, RpS_))
                        for c in chunks[:LOOK]:
                            issue_s(c)
                        for ci, c in enumerate(chunks):
                            if ci + LOOK < len(chunks):
                                issue_s(chunks[ci + LOOK])
                            pS, RpS = pend.pop(0)
                            PT, RPT = PT_r.get()
                            op("act", lambda e: e.activation(out=PT[:, 0:n], in_=pS[:, 0:n], func=AF.Exp, scale=QS), [RpS], [RPT])
                            op("pe", lambda e: e.matmul(pO[0:65, 0:n], lhsT=V[:, c, 0:65], rhs=PT[:, 0:n], start=(ci == 0), stop=(ci == len(chunks) - 1)),
                               [RV, RPT], [RpO])
                        oT, RoT = oT_r.get()
                        op("dve", lambda e: e.tensor_copy(out=oT[:, 0:n], in_=pO[0:65, 0:n]), [RpO], [RoT])
                        pb, Rpb = PBK[6 + bi % 2]
                        bi += 1
                        nj = n // 128
                        for j in range(nj):
                            op("pe", lambda e: e.transpose(out=pb[:, j * 65:(j + 1) * 65], in_=oT[0:65, j * 128:(j + 1) * 128], identity=ident_f[0:65, 0:65]),
                               [RoT, R_idf], [Rpb])
                        pv = pb[:, 0:nj * 65].rearrange("p (j d) -> p j d", d=65)
                        rec, Rrec = rec_r.get()
                        op("dve", lambda e: e.reciprocal(out=rec[:, 0:nj], in_=pv[:, :, 64]), [Rpb], [Rrec])
                        i0 = g0 // 128
                        Rs_ = R_attn[i0:i0 + nj]
                        op("dve", lambda e: e.tensor_tensor(out=attn_sb[:, i0:i0 + nj, h * 64:(h + 1) * 64], in0=pv[:, :, 0:64],
                                                            in1=rec[:, 0:nj].unsqueeze(2).to_broadcast([128, nj, 64]), op=ALU.mult),
                           [Rpb, Rrec] + Rs_, Rs_)
                S.barrier()
                S.release(mk_stB)
            if stop_after == "B":
                dbg = nc.dram_tensor("attn_dbg", [128, NTT, 512], BF16, kind="ExternalOutput").ap()
                Rd = Res("attn_dbg")
                dma("sp", dbg, attn_sb[:], R_attn, [Rd])
                return finish(nc, S, [Rd])

            with ExitStack() as stD:
                mk_stD = S.mark()
                def aD(name, shape, dt=F32):
                    return stD.enter_context(nc.sbuf_tensor("%s_L%d" % (name, l), shape, dt))
                w_out_sb, R_wout = aD("w_out_sb", [128, 8, D], BF16), Res("w_out_sb")
                dma("pool", w_out_sb[:], w_out[l].rearrange("(k p) n -> p k n", p=128), [], [R_wout])
                wr_sb, R_wr = aD("wr_sb", [128, 8, 36]), Res("wr_sb")
                dma("sp", wr_sb[:, :, 0:4], w_rg[l].rearrange("(k p) n -> p k n", p=128), [], [R_wr])
                dma("sp", wr_sb[:, :, 4:36], w_re[l].rearrange("(k p) n -> p k n", p=128), [], [R_wr])
                br_bc, R_br = aD("br_bc", [128, 36]), Res("br_bc")
                dma("sp", br_bc[:, 0:4], b_rg[l].partition_broadcast(128), [], [R_br])
                dma("sp", br_bc[:, 4:36], b_re[l].partition_broadcast(128), [], [R_br])
                bcs = {}
                for (nm, j) in (("g1", 2), ("sh2", 3), ("sc2", 4)):
                    for r in range(2):
                        if r == 1 and last:
                            continue
                        t = aD("%s_%d" % (nm, r), [128, D])
                        Rr = Res("%s_%d" % (nm, r))
                        load_bc(t, Rr, ada_vec(l, r, j), D, [R_ada])
                        bcs[(nm, r)] = (t, Rr)
                ln1g_bc, R_l1g = aD("ln1g_bc", [128, D]), Res("ln1g_bc")
                ln1b_bc, R_l1b = aD("ln1b_bc", [128, D]), Res("ln1b_bc")
                load_bc(ln1g_bc, R_l1g, ln1_g[l], D)
                load_bc(ln1b_bc, R_l1b, ln1_b[l], D)
                catcp_r = Ring(aD, "catcpD", [128, 512], BF16, 2)
                catT_r = Ring(aD, "catT", [128, 8, 128], BF16, 2)
                xt_r = Ring(aD, "xtD", [128, D], F32, 3)
                y_r = Ring(aD, "yD", [128, D], F32, 2)
                x1_r = Ring(aD, "x1D", [128, D], F32, 2)
                h2_r = Ring(aD, "h2D", [128, D], F32, 2)
                h2Tf_r = Ring(aD, "h2Tf", [128, 8, 128], F32, 2)
                h2Tb_r = Ring(aD, "h2b", [128, D], BF16, 2)
                lg_r = Ring(aD, "lg", [128, 36], F32, 2)
                rs_r = Ring(aD, "rs", [128, 16], F32, 2)
                oh_r = Ring(aD, "oh", [128, 3, 32], F32, 2)
                comb_r = Ring(aD, "comb", [128, 32], F32, 2)
                combT_r = Ring(aD, "combT", [32, 128], F32, 2)
                lnr = {"st": Ring(aD, "dst", [128, 12], F32, 2), "mv": Ring(aD, "dmv", [128, 4], F32, 2)}
                cut("D1")
                def tileD(i, tcnt):
                    isctx = i < NTC
                    typ = 1 if isctx else 0
                    g0 = i * 128
                    catcp, Rcatcp = catcp_r.get()
                    dma("sp", catcp[:], catcp_scr[g0:g0 + 128, :], [R_catcp], [Rcatcp])
                    src, Rsrc = x_src(l, i)
                    xt, Rxt = xt_r.get()
                    dma("sp", xt[:], src, Rsrc, [Rxt])
                    yield
                    pb, Rpb = PBK[tcnt % 2]
                    pbv = bfv(pb)
                    for k in range(4):
                        op("pe", lambda e: e.transpose(out=pbv[:, k * 128:(k + 1) * 128], in_=attn_sb[:, i, k * 128:(k + 1) * 128], identity=ident_b[:]),
                           [R_attn[i], R_idb], [Rpb])
                    for k in range(4):
                        op("pe", lambda e: e.transpose(out=pbv[:, (4 + k) * 128:(5 + k) * 128], in_=catcp[:, k * 128:(k + 1) * 128], identity=ident_b[:]),
                           [Rcatcp, R_idb], [Rpb])
                    catT, RcatT = catT_r.get()
                    op("act", lambda e: e.copy(out=catT[:].rearrange("p k t -> p (k t)"), in_=pbv[:, 0:1024]), [Rpb], [RcatT])
                    cut("D2")
                    M = [PBK[2 + 2 * (tcnt % 2)], PBK[3 + 2 * (tcnt % 2)]]
                    for hf in range(2):
                        mt, Rm = M[hf]
                        for k in range(8):
                            op("pe", lambda e: e.matmul(mt[:, :], lhsT=catT[:, k, :], rhs=w_out_sb[:, k, hf * 512:(hf + 1) * 512], start=(k == 0), stop=(k == 7)),
                               [RcatT, R_wout], [Rm])
                    cut("D3")
                    yield
                    y, Ry = y_r.get()
                    g1t, Rg1 = bcs[("g1", typ)]
                    for hf in range(2):
                        mt, Rm = M[hf]
                        op("dve", lambda e: e.tensor_tensor(out=y[:, hf * 512:(hf + 1) * 512], in0=mt[:, :], in1=g1t[:, hf * 512:(hf + 1) * 512], op=ALU.mult),
                           [Rm, Rg1], [Ry])
                    op("dve", lambda e: e.scalar_tensor_tensor(out=y[:], in0=xt[:], scalar=ALPHA, in1=y[:], op0=ALU.mult, op1=ALU.add),
                       [Rxt, Ry], [Ry])
                    x1, Rx1 = x1_r.get()
                    layer_norm_tile(None, "pool", y, Ry, D, ln1g_bc, R_l1g, ln1b_bc, R_l1b, x1, Rx1, lnr)
                    dma("sp", xs_mix[g0:g0 + 128, :], x1[:], [Rx1], [R_xs_mix])
                    cut("D4")
                    h2, Rh2 = h2_r.get()
                    sc2t, Rsc2 = bcs[("sc2", typ)]
                    sh2t, Rsh2 = bcs[("sh2", typ)]
                    op("pool", lambda e: e.tensor_tensor(out=h2[:], in0=x1[:], in1=sc2t[:], op=ALU.mult), [Rx1, Rsc2], [Rh2])
                    op("dve", lambda e: e.tensor_tensor(out=h2[:], in0=h2[:], in1=sh2t[:], op=ALU.add), [Rh2, Rsh2], [Rh2])
                    yield
                    T6, RT6 = PBK[6]
                    T7, RT7 = PBK[7]
                    for k in range(8):
                        tb, Rtb = (T6, RT6) if k < 4 else (T7, RT7)
                        op("pe", lambda e: e.transpose(out=tb[:, (k % 4) * 128:(k % 4 + 1) * 128], in_=h2[:, k * 128:(k + 1) * 128], identity=ident_f[:]),
                           [Rh2, R_idf], [Rtb])
                    h2Tf, Rh2Tf = h2Tf_r.get()
                    h2b, Rh2b = h2Tb_r.get()
                    op("pool", lambda e: e.tensor_copy(out=h2b[:], in_=h2[:]), [Rh2], [Rh2b])
                    dma("sp", h2tok_scr[g0:g0 + 128, :], h2b[:], [Rh2b], [R_h2tok])
                    for hf, (tb, Rtb) in enumerate(((T6, RT6), (T7, RT7))):
                        op("act", lambda e: e.copy(out=h2Tf[:, hf * 4:hf * 4 + 4, :].rearrange("p k t -> p (k t)"), in_=tb[:, :]), [Rtb], [Rh2Tf])
                    cut("D5")
                    pr, Rpr = PBK[tcnt % 2]
                    for k in range(8):
                        op("pe", lambda e: e.matmul(pr[:, 0:36], lhsT=h2Tf[:, k, :], rhs=wr_sb[:, k, :], start=(k == 0), stop=(k == 7)),
                           [Rh2Tf, R_wr], [Rpr])
                    lg, Rlg = lg_r.get()
                    rs, Rrs = rs_r.get()
                    oh, Roh = oh_r.get()
                    op("dve", lambda e: e.tensor_tensor(out=lg[:], in0=pr[:, 0:36], in1=br_bc[:], op=ALU.add), [Rpr, R_br], [Rlg])
                    cut("D6")
                    yield
                    op("dve", lambda e: e.tensor_reduce(out=rs[:, 0:1], in_=lg[:, 0:4], axis=AX.X, op=ALU.max), [Rlg], [Rrs])
                    op("dve", lambda e: e.tensor_scalar(out=rs[:, 8:12], in0=lg[:, 0:4], scalar1=rs[:, 0:1], scalar2=None, op0=ALU.is_equal), [Rlg, Rrs], [Rrs])
                    op("dve", lambda e: e.tensor_copy(out=goh_all[:, i, :], in_=rs[:, 8:12]), [Rrs], [R_goh])
                    op("dve", lambda e: e.tensor_scalar(out=rs[:, 1:2], in0=rs[:, 0:1], scalar1=-1.0, scalar2=None, op0=ALU.mult), [Rrs], [Rrs])
                    op("act", lambda e: e.activation(out=rs[:, 12:16], in_=lg[:, 0:4], func=AF.Exp, bias=rs[:, 1:2], scale=1.0, accum_out=rs[:, 2:3]),
                       [Rlg, Rrs], [Rrs])
                    op("dve", lambda e: e.reciprocal(out=rs[:, 2:3], in_=rs[:, 2:3]), [Rrs], [Rrs])
                    op("dve", lambda e: e.tensor_scalar(out=rs[:, 8:12], in0=rs[:, 8:12], scalar1=-1.0, scalar2=-NEG, op0=ALU.add, op1=ALU.mult), [Rrs], [Rrs])
                    elm = oh[:, 0, :]
                    op("dve", lambda e: e.tensor_tensor(out=elm.rearrange("p (g x) -> p g x", g=4), in0=lg[:, 4:36].rearrange("p (g x) -> p g x", g=4),
                                                        in1=rs[:, 8:12].unsqueeze(2).to_broadcast([128, 4, 8]), op=ALU.add), [Rlg, Rrs], [Roh])
                    op("dve", lambda e: e.tensor_reduce(out=rs[:, 3:4], in_=elm, axis=AX.X, op=ALU.max), [Roh], [Rrs])
                    op("dve", lambda e: e.tensor_scalar(out=oh[:, 1, :], in0=elm, scalar1=rs[:, 3:4], scalar2=None, op0=ALU.is_equal), [Roh, Rrs], [Roh])
                    op("dve", lambda e: e.scalar_tensor_tensor(out=elm, in0=oh[:, 1, :], scalar=NEG, in1=elm, op0=ALU.mult, op1=ALU.add), [Roh], [Roh])
                    op("dve", lambda e: e.tensor_reduce(out=rs[:, 4:5], in_=elm, axis=AX.X, op=ALU.max), [Roh], [Rrs])
                    op("dve", lambda e: e.tensor_scalar(out=oh[:, 2, :], in0=elm, scalar1=rs[:, 4:5], scalar2=None, op0=ALU.is_equal), [Roh, Rrs], [Roh])
                    op("dve", lambda e: e.tensor_tensor(out=rs[:, 5:6], in0=rs[:, 4:5], in1=rs[:, 3:4], op=ALU.subtract), [Rrs], [Rrs])
                    op("act", lambda e: e.activation(out=rs[:, 5:6], in_=rs[:, 5:6], func=AF.Exp), [Rrs], [Rrs])
                    op("dve", lambda e: e.tensor_scalar(out=rs[:, 6:7], in0=rs[:, 5:6], scalar1=1.0, scalar2=None, op0=ALU.add), [Rrs], [Rrs])
                    op("dve", lambda e: e.reciprocal(out=rs[:, 6:7], in_=rs[:, 6:7]), [Rrs], [Rrs])
                    op("dve", lambda e: e.tensor_tensor(out=rs[:, 6:7], in0=rs[:, 6:7], in1=rs[:, 2:3], op=ALU.mult), [Rrs], [Rrs])
                    op("dve", lambda e: e.tensor_tensor(out=rs[:, 7:8], in0=rs[:, 6:7], in1=rs[:, 5:6], op=ALU.mult), [Rrs], [Rrs])
                    cut("D7")
                    comb, Rcomb = comb_r.get()
                    op("dve", lambda e: e.tensor_scalar(out=comb[:], in0=oh[:, 1, :], scalar1=rs[:, 6:7], scalar2=None, op0=ALU.mult), [Roh, Rrs], [Rcomb])
                    op("dve", lambda e: e.scalar_tensor_tensor(out=comb[:], in0=oh[:, 2, :], scalar=rs[:, 7:8], in1=comb[:], op0=ALU.mult, op1=ALU.add),
                       [Roh, Rrs, Rcomb], [Rcomb])
                    cut("D8")
                    op("dve", lambda e: e.tensor_reduce(out=c8_all[:, i, :], in_=comb[:].rearrange("p (g j) -> p j g", g=4), axis=AX.X, op=ALU.add),
                       [Rcomb], [R_c8])

                op("dve", lambda e: e.memset(goh_all[:], 0.0), [], [R_goh])
                tilesD = [i for i in range(NTT) if not (i < NTC and last)]
                run_skewed([tileD(i, tc) for tc, i in enumerate(tilesD)])
                S.barrier()
                S.release(mk_stD)
        if stop_after == "D":
            return finish(nc, S, [R_xs_mix, R_h2tok])

        tilesD = [i for i in range(NTT) if not (i < NTC and last)]
        with ExitStack() as stS:
            mk_stS = S.mark()

            def aS(name, shape, dt=F32):
                return stS.enter_context(nc.sbuf_tensor("%s_L%d" % (name, l), shape, dt))
            CUM = srt[:, 0:4]
            op("dve", lambda e: e.memset(srt[:], 0.0), [], [R_srt])
            op("dve", lambda e: e.memset(dest_f[:], 0.0), [], [R_destf])
            tmp4_r = Ring(aS, "tmp4", [128, 4], F32, 2)
            for n_, i in enumerate(tilesD):
                pr, Rpr = PBK[n_ % 2]
                op("pe", lambda e: e.matmul(pr[:, 0:4], lhsT=tri_sb[:], rhs=goh_all[:, i, :], start=True, stop=True), [R_tri, R_goh], [Rpr])
                op("pe", lambda e: e.matmul(pr[:, 4:8], lhsT=ones_sb[:], rhs=goh_all[:, i, :], start=True, stop=True), [R_ones, R_goh], [Rpr])
                t4, Rt4 = tmp4_r.get()
                op("dve", lambda e: e.tensor_tensor(out=t4[:], in0=pr[:, 0:4], in1=CUM, op=ALU.add), [Rpr, R_srt], [Rt4])
                op("dve", lambda e: e.tensor_tensor(out=t4[:], in0=t4[:], in1=goh_all[:, i, :], op=ALU.mult), [Rt4, R_goh], [Rt4])
                op("dve", lambda e: e.tensor_reduce(out=dest_f[:, i:i + 1], in_=t4[:], axis=AX.X, op=ALU.add), [Rt4], [R_destf])
                op("dve", lambda e: e.tensor_tensor(out=CUM, in0=pr[:, 4:8], in1=CUM, op=ALU.add), [Rpr, R_srt], [R_srt])
            tk, Rtk = aS("tk", [128, NB]), Res("tk")
            for g in range(4):
                op("dve", lambda e: e.tensor_scalar(out=tk[:], in0=thr_sb[:], scalar1=srt[:, g:g + 1], scalar2=None, op0=ALU.is_lt), [R_thr, R_srt], [Rtk])
                op("dve", lambda e: e.tensor_reduce(out=srt[:, 8 + g:9 + g], in_=tk[:], axis=AX.X, op=ALU.add), [Rtk], [R_srt])
            op("dve", lambda e: e.memset(srt[:, 16:17], 0.0), [R_srt], [R_srt])
            for g in range(1, 4):
                op("dve", lambda e: e.tensor_tensor(out=srt[:, 16 + g:17 + g], in0=srt[:, 15 + g:16 + g], in1=srt[:, 7 + g:8 + g], op=ALU.add), [R_srt], [R_srt])
            op("dve", lambda e: e.tensor_scalar(out=srt[:, 24:28], in0=srt[:, 16:20], scalar1=512.0, scalar2=None, op0=ALU.mult), [R_srt], [R_srt])
            for g in range(4):
                op("dve", lambda e: e.scalar_tensor_tensor(out=dest_f[:], in0=goh_all[:, :, g], scalar=srt[:, 24 + g:25 + g], in1=dest_f[:],
                                                           op0=ALU.mult, op1=ALU.add), [R_goh, R_srt, R_destf], [R_destf])
            op("dve", lambda e: e.tensor_copy(out=dest_i[:], in_=dest_f[:]), [R_destf], [R_desti])
            gb, Rgb = aS("gb", [128, NB]), Res("gb")
            op("dve", lambda e: e.memset(gb[:], 0.0), [], [Rgb])
            for g in range(1, 4):
                op("dve", lambda e: e.tensor_scalar(out=tk[:], in0=blk_sb[:], scalar1=srt[:, 16 + g:17 + g], scalar2=None, op0=ALU.is_ge), [R_blk, R_srt], [Rtk])
                op("dve", lambda e: e.tensor_tensor(out=gb[:], in0=gb[:], in1=tk[:], op=ALU.add), [Rtk, Rgb], [Rgb])
            op("dve", lambda e: e.tensor_scalar(out=gb[:], in0=gb[:], scalar1=1024.0, scalar2=float(l * NE * 128), op0=ALU.mult, op1=ALU.add), [Rgb], [Rgb])
            op("dve", lambda e: e.tensor_tensor(out=widx_f[:], in0=gb[:].unsqueeze(2).to_broadcast([128, NB, 8]),
                                                in1=jp_sb[:].unsqueeze(1).to_broadcast([128, NB, 8]), op=ALU.add), [Rgb, R_jp], [R_widxf])
            op("dve", lambda e: e.tensor_copy(out=widx_i[:], in_=widx_f[:].rearrange("p b j -> p (b j)")), [R_widxf], [R_widxi])
            h2r_r = Ring(aS, "h2r", [128, D], BF16, len(tilesD))
            for i in tilesD:
                h2r, Rh2r = h2r_r.get()
                dma("sp", h2r[:], h2tok_scr[i * 128:(i + 1) * 128, :], [R_h2tok], [Rh2r])
                S.idma(h2perm_scr, h2r[:], dest_i[:, i:i + 1], True, [Rh2r, R_desti, R_h2pz], [R_h2perm])
                S.idma(c8perm_scr, c8_all[:, i, :], dest_i[:, i:i + 1], True, [R_c8, R_desti, R_c8pz], [R_c8perm])
            S.barrier()
            S.release(mk_stS)
        if stop_after == "S":
            return finish(nc, S, [R_h2perm, R_c8perm])

        with ExitStack() as stE:
            mk_stE = S.mark()

            def aE(name, shape, dt=F32):
                return stE.enter_context(nc.sbuf_tensor("%s_L%d" % (name, l), shape, dt))
            if not last:
                load_mix_weights(l + 1)
            hp_r = Ring(aE, "hp", [128, 4, D], BF16, 2)
            h2Tb_r = Ring(aE, "h2TbE", [128, 8, 512], BF16, 2)
            c8_r = Ring(aE, "c8b", [128, 4, 8], F32, 2)
            c8T_r = Ring(aE, "c8T", [8, 512], F32, 2)
            wg_r = Ring(aE, "wg", [128, 8 * DE], BF16, 3)
            wu_r = Ring(aE, "wu", [128, 8 * DE], BF16, 3)
            wd_r = Ring(aE, "wd", [128, 2 * D], BF16, 3)
            cb_r = Ring(aE, "cb", [128, 512], F32, 3)
            sg_r = Ring(aE, "sg", [128, 512], F32, 3)
            hid = aE("hidT_all", [128, 16, 512], BF16)
            R_hid = [Res("hid%d" % e) for e in range(8)]
            yo_r = Ring(aE, "yo", [128, D], F32, 3)
            ecnt = 0
            NB_l = ((T if last else TT) + 4 * 511) // 512
            for b in range(NB_l):
                hp, Rhp = hp_r.get()
                dma("sp", hp[:], h2perm_scr[b * 512:(b + 1) * 512, :].rearrange("(j p) d -> p j d", p=128), [R_h2perm], [Rhp])
                c8, Rc8 = c8_r.get()
                dma("sp", c8[:], c8perm_scr[b * 512:(b + 1) * 512, :].rearrange("(j p) e -> p j e", p=128), [R_c8perm], [Rc8])
                hb_, Rhb_ = h2Tb_r.get()
                for j in range(4):
                    pb, Rpb = PBK[j]
                    pbv = bfv(pb)
                    for k in range(8):
                        op("pe", lambda e: e.transpose(out=pbv[:, k * 128:(k + 1) * 128], in_=hp[:, j, k * 128:(k + 1) * 128], identity=ident_b[:]),
                           [Rhp, R_idb], [Rpb])
                    eng = "act" if j % 2 == 0 else "dve"
                    if eng == "act":
                        op("act", lambda e: e.copy(out=hb_[:, :, j * 128:(j + 1) * 128], in_=pbv[:, 0:1024].rearrange("p (k t) -> p k t", k=8)), [Rpb], [Rhb_])
                    else:
                        op("dve", lambda e: e.tensor_copy(out=hb_[:, :, j * 128:(j + 1) * 128], in_=pbv[:, 0:1024].rearrange("p (k t) -> p k t", k=8)), [Rpb], [Rhb_])
                pc, Rpc = PBK[4]
                for j in range(4):
                    op("pe", lambda e: e.transpose(out=pc[0:8, j * 128:(j + 1) * 128], in_=c8[:, j, :], identity=ident_f[:]), [Rc8, R_idf], [Rpc])
                c8T, Rc8T = c8T_r.get()
                op("dve", lambda e: e.tensor_copy(out=c8T[:], in_=pc[0:8, 0:512]), [Rpc], [Rc8T])
                dma("sp", cbT_scr[b], c8T[:], [Rc8T], [R_cbT])
                for j_ in range(8):
                    wg, Rwg = wg_r.get()
                    wu, Rwu = wu_r.get()
                    cb, Rcb_ = cb_r.get()
                    ix = widx_i[:, b * 8 + j_:b * 8 + j_ + 1]
                    S.idma(wg[:], wg_scr, ix, False, [R_wg, R_widxi], [Rwg])
                    S.idma(wu[:], wu_scr, ix, False, [R_wu, R_widxi], [Rwu])
                    dma("sp", cb[:], cbT_scr[b, j_, :].partition_broadcast(128), [R_cbT], [Rcb_])
                    wgv = wg[:].rearrange("p (k h) -> p k h", k=8)
                    wuv = wu[:].rearrange("p (k h) -> p k h", k=8)
                    base = 4 * (ecnt % 2)
                    ecnt += 1
                    for hc in range(2):
                        gt, Rg = PBK[base + hc]
                        ut, Ru = PBK[base + 2 + hc]
                        for k in range(8):
                            op("pe", lambda e: e.matmul(gt[:, :], lhsT=wgv[:, k, hc * 128:(hc + 1) * 128], rhs=hb_[:, k, :], start=(k == 0), stop=(k == 7)),
                               [Rwg, Rhb_], [Rg])
                        for k in range(8):
                            op("pe", lambda e: e.matmul(ut[:, :], lhsT=wuv[:, k, hc * 128:(hc + 1) * 128], rhs=hb_[:, k, :], start=(k == 0), stop=(k == 7)),
                               [Rwu, Rhb_], [Ru])
                    for hc in range(2):
                        gt, Rg = PBK[base + hc]
                        ut, Ru = PBK[base + 2 + hc]
                        sg, Rsg = sg_r.get()
                        op("act", lambda e: e.activation(out=sg[:], in_=gt[:, :], func=AF.Silu), [Rg], [Rsg])
                        op("dve", lambda e: e.tensor_tensor(out=sg[:], in0=ut[:, :], in1=sg[:], op=ALU.mult), [Ru, Rsg], [Rsg])
                        op("dve", lambda e: e.tensor_tensor(out=hid[:, 2 * j_ + hc, :], in0=sg[:], in1=cb[:], op=ALU.mult),
                           [Rsg, Rcb_], [R_hid[j_]])
                for j_ in range(8):
                    wd, Rwd = wd_r.get()
                    ix = widx_i[:, b * 8 + j_:b * 8 + j_ + 1]
                    S.idma(wd[:], wd_scr, ix, False, [R_wd, R_widxi], [Rwd])
                    wdv = wd[:].rearrange("p (c d) -> p c d", c=2)
                    for hc in range(2):
                        for j in range(4):
                            for dh in range(2):
                                yt, Ry_ = PBK[j * 2 + dh]
                                op("pe", lambda e: e.matmul(yt[:, :], lhsT=hid[:, 2 * j_ + hc, j * 128:(j + 1) * 128], rhs=wdv[:, hc, dh * 512:(dh + 1) * 512],
                                                            start=(j_ == 0 and hc == 0), stop=(j_ == 7 and hc == 1)), [R_hid[j_], Rwd], [Ry_])
                for j in range(4):
                    yo, Ryo = yo_r.get()
                    for dh in range(2):
                        yt, Ry_ = PBK[j * 2 + dh]
                        if dh == 0:
                            op("act", lambda e: e.copy(out=yo[:, 0:512], in_=yt[:, :]), [Ry_], [Ryo])
                        else:
                            op("dve", lambda e: e.tensor_copy(out=yo[:, 512:1024], in_=yt[:, :]), [Ry_], [Ryo])
                    r0 = b * 512 + j * 128
                    dma("sp", yperm_scr[r0:r0 + 128, :], yo[:], [Ryo], [R_yperm])
            S.barrier()
            S.release(mk_stE)
        if stop_after == "E":
            return finish(nc, S, [R_yperm])

        with ExitStack() as stF:
            mk_stF = S.mark()

            def aF(name, shape, dt=F32):
                return stF.enter_context(nc.sbuf_tensor("%s_L%d" % (name, l), shape, dt))
            g2bc = {}
            for r in range(2):
                if r == 1 and last:
                    continue
                t = aF("g2_%d" % r, [128, D])
                Rr = Res("g2_%d" % r)
                load_bc(t, Rr, ada_vec(l, r, 5), D, [R_ada])
                g2bc[r] = (t, Rr)
            ln2g_bc, R_l2g = aF("ln2g_bc", [128, D]), Res("ln2g_bc")
            ln2b_bc, R_l2b = aF("ln2b_bc", [128, D]), Res("ln2b_bc")
            load_bc(ln2g_bc, R_l2g, ln2_g[l], D)
            load_bc(ln2b_bc, R_l2b, ln2_b[l], D)
            xt_r = Ring(aF, "xtF", [128, D], F32, 3)
            yg_r = Ring(aF, "ygF", [128, D], F32, 3)
            o_r = Ring(aF, "oF", [128, D], F32, 3)
            lnr = {"st": Ring(aF, "fst", [128, 12], F32, 3), "mv": Ring(aF, "fmv", [128, 4], F32, 3)}

            def tileF(i):
                typ = 1 if i < NTC else 0
                gg = i * 128
                xt, Rxt = xt_r.get()
                dma("sp", xt[:], xs_mix[gg:gg + 128, :], [R_xs_mix], [Rxt])
                yg, Ryg = yg_r.get()
                S.idma(yg[:], yperm_scr, dest_i[:, i:i + 1], False, [R_yperm, R_desti], [Ryg])
                yield
                g2t, Rg2 = g2bc[typ]
                op("dve", lambda e: e.tensor_tensor(out=yg[:], in0=yg[:], in1=g2t[:], op=ALU.mult), [Ryg, Rg2], [Ryg])
                op("dve", lambda e: e.scalar_tensor_tensor(out=yg[:], in0=xt[:], scalar=ALPHA, in1=yg[:], op0=ALU.mult, op1=ALU.add), [Rxt, Ryg], [Ryg])
                o, Ro = o_r.get()
                layer_norm_tile(None, "dve", yg, Ryg, D, ln2g_bc, R_l2g, ln2b_bc, R_l2b, o, Ro, lnr)
                if last:
                    dma("sp", out_d[gg - C:gg - C + 128, :], o[:], [Ro], [R_out])
                else:
                    dma("sp", xs_out[l % 2][gg:gg + 128, :], o[:], [Ro], [R_xs_out[l % 2]])

            run_skewed([tileF(i) for i in tilesD])
            S.barrier()
            S.release(mk_stF)
    return finish(nc, S, [R_out])


def finish(nc, S, ress):
    S.barrier()
    S.wait_all("sp", ress)
    return nc


def _rope_tables(T, C):
    rows = T // GRID_W
    row = np.repeat(np.arange(rows), GRID_W).astype(np.float32)
    col = np.tile(np.arange(GRID_W), rows).astype(np.float32)
    d_axis = DR // 2
    inv_freq = np.power(np.float32(10000.0), -np.arange(0, d_axis, 2, dtype=np.float32) / np.float32(d_axis)).astype(np.float32)

    def ax(p):
        a = p[:, None] * inv_freq[None, :]
        return np.concatenate([a, a], -1)

    ang = np.concatenate([ax(row), ax(col)], -1).astype(np.float32)
    cos = np.cos(ang).astype(np.float32)
    sin = np.sin(ang).astype(np.float32)
    sgn = np.tile(np.concatenate([-np.ones(8), np.ones(8)]), 2).astype(np.float32)
    tab = np.zeros((T + C, 2, DR), np.float32)
    tab[:C, 0, :] = 1.0
    tab[C:, 0, :] = cos
    tab[C:, 1, :] = sin * sgn[None, :]
    return tab


def _pool_tables():
    wins = (2, 4, 8, 16)
    edge = np.zeros((128, 2, 2, 8), np.float32)
    invw = np.zeros((128, 2), np.float32)
    for k in range(2):
        for ph in range(2):
            w = wins[2 * k + ph]
            ps = slice(ph * 64, ph * 64 + 64)
            invw[ps, k] = 1.0 / w
            for j in range(8):
                t = j
                cnt = (t + w // 2 - 1) - max(t - w // 2, 0) + 1
                edge[ps, k, 0, j] = 1.0 / cnt
                r = 7 - j
                hi = min(w // 2 - 1, r)
                cnt = hi + w // 2 + 1
                edge[ps, k, 1, j] = 1.0 / cnt
    return edge, invw


def _sort_tables(T, C):
    TT = T + C
    NB = (TT + 4 * 511 + 511) // 512
    tri = np.triu(np.ones((128, 128), np.float32), k=1)
    thr = np.broadcast_to((np.arange(NB, dtype=np.float32) * 512.0)[None, :], (128, NB)).copy()
    blk = np.broadcast_to(np.arange(NB, dtype=np.float32)[None, :], (128, NB)).copy()
    jp = (np.arange(8, dtype=np.float32)[None, :] * 128.0 + np.arange(128, dtype=np.float32)[:, None]).astype(np.float32)
    return {"tri": tri, "thr_bc": thr, "blk_bc": blk, "jp": jp}


_CACHE = {}


def _consts(T, C):
    edge, invw = _pool_tables()
    return {
        "ident": np.eye(128, dtype=np.float32),
        "rope_cs": _rope_tables(T, C),
        "pool_edge": edge,
        "pool_invw": invw,
        **_sort_tables(T, C),
    }


_WKEYS = ["w_ada", "b_ada", "w_in", "g_q", "w_uq", "g_kv", "w_ukv", "conv_w", "conv_b", "conv_ln_g", "conv_ln_b", "pool_w",
          "pool_scale", "w_out", "ln1_g", "ln1_b", "w_router_group", "b_router_group", "w_router_expert", "b_router_expert",
          "w_gate", "w_up", "w_down", "ln2_g", "ln2_b"]


def make_in_maps(inputs, T, C, ncores):
    consts = _consts(T, C)
    shared = {k: np.ascontiguousarray(np.asarray(inputs[k], dtype=np.float32)) for k in _WKEYS}
    maps = []
    for b in range(ncores):
        m = dict(shared)
        m.update(consts)
        m["x"] = np.ascontiguousarray(np.asarray(inputs["x"][b], dtype=np.float32))
        m["ctx"] = np.ascontiguousarray(np.asarray(inputs["ctx"][b], dtype=np.float32))
        m["cvec"] = np.ascontiguousarray(np.stack([np.asarray(inputs["c"][b]), np.asarray(inputs["c_ctx"])]).astype(np.float32))
        maps.append(m)
    return maps


def kernel(**inputs):
    x = np.asarray(inputs["x"])
    B, T, _ = x.shape
    C = np.asarray(inputs["ctx"]).shape[1]
    L = np.asarray(inputs["w_ada"]).shape[0]
    key = (T, C, L)
    if key not in _CACHE:
        _CACHE[key] = build(T, C, L)
    nc = _CACHE[key]
    maps = make_in_maps(inputs, T, C, B)
    res = run_bass_kernel_spmd(nc, maps, core_ids=list(range(B)))
    return np.stack([np.asarray(r["out"]) for r in res.results], axis=0).astype(np.float32)
```

```python
import math
from contextlib import ExitStack
import numpy as np
import concourse.bass as bass
import concourse.mybir as mybir
from concourse.bass_utils import run_bass_kernel_spmd

F32 = mybir.dt.float32
BF16 = mybir.dt.bfloat16
AF = mybir.ActivationFunctionType
ALU = mybir.AluOpType
AX = mybir.AxisListType

D = 1024
H = 8
DQ = 384
DKV = 256
DR = 32
DIN = 1440
NE = 32
DE = 256
GRID_W = 64
CONVW = 31
LN_EPS = 1e-5
RMS_EPS = 1e-6
NEG = -1.0e30


class Res:
    __slots__ = ("name", "w", "r", "sem", "cnt", "excl", "multi")

    def __init__(self, name, excl=False, multi=False):
        self.multi = False
        self.name = name
        self.w = None
        self.r = []
        self.sem = None
        self.cnt = 0
        self.excl = excl


class Sched:
    def __init__(self, nc):
        self.nc = nc
        self.eng = {"pe": nc.tensor, "act": nc.scalar, "dve": nc.vector, "pool": nc.gpsimd, "sp": nc.sync}
        self.sems = {}
        self.cnt = {}
        self.known = {}
        self.dma_res = []
        self.free_sems = []
        self.free_sw = []
        self.is_sw = {}
        self.nalloc = 0
        self.nwait = 0
        for e in self.eng:
            self.sems[e] = nc.alloc_semaphore("e_" + e)
            self.cnt[e] = 0
            self.known[e] = {}

    def _waits(self, e, reads, writes):
        deps = {}

        def add(ev):
            if ev is None:
                return
            k, v = ev
            if deps.get(k, 0) < v:
                deps[k] = v

        for r in reads:
            add(r.w)
        for w in writes:
            if not w.multi:
                add(w.w)
            for ev in w.r:
                add(ev)
        kn = self.known[e]
        for k, v in deps.items():
            if kn.get(k, 0) >= v:
                continue
            if e == "pe" and k == "pe":
                continue
            kn[k] = v
            sem = self.sems[k] if isinstance(k, str) else k.sem
            self.eng[e].wait_ge(sem, v)
            self.nwait += 1

    @staticmethod
    def _commit(ev, reads, writes):
        for r in reads:
            r.r.append(ev)
            if len(r.r) > 48:
                best = {}
                for k, v in r.r:
                    if best.get(k, 0) < v:
                        best[k] = v
                r.r = list(best.items())
        for w in writes:
            w.w = ev
            w.r = []

    def op(self, e, fn, reads=(), writes=()):
        if any(r.excl for r in reads):
            writes = list(writes) + [r for r in reads if r.excl and r not in writes]
            reads = [r for r in reads if not r.excl]
        self._waits(e, reads, writes)
        ins = fn(self.eng[e])
        self.cnt[e] += 1
        ins.then_inc(self.sems[e], 1)
        self._commit((e, self.cnt[e]), reads, writes)

    def dma(self, e, out, in_, reads, writes, **kw):
        dst = writes[0]
        self._waits(e, reads, writes)
        self.ensure(dst, sw=(e == "pool"))
        dst.cnt += 16
        self.eng[e].dma_start(out=out, in_=in_, **kw).then_inc(dst.sem, 16)
        self._commit((dst, dst.cnt), reads, writes)

    def idma(self, out, in_, idx_ap, scatter, reads, writes):
        import concourse.bass as _b
        dst = writes[0]
        self._waits("pool", reads, writes)
        self.ensure(dst, sw=True)
        dst.cnt += 16
        off = _b.IndirectOffsetOnAxis(ap=idx_ap, axis=0)
        if scatter:
            ins = self.nc.gpsimd.indirect_dma_start(out=out, out_offset=off, in_=in_, in_offset=None)
        else:
            ins = self.nc.gpsimd.indirect_dma_start(out=out, out_offset=None, in_=in_, in_offset=off)
        ins.then_inc(dst.sem, 16)
        self._commit((dst, dst.cnt), reads, writes)

    def ensure(self, dst, sw=False):
        if dst.sem is None:
            fl = self.free_sw if sw else self.free_sems
            self.is_sw[id(dst)] = sw
            if fl:
                dst.sem, dst.cnt = fl.pop()
            else:
                self.nalloc += 1
                dst.sem = self.nc.alloc_semaphore("d%d_%s" % (self.nalloc, dst.name))
                dst.cnt = 0
            self.dma_res.append(dst)

    def mark(self):
        return len(self.dma_res)

    def release(self, mark):
        for r in self.dma_res[mark:]:
            (self.free_sw if self.is_sw.get(id(r)) else self.free_sems).append((r.sem, r.cnt))
            r.sem = None
        del self.dma_res[mark:]

    def barrier(self):
        for e in self.eng:
            kn = self.known[e]
            for k in self.eng:
                if k == e:
                    continue
                v = self.cnt[k]
                if v > 0 and kn.get(k, 0) < v:
                    kn[k] = v
                    self.eng[e].wait_ge(self.sems[k], v)
            for r in self.dma_res:
                if r.cnt > 0 and kn.get(r, 0) < r.cnt:
                    kn[r] = r.cnt
                    self.eng[e].wait_ge(r.sem, r.cnt)

    def wait_all(self, e, ress):
        self._waits(e, ress, ())


class Ring:
    def __init__(self, alloc, name, shape, dt, n):
        self.bufs = []
        for i in range(n):
            nm = "%s_%d" % (name, i)
            self.bufs.append((alloc(nm, shape, dt), Res(nm)))
        self.i = 0

    def get(self):
        b = self.bufs[self.i % len(self.bufs)]
        self.i += 1
        return b


class _Cut(Exception):
    pass


def run_skewed(gens):
    active = []
    it = iter(gens)
    while True:
        g = next(it, None)
        if g is not None:
            active.append(g)
        elif not active:
            break
        for g_ in list(reversed(active)):
            try:
                next(g_)
            except StopIteration:
                active.remove(g_)


def build(T, C, L, debug=False, stop_after=None):
    st = {}
    try:
        return _build(T, C, L, debug, stop_after, st)
    except _Cut:
        return finish(st["nc"], st["S"], [])


def _build(T, C, L, debug, stop_after, st_):
    NTL = T // 128
    NTC = C // 128
    TT = T + C
    NTT = NTL + NTC
    ALPHA = float((2 * L) ** 0.25)
    QS = 1.0 / math.sqrt(96.0)
    SEG = min(1024, T)

    nc = bass.Bass("TRN2", target_bir_lowering=False)
    S = Sched(nc)
    op = S.op
    dma = S.dma
    st_["nc"] = nc
    st_["S"] = S

    def cut(tag):
        if stop_after == tag:
            raise _Cut()

    def din(name, shape, dt=F32):
        return nc.dram_tensor(name, shape, dt, kind="ExternalInput").ap()

    def dscr(name, shape, dt=F32):
        return nc.dram_tensor(name, shape, dt, kind=("ExternalOutput" if debug else "Internal")).ap()

    x_in = din("x", [T, D])
    ctx_in = din("ctx", [C, D])
    cvec = din("cvec", [2, D])
    w_ada = din("w_ada", [L, D, 6 * D])
    b_ada = din("b_ada", [L, 6 * D])
    w_in = din("w_in", [L, D, DIN])
    g_q = din("g_q", [L, DQ])
    w_uq = din("w_uq", [L, DQ, 768])
    g_kv = din("g_kv", [L, DKV])
    w_ukv = din("w_ukv", [L, DKV, 1024])
    conv_w = din("conv_w", [L, CONVW, 256])
    conv_b = din("conv_b", [L, 256])
    conv_ln_g = din("conv_ln_g", [L, 256])
    conv_ln_b = din("conv_ln_b", [L, 256])
    pool_w = din("pool_w", [L, 4, 64, 64])
    pool_scale = din("pool_scale", [L, 256])
    w_out = din("w_out", [L, D, D])
    ln1_g = din("ln1_g", [L, D])
    ln1_b = din("ln1_b", [L, D])
    w_rg = din("w_router_group", [L, D, 4])
    b_rg = din("b_router_group", [L, 4])
    w_re = din("w_router_expert", [L, D, NE])
    b_re = din("b_router_expert", [L, NE])
    w_gate = din("w_gate", [L, NE, D, DE])
    w_up = din("w_up", [L, NE, D, DE])
    w_down = din("w_down", [L, NE, DE, D])
    ln2_g = din("ln2_g", [L, D])
    ln2_b = din("ln2_b", [L, D])
    ident_d = din("ident", [128, 128])
    rope_d = din("rope_cs", [TT, 2, DR])
    pedge_d = din("pool_edge", [128, 2, 2, 8])
    pinvw_d = din("pool_invw", [128, 2])

    NB = (TT + 4 * 511 + 511) // 512
    NP = NB * 512
    I32 = mybir.dt.int32
    tri_d = din("tri", [128, 128])
    thr_d = din("thr_bc", [128, NB])
    blk_d = din("blk_bc", [128, NB])
    jp_d = din("jp", [128, 8])
    out_d = nc.dram_tensor("out", [T, D], F32, kind="ExternalOutput").ap()
    h2tok_scr = dscr("h2tok_scr", [TT, D], BF16)
    h2perm_scr = dscr("h2perm_scr", [NP, D], BF16)
    c8perm_scr = dscr("c8perm_scr", [NP, 8])
    cbT_scr = dscr("cbT_scr", [NB, 8, 512])
    yperm_scr = dscr("yperm_scr", [NP, D])
    R_h2tok = Res("h2tok_scr", multi=True)
    R_h2perm = Res("h2perm_scr", multi=True)
    R_c8perm = Res("c8perm_scr", multi=True)
    R_cbT = Res("cbT_scr", multi=True)
    R_yperm = Res("yperm_scr", multi=True)

    ada_scr = dscr("ada_scr", [L, 2, 6 * D])
    xs_mix = dscr("xs_mix", [TT, D])
    xs_out = [dscr("xs_out0", [TT, D]), dscr("xs_out1", [TT, D])]
    kT_scr = dscr("kT_scr", [H, 97, TT], BF16)
    qT_scr = dscr("qT_scr", [H, 96, TT], BF16)
    mT_scr = dscr("mT_scr", [H, TT], BF16)
    v_scr = dscr("v_scr", [H, 128, NTT, 80], BF16)
    catcp_scr = dscr("catcp_scr", [TT, 512], BF16)
    h2T_scr = dscr("h2T_scr", [8, 128, TT], BF16)
    combT_scr = dscr("combT_scr", [NE, TT])
    wg_scr = nc.dram_tensor("wg_scr", [L * NE * 128, 8 * DE], BF16, kind="Internal").ap()
    wu_scr = nc.dram_tensor("wu_scr", [L * NE * 128, 8 * DE], BF16, kind="Internal").ap()
    wd_scr = nc.dram_tensor("wd_scr", [L * NE * 128, 2 * D], BF16, kind="Internal").ap()
    R_ada = Res("ada_scr")
    R_xs_mix = Res("xs_mix", multi=True)
    R_xs_out = [Res("xs_out0", multi=True), Res("xs_out1", multi=True)]
    R_kT = Res("kT_scr", multi=True)
    R_qT = Res("qT_scr", multi=True)
    R_mT = Res("mT_scr")
    R_v = Res("v_scr", multi=True)
    R_catcp = Res("catcp_scr", multi=True)
    R_h2T = Res("h2T_scr", multi=True)
    R_combT = Res("combT_scr", multi=True)
    R_wg = Res("wg_scr")
    R_wu = Res("wu_scr")
    R_wd = Res("wd_scr")
    R_out = Res("out", multi=True)
    for R_ in [R_h2perm, R_c8perm]:
        S.ensure(R_, sw=True)
    R_h2pz = Res("h2perm_zero")
    R_c8pz = Res("c8perm_zero")
    S.ensure(R_h2pz)
    S.ensure(R_c8pz)
    for R_ in [R_h2tok, R_cbT, R_yperm]:
        S.ensure(R_)
    for R_ in [R_ada, R_xs_mix, R_xs_out[0], R_xs_out[1], R_kT, R_qT, R_mT, R_v, R_catcp, R_h2T, R_combT, R_out]:
        S.ensure(R_)
    for R_ in [R_wg, R_wu, R_wd]:
        S.ensure(R_, sw=True)

    PBK = []
    for i in range(8):
        PBK.append((nc.alloc_psum_tensor("pb%d" % i, [128, 512], F32), Res("pb%d" % i, excl=True)))

    def bfv(t):
        return t[:].bitcast(BF16)

    def palloc(name, shape, dt=F32):
        return nc.alloc_sbuf_tensor(name, shape, dt)

    def sb(name, shape, dt=F32):
        return nc.alloc_sbuf_tensor(name, shape, dt), Res(name)

    ident_f, R_idf = sb("ident_f", [128, 128])
    ident_b, R_idb = sb("ident_b", [128, 128], BF16)
    dma("sp", ident_f[:], ident_d, [], [R_idf])
    op("dve", lambda e: e.tensor_copy(out=ident_b[:], in_=ident_f[:]), [R_idf], [R_idb])
    eps_t, R_epst = sb("eps_t", [128, 2])
    op("dve", lambda e: e.memset(eps_t[:, 0:1], LN_EPS), [], [R_epst])
    op("dve", lambda e: e.memset(eps_t[:, 1:2], RMS_EPS), [R_epst], [R_epst])
    tri_sb, R_tri = sb("tri_sb", [128, 128])
    dma("sp", tri_sb[:], tri_d, [], [R_tri])
    ones_sb, R_ones = sb("ones_sb", [128, 128])
    op("dve", lambda e: e.memset(ones_sb[:], 1.0), [], [R_ones])
    thr_sb, R_thr = sb("thr_sb", [128, NB])
    blk_sb, R_blk = sb("blk_sb", [128, NB])
    jp_sb, R_jp = sb("jp_sb", [128, 8])
    dma("sp", thr_sb[:], thr_d, [], [R_thr])
    dma("sp", blk_sb[:], blk_d, [], [R_blk])
    dma("sp", jp_sb[:], jp_d, [], [R_jp])
    zer_b, R_zerb = sb("zer_b", [128, D], BF16)
    zer_f, R_zerf = sb("zer_f", [128, 8])
    op("dve", lambda e: e.memset(zer_b[:], 0.0), [], [R_zerb])
    op("dve", lambda e: e.memset(zer_f[:], 0.0), [], [R_zerf])
    goh_all, R_goh = sb("goh_all", [128, NTT, 4])
    c8_all, R_c8 = sb("c8_all", [128, NTT, 8])
    dest_f, R_destf = sb("dest_f", [128, NTT])
    dest_i, R_desti = sb("dest_i", [128, NTT], I32)
    widx_f, R_widxf = sb("widx_f", [128, NB, 8])
    widx_i, R_widxi = sb("widx_i", [128, NB * 8], I32)
    srt, R_srt = sb("srt", [128, 64])
    pedge, R_pedge = sb("pedge", [128, 2, 2, 8])
    pinvw, R_pinvw = sb("pinvw", [128, 2])
    dma("sp", pedge[:], pedge_d, [], [R_pedge])
    dma("sp", pinvw[:], pinvw_d, [], [R_pinvw])

    w_in_sb, R_win = sb("w_in_sb", [128, 8, DIN], BF16)
    w_uq_sb, R_wuq = sb("w_uq_sb", [128, 3, 768], BF16)
    w_ukv_sb, R_wukv = sb("w_ukv_sb", [128, 2, 1024], BF16)
    for R_ in (R_win, R_wuq, R_wukv):
        S.ensure(R_, sw=True)

    def load_mix_weights(l_):
        dma("pool", w_in_sb[:], w_in[l_].rearrange("(k p) n -> p k n", p=128), [], [R_win])
        dma("pool", w_uq_sb[:], w_uq[l_].rearrange("(k p) n -> p k n", p=128), [], [R_wuq])
        dma("pool", w_ukv_sb[:], w_ukv[l_].rearrange("(k p) n -> p k n", p=128), [], [R_wukv])

    load_mix_weights(0)

    for l in range(L if stop_after not in ("0", "A", "C", "B", "D") else 0):
        for e0 in range(0, NE, 8):
            for e1 in range(e0, e0 + 8):
                r0 = (l * NE + e1) * 128
                dma("pool", wg_scr[r0:r0 + 128, :].rearrange("p (k h) -> p k h", k=8), w_gate[l, e1].rearrange("(k p) h -> p k h", p=128), [], [R_wg])
                dma("pool", wu_scr[r0:r0 + 128, :].rearrange("p (k h) -> p k h", k=8), w_up[l, e1].rearrange("(k p) h -> p k h", p=128), [], [R_wu])
                dma("pool", wd_scr[r0:r0 + 128, :].rearrange("p (c d) -> p c d", c=2), w_down[l, e1].rearrange("(c p) d -> p c d", p=128), [], [R_wd])

    with ExitStack() as st0:
        mk0 = S.mark()

        def a0(name, shape, dt=F32):
            return st0.enter_context(nc.sbuf_tensor(name, shape, dt))
        cs, R_cs = a0("cs", [2, D]), Res("cs")
        csT, R_csT = a0("csT", [128, 8, 2]), Res("csT")
        bada, R_bada = a0("bada", [2, 6 * D]), Res("bada")
        adas, R_adas = a0("adas", [2, 6 * D]), Res("adas")
        wblk = Ring(a0, "wblk", [128, 8, 512], F32, 2)
        dma("sp", cs[:], cvec, [], [R_cs])
        op("act", lambda e: e.activation(out=cs[:], in_=cs[:], func=AF.Silu), [R_cs], [R_cs])
        pb, Rpb = PBK[0]
        for k in range(8):
            op("pe", lambda e: e.transpose(out=pb[:, 2 * k:2 * k + 2], in_=cs[0:2, k * 128:(k + 1) * 128],
                                           identity=ident_f[0:2, 0:2]), [R_cs, R_idf], [Rpb])
        op("dve", lambda e: e.tensor_copy(out=csT[:].rearrange("p k r -> p (k r)"), in_=pb[:, 0:16]), [Rpb], [R_csT])
        nb_i = 0
        for l in range(L):
            dma("sp", bada[:], b_ada[l].partition_broadcast(2), [], [R_bada])
            for nb in range(12):
                wb, Rwb = wblk.get()
                dma("sp", wb[:], w_ada[l, :, nb * 512:(nb + 1) * 512].rearrange("(k p) n -> p k n", p=128), [], [Rwb])
                pb, Rpb = PBK[1 + (nb_i % 2)]
                nb_i += 1
                for k in range(8):
                    op("pe", lambda e: e.matmul(pb[0:2, :], lhsT=csT[:, k, :], rhs=wb[:, k, :], start=(k == 0), stop=(k == 7)),
                       [R_csT, Rwb], [Rpb])
                op("dve", lambda e: e.tensor_tensor(out=adas[:, nb * 512:(nb + 1) * 512], in0=pb[0:2, :],
                                                    in1=bada[:, nb * 512:(nb + 1) * 512], op=ALU.add), [Rpb, R_bada], [R_adas])
            for j in (1, 4):
                op("dve", lambda e: e.tensor_scalar_add(out=adas[:, j * D:(j + 1) * D], in0=adas[:, j * D:(j + 1) * D], scalar1=1.0),
                   [R_adas], [R_adas])
            dma("sp", ada_scr[l], adas[:], [R_adas], [R_ada])
        S.barrier()
        S.release(mk0)

    if stop_after == "0":
        return finish(nc, S, [R_ada])

    def ada_vec(l, r, j):
        return ada_scr[l, r, j * D:(j + 1) * D]

    def load_bc(t, R, src1d, n, rd=()):
        dma("sp", t[:, 0:n], src1d.partition_broadcast(128), list(rd), [R])

    def x_src(l, i):
        if l == 0:
            if i < NTC:
                return ctx_in[i * 128:(i + 1) * 128, :], []
            return x_in[(i - NTC) * 128:(i - NTC + 1) * 128, :], []
        return xs_out[(l - 1) % 2][i * 128:(i + 1) * 128, :], [R_xs_out[(l - 1) % 2]]

    def layer_norm_tile(st, eng2, y, Ry, n, gbc, Rg, bbc, Rb, outt, Rout, rings):
        stt, Rst = rings["st"].get()
        mv, Rmv = rings["mv"].get()
        nch = (n + 511) // 512
        for c in range(nch):
            a, b_ = c * 512, min(n, (c + 1) * 512)
            op("dve", lambda e: e.bn_stats(out=stt[:, c * 6:(c + 1) * 6], in_=y[:, a:b_]), [Ry], [Rst])
        op("dve", lambda e: e.bn_aggr(out=mv[:, 0:2], in_=stt[:, 0:nch * 6]), [Rst], [Rmv])
        op("act", lambda e: e.activation(out=mv[:, 2:3], in_=mv[:, 1:2], func=AF.Ln, bias=eps_t[:, 0:1], scale=1.0), [Rmv], [Rmv])
        op("act", lambda e: e.activation(out=mv[:, 2:3], in_=mv[:, 2:3], func=AF.Exp, scale=-0.5), [Rmv], [Rmv])
        op("dve", lambda e: e.scalar_tensor_tensor(out=mv[:, 3:4], in0=mv[:, 0:1], scalar=-1.0, in1=mv[:, 2:3],
                                                   op0=ALU.mult, op1=ALU.mult), [Rmv], [Rmv])
        op("act", lambda e: e.activation(out=y[:, 0:n], in_=y[:, 0:n], func=AF.Identity, bias=mv[:, 3:4], scale=mv[:, 2:3]),
           [Ry, Rmv], [Ry])
        op(eng2, lambda e: e.tensor_tensor(out=y[:, 0:n], in0=y[:, 0:n], in1=gbc[:, 0:n], op=ALU.mult), [Ry, Rg], [Ry])
        op("dve", lambda e: e.tensor_tensor(out=outt[:, 0:n], in0=y[:, 0:n], in1=bbc[:, 0:n], op=ALU.add), [Ry, Rb], [Rout])

    for l in range(L):
        last = (l == L - 1)
        S.barrier()

        with ExitStack() as stAC:
            def aAC(name, shape, dt=F32):
                return stAC.enter_context(nc.sbuf_tensor("%s_L%d" % (name, l), shape, dt))
            cpT_l, R_cpl = aAC("cpT_l", [128, 4, T + 32]), Res("cpT_l")
            cpT_c, R_cpc = aAC("cpT_c", [128, 4, C + 32]), Res("cpT_c")
            for (t_, R_, n_) in ((cpT_l, R_cpl, T), (cpT_c, R_cpc, C)):
                op("pool", lambda e: e.memset(t_[:, :, 0:16], 0.0), [], [R_])
                op("pool", lambda e: e.memset(t_[:, :, 16 + n_:32 + n_], 0.0), [R_], [R_])

            with ExitStack() as stA:
                mk_stA = S.mark()
                def aA(name, shape, dt=F32):
                    return stA.enter_context(nc.sbuf_tensor("%s_L%d" % (name, l), shape, dt))
                gq_bc, R_gq = aA("gq_bc", [128, DQ]), Res("gq_bc")
                gkv_bc, R_gkv = aA("gkv_bc", [128, DKV]), Res("gkv_bc")
                load_bc(gq_bc, R_gq, g_q[l], DQ)
                load_bc(gkv_bc, R_gkv, g_kv[l], DKV)
                sc1, sh1, R_sc1, R_sh1 = [], [], [], []
                for r in range(2):
                    t = aA("sc1_%d" % r, [128, D])
                    Rr = Res("sc1_%d" % r)
                    load_bc(t, Rr, ada_vec(l, r, 1), D, [R_ada])
                    sc1.append(t)
                    R_sc1.append(Rr)
                    t = aA("sh1_%d" % r, [128, D])
                    Rr = Res("sh1_%d" % r)
                    load_bc(t, Rr, ada_vec(l, r, 0), D, [R_ada])
                    sh1.append(t)
                    R_sh1.append(Rr)
                rope_sb, R_rope = aA("rope_sb", [128, NTT, 2, DR]), Res("rope_sb")
                dma("sp", rope_sb[:], rope_d.rearrange("(i p) a d -> p i a d", p=128), [], [R_rope])
                nq_all, R_nq = aA("nq_all", [128, NTT, H]), Res("nq_all")
                kmax2, R_kmax2 = aA("kmax2", [128, H]), Res("kmax2")
                op("dve", lambda e: e.memset(kmax2[:], 0.0), [], [R_kmax2])
                op("dve", lambda e: e.memset(nq_all[:], 0.0), [], [R_nq])

                xt_r = Ring(aA, "xt", [128, D], F32, 2)
                tmp_r = Ring(aA, "tmp32", [128, D], F32, 2)
                hb_r = Ring(aA, "hb", [128, D], BF16, 2)
                hT_r = Ring(aA, "hT", [128, 8, 128], BF16, 2)
                junk_r = Ring(aA, "junk", [128, 512], F32, 2)
                stat_r = Ring(aA, "stat", [128, 8], F32, 4)
                qn_r = Ring(aA, "qn", [128, DQ], BF16, 2)
                qnT_r = Ring(aA, "qnT", [128, 3, 128], BF16, 2)
                ckvn_r = Ring(aA, "ckvn", [128, DKV], BF16, 2)
                ckvT_r = Ring(aA, "ckvT", [128, 2, 128], BF16, 2)
                qaug_r = Ring(aA, "qaug", [128, H, 96], BF16, 2)
                kaug_r = Ring(aA, "kaug", [128, H, 112], BF16, 2)
                vaug_r = Ring(aA, "vaug", [128, H, 80], BF16, 2)
                for (t_, R_) in kaug_r.bufs:
                    op("dve", lambda e: e.memset(t_[:, :, 96:112], 1.0), [], [R_])
                for (t_, R_) in vaug_r.bufs:
                    op("dve", lambda e: e.memset(t_[:, :, 64:80], 1.0), [], [R_])
                kTst_r = Ring(aA, "kTst", [97, H, 128], BF16, 2)
                qTst_r = Ring(aA, "qTst", [96, H, 128], BF16, 2)
                cptok_r = Ring(aA, "cptok", [128, 512], F32, 2)
                sig_r = Ring(aA, "sig", [128, 256], F32, 2)
                rt_r = Ring(aA, "rt", [128, H, 2, DR], F32, 2)
                krr_r = Ring(aA, "krr", [128, 2, DR], F32, 2)
                nk_r = Ring(aA, "nk", [128, 16], F32, 2)

                def rope_apply(i, src_view, Rsrc, nh, t1, t2, Rt):
                    cosb = rope_sb[:, i, 0:1, :].to_broadcast([128, nh, DR])
                    op("dve", lambda e: e.tensor_tensor(out=t1[:, 0:nh, :], in0=src_view, in1=cosb, op=ALU.mult),
                       [Rsrc, R_rope], [Rt])
                    sv = src_view.rearrange("p h (a b c) -> p h a b c", a=2, b=2)
                    t2v = t2[:, 0:nh, :].rearrange("p h (a b c) -> p h a b c", a=2, b=2)
                    sn = rope_sb[:, i, 1, :].rearrange("p (a b c) -> p a b c", a=2, b=2)
                    for b_ in range(2):
                        snb = sn[:, :, b_, :].unsqueeze(1).to_broadcast([128, nh, 2, 8])
                        op("dve", lambda e: e.tensor_tensor(out=t2v[:, :, :, b_, :], in0=sv[:, :, :, 1 - b_, :], in1=snb, op=ALU.mult),
                           [Rsrc, R_rope], [Rt])
                    op("dve", lambda e: e.tensor_tensor(out=t1[:, 0:nh, :], in0=t1[:, 0:nh, :], in1=t2[:, 0:nh, :], op=ALU.add),
                       [Rt], [Rt])

                if stop_after == "Apre":
                    return finish(nc, S, [R_win, R_wuq, R_wukv, R_gq, R_gkv, R_rope] + R_sc1 + R_sh1)
                COLS = [(0, 384), (384, 672), (672, 1184), (1184, 1440)]
                def tileA(i):
                    isctx = i < NTC
                    typ = 1 if isctx else 0
                    full = not (last and isctx)
                    g0 = i * 128
                    src, Rsrc = x_src(l, i)
                    xt, Rxt = xt_r.get()
                    dma("sp", xt[:], src, Rsrc, [Rxt])
                    tmp, Rtmp = tmp_r.get()
                    hb, Rhb = hb_r.get()
                    op("pool", lambda e: e.tensor_tensor(out=tmp[:], in0=xt[:], in1=sc1[typ][:], op=ALU.mult), [Rxt, R_sc1[typ]], [Rtmp])
                    op("dve", lambda e: e.tensor_tensor(out=hb[:], in0=tmp[:], in1=sh1[typ][:], op=ALU.add), [Rtmp, R_sh1[typ]], [Rhb])
                    yield
                    pb, Rpb = PBK[0]
                    pbv = bfv(pb)
                    for k in range(8):
                        op("pe", lambda e: e.transpose(out=pbv[:, k * 128:(k + 1) * 128], in_=hb[:, k * 128:(k + 1) * 128], identity=ident_b[:]),
                           [Rhb, R_idb], [Rpb])
                    hT, RhT = hT_r.get()
                    op("act", lambda e: e.copy(out=hT[:].rearrange("p k t -> p (k t)"), in_=pbv[:, 0:1024]), [Rpb], [RhT])
                    cut("A1")
                    G = [PBK[2], PBK[3], PBK[4], PBK[5]]
                    for gi, (c0, c1) in enumerate(COLS):
                        if not full and gi != 1:
                            continue
                        gt, Rg = G[gi]
                        for k in range(8):
                            op("pe", lambda e: e.matmul(gt[:, 0:c1 - c0], lhsT=hT[:, k, :], rhs=w_in_sb[:, k, c0:c1], start=(k == 0), stop=(k == 7)),
                               [RhT, R_win], [Rg])
                    g1t, Rg1 = G[0]
                    g2t, Rg2 = G[1]
                    g3t, Rg3 = G[2]
                    g4t, Rg4 = G[3]
                    stt, Rstt = stat_r.get()
                    junk, Rjunk = junk_r.get()
                    cut("A2")
                    op("act", lambda e: e.activation(out=junk[:, 0:DKV], in_=g2t[:, 0:DKV], func=AF.Square, accum_out=stt[:, 0:1]),
                       [Rg2], [Rjunk, Rstt])
                    op("act", lambda e: e.activation(out=stt[:, 1:2], in_=stt[:, 0:1], func=AF.Ln, bias=eps_t[:, 1:2], scale=1.0 / DKV),
                       [Rstt], [Rstt])
                    op("act", lambda e: e.activation(out=stt[:, 1:2], in_=stt[:, 1:2], func=AF.Exp, scale=-0.5), [Rstt], [Rstt])
                    ckvn, Rckvn = ckvn_r.get()
                    op("dve", lambda e: e.scalar_tensor_tensor(out=ckvn[:], in0=g2t[:, 0:DKV], scalar=stt[:, 1:2], in1=gkv_bc[:],
                                                               op0=ALU.mult, op1=ALU.mult), [Rg2, Rstt, R_gkv], [Rckvn])
                    cut("A3")
                    krr, Rkrr = krr_r.get()
                    rt, Rrt = rt_r.get()
                    rope_apply(i, g2t[:, DKV:DKV + DR].unsqueeze(1), Rg2, 1, krr[:, 0:1, :], krr[:, 1:2, :], Rkrr)
                    cut("A4")
                    if full:
                        op("act", lambda e: e.activation(out=junk[:, 0:DQ], in_=g1t[:, 0:DQ], func=AF.Square, accum_out=stt[:, 2:3]),
                           [Rg1], [Rjunk, Rstt])
                        op("act", lambda e: e.activation(out=stt[:, 3:4], in_=stt[:, 2:3], func=AF.Ln, bias=eps_t[:, 1:2], scale=1.0 / DQ),
                           [Rstt], [Rstt])
                        op("act", lambda e: e.activation(out=stt[:, 3:4], in_=stt[:, 3:4], func=AF.Exp, scale=-0.5), [Rstt], [Rstt])
                        qn, Rqn = qn_r.get()
                        op("dve", lambda e: e.scalar_tensor_tensor(out=qn[:], in0=g1t[:, 0:DQ], scalar=stt[:, 3:4], in1=gq_bc[:],
                                                                   op0=ALU.mult, op1=ALU.mult), [Rg1, Rstt, R_gq], [Rqn])
                        sig, Rsig = sig_r.get()
                        cptok, Rcptok = cptok_r.get()
                        op("act", lambda e: e.activation(out=sig[:], in_=g3t[:, 256:512], func=AF.Exp, scale=-1.0), [Rg3], [Rsig])
                        op("act", lambda e: e.activation(out=sig[:], in_=sig[:], func=AF.Ln, bias=1.0, scale=1.0), [Rsig], [Rsig])
                        op("act", lambda e: e.activation(out=sig[:], in_=sig[:], func=AF.Exp, scale=-1.0), [Rsig], [Rsig])
                        op("dve", lambda e: e.tensor_tensor(out=cptok[:, 0:256], in0=g3t[:, 0:256], in1=sig[:], op=ALU.mult),
                           [Rg3, Rsig], [Rcptok])
                        op("act", lambda e: e.copy(out=cptok[:, 256:512], in_=g4t[:, 0:256]), [Rg4, Rcptok], [Rcptok])
                        pb1, Rpb1 = PBK[1]
                        pb1v = bfv(pb1)
                        for k in range(3):
                            op("pe", lambda e: e.transpose(out=pb1v[:, k * 128:(k + 1) * 128], in_=qn[:, k * 128:(k + 1) * 128], identity=ident_b[:]),
                               [Rqn, R_idb], [Rpb1])
                        qnT, RqnT = qnT_r.get()
                        op("act", lambda e: e.copy(out=qnT[:].rearrange("p k t -> p (k t)"), in_=pb1v[:, 0:384]), [Rpb1], [RqnT])
                    cut("A5")
                    pb0, Rpb0 = PBK[0]
                    pb0v = bfv(pb0)
                    for k in range(2):
                        op("pe", lambda e: e.transpose(out=pb0v[:, k * 128:(k + 1) * 128], in_=ckvn[:, k * 128:(k + 1) * 128], identity=ident_b[:]),
                           [Rckvn, R_idb], [Rpb0])
                    ckvT, RckvT = ckvT_r.get()
                    op("dve", lambda e: e.tensor_copy(out=ckvT[:].rearrange("p k t -> p (k t)"), in_=pb0v[:, 0:256]), [Rpb0], [RckvT])
                    yield
                    if full:
                        Q = [(PBK[6], 0, 5), (PBK[7], 5, 3)]
                        for ((qt, Rq), h0, nh) in Q:
                            for k in range(3):
                                op("pe", lambda e: e.matmul(qt[:, 0:nh * 96], lhsT=qnT[:, k, :], rhs=w_uq_sb[:, k, h0 * 96:(h0 + nh) * 96],
                                                            start=(k == 0), stop=(k == 2)), [RqnT, R_wuq], [Rq])
                    cut("A6")
                    KV = [(PBK[2], 0), (PBK[4], 4)]
                    for ((kt, Rk), h0) in KV:
                        for k in range(2):
                            op("pe", lambda e: e.matmul(kt[:, 0:512], lhsT=ckvT[:, k, :], rhs=w_ukv_sb[:, k, h0 * 128:(h0 + 4) * 128],
                                                        start=(k == 0), stop=(k == 1)), [RckvT, R_wukv], [Rk])
                    if full:
                        pb5, Rpb5 = PBK[5]
                        for k in range(4):
                            op("pe", lambda e: e.transpose(out=pb5[:, k * 128:(k + 1) * 128], in_=cptok[:, k * 128:(k + 1) * 128], identity=ident_f[:]),
                               [Rcptok, R_idf], [Rpb5])
                        cpd, Rcpd, off = (cpT_c, R_cpc, g0) if isctx else (cpT_l, R_cpl, g0 - C)
                        op("act", lambda e: e.copy(out=cpd[:, :, 16 + off:16 + off + 128], in_=pb5[:].rearrange("p (k t) -> p k t", k=4)),
                           [Rpb5], [Rcpd])
                        qaug, Rqaug = qaug_r.get()
                        nk, Rnk = nk_r.get()
                        for ((qt, Rq), h0, nh) in Q:
                            qv = qt[:, 0:nh * 96].rearrange("p (h d) -> p h d", d=96)
                            op("act", lambda e: e.copy(out=qaug[:, h0:h0 + nh, 0:64], in_=qv[:, :, 0:64]), [Rq], [Rqaug])
                            rope_apply(i, qv[:, :, 64:96], Rq, nh, rt[:, h0:h0 + nh, 0, :], rt[:, h0:h0 + nh, 1, :], Rrt)
                            op("dve", lambda e: e.tensor_copy(out=qaug[:, h0:h0 + nh, 64:96], in_=rt[:, h0:h0 + nh, 0, :]), [Rrt], [Rqaug])
                            jv = junk[:, 0:nh * 96].rearrange("p (h d) -> p h d", d=96)
                            op("act", lambda e: e.activation(out=jv, in_=qv, func=AF.Square), [Rq], [Rjunk])
                            op("dve", lambda e: e.tensor_reduce(out=nq_all[:, i, h0:h0 + nh], in_=jv, axis=AX.X, op=ALU.add), [Rjunk], [R_nq])
                    else:
                        nk, Rnk = nk_r.get()
                    cut("A7")
                    kaug, Rkaug = kaug_r.get()
                    vaug, Rvaug = vaug_r.get()
                    for ((kt, Rk), h0) in KV:
                        kv = kt[:, 0:512].rearrange("p (h d) -> p h d", d=128)
                        cut("KV0")
                        op("act", lambda e: e.copy(out=kaug[:, h0:h0 + 4, 0:64], in_=kv[:, :, 0:64]), [Rk], [Rkaug])
                        cut("KV1")
                        op("dve", lambda e: e.tensor_copy(out=vaug[:, h0:h0 + 4, 0:64], in_=kv[:, :, 64:128]), [Rk], [Rvaug])
                        cut("KV2")
                        jv = junk[:, 0:256].rearrange("p (h d) -> p h d", d=64)
                        op("act", lambda e: e.activation(out=jv, in_=kv[:, :, 0:64], func=AF.Square), [Rk], [Rjunk])
                        cut("KV3")
                        op("dve", lambda e: e.tensor_reduce(out=nk[:, h0:h0 + 4], in_=jv, axis=AX.X, op=ALU.add), [Rjunk], [Rnk])
                        cut("KV4")
                    cut("K1")
                    op("dve", lambda e: e.tensor_copy(out=kaug[:, :, 64:96], in_=krr[:, 0:1, :].to_broadcast([128, H, DR])), [Rkrr], [Rkaug])
                    cut("K2")
                    op("dve", lambda e: e.tensor_tensor(out=krr[:, 1, :], in0=krr[:, 0, :], in1=krr[:, 0, :], op=ALU.mult), [Rkrr], [Rkrr])
                    op("dve", lambda e: e.tensor_reduce(out=nk[:, 8:9], in_=krr[:, 1, :], axis=AX.X, op=ALU.add), [Rkrr], [Rnk])
                    cut("K3")
                    op("dve", lambda e: e.tensor_scalar(out=nk[:, 0:8], in0=nk[:, 0:8], scalar1=nk[:, 8:9], scalar2=None, op0=ALU.add), [Rnk], [Rnk])
                    cut("K4")
                    op("dve", lambda e: e.tensor_tensor(out=kmax2[:], in0=kmax2[:], in1=nk[:, 0:8], op=ALU.max), [Rnk, R_kmax2], [R_kmax2])
                    cut("A7b")
                    yield
                    if full:
                        pb0, Rpb0 = PBK[0]
                        pb0v = bfv(pb0)
                        for h in range(H):
                            op("pe", lambda e: e.transpose(out=pb0v[0:96, h * 128:(h + 1) * 128], in_=qaug[:, h, :], identity=ident_b[:]),
                               [Rqaug, R_idb], [Rpb0])
                        qTst, RqTst = qTst_r.get()
                        op("act", lambda e: e.copy(out=qTst[:].rearrange("p h t -> p (h t)"), in_=pb0v[0:96, 0:1024]), [Rpb0], [RqTst])
                        dma("sp", qT_scr[:, :, g0:g0 + 128].rearrange("h d t -> d h t"), qTst[:], [RqTst], [R_qT])
                    pb1, Rpb1 = PBK[1]
                    pb1v = bfv(pb1)
                    for h in range(H):
                        op("pe", lambda e: e.transpose(out=pb1v[0:97, h * 128:(h + 1) * 128], in_=kaug[:, h, 0:97], identity=ident_b[:]),
                           [Rkaug, R_idb], [Rpb1])
                    kTst, RkTst = kTst_r.get()
                    op("dve", lambda e: e.tensor_copy(out=kTst[:].rearrange("p h t -> p (h t)"), in_=pb1v[0:97, 0:1024]), [Rpb1], [RkTst])
                    dma("sp", kT_scr[:, :, g0:g0 + 128].rearrange("h d t -> d h t"), kTst[:], [RkTst], [R_kT])
                    dma("sp", v_scr[:, :, i, :].rearrange("h p d -> p h d"), vaug[:], [Rvaug], [R_v])
                    cut("AT%d" % i)

                run_skewed([tileA(i) for i in range(NTT)])
                cut("A8")
                pb, Rpb = PBK[0]
                op("pe", lambda e: e.transpose(out=pb[0:8, 0:128], in_=kmax2[:, 0:8], identity=ident_f[:]), [R_kmax2, R_idf], [Rpb])
                km, Rkm = aA("km", [8, 16]), Res("km")
                op("dve", lambda e: e.tensor_reduce(out=km[:, 0:1], in_=pb[0:8, 0:128], axis=AX.X, op=ALU.max), [Rpb], [Rkm])
                op("act", lambda e: e.activation(out=km[:, 1:2], in_=km[:, 0:1], func=AF.Sqrt, scale=1.0404), [Rkm], [Rkm])
                op("dve", lambda e: e.tensor_scalar(out=km[:, 8:16], in0=ident_f[0:8, 0:8], scalar1=km[:, 1:2], scalar2=-1.0,
                                                    op0=ALU.mult, op1=ALU.mult), [Rkm, R_idf], [Rkm])
                ones8, Rones8 = aA("ones8", [8, 128]), Res("ones8")
                op("dve", lambda e: e.memset(ones8[:], 1.0), [], [Rones8])
                pb, Rpb = PBK[1]
                op("pe", lambda e: e.matmul(pb[:, 0:8], lhsT=ones8[:], rhs=km[:, 8:16], start=True, stop=True), [Rones8, Rkm], [Rpb])
                kmbc, Rkmbc = aA("kmbc", [128, 8]), Res("kmbc")
                op("dve", lambda e: e.tensor_copy(out=kmbc[:], in_=pb[:, 0:8]), [Rpb], [Rkmbc])
                op("act", lambda e: e.activation(out=nq_all[:], in_=nq_all[:], func=AF.Sqrt), [R_nq], [R_nq])
                op("dve", lambda e: e.tensor_tensor(out=nq_all[:], in0=nq_all[:], in1=kmbc[:].unsqueeze(1).to_broadcast([128, NTT, H]), op=ALU.mult),
                   [R_nq, Rkmbc], [R_nq])
                mT_sb, RmT = aA("mT_sb", [8, TT], BF16), Res("mT_sb")
                for i0 in range(0, NTT, 4):
                    pb, Rpb = PBK[(i0 // 4) % 2]
                    ni = min(4, NTT - i0)
                    for j in range(ni):
                        op("pe", lambda e: e.transpose(out=pb[0:8, j * 128:(j + 1) * 128], in_=nq_all[:, i0 + j, :], identity=ident_f[:]),
                           [R_nq, R_idf], [Rpb])
                    op("dve", lambda e: e.tensor_copy(out=mT_sb[:, i0 * 128:(i0 + ni) * 128], in_=pb[0:8, 0:ni * 128]), [Rpb], [RmT])
                dma("sp", mT_scr, mT_sb[:], [RmT], [R_mT])
                S.barrier()
                S.release(mk_stA)
            if stop_after == "A":
                return finish(nc, S, [R_kT, R_qT, R_mT, R_v])

            with ExitStack() as stC:
                mk_stC = S.mark()
                def aC(name, shape, dt=F32):
                    return stC.enter_context(nc.sbuf_tensor("%s_L%d" % (name, l), shape, dt))
                cwr, Rcwr = aC("cwr", [CONVW, 256]), Res("cwr")
                cw_sb, Rcw = aC("cw_sb", [128, 2, CONVW]), Res("cw_sb")
                dma("sp", cwr[:], conv_w[l], [], [Rcwr])
                pb, Rpb = PBK[0]
                for k in range(2):
                    op("pe", lambda e: e.transpose(out=pb[:, k * 32:k * 32 + CONVW], in_=cwr[0:CONVW, k * 128:(k + 1) * 128],
                                                   identity=ident_f[0:CONVW, 0:CONVW]), [Rcwr, R_idf], [Rpb])
                op("dve", lambda e: e.tensor_copy(out=cw_sb[:], in_=pb[:, 0:64].rearrange("p (k j) -> p k j", k=2)[:, :, 0:CONVW]), [Rpb], [Rcw])
                convb_bc, Rcb = aC("convb_bc", [128, 256]), Res("convb_bc")
                clng_bc, Rclg = aC("clng_bc", [128, 256]), Res("clng_bc")
                clnb_bc, Rclb = aC("clnb_bc", [128, 256]), Res("clnb_bc")
                psc_bc, Rpsc = aC("psc_bc", [128, 256]), Res("psc_bc")
                load_bc(convb_bc, Rcb, conv_b[l], 256)
                load_bc(clng_bc, Rclg, conv_ln_g[l], 256)
                load_bc(clnb_bc, Rclb, conv_ln_b[l], 256)
                load_bc(psc_bc, Rpsc, pool_scale[l], 256)
                poolw_sb, Rpw = aC("poolw_sb", [128, 2, 128], BF16), Res("poolw_sb")
                op("dve", lambda e: e.memset(poolw_sb[:], 0.0), [], [Rpw])
                for ph in range(2):
                    dma("pool", poolw_sb[ph * 64:(ph + 1) * 64, :, ph * 64:(ph + 1) * 64],
                        pool_w[l].rearrange("(k two) i o -> two i k o", two=2)[ph], [], [Rpw])
                acc_t = aC("acc", [128, 2, SEG])
                R_acc = [Res("acc0"), Res("acc1")]
                P2, RP2 = aC("P2", [128, 2, SEG + 16]), Res("P2")
                P4, RP4 = aC("P4", [128, 2, SEG + 16]), Res("P4")
                P8, RP8 = aC("P8", [128, SEG + 16]), Res("P8")
                P16, RP16 = aC("P16", [128, SEG + 16]), Res("P16")
                mixed, Rmixed = aC("mixed", [128, 2, SEG], BF16), Res("mixed")
                etmp, Retmp = aC("etmp", [128, 2, 8]), Res("etmp")
                ctmp_r = Ring(aC, "ctmp", [128, SEG], F32, 3)
                cv_r = Ring(aC, "cv", [128, 256], F32, 2)
                sgc_r = Ring(aC, "sgc", [128, 256], F32, 2)
                catcp_r = Ring(aC, "catcp", [128, 512], BF16, 2)
                lnr = {"st": Ring(aC, "cst", [128, 12], F32, 2), "mv": Ring(aC, "cmv", [128, 4], F32, 2)}
                cut("C1")
                seqs = [(cpT_l, R_cpl, T, C)]
                if not last:
                    seqs.append((cpT_c, R_cpc, C, 0))
                tile_ctr = 0
                for (buf, Rbuf, n, goff) in seqs:
                    for s0 in range(0, n, SEG):
                        seg = min(SEG, n - s0)
                        b0 = 16 + s0
                        for j in range(CONVW):
                            if j == 0:
                                op("dve", lambda e: e.tensor_scalar(out=acc_t[:, 0, 0:seg], in0=buf[:, 0, b0 - 15:b0 - 15 + seg], scalar1=cw_sb[:, 0, 0:1],
                                                                    scalar2=None, op0=ALU.mult), [Rbuf, Rcw], [R_acc[0]])
                                op("act", lambda e: e.activation(out=acc_t[:, 1, 0:seg], in_=buf[:, 1, b0 - 15:b0 - 15 + seg], func=AF.Copy,
                                                                 scale=cw_sb[:, 1, 0:1]), [Rbuf, Rcw], [R_acc[1]])
                                continue
                            op("dve", lambda e: e.scalar_tensor_tensor(out=acc_t[:, 0, 0:seg], in0=buf[:, 0, b0 - 15 + j:b0 - 15 + j + seg],
                                                                       scalar=cw_sb[:, 0, j:j + 1], in1=acc_t[:, 0, 0:seg],
                                                                       op0=ALU.mult, op1=ALU.add), [Rbuf, Rcw, R_acc[0]], [R_acc[0]])
                            ct, Rct = ctmp_r.get()
                            op("act", lambda e: e.activation(out=ct[:, 0:seg], in_=buf[:, 1, b0 - 15 + j:b0 - 15 + j + seg], func=AF.Copy,
                                                             scale=cw_sb[:, 1, j:j + 1]), [Rbuf, Rcw], [Rct])
                            op("pool", lambda e: e.tensor_tensor(out=acc_t[:, 1, 0:seg], in0=acc_t[:, 1, 0:seg], in1=ct[:, 0:seg], op=ALU.add),
                               [Rct, R_acc[1]], [R_acc[1]])
                        cut("C2")
                        n2 = seg + 16
                        op("dve", lambda e: e.tensor_tensor(out=P2[:, :, 0:n2], in0=buf[:, 2:4, b0 - 9:b0 - 9 + n2], in1=buf[:, 2:4, b0 - 8:b0 - 8 + n2],
                                                            op=ALU.add), [Rbuf], [RP2])
                        op("dve", lambda e: e.tensor_tensor(out=P4[:, :, 2:n2 - 2], in0=P2[:, :, 1:n2 - 3], in1=P2[:, :, 3:n2 - 1], op=ALU.add),
                           [RP2], [RP4])
                        op("dve", lambda e: e.tensor_tensor(out=P8[:, 4:n2 - 4], in0=P4[:, 1, 2:n2 - 6], in1=P4[:, 1, 6:n2 - 2], op=ALU.add),
                           [RP4], [RP8])
                        op("dve", lambda e: e.tensor_tensor(out=P16[:, 8:n2 - 8], in0=P8[:, 4:n2 - 12], in1=P8[:, 12:n2 - 4], op=ALU.add),
                           [RP8], [RP16])
                        srcs = {(0, 0): (P2[0:64, 0, 8:8 + seg], RP2), (1, 0): (P4[64:128, 0, 8:8 + seg], RP4),
                                (0, 1): (P8[0:64, 8:8 + seg], RP8), (1, 1): (P16[64:128, 8:8 + seg], RP16)}
                        for (ph, k), (sap, Rs) in srcs.items():
                            ps = slice(ph * 64, ph * 64 + 64)
                            op("dve", lambda e: e.scalar_tensor_tensor(out=mixed[ps, k, 0:seg], in0=sap, scalar=pinvw[ps, k:k + 1],
                                                                       in1=buf[ps, 2 + k, b0:b0 + seg], op0=ALU.mult, op1=ALU.subtract),
                               [Rs, R_pinvw, Rbuf], [Rmixed])
                            for (side, cond, c0) in ((0, s0 == 0, 0), (1, s0 + seg == n, seg - 8)):
                                if not cond:
                                    continue
                                sap8 = sap[:, c0:c0 + 8]
                                op("dve", lambda e: e.tensor_tensor(out=etmp[ps, k, :], in0=sap8, in1=pedge[ps, k, side, :], op=ALU.mult),
                                   [Rs, R_pedge], [Retmp])
                                op("dve", lambda e: e.tensor_tensor(out=mixed[ps, k, c0:c0 + 8], in0=etmp[ps, k, :], in1=buf[ps, 2 + k, b0 + c0:b0 + c0 + 8],
                                                                    op=ALU.subtract), [Retmp, Rbuf], [Rmixed])
                        cut("C3")
                        for j in range(seg // 128):
                            g0 = goff + s0 + j * 128
                            pb, Rpb = PBK[tile_ctr % 2]
                            pb2, Rpb2 = PBK[2 + tile_ctr % 2]
                            tile_ctr += 1
                            for k in range(2):
                                op("pe", lambda e: e.transpose(out=pb[:, k * 128:(k + 1) * 128], in_=acc_t[:, k, j * 128:(j + 1) * 128], identity=ident_f[:]),
                                   [R_acc[k], R_idf], [Rpb])
                            cv, Rcv = cv_r.get()
                            op("dve", lambda e: e.tensor_tensor(out=cv[:], in0=pb[:, 0:256], in1=convb_bc[:], op=ALU.add), [Rpb, Rcb], [Rcv])
                            layer_norm_tile(None, "pool", cv, Rcv, 256, clng_bc, Rclg, clnb_bc, Rclb, cv, Rcv, lnr)
                            catcp, Rcatcp = catcp_r.get()
                            sgc, Rsgc = sgc_r.get()
                            op("act", lambda e: e.activation(out=sgc[:], in_=cv[:], func=AF.Exp, scale=-1.0), [Rcv], [Rsgc])
                            op("act", lambda e: e.activation(out=sgc[:], in_=sgc[:], func=AF.Ln, bias=1.0, scale=1.0), [Rsgc], [Rsgc])
                            op("act", lambda e: e.activation(out=sgc[:], in_=sgc[:], func=AF.Exp, scale=-1.0), [Rsgc], [Rsgc])
                            op("dve", lambda e: e.tensor_tensor(out=catcp[:, 0:256], in0=cv[:], in1=sgc[:], op=ALU.mult), [Rcv, Rsgc], [Rcatcp])
                            cut("C3b")
                            for k in range(2):
                                op("pe", lambda e: e.matmul(pb2[:, k * 128:(k + 1) * 128], lhsT=mixed[:, k, j * 128:(j + 1) * 128],
                                                            rhs=poolw_sb[:, k, :], start=True, stop=True), [Rmixed, Rpw], [Rpb2])
                            cut("C3c")
                            op("dve", lambda e: e.tensor_tensor(out=catcp[:, 256:512], in0=pb2[:, 0:256], in1=psc_bc[:], op=ALU.mult),
                               [Rpb2, Rpsc, Rcatcp], [Rcatcp])
                            cut("C3d")
                            dma("sp", catcp_scr[g0:g0 + 128, :], catcp[:], [Rcatcp], [R_catcp])
                            cut("C4")
                S.barrier()
                S.release(mk_stC)
        if stop_after == "C":
            return finish(nc, S, [R_catcp])

        with ExitStack() as stBD:
            def aBD(name, shape, dt=F32):
                return stBD.enter_context(nc.sbuf_tensor("%s_L%d" % (name, l), shape, dt))
            attn_sb = aBD("attn_sb", [128, NTT, 512], BF16)
            R_attn = [Res("attn%d" % i) for i in range(NTT)]
            with ExitStack() as stB:
                mk_stB = S.mark()
                def aB(name, shape, dt=F32):
                    return stB.enter_context(nc.sbuf_tensor("%s_L%d" % (name, l), shape, dt))
                NJ = NP // 128
                dma("act", h2perm_scr.rearrange("(j p) d -> p j d", p=128), zer_b[:].unsqueeze(1).to_broadcast([128, NJ, D]), [R_zerb], [R_h2pz])
                dma("act", c8perm_scr.rearrange("(j p) e -> p j e", p=128), zer_f[:].unsqueeze(1).to_broadcast([128, NJ, 8]), [R_zerf], [R_c8pz])
                KT_r = Ring(aB, "KT", [97, TT], BF16, 2)
                V_r = Ring(aB, "V", [128, NTT, 80], BF16, 2)
                qT_r = Ring(aB, "qT", [97, 512], BF16, 3)
                PT_r = Ring(aB, "PT", [128, 512], BF16, 4)
                oT_r = Ring(aB, "oT", [65, 512], F32, 2)
                rec_r = Ring(aB, "rec", [128, 4], F32, 2)
                blocks = [(C + b * 512, 512, list(range(NTT))) for b in range(T // 512)]
                if not last:
                    blocks.append((0, C, list(range(NTC))))
                bi = 0
                si = 0
                for h in range(H):
                    KT, RKT = KT_r.get()
                    V, RV = V_r.get()
                    dma("sp", KT[:], kT_scr[h], [R_kT], [RKT])
                    dma("sp", V[:], v_scr[h], [R_v], [RV])
                    for (g0, n, chunks) in blocks:
                        qT, RqT = qT_r.get()
                        dma("sp", qT[0:96, 0:n], qT_scr[h, :, g0:g0 + n], [R_qT], [RqT])
                        dma("sp", qT[96:97, 0:n], mT_scr[h:h + 1, g0:g0 + n], [R_mT], [RqT])
                        pO, RpO = PBK[bi % 2]
                        LOOK = 3
                        pend = []

                        def issue_s(c):
                            nonlocal si
                            pS_, RpS_ = PBK[2 + si % 4]
                            si += 1
                            op("pe", lambda e: e.matmul(pS_[:, 0:n], lhsT=KT[:, c * 128:(c + 1) * 128], rhs=qT[:, 0:n], start=True, stop=True),
                               [RKT, RqT], [RpS_])
                            pend.append((pS_, RpS_))
                        for c in chunks[:LOOK]:
                            issue_s(c)
                        for ci, c in enumerate(chunks):
                            if ci + LOOK < len(chunks):
                                issue_s(chunks[ci + LOOK])
                            pS, RpS = pend.pop(0)
                            PT, RPT = PT_r.get()
                            op("act", lambda e: e.activation(out=PT[:, 0:n], in_=pS[:, 0:n], func=AF.Exp, scale=QS), [RpS], [RPT])
                            op("pe", lambda e: e.matmul(pO[0:65, 0:n], lhsT=V[:, c, 0:65], rhs=PT[:, 0:n], start=(ci == 0), stop=(ci == len(chunks) - 1)),
                               [RV, RPT], [RpO])
                        oT, RoT = oT_r.get()
                        op("dve", lambda e: e.tensor_copy(out=oT[:, 0:n], in_=pO[0:65, 0:n]), [RpO], [RoT])
                        pb, Rpb = PBK[6 + bi % 2]
                        bi += 1
                        nj = n // 128
                        for j in range(nj):
                            op("pe", lambda e: e.transpose(out=pb[:, j * 65:(j + 1) * 65], in_=oT[0:65, j * 128:(j + 1) * 128], identity=ident_f[0:65, 0:65]),
                               [RoT, R_idf], [Rpb])
                        pv = pb[:, 0:nj * 65].rearrange("p (j d) -> p j d", d=65)
                        rec, Rrec = rec_r.get()
                        op("dve", lambda e: e.reciprocal(out=rec[:, 0:nj], in_=pv[:, :, 64]), [Rpb], [Rrec])
                        i0 = g0 // 128
                        Rs_ = R_attn[i0:i0 + nj]
                        op("dve", lambda e: e.tensor_tensor(out=attn_sb[:, i0:i0 + nj, h * 64:(h + 1) * 64], in0=pv[:, :, 0:64],
                                                            in1=rec[:, 0:nj].unsqueeze(2).to_broadcast([128, nj, 64]), op=ALU.mult),
                           [Rpb, Rrec] + Rs_, Rs_)
                S.barrier()
                S.release(mk_stB)
            if stop_after == "B":
                dbg = nc.dram_tensor("attn_dbg", [128, NTT, 512], BF16, kind="ExternalOutput").ap()
                Rd = Res("attn_dbg")
                dma("sp", dbg, attn_sb[:], R_attn, [Rd])
                return finish(nc, S, [Rd])

            with ExitStack() as stD:
                mk_stD = S.mark()
                def aD(name, shape, dt=F32):
                    return stD.enter_context(nc.sbuf_tensor("%s_L%d" % (name, l), shape, dt))
                w_out_sb, R_wout = aD("w_out_sb", [128, 8, D], BF16), Res("w_out_sb")
                dma("pool", w_out_sb[:], w_out[l].rearrange("(k p) n -> p k n", p=128), [], [R_wout])
                wr_sb, R_wr = aD("wr_sb", [128, 8, 36]), Res("wr_sb")
                dma("sp", wr_sb[:, :, 0:4], w_rg[l].rearrange("(k p) n -> p k n", p=128), [], [R_wr])
                dma("sp", wr_sb[:, :, 4:36], w_re[l].rearrange("(k p) n -> p k n", p=128), [], [R_wr])
                br_bc, R_br = aD("br_bc", [128, 36]), Res("br_bc")
                dma("sp", br_bc[:, 0:4], b_rg[l].partition_broadcast(128), [], [R_br])
                dma("sp", br_bc[:, 4:36], b_re[l].partition_broadcast(128), [], [R_br])
                bcs = {}
                for (nm, j) in (("g1", 2), ("sh2", 3), ("sc2", 4)):
                    for r in range(2):
                        if r == 1 and last:
                            continue
                        t = aD("%s_%d" % (nm, r), [128, D])
                        Rr = Res("%s_%d" % (nm, r))
                        load_bc(t, Rr, ada_vec(l, r, j), D, [R_ada])
                        bcs[(nm, r)] = (t, Rr)
                ln1g_bc, R_l1g = aD("ln1g_bc", [128, D]), Res("ln1g_bc")
                ln1b_bc, R_l1b = aD("ln1b_bc", [128, D]), Res("ln1b_bc")
                load_bc(ln1g_bc, R_l1g, ln1_g[l], D)
                load_bc(ln1b_bc, R_l1b, ln1_b[l], D)
                catcp_r = Ring(aD, "catcpD", [128, 512], BF16, 2)
                catT_r = Ring(aD, "catT", [128, 8, 128], BF16, 2)
                xt_r = Ring(aD, "xtD", [128, D], F32, 3)
                y_r = Ring(aD, "yD", [128, D], F32, 2)
                x1_r = Ring(aD, "x1D", [128, D], F32, 2)
                h2_r = Ring(aD, "h2D", [128, D], F32, 2)
                h2Tf_r = Ring(aD, "h2Tf", [128, 8, 128], F32, 2)
                h2Tb_r = Ring(aD, "h2b", [128, D], BF16, 2)
                lg_r = Ring(aD, "lg", [128, 36], F32, 2)
                rs_r = Ring(aD, "rs", [128, 16], F32, 2)
                oh_r = Ring(aD, "oh", [128, 3, 32], F32, 2)
                comb_r = Ring(aD, "comb", [128, 32], F32, 2)
                combT_r = Ring(aD, "combT", [32, 128], F32, 2)
                lnr = {"st": Ring(aD, "dst", [128, 12], F32, 2), "mv": Ring(aD, "dmv", [128, 4], F32, 2)}
                cut("D1")
                def tileD(i, tcnt):
                    isctx = i < NTC
                    typ = 1 if isctx else 0
                    g0 = i * 128
                    catcp, Rcatcp = catcp_r.get()
                    dma("sp", catcp[:], catcp_scr[g0:g0 + 128, :], [R_catcp], [Rcatcp])
                    src, Rsrc = x_src(l, i)
                    xt, Rxt = xt_r.get()
                    dma("sp", xt[:], src, Rsrc, [Rxt])
                    yield
                    pb, Rpb = PBK[tcnt % 2]
                    pbv = bfv(pb)
                    for k in range(4):
                        op("pe", lambda e: e.transpose(out=pbv[:, k * 128:(k + 1) * 128], in_=attn_sb[:, i, k * 128:(k + 1) * 128], identity=ident_b[:]),
                           [R_attn[i], R_idb], [Rpb])
                    for k in range(4):
                        op("pe", lambda e: e.transpose(out=pbv[:, (4 + k) * 128:(5 + k) * 128], in_=catcp[:, k * 128:(k + 1) * 128], identity=ident_b[:]),
                           [Rcatcp, R_idb], [Rpb])
                    catT, RcatT = catT_r.get()
                    op("act", lambda e: e.copy(out=catT[:].rearrange("p k t -> p (k t)"), in_=pbv[:, 0:1024]), [Rpb], [RcatT])
                    cut("D2")
                    M = [PBK[2 + 2 * (tcnt % 2)], PBK[3 + 2 * (tcnt % 2)]]
                    for hf in range(2):
                        mt, Rm = M[hf]
                        for k in range(8):
                            op("pe", lambda e: e.matmul(mt[:, :], lhsT=catT[:, k, :], rhs=w_out_sb[:, k, hf * 512:(hf + 1) * 512], start=(k == 0), stop=(k == 7)),
                               [RcatT, R_wout], [Rm])
                    cut("D3")
                    yield
                    y, Ry = y_r.get()
                    g1t, Rg1 = bcs[("g1", typ)]
                    for hf in range(2):
                        mt, Rm = M[hf]
                        op("dve", lambda e: e.tensor_tensor(out=y[:, hf * 512:(hf + 1) * 512], in0=mt[:, :], in1=g1t[:, hf * 512:(hf + 1) * 512], op=ALU.mult),
                           [Rm, Rg1], [Ry])
                    op("dve", lambda e: e.scalar_tensor_tensor(out=y[:], in0=xt[:], scalar=ALPHA, in1=y[:], op0=ALU.mult, op1=ALU.add),
                       [Rxt, Ry], [Ry])
                    x1, Rx1 = x1_r.get()
                    layer_norm_tile(None, "pool", y, Ry, D, ln1g_bc, R_l1g, ln1b_bc, R_l1b, x1, Rx1, lnr)
                    dma("sp", xs_mix[g0:g0 + 128, :], x1[:], [Rx1], [R_xs_mix])
                    cut("D4")
                    h2, Rh2 = h2_r.get()
                    sc2t, Rsc2 = bcs[("sc2", typ)]
                    sh2t, Rsh2 = bcs[("sh2", typ)]
                    op("pool", lambda e: e.tensor_tensor(out=h2[:], in0=x1[:], in1=sc2t[:], op=ALU.mult), [Rx1, Rsc2], [Rh2])
                    op("dve", lambda e: e.tensor_tensor(out=h2[:], in0=h2[:], in1=sh2t[:], op=ALU.add), [Rh2, Rsh2], [Rh2])
                    yield
                    T6, RT6 = PBK[6]
                    T7, RT7 = PBK[7]
                    for k in range(8):
                        tb, Rtb = (T6, RT6) if k < 4 else (T7, RT7)
                        op("pe", lambda e: e.transpose(out=tb[:, (k % 4) * 128:(k % 4 + 1) * 128], in_=h2[:, k * 128:(k + 1) * 128], identity=ident_f[:]),
                           [Rh2, R_idf], [Rtb])
                    h2Tf, Rh2Tf = h2Tf_r.get()
                    h2b, Rh2b = h2Tb_r.get()
                    op("pool", lambda e: e.tensor_copy(out=h2b[:], in_=h2[:]), [Rh2], [Rh2b])
                    dma("sp", h2tok_scr[g0:g0 + 128, :], h2b[:], [Rh2b], [R_h2tok])
                    for hf, (tb, Rtb) in enumerate(((T6, RT6), (T7, RT7))):
                        op("act", lambda e: e.copy(out=h2Tf[:, hf * 4:hf * 4 + 4, :].rearrange("p k t -> p (k t)"), in_=tb[:, :]), [Rtb], [Rh2Tf])
                    cut("D5")
                    pr, Rpr = PBK[tcnt % 2]
                    for k in range(8):
                        op("pe", lambda e: e.matmul(pr[:, 0:36], lhsT=h2Tf[:, k, :], rhs=wr_sb[:, k, :], start=(k == 0), stop=(k == 7)),
                           [Rh2Tf, R_wr], [Rpr])
                    lg, Rlg = lg_r.get()
                    rs, Rrs = rs_r.get()
                    oh, Roh = oh_r.get()
                    op("dve", lambda e: e.tensor_tensor(out=lg[:], in0=pr[:, 0:36], in1=br_bc[:], op=ALU.add), [Rpr, R_br], [Rlg])
                    cut("D6")
                    yield
                    op("dve", lambda e: e.tensor_reduce(out=rs[:, 0:1], in_=lg[:, 0:4], axis=AX.X, op=ALU.max), [Rlg], [Rrs])
                    op("dve", lambda e: e.tensor_scalar(out=rs[:, 8:12], in0=lg[:, 0:4], scalar1=rs[:, 0:1], scalar2=None, op0=ALU.is_equal), [Rlg, Rrs], [Rrs])
                    op("dve", lambda e: e.tensor_copy(out=goh_all[:, i, :], in_=rs[:, 8:12]), [Rrs], [R_goh])
                    op("dve", lambda e: e.tensor_scalar(out=rs[:, 1:2], in0=rs[:, 0:1], scalar1=-1.0, scalar2=None, op0=ALU.mult), [Rrs], [Rrs])
                    op("act", lambda e: e.activation(out=rs[:, 12:16], in_=lg[:, 0:4], func=AF.Exp, bias=rs[:, 1:2], scale=1.0, accum_out=rs[:, 2:3]),
                       [Rlg, Rrs], [Rrs])
                    op("dve", lambda e: e.reciprocal(out=rs[:, 2:3], in_=rs[:, 2:3]), [Rrs], [Rrs])
                    op("dve", lambda e: e.tensor_scalar(out=rs[:, 8:12], in0=rs[:, 8:12], scalar1=-1.0, scalar2=-NEG, op0=ALU.add, op1=ALU.mult), [Rrs], [Rrs])
                    elm = oh[:, 0, :]
                    op("dve", lambda e: e.tensor_tensor(out=elm.rearrange("p (g x) -> p g x", g=4), in0=lg[:, 4:36].rearrange("p (g x) -> p g x", g=4),
                                                        in1=rs[:, 8:12].unsqueeze(2).to_broadcast([128, 4, 8]), op=ALU.add), [Rlg, Rrs], [Roh])
                    op("dve", lambda e: e.tensor_reduce(out=rs[:, 3:4], in_=elm, axis=AX.X, op=ALU.max), [Roh], [Rrs])
                    op("dve", lambda e: e.tensor_scalar(out=oh[:, 1, :], in0=elm, scalar1=rs[:, 3:4], scalar2=None, op0=ALU.is_equal), [Roh, Rrs], [Roh])
                    op("dve", lambda e: e.scalar_tensor_tensor(out=elm, in0=oh[:, 1, :], scalar=NEG, in1=elm, op0=ALU.mult, op1=ALU.add), [Roh], [Roh])
                    op("dve", lambda e: e.tensor_reduce(out=rs[:, 4:5], in_=elm, axis=AX.X, op=ALU.max), [Roh], [Rrs])
                    op("dve", lambda e: e.tensor_scalar(out=oh[:, 2, :], in0=elm, scalar1=rs[:, 4:5], scalar2=None, op0=ALU.is_equal), [Roh, Rrs], [Roh])
                    op("dve", lambda e: e.tensor_tensor(out=rs[:, 5:6], in0=rs[:, 4:5], in1=rs[:, 3:4], op=ALU.subtract), [Rrs], [Rrs])
                    op("act", lambda e: e.activation(out=rs[:, 5:6], in_=rs[:, 5:6], func=AF.Exp), [Rrs], [Rrs])
                    op("dve", lambda e: e.tensor_scalar(out=rs[:, 6:7], in0=rs[:, 5:6], scalar1=1.0, scalar2=None, op0=ALU.add), [Rrs], [Rrs])
                    op("dve", lambda e: e.reciprocal(out=rs[:, 6:7], in_=rs[:, 6:7]), [Rrs], [Rrs])
                    op("dve", lambda e: e.tensor_tensor(out=rs[:, 6:7], in0=rs[:, 6:7], in1=rs[:, 2:3], op=ALU.mult), [Rrs], [Rrs])
                    op("dve", lambda e: e.tensor_tensor(out=rs[:, 7:8], in0=rs[:, 6:7], in1=rs[:, 5:6], op=ALU.mult), [Rrs], [Rrs])
                    cut("D7")
                    comb, Rcomb = comb_r.get()
                    op("dve", lambda e: e.tensor_scalar(out=comb[:], in0=oh[:, 1, :], scalar1=rs[:, 6:7], scalar2=None, op0=ALU.mult), [Roh, Rrs], [Rcomb])
                    op("dve", lambda e: e.scalar_tensor_tensor(out=comb[:], in0=oh[:, 2, :], scalar=rs[:, 7:8], in1=comb[:], op0=ALU.mult, op1=ALU.add),
                       [Roh, Rrs, Rcomb], [Rcomb])
                    cut("D8")
                    op("dve", lambda e: e.tensor_reduce(out=c8_all[:, i, :], in_=comb[:].rearrange("p (g j) -> p j g", g=4), axis=AX.X, op=ALU.add),
                       [Rcomb], [R_c8])

                op("dve", lambda e: e.memset(goh_all[:], 0.0), [], [R_goh])
                tilesD = [i for i in range(NTT) if not (i < NTC and last)]
                run_skewed([tileD(i, tc) for tc, i in enumerate(tilesD)])
                S.barrier()
                S.release(mk_stD)
        if stop_after == "D":
            return finish(nc, S, [R_xs_mix, R_h2tok])

        tilesD = [i for i in range(NTT) if not (i < NTC and last)]
        with ExitStack() as stS:
            mk_stS = S.mark()

            def aS(name, shape, dt=F32):
                return stS.enter_context(nc.sbuf_tensor("%s_L%d" % (name, l), shape, dt))
            CUM = srt[:, 0:4]
            op("dve", lambda e: e.memset(srt[:], 0.0), [], [R_srt])
            op("dve", lambda e: e.memset(dest_f[:], 0.0), [], [R_destf])
            tmp4_r = Ring(aS, "tmp4", [128, 4], F32, 2)
            for n_, i in enumerate(tilesD):
                pr, Rpr = PBK[n_ % 2]
                op("pe", lambda e: e.matmul(pr[:, 0:4], lhsT=tri_sb[:], rhs=goh_all[:, i, :], start=True, stop=True), [R_tri, R_goh], [Rpr])
                op("pe", lambda e: e.matmul(pr[:, 4:8], lhsT=ones_sb[:], rhs=goh_all[:, i, :], start=True, stop=True), [R_ones, R_goh], [Rpr])
                t4, Rt4 = tmp4_r.get()
                op("dve", lambda e: e.tensor_tensor(out=t4[:], in0=pr[:, 0:4], in1=CUM, op=ALU.add), [Rpr, R_srt], [Rt4])
                op("dve", lambda e: e.tensor_tensor(out=t4[:], in0=t4[:], in1=goh_all[:, i, :], op=ALU.mult), [Rt4, R_goh], [Rt4])
                op("dve", lambda e: e.tensor_reduce(out=dest_f[:, i:i + 1], in_=t4[:], axis=AX.X, op=ALU.add), [Rt4], [R_destf])
                op("dve", lambda e: e.tensor_tensor(out=CUM, in0=pr[:, 4:8], in1=CUM, op=ALU.add), [Rpr, R_srt], [R_srt])
            tk, Rtk = aS("tk", [128, NB]), Res("tk")
            for g in range(4):
                op("dve", lambda e: e.tensor_scalar(out=tk[:], in0=thr_sb[:], scalar1=srt[:, g:g + 1], scalar2=None, op0=ALU.is_lt), [R_thr, R_srt], [Rtk])
                op("dve", lambda e: e.tensor_reduce(out=srt[:, 8 + g:9 + g], in_=tk[:], axis=AX.X, op=ALU.add), [Rtk], [R_srt])
            op("dve", lambda e: e.memset(srt[:, 16:17], 0.0), [R_srt], [R_srt])
            for g in range(1, 4):
                op("dve", lambda e: e.tensor_tensor(out=srt[:, 16 + g:17 + g], in0=srt[:, 15 + g:16 + g], in1=srt[:, 7 + g:8 + g], op=ALU.add), [R_srt], [R_srt])
            op("dve", lambda e: e.tensor_scalar(out=srt[:, 24:28], in0=srt[:, 16:20], scalar1=512.0, scalar2=None, op0=ALU.mult), [R_srt], [R_srt])
            for g in range(4):
                op("dve", lambda e: e.scalar_tensor_tensor(out=dest_f[:], in0=goh_all[:, :, g], scalar=srt[:, 24 + g:25 + g], in1=dest_f[:],
                                                           op0=ALU.mult, op1=ALU.add), [R_goh, R_srt, R_destf], [R_destf])
            op("dve", lambda e: e.tensor_copy(out=dest_i[:], in_=dest_f[:]), [R_destf], [R_desti])
            gb, Rgb = aS("gb", [128, NB]), Res("gb")
            op("dve", lambda e: e.memset(gb[:], 0.0), [], [Rgb])
            for g in range(1, 4):
                op("dve", lambda e: e.tensor_scalar(out=tk[:], in0=blk_sb[:], scalar1=srt[:, 16 + g:17 + g], scalar2=None, op0=ALU.is_ge), [R_blk, R_srt], [Rtk])
                op("dve", lambda e: e.tensor_tensor(out=gb[:], in0=gb[:], in1=tk[:], op=ALU.add), [Rtk, Rgb], [Rgb])
            op("dve", lambda e: e.tensor_scalar(out=gb[:], in0=gb[:], scalar1=1024.0, scalar2=float(l * NE * 128), op0=ALU.mult, op1=ALU.add), [Rgb], [Rgb])
            op("dve", lambda e: e.tensor_tensor(out=widx_f[:], in0=gb[:].unsqueeze(2).to_broadcast([128, NB, 8]),
                                                in1=jp_sb[:].unsqueeze(1).to_broadcast([128, NB, 8]), op=ALU.add), [Rgb, R_jp], [R_widxf])
            op("dve", lambda e: e.tensor_copy(out=widx_i[:], in_=widx_f[:].rearrange("p b j -> p (b j)")), [R_widxf], [R_widxi])
            h2r_r = Ring(aS, "h2r", [128, D], BF16, 3)
            for i in tilesD:
                h2r, Rh2r = h2r_r.get()
                dma("sp", h2r[:], h2tok_scr[i * 128:(i + 1) * 128, :], [R_h2tok], [Rh2r])
                S.idma(h2perm_scr, h2r[:], dest_i[:, i:i + 1], True, [Rh2r, R_desti, R_h2pz], [R_h2perm])
                S.idma(c8perm_scr, c8_all[:, i, :], dest_i[:, i:i + 1], True, [R_c8, R_desti, R_c8pz], [R_c8perm])
            S.barrier()
            S.release(mk_stS)
        if stop_after == "S":
            return finish(nc, S, [R_h2perm, R_c8perm])

        with ExitStack() as stE:
            mk_stE = S.mark()

            def aE(name, shape, dt=F32):
                return stE.enter_context(nc.sbuf_tensor("%s_L%d" % (name, l), shape, dt))
            if not last:
                load_mix_weights(l + 1)
            hp_r = Ring(aE, "hp", [128, 4, D], BF16, 2)
            h2Tb_r = Ring(aE, "h2TbE", [128, 8, 512], BF16, 2)
            c8_r = Ring(aE, "c8b", [128, 4, 8], F32, 2)
            c8T_r = Ring(aE, "c8T", [8, 512], F32, 2)
            wg_r = Ring(aE, "wg", [128, 8 * DE], BF16, 4)
            wu_r = Ring(aE, "wu", [128, 8 * DE], BF16, 4)
            wd_r = Ring(aE, "wd", [128, 2 * D], BF16, 4)
            cb_r = Ring(aE, "cb", [128, 512], F32, 4)
            sg_r = Ring(aE, "sg", [128, 512], F32, 3)
            hid = aE("hidT_all", [128, 16, 512], BF16)
            R_hid = [Res("hid%d" % e) for e in range(8)]
            yo_r = Ring(aE, "yo", [128, D], F32, 3)
            ecnt = 0
            NB_l = ((T if last else TT) + 4 * 511) // 512
            for b in range(NB_l):
                hp, Rhp = hp_r.get()
                dma("sp", hp[:], h2perm_scr[b * 512:(b + 1) * 512, :].rearrange("(j p) d -> p j d", p=128), [R_h2perm], [Rhp])
                c8, Rc8 = c8_r.get()
                dma("sp", c8[:], c8perm_scr[b * 512:(b + 1) * 512, :].rearrange("(j p) e -> p j e", p=128), [R_c8perm], [Rc8])
                hb_, Rhb_ = h2Tb_r.get()
                for j in range(4):
                    pb, Rpb = PBK[j]
                    pbv = bfv(pb)
                    for k in range(8):
                        op("pe", lambda e: e.transpose(out=pbv[:, k * 128:(k + 1) * 128], in_=hp[:, j, k * 128:(k + 1) * 128], identity=ident_b[:]),
                           [Rhp, R_idb], [Rpb])
                    eng = "act" if j % 2 == 0 else "dve"
                    if eng == "act":
                        op("act", lambda e: e.copy(out=hb_[:, :, j * 128:(j + 1) * 128], in_=pbv[:, 0:1024].rearrange("p (k t) -> p k t", k=8)), [Rpb], [Rhb_])
                    else:
                        op("dve", lambda e: e.tensor_copy(out=hb_[:, :, j * 128:(j + 1) * 128], in_=pbv[:, 0:1024].rearrange("p (k t) -> p k t", k=8)), [Rpb], [Rhb_])
                pc, Rpc = PBK[4]
                for j in range(4):
                    op("pe", lambda e: e.transpose(out=pc[0:8, j * 128:(j + 1) * 128], in_=c8[:, j, :], identity=ident_f[:]), [Rc8, R_idf], [Rpc])
                c8T, Rc8T = c8T_r.get()
                op("dve", lambda e: e.tensor_copy(out=c8T[:], in_=pc[0:8, 0:512]), [Rpc], [Rc8T])
                dma("sp", cbT_scr[b], c8T[:], [Rc8T], [R_cbT])
                for j_ in range(8):
                    wg, Rwg = wg_r.get()
                    wu, Rwu = wu_r.get()
                    cb, Rcb_ = cb_r.get()
                    ix = widx_i[:, b * 8 + j_:b * 8 + j_ + 1]
                    S.idma(wg[:], wg_scr, ix, False, [R_wg, R_widxi], [Rwg])
                    S.idma(wu[:], wu_scr, ix, False, [R_wu, R_widxi], [Rwu])
                    dma("sp", cb[:], cbT_scr[b, j_, :].partition_broadcast(128), [R_cbT], [Rcb_])
                    wgv = wg[:].rearrange("p (k h) -> p k h", k=8)
                    wuv = wu[:].rearrange("p (k h) -> p k h", k=8)
                    base = 4 * (ecnt % 2)
                    ecnt += 1
                    for hc in range(2):
                        gt, Rg = PBK[base + hc]
                        ut, Ru = PBK[base + 2 + hc]
                        for k in range(8):
                            op("pe", lambda e: e.matmul(gt[:, :], lhsT=wgv[:, k, hc * 128:(hc + 1) * 128], rhs=hb_[:, k, :], start=(k == 0), stop=(k == 7)),
                               [Rwg, Rhb_], [Rg])
                        for k in range(8):
                            op("pe", lambda e: e.matmul(ut[:, :], lhsT=wuv[:, k, hc * 128:(hc + 1) * 128], rhs=hb_[:, k, :], start=(k == 0), stop=(k == 7)),
                               [Rwu, Rhb_], [Ru])
                    for hc in range(2):
                        gt, Rg = PBK[base + hc]
                        ut, Ru = PBK[base + 2 + hc]
                        sg, Rsg = sg_r.get()
                        op("act", lambda e: e.activation(out=sg[:], in_=gt[:, :], func=AF.Silu), [Rg], [Rsg])
                        op("dve", lambda e: e.tensor_tensor(out=sg[:], in0=ut[:, :], in1=sg[:], op=ALU.mult), [Ru, Rsg], [Rsg])
                        op("dve", lambda e: e.tensor_tensor(out=hid[:, 2 * j_ + hc, :], in0=sg[:], in1=cb[:], op=ALU.mult),
                           [Rsg, Rcb_], [R_hid[j_]])
                for j_ in range(8):
                    wd, Rwd = wd_r.get()
                    ix = widx_i[:, b * 8 + j_:b * 8 + j_ + 1]
                    S.idma(wd[:], wd_scr, ix, False, [R_wd, R_widxi], [Rwd])
                    wdv = wd[:].rearrange("p (c d) -> p c d", c=2)
                    for hc in range(2):
                        for j in range(4):
                            for dh in range(2):
                                yt, Ry_ = PBK[j * 2 + dh]
                                op("pe", lambda e: e.matmul(yt[:, :], lhsT=hid[:, 2 * j_ + hc, j * 128:(j + 1) * 128], rhs=wdv[:, hc, dh * 512:(dh + 1) * 512],
                                                            start=(j_ == 0 and hc == 0), stop=(j_ == 7 and hc == 1)), [R_hid[j_], Rwd], [Ry_])
                for j in range(4):
                    yo, Ryo = yo_r.get()
                    for dh in range(2):
                        yt, Ry_ = PBK[j * 2 + dh]
                        if dh == 0:
                            op("act", lambda e: e.copy(out=yo[:, 0:512], in_=yt[:, :]), [Ry_], [Ryo])
                        else:
                            op("dve", lambda e: e.tensor_copy(out=yo[:, 512:1024], in_=yt[:, :]), [Ry_], [Ryo])
                    r0 = b * 512 + j * 128
                    dma("sp", yperm_scr[r0:r0 + 128, :], yo[:], [Ryo], [R_yperm])
            S.barrier()
            S.release(mk_stE)
        if stop_after == "E":
            return finish(nc, S, [R_yperm])

        with ExitStack() as stF:
            mk_stF = S.mark()

            def aF(name, shape, dt=F32):
                return stF.enter_context(nc.sbuf_tensor("%s_L%d" % (name, l), shape, dt))
            g2bc = {}
            for r in range(2):
                if r == 1 and last:
                    continue
                t = aF("g2_%d" % r, [128, D])
                Rr = Res("g2_%d" % r)
                load_bc(t, Rr, ada_vec(l, r, 5), D, [R_ada])
                g2bc[r] = (t, Rr)
            ln2g_bc, R_l2g = aF("ln2g_bc", [128, D]), Res("ln2g_bc")
            ln2b_bc, R_l2b = aF("ln2b_bc", [128, D]), Res("ln2b_bc")
            load_bc(ln2g_bc, R_l2g, ln2_g[l], D)
            load_bc(ln2b_bc, R_l2b, ln2_b[l], D)
            xt_r = Ring(aF, "xtF", [128, D], F32, 3)
            yg_r = Ring(aF, "ygF", [128, D], F32, 3)
            o_r = Ring(aF, "oF", [128, D], F32, 3)
            lnr = {"st": Ring(aF, "fst", [128, 12], F32, 3), "mv": Ring(aF, "fmv", [128, 4], F32, 3)}

            def tileF(i):
                typ = 1 if i < NTC else 0
                gg = i * 128
                xt, Rxt = xt_r.get()
                dma("sp", xt[:], xs_mix[gg:gg + 128, :], [R_xs_mix], [Rxt])
                yg, Ryg = yg_r.get()
                S.idma(yg[:], yperm_scr, dest_i[:, i:i + 1], False, [R_yperm, R_desti], [Ryg])
                yield
                g2t, Rg2 = g2bc[typ]
                op("dve", lambda e: e.tensor_tensor(out=yg[:], in0=yg[:], in1=g2t[:], op=ALU.mult), [Ryg, Rg2], [Ryg])
                op("dve", lambda e: e.scalar_tensor_tensor(out=yg[:], in0=xt[:], scalar=ALPHA, in1=yg[:], op0=ALU.mult, op1=ALU.add), [Rxt, Ryg], [Ryg])
                o, Ro = o_r.get()
                layer_norm_tile(None, "dve", yg, Ryg, D, ln2g_bc, R_l2g, ln2b_bc, R_l2b, o, Ro, lnr)
                if last:
                    dma("sp", out_d[gg - C:gg - C + 128, :], o[:], [Ro], [R_out])
                else:
                    dma("sp", xs_out[l % 2][gg:gg + 128, :], o[:], [Ro], [R_xs_out[l % 2]])

            run_skewed([tileF(i) for i in tilesD])
            S.barrier()
            S.release(mk_stF)
    return finish(nc, S, [R_out])


def finish(nc, S, ress):
    S.barrier()
    S.wait_all("sp", ress)
    return nc


def _rope_tables(T, C):
    rows = T // GRID_W
    row = np.repeat(np.arange(rows), GRID_W).astype(np.float32)
    col = np.tile(np.arange(GRID_W), rows).astype(np.float32)
    d_axis = DR // 2
    inv_freq = np.power(np.float32(10000.0), -np.arange(0, d_axis, 2, dtype=np.float32) / np.float32(d_axis)).astype(np.float32)

    def ax(p):
        a = p[:, None] * inv_freq[None, :]
        return np.concatenate([a, a], -1)

    ang = np.concatenate([ax(row), ax(col)], -1).astype(np.float32)
    cos = np.cos(ang).astype(np.float32)
    sin = np.sin(ang).astype(np.float32)
    sgn = np.tile(np.concatenate([-np.ones(8), np.ones(8)]), 2).astype(np.float32)
    tab = np.zeros((T + C, 2, DR), np.float32)
    tab[:C, 0, :] = 1.0
    tab[C:, 0, :] = cos
    tab[C:, 1, :] = sin * sgn[None, :]
    return tab


def _pool_tables():
    wins = (2, 4, 8, 16)
    edge = np.zeros((128, 2, 2, 8), np.float32)
    invw = np.zeros((128, 2), np.float32)
    for k in range(2):
        for ph in range(2):
            w = wins[2 * k + ph]
            ps = slice(ph * 64, ph * 64 + 64)
            invw[ps, k] = 1.0 / w
            for j in range(8):
                t = j
                cnt = (t + w // 2 - 1) - max(t - w // 2, 0) + 1
                edge[ps, k, 0, j] = 1.0 / cnt
                r = 7 - j
                hi = min(w // 2 - 1, r)
                cnt = hi + w // 2 + 1
                edge[ps, k, 1, j] = 1.0 / cnt
    return edge, invw


def _sort_tables(T, C):
    TT = T + C
    NB = (TT + 4 * 511 + 511) // 512
    tri = np.triu(np.ones((128, 128), np.float32), k=1)
    thr = np.broadcast_to((np.arange(NB, dtype=np.float32) * 512.0)[None, :], (128, NB)).copy()
    blk = np.broadcast_to(np.arange(NB, dtype=np.float32)[None, :], (128, NB)).copy()
    jp = (np.arange(8, dtype=np.float32)[None, :] * 128.0 + np.arange(128, dtype=np.float32)[:, None]).astype(np.float32)
    return {"tri": tri, "thr_bc": thr, "blk_bc": blk, "jp": jp}


_CACHE = {}


def _consts(T, C):
    edge, invw = _pool_tables()
    return {
        "ident": np.eye(128, dtype=np.float32),
        "rope_cs": _rope_tables(T, C),
        "pool_edge": edge,
        "pool_invw": invw,
        **_sort_tables(T, C),
    }


_WKEYS = ["w_ada", "b_ada", "w_in", "g_q", "w_uq", "g_kv", "w_ukv", "conv_w", "conv_b", "conv_ln_g", "conv_ln_b", "pool_w",
          "pool_scale", "w_out", "ln1_g", "ln1_b", "w_router_group", "b_router_group", "w_router_expert", "b_router_expert",
          "w_gate", "w_up", "w_down", "ln2_g", "ln2_b"]


def make_in_maps(inputs, T, C, ncores):
    consts = _consts(T, C)
    shared = {k: np.ascontiguousarray(np.asarray(inputs[k], dtype=np.float32)) for k in _WKEYS}
    maps = []
    for b in range(ncores):
        m = dict(shared)
        m.update(consts)
        m["x"] = np.ascontiguousarray(np.asarray(inputs["x"][b], dtype=np.float32))
        m["ctx"] = np.ascontiguousarray(np.asarray(inputs["ctx"][b], dtype=np.float32))
        m["cvec"] = np.ascontiguousarray(np.stack([np.asarray(inputs["c"][b]), np.asarray(inputs["c_ctx"])]).astype(np.float32))
        maps.append(m)
    return maps


def kernel(**inputs):
    x = np.asarray(inputs["x"])
    B, T, _ = x.shape
    C = np.asarray(inputs["ctx"]).shape[1]
    L = np.asarray(inputs["w_ada"]).shape[0]
    key = (T, C, L)
    if key not in _CACHE:
        _CACHE[key] = build(T, C, L)
    nc = _CACHE[key]
    maps = make_in_maps(inputs, T, C, B)
    res = run_bass_kernel_spmd(nc, maps, core_ids=list(range(B)))
    return np.stack([np.asarray(r["out"]) for r in res.results], axis=0).astype(np.float32)
```
